# Optimizing a Trainium2 kernel written in Bass

```python
import math
import jax, jax.numpy as jnp
from jax import lax
import numpy as np

D_MODEL = 2048
BATCH = 2
SEQ = 4096
DEPTH = 1

D_PLE = 256
D_MIX = D_MODEL
POOL_WIDTH = D_MIX // 2
POOL_GROUPS = 4
POOL_GC = POOL_WIDTH // POOL_GROUPS
POOL_WINDOWS = (2, 4, 8, 16)
ATTN_HEADS = 8
HEAD_DIM = 128
ATTN_WIDTH = ATTN_HEADS * HEAD_DIM
IDX_HEADS = 16
IDX_DIM = 64
TOPK_MAX = 256
N_BUCKETS = 32
MAX_DISTANCE = 128
D_FF = 5632
CONV_WIDTH = 3
Q_BLOCK = 128
EPS = 1e-6
IN_SIZES = (POOL_WIDTH, ATTN_WIDTH, ATTN_WIDTH, ATTN_WIDTH,
            IDX_HEADS * IDX_DIM, IDX_DIM, IDX_HEADS)
D_IN = sum(IN_SIZES)

kernel_name = "hymba_pool_dsa_convffn_ple"


def rmsnorm(x, g):
    xf = x.astype(jnp.float32)
    y = xf * lax.rsqrt(jnp.mean(xf * xf, axis=-1, keepdims=True) + EPS)
    return (y * g.astype(jnp.float32)).astype(x.dtype)


def t5_bucket(dist):
    n = jnp.maximum(dist, 0)
    max_exact = N_BUCKETS // 2
    nf = jnp.maximum(n, 1).astype(jnp.float32)
    large = max_exact + (jnp.log(nf / max_exact) / math.log(MAX_DISTANCE / max_exact)
                         * (N_BUCKETS - max_exact)).astype(jnp.int32)
    large = jnp.minimum(large, N_BUCKETS - 1)
    return jnp.where(n < max_exact, n, large)


def pool_mixer(u, w_pool, pool_scale):
    B, S, _ = u.shape
    uf = u.astype(jnp.float32).reshape(B, S, POOL_GROUPS, POOL_GC)
    cs0 = jnp.concatenate([jnp.zeros((B, 1, POOL_GROUPS, POOL_GC), jnp.float32),
                           jnp.cumsum(uf, axis=1)], axis=1)
    t = jnp.arange(S)
    means = []
    for g, w in enumerate(POOL_WINDOWS):
        lo = jnp.maximum(t + 1 - w, 0)
        wsum = cs0[:, t + 1, g] - cs0[:, lo, g]
        count = (t + 1 - lo).astype(jnp.float32)
        means.append(wsum / count[None, :, None])
    pooled = (jnp.stack(means, axis=2) - uf).astype(u.dtype)
    y = jnp.einsum('bsgc,gcd->bsgd', pooled, w_pool).reshape(B, S, POOL_WIDTH)
    return y * pool_scale


def dsa_attention(q, k, v, q_idx, k_idx, w_idx, rel_bias):
    B, S = q.shape[0], q.shape[1]
    topk = min(TOPK_MAX, S // 4)
    nb = S // Q_BLOCK
    s_pos = jnp.arange(S)
    idx_scale = (IDX_HEADS ** -0.5) * (IDX_DIM ** -0.5)
    k_idx_f = k_idx.astype(jnp.float32)

    def to_blocks(a):
        return a.reshape(B, nb, Q_BLOCK, *a.shape[2:]).swapaxes(0, 1)

    gather = jax.vmap(lambda kb, ib: kb[ib])

    def one_block(args):
        blk, qb, qib, wb = args
        t_pos = blk * Q_BLOCK + jnp.arange(Q_BLOCK)
        dots = jnp.einsum('bqhd,bsd->bhqs', qib.astype(jnp.float32), k_idx_f)
        score = jnp.einsum('bqh,bhqs->bqs', wb.astype(jnp.float32) * idx_scale,
                           jax.nn.relu(dots))
        causal = s_pos[None, :] <= t_pos[:, None]
        score = jnp.where(causal[None], score, -jnp.inf)
        _, sel = lax.top_k(score, topk)
        k_sel = gather(k, sel)
        v_sel = gather(v, sel)
        dist = t_pos[None, :, None] - sel
        valid = dist >= 0
        bias = rel_bias[t5_bucket(dist)].astype(jnp.float32)
        logits = (jnp.einsum('bqhd,bqkhd->bhqk', qb.astype(jnp.float32),
                             k_sel.astype(jnp.float32)) * (HEAD_DIM ** -0.5)
                  + bias.transpose(0, 3, 1, 2))
        logits = jnp.where(valid[:, None], logits, -jnp.inf)
        probs = jax.nn.softmax(logits, axis=-1)
        out = jnp.einsum('bhqk,bqkhd->bqhd', probs, v_sel.astype(jnp.float32))
        return out.astype(q.dtype)

    outs = lax.map(one_block, (jnp.arange(nb), to_blocks(q), to_blocks(q_idx), to_blocks(w_idx)))
    return outs.swapaxes(0, 1).reshape(B, S, ATTN_HEADS * HEAD_DIM)


def conv_ffn(h, w_up, conv_w, conv_b, w_down):
    S = h.shape[1]
    u = h @ w_up
    upad = jnp.pad(u, ((0, 0), (CONV_WIDTH - 1, 0), (0, 0)))
    uc = sum(upad[:, j:j + S] * conv_w[j] for j in range(CONV_WIDTH)) + conv_b
    gate, val = jnp.split(uc, 2, axis=-1)
    return (jax.nn.silu(gate) * val) @ w_down


def setup_inputs(seed: int = 0) -> dict:
    key = jax.random.key(seed)
    ks = jax.random.split(key, 20)
    f32 = jnp.float32
    nrm = lambda k, shape, scale: jax.random.normal(k, shape, f32) * scale
    return {
        "x": nrm(ks[0], (BATCH, SEQ, D_MODEL), 1.0),
        "p": nrm(ks[1], (DEPTH, BATCH, SEQ, D_PLE), 1.0),
        "g_mix": 1.0 + nrm(ks[2], (DEPTH, D_MODEL), 0.02),
        "w_in": nrm(ks[3], (DEPTH, D_MODEL, D_IN), D_MODEL ** -0.5),
        "w_pool": nrm(ks[4], (DEPTH, POOL_GROUPS, POOL_GC, POOL_GC), POOL_GC ** -0.5),
        "pool_scale": 1.0 + nrm(ks[5], (DEPTH, POOL_WIDTH), 0.02),
        "rel_bias": nrm(ks[6], (N_BUCKETS, ATTN_HEADS), 0.5),
        "w_out": nrm(ks[7], (DEPTH, D_MIX, D_MODEL), D_MIX ** -0.5),
        "g_ffn": 1.0 + nrm(ks[8], (DEPTH, D_MODEL), 0.02),
        "w_up": nrm(ks[9], (DEPTH, D_MODEL, 2 * D_FF), D_MODEL ** -0.5),
        "conv_w": 1.0 / CONV_WIDTH + nrm(ks[10], (DEPTH, CONV_WIDTH, 2 * D_FF), 0.2),
        "conv_b": nrm(ks[11], (DEPTH, 2 * D_FF), 0.01),
        "w_down": nrm(ks[12], (DEPTH, D_FF, D_MODEL), D_FF ** -0.5),
        "g_ple": 1.0 + nrm(ks[13], (DEPTH, D_MODEL), 0.02),
        "w_ple_gate": nrm(ks[14], (DEPTH, D_MODEL, D_MODEL), D_MODEL ** -0.5),
        "w_ple_proj": nrm(ks[15], (DEPTH, D_PLE, D_MODEL), D_PLE ** -0.5),
        "g_final": 1.0 + nrm(ks[16], (D_MODEL,), 0.02),
    }


def reference(x, p, g_mix, w_in, w_pool, pool_scale, rel_bias, w_out, g_ffn, w_up,
              conv_w, conv_b, w_down, g_ple, w_ple_gate, w_ple_proj, g_final):
    B, S, _ = x.shape
    offs = np.cumsum((0,) + IN_SIZES).tolist()
    for i in range(DEPTH):
        h = rmsnorm(x, g_mix[i])
        proj = h @ w_in[i]
        u_pool, q, k, v, q_idx, k_idx, w_idx = [proj[..., offs[j]:offs[j + 1]]
                                                 for j in range(len(IN_SIZES))]
        pool_out = pool_mixer(u_pool, w_pool[i], pool_scale[i])
        attn_out = dsa_attention(q.reshape(B, S, ATTN_HEADS, HEAD_DIM),
                                 k.reshape(B, S, ATTN_HEADS, HEAD_DIM),
                                 v.reshape(B, S, ATTN_HEADS, HEAD_DIM),
                                 q_idx.reshape(B, S, IDX_HEADS, IDX_DIM),
                                 k_idx, w_idx, rel_bias)
        x = x + jnp.concatenate([pool_out, attn_out], axis=-1) @ w_out[i]
        x = x + conv_ffn(rmsnorm(x, g_ffn[i]), w_up[i], conv_w[i], conv_b[i], w_down[i])
        gate = jax.nn.sigmoid(rmsnorm(x, g_ple[i]) @ w_ple_gate[i])
        x = x + (p[i] @ w_ple_proj[i]) * gate
    return rmsnorm(x, g_final)
```

```python
import contextlib
import math
import numpy as np
import concourse.bass as bass
import concourse.mybir as mybir
from concourse.bass_utils import run_bass_kernel_spmd

F32 = mybir.dt.float32
BF16 = mybir.dt.bfloat16
AF = mybir.ActivationFunctionType
ALU = mybir.AluOpType
AX = mybir.AxisListType

D = 2048
SEQ = 4096
CTX = 4096
NQB = 9
QS0 = 2944
NQ = NQB * 128
HM0 = 2816
NHM = 1280
D_FF = 5632
NFC = 44
EPS = 1e-6
NEG = -1.0e30
NIT = 20
OFF = dict(pool=0, q=1024, k=2048, v=3072, qi=4096, ki=5120, wi=5184)


class Prog:
    NDS = 12

    def __init__(self, nc, es, same_sync=True):
        self.nc = nc
        self.same_sync = same_sync
        self.eng = {'pe': nc.tensor, 'act': nc.scalar, 'dve': nc.vector, 'pool': nc.gpsimd, 'sp': nc.sync}
        self.csem = {e: es.enter_context(nc.semaphore('c_' + e)) for e in ['pe', 'act', 'dve', 'pool']}
        self.cnt = {e: 0 for e in self.csem}
        self.dsem = {q: [es.enter_context(nc.semaphore('d_%s%d' % (q, i))) for i in range(self.NDS)]
                     for q in ['sp', 'pool']}
        self.duse = {q: [0] * self.NDS for q in self.dsem}
        self.dnext = {q: 0 for q in self.dsem}
        self.seen = {e: {} for e in self.eng}
        self.lastw = {}
        self.rds = {}
        self.nwait = 0

    def _wait(self, e, tok):
        sem, val, sid = tok
        if sid == e and (e == 'pe' or not self.same_sync):
            return
        if self.seen[e].get(sid, 0) >= val:
            return
        self.eng[e].wait_ge(sem, val)
        self.seen[e][sid] = val
        self.nwait += 1

    def _deps(self, reads, writes):
        deps = {}

        def add(tok):
            sid = tok[2]
            if sid not in deps or deps[sid][1] < tok[1]:
                deps[sid] = tok
        for k in reads:
            if k in self.lastw:
                add(self.lastw[k])
        for k in writes:
            if k in self.lastw:
                add(self.lastw[k])
            for tok in self.rds.get(k, {}).values():
                add(tok)
        return deps.values()

    def _record(self, tok, reads, writes):
        sid = tok[2]
        for k in reads:
            d = self.rds.setdefault(k, {})
            if sid not in d or d[sid][1] < tok[1]:
                d[sid] = tok
        for k in writes:
            self.lastw[k] = tok
            self.rds[k] = {}

    def op(self, e, fn, reads=(), writes=()):
        for tok in self._deps(reads, writes):
            self._wait(e, tok)
        ins = fn()
        self.cnt[e] += 1
        ins.then_inc(self.csem[e], 1)
        self._record((self.csem[e], self.cnt[e], e), reads, writes)

    def dma(self, q, out, in_, reads=(), writes=(), **kw):
        for tok in self._deps(reads, writes):
            self._wait(q, tok)
        k = self.dnext[q]
        self.dnext[q] = (k + 1) % self.NDS
        u = self.duse[q][k]
        sem = self.dsem[q][k]
        if u > 0:
            self._wait(q, (sem, 16 * u, (q, k)))
        ins = self.eng[q].dma_start(out=out, in_=in_, **kw)
        ins.then_inc(sem, 16)
        self.duse[q][k] = u + 1
        self._record((sem, 16 * (u + 1), (q, k)), reads, writes)

    def barrier(self):
        for e in self.eng:
            for c in self.csem:
                if self.cnt[c] > 0:
                    self._wait_force(e, (self.csem[c], self.cnt[c], c))
            for q in self.dsem:
                for k in range(self.NDS):
                    if self.duse[q][k] > 0:
                        self._wait_force(e, (self.dsem[q][k], 16 * self.duse[q][k], (q, k)))
        self.lastw.clear()
        self.rds.clear()

    def _wait_force(self, e, tok):
        sem, val, sid = tok
        if self.seen[e].get(sid, 0) >= val:
            return
        self.eng[e].wait_ge(sem, val)
        self.seen[e][sid] = val
        self.nwait += 1


def build(debug=False, stop_after='Z'):
    nc = bass.Bass("TRN2", target_bir_lowering=False)

    def din(name, shape, dt=F32):
        return nc.dram_tensor(name, list(shape), dt, kind="ExternalInput").ap()

    xctx = din("xctx", [CTX, D])
    p_own = din("p_own", [1024, 256])
    w_in = din("w_in", [D, 5200])
    w_pool = din("w_pool", [4, 256, 256])
    w_out = din("w_out", [D, D])
    w_up = din("w_up", [D, 2 * D_FF])
    w_down = din("w_down", [D_FF, D])
    w_pg = din("w_ple_gate", [D, D])
    w_pp = din("w_ple_proj", [256, D])
    gT3 = din("gT3", [3, 128, 2048])
    gfin = din("gfin", [128, 2048])
    pscale = din("pscale", [128, 8])
    convp = din("convp", [128, 2 * NFC, 4])
    strip = din("strip", [128, 8, 896])
    cfar = din("cfar", [128, 8])
    slotb = din("slotb", [128, CTX])
    causal = din("causal", [128, 128])
    ident = din("ident", [128, 128])
    invc = din("invc", [128, 8, 16])
    hflag = din("hflag", [128, 1])
    halfs = din("halfs", [128, NIT])
    out = nc.dram_tensor("out", [1024, D], F32, kind="ExternalOutput").ap()
    ks = "ExternalOutput" if debug else "Internal"
    KT_d = nc.dram_tensor("KT_d", [8, 128, CTX], BF16, kind=ks).ap()
    V_d = nc.dram_tensor("V_d", [8, CTX, 128], BF16, kind=ks).ap()
    x1_d = nc.dram_tensor("x1_d", [NQ, D], F32, kind=ks).ap()
    x2_d = nc.dram_tensor("x2_d", [1024, D], F32, kind=ks).ap()
    kidxT_d = nc.dram_tensor("kidxT_d", [128, CTX], BF16, kind=ks).ap()
    QT_d = nc.dram_tensor("QT_d", [128, 8, NQ], BF16, kind=ks).ap()
    qiT_d = nc.dram_tensor("qiT_d", [128, 8, NQ], BF16, kind=ks).ap()
    ypT_d = nc.dram_tensor("ypT_d", [128, 8, NQ], BF16, kind=ks).ap()
    attnT_d = nc.dram_tensor("attnT_d", [128, 8, NQ], BF16, kind=ks).ap()
    widx_d = nc.dram_tensor("widx_d", [128, NQB, 16], F32, kind=ks).ap()
    dbg = {}
    if debug:
        dbg['maskT'] = nc.dram_tensor("dbg_maskT", [128, 32, NQ], BF16, kind="ExternalOutput").ap()
        dbg['thr'] = nc.dram_tensor("dbg_thr", [128, NQB], F32, kind="ExternalOutput").ap()
        dbg['gT'] = nc.dram_tensor("dbg_gT", [128, NFC, 1024], BF16, kind="ExternalOutput").ap()

    with contextlib.ExitStack() as es:
        P = Prog(nc, es)
        T = nc.tensor
        A = nc.scalar
        V = nc.vector
        G = nc.gpsimd

        def sb(name, shape, dt, stack=es):
            return stack.enter_context(nc.sbuf_tensor(name, list(shape), dt))

        def ps(name, shape, dt, stack):
            return stack.enter_context(nc.psum_tensor(name, list(shape), dt))

        ident_f = sb("ident_f", [128, 128], F32)
        ident_b = sb("ident_b", [128, 128], BF16)
        ones_b = sb("ones_b", [128, 128], BF16)
        st = sb("st", [128, 16], F32)
        P.dma('sp', ident_f[:], ident[:, :], writes=['ident_f'])
        P.op('dve', lambda: V.tensor_copy(out=ident_b[:], in_=ident_f[:]), reads=['ident_f'], writes=['ident_b'])
        P.op('dve', lambda: V.memset(ones_b[:], 1.0), writes=['ones_b'])

        def alloc(name, shape, dt):
            cm = nc.sbuf_tensor(name, list(shape), dt)
            return cm, cm.__enter__()

        def norm_T(i, xin, xin_key, xn, pT, gsb, dst_fn, dst_keys, extra=None):
            ss = st[:, 3 * i:3 * i + 1]
            sd = st[:, 3 * i + 1:3 * i + 2]
            rs = st[:, 3 * i + 2:3 * i + 3]
            P.op('act', lambda: A.activation(out=xn[i][:], in_=xin, func=AF.Square, accum_out=ss),
                 reads=[xin_key], writes=[('xn', i), ('ss', i)])
            P.op('dve', lambda: V.tensor_scalar(out=sd, in0=ss, scalar1=1.0 / D, scalar2=EPS, op0=ALU.mult, op1=ALU.add),
                 reads=[('ss', i)], writes=[('sd', i)])
            P.op('act', lambda: A.activation(out=sd, in_=sd, func=AF.Sqrt), reads=[('sd', i)], writes=[('sd', i)])
            P.op('dve', lambda: V.reciprocal(out=rs, in_=sd), reads=[('sd', i)], writes=[('rs', i)])
            if extra is not None:
                P.op('dve', lambda: V.tensor_tensor(out=rs, in0=rs, in1=extra, op=ALU.mult),
                     reads=[('rs', i), 'hflag'], writes=[('rs', i)])
            P.op('dve', lambda: V.tensor_scalar(out=xn[i][:], in0=xin, scalar1=rs, scalar2=None, op0=ALU.mult),
                 reads=[xin_key, ('rs', i)], writes=[('xn', i)])
            for c in range(16):
                P.op('pe', lambda c=c: T.transpose(out=pT[i][c // 8][:, (c % 8) * 128:(c % 8 + 1) * 128],
                                                   in_=xn[i][:, c * 128:(c + 1) * 128], identity=ident_b[:]),
                     reads=[('xn', i), 'ident_b'], writes=[('pT', i, c // 8)])
            for h in range(2):
                P.op('dve', lambda h=h: V.tensor_tensor(
                    out=dst_fn(h), in0=pT[i][h][:, :].rearrange("p (c t) -> p c t", t=128),
                    in1=gsb[:, 8 * h:8 * h + 8, :], op=ALU.mult),
                    reads=[('pT', i, h), 'gsb'], writes=dst_keys)

        def wload(dst, dkey, src_rows, c0, c1, col0, ncol, q='pool'):
            P.dma(q, dst[:, c0:c1, 0:ncol],
                  src_rows[c0 * 128:c1 * 128, col0:col0 + ncol].rearrange("(c p) n -> p c n", p=128),
                  writes=[dkey])

        with contextlib.ExitStack() as pa:
            kidxT = sb("kidxT", [128, CTX], BF16, pa)
            Wk = sb("Wk", [128, 16, 1024], BF16, pa)
            Wv = sb("Wv", [128, 16, 1024], BF16, pa)
            Wki = sb("Wki", [128, 16, 128], BF16, pa)
            gsb = sb("gsbA", [128, 16, 128], F32, pa)
            xb = [sb("xbA%d" % i, [128, D], F32, pa) for i in range(2)]
            xn = [sb("xnA%d" % i, [128, D], BF16, pa) for i in range(2)]
            hT = [sb("hTA%d" % i, [128, 16, 512], BF16, pa) for i in range(2)]
            KTst = [sb("KTst%d" % i, [128, 8, 512], BF16, pa) for i in range(2)]
            Vst = [sb("Vst%d" % i, [128, 4, 1024], BF16, pa) for i in range(2)]
            pT = [[ps("pTA%d%d" % (i, h), [128, 1024], BF16, pa) for h in range(2)] for i in range(2)]
            pm = [ps("pmA%d" % i, [128, 512], F32, pa) for i in range(4)]

            P.dma('sp', gsb[:].rearrange("p c t -> p (c t)"), gT3[0, :, :], writes=['gsb'])
            for cg in range(4):
                wload(Wk, ('Wk', cg), w_in, 4 * cg, 4 * cg + 4, OFF['k'], 1024)
            for cg in range(4):
                wload(Wv, ('Wv', cg), w_in, 4 * cg, 4 * cg + 4, OFF['v'], 1024)
            P.dma('pool', Wki[:, :, 0:64], w_in[:, OFF['ki']:OFF['ki'] + 64].rearrange("(c p) n -> p c n", p=128),
                  writes=['Wki'])
            P.dma('pool', Wki[:, :, 64:128], w_in[:, OFF['ki']:OFF['ki'] + 64].rearrange("(c p) n -> p c n", p=128),
                  writes=['Wki'])
            pmi = 0
            for ct in range(CTX // 512):
                b = ct % 2
                for sbl in range(4):
                    j = ct * 4 + sbl
                    i = j % 2
                    P.dma('sp', xb[i][:], xctx[j * 128:(j + 1) * 128, :], writes=[('xb', i)])
                    norm_T(i, xb[i][:], ('xb', i), xn, pT, gsb,
                           lambda h, b=b, sbl=sbl: hT[b][:, 8 * h:8 * h + 8, sbl * 128:(sbl + 1) * 128],
                           [('hT', b, sbl)])
                hkeys = [('hT', b, s_) for s_ in range(4)]
                for h in range(8):
                    pq = pm[pmi % 4]
                    pk = ('pm', pmi % 4)
                    pmi += 1
                    for c in range(16):
                        P.op('pe', lambda c=c, h=h, pq=pq: T.matmul(pq[:, :], lhsT=Wk[:, c, h * 128:(h + 1) * 128],
                                                                    rhs=hT[b][:, c, :], start=(c == 0), stop=(c == 15)),
                             reads=hkeys + [('Wk', c // 4)], writes=[pk])
                    P.op('act', lambda h=h, pq=pq: A.activation(out=KTst[b][:, h, :], in_=pq[:, :], func=AF.Copy),
                         reads=[pk], writes=[('KTst', b)])
                pq = pm[pmi % 4]
                pk = ('pm', pmi % 4)
                pmi += 1
                for c in range(16):
                    P.op('pe', lambda c=c, pq=pq: T.matmul(pq[:, :], lhsT=Wki[:, c, :], rhs=hT[b][:, c, :],
                                                           start=(c == 0), stop=(c == 15)),
                         reads=hkeys + ['Wki'], writes=[pk])
                P.op('act', lambda pq=pq, ct=ct: A.activation(out=kidxT[:, ct * 512:(ct + 1) * 512], in_=pq[:, :], func=AF.Copy),
                     reads=[pk], writes=[('kidxT', ct)])
                for sbl in range(4):
                    for hf in range(2):
                        pq = pm[pmi % 4]
                        pk = ('pm', pmi % 4)
                        pmi += 1
                        for c in range(16):
                            P.op('pe', lambda c=c, pq=pq, sbl=sbl, hf=hf: T.matmul(
                                pq[:, :], lhsT=hT[b][:, c, sbl * 128:(sbl + 1) * 128],
                                rhs=Wv[:, c, hf * 512:(hf + 1) * 512], start=(c == 0), stop=(c == 15)),
                                reads=[('hT', b, sbl), ('Wv', c // 4)], writes=[pk])
                        eng = 'act' if hf == 0 else 'dve'
                        if eng == 'act':
                            P.op('act', lambda pq=pq, sbl=sbl, hf=hf: A.activation(
                                out=Vst[b][:, sbl, hf * 512:(hf + 1) * 512], in_=pq[:, :], func=AF.Copy),
                                reads=[pk], writes=[('Vst', b)])
                        else:
                            P.op('dve', lambda pq=pq, sbl=sbl, hf=hf: V.tensor_copy(
                                out=Vst[b][:, sbl, hf * 512:(hf + 1) * 512], in_=pq[:, :]),
                                reads=[pk], writes=[('Vst', b)])
                P.dma('sp', KT_d[:, :, ct * 512:(ct + 1) * 512].rearrange("h d s -> d h s"), KTst[b][:, :, :],
                      reads=[('KTst', b)], writes=[('KT_d', ct)])
                for sbl in range(4):
                    r0 = ct * 512 + sbl * 128
                    P.dma('sp', V_d[:, r0:r0 + 128, :].rearrange("h p d -> p h d"),
                          Vst[b][:, sbl, :].rearrange("p (h d) -> p h d", d=128),
                          reads=[('Vst', b)], writes=[('V_d', ct)])
            P.dma('sp', kidxT_d[:, :], kidxT[:, :], reads=[('kidxT', c_) for c_ in range(8)])
            P.barrier()
        if stop_after == 'A':
            return nc


        with contextlib.ExitStack() as pb:
            QT = sb("QT", [128, 8, NQ], BF16, pb)
            qiT = sb("qiT", [128, 8, NQ], BF16, pb)
            ypT = sb("ypT", [128, 8, NQ], BF16, pb)
            widx = sb("widx", [128, NQB, 16], F32, pb)
            hM = sb("hM", [128, 16, NHM], BF16, pb)
            with contextlib.ExitStack() as pb1:
                gsb = sb("gsbB", [128, 16, 128], F32, pb1)
                xb = [sb("xbB%d" % i, [128, D], F32, pb1) for i in range(2)]
                xn = [sb("xnB%d" % i, [128, D], BF16, pb1) for i in range(2)]
                pT = [[ps("pTB%d%d" % (i, h), [128, 1024], BF16, pb1) for h in range(2)] for i in range(2)]
                P.dma('sp', gsb[:].rearrange("p c t -> p (c t)"), gT3[0, :, :], writes=['gsb'])
                for j in range(NHM // 128):
                    i = j % 2
                    P.dma('sp', xb[i][:], xctx[HM0 + j * 128:HM0 + (j + 1) * 128, :], writes=[('xb', i)])
                    norm_T(i, xb[i][:], ('xb', i), xn, pT, gsb,
                           lambda h, j=j: hM[:, 8 * h:8 * h + 8, j * 128:(j + 1) * 128], [('hM', j)])
                P.barrier()
            Ws = [sb("WsB%d" % i, [128, 16, 256], BF16, pb) for i in range(2)]
            pp = [ps("ppB%d" % i, [128, 4, 512], F32, pb) for i in range(2)]
            Wpl = sb("Wpl", [128, 4, 2, 256], BF16, pb)
            Ww = sb("Ww", [128, 16, 16], BF16, pb)
            psc_sb = sb("pscale_sb", [128, 8], F32, pb)
            invc_sb = sb("invc_sb", [128, 8, 16], F32, pb)
            pooledT = sb("pooledT", [128, 8, NQ], BF16, pb)
            uk = [sb("uk%d" % i, [128, 1168], F32, pb) for i in range(2)]
            sA = sb("sA", [128, 1168], F32, pb)
            sB = sb("sB", [128, 1168], F32, pb)
            t16 = sb("t16", [128, 16], F32, pb)
            P.dma('sp', psc_sb[:], pscale[:, :], writes=['pscale'])
            P.dma('sp', invc_sb[:], invc[:, :, :], writes=['invc'])
            for g in range(4):
                P.dma('pool', Wpl[:, g, :, :], w_pool[g, :, :].rearrange("(cc p) d -> p cc d", p=128), writes=['Wpl'])
            P.dma('pool', Ww[:, :, :], w_in[:, OFF['wi']:OFF['wi'] + 16].rearrange("(c p) n -> p c n", p=128), writes=['Ww'])
            wcnt = [0]
            pcnt = [0]

            def next_w(col0):
                b = wcnt[0] % 2
                wcnt[0] += 1
                wload(Ws[b], ('Ws', b), w_in, 0, 16, col0, 256)
                return b

            def next_pp():
                b = pcnt[0] % 2
                pcnt[0] += 1
                return b

            for (dst, off) in ((QT, OFF['q']), (qiT, OFF['qi'])):
                for g in range(4):
                    wb = next_w(off + 256 * g)
                    for m_ in range(2):
                        pb_ = next_pp()
                        for n in range(3):
                            for c in range(16):
                                P.op('pe', lambda c=c, n=n, wb=wb, m_=m_, pb_=pb_: T.matmul(
                                    pp[pb_][:, n, 0:384], lhsT=Ws[wb][:, c, m_ * 128:(m_ + 1) * 128],
                                    rhs=hM[:, c, 128 + n * 384:128 + (n + 1) * 384], start=(c == 0), stop=(c == 15)),
                                    reads=[('Ws', wb)], writes=[('pp', pb_)])
                        P.op('act', lambda dst=dst, g=g, m_=m_, pb_=pb_: A.activation(
                            out=dst[:, 2 * g + m_, :].rearrange("p (n t) -> p n t", t=384),
                            in_=pp[pb_][:, 0:3, 0:384], func=AF.Copy),
                            reads=[('pp', pb_)], writes=[('fm', id(dst), 2 * g + m_)])
            for g in range(4):
                wb = next_w(OFF['pool'] + 256 * g)
                for m_ in range(2):
                    k = 2 * g + m_
                    pb_ = next_pp()
                    for n in range(4):
                        for c in range(16):
                            P.op('pe', lambda c=c, n=n, wb=wb, m_=m_, pb_=pb_: T.matmul(
                                pp[pb_][:, n, 0:292], lhsT=Ws[wb][:, c, m_ * 128:(m_ + 1) * 128],
                                rhs=hM[:, c, 112 + n * 292:112 + (n + 1) * 292], start=(c == 0), stop=(c == 15)),
                                reads=[('Ws', wb)], writes=[('pp', pb_)])
                    u = uk[k % 2]
                    P.op('act', lambda u=u, pb_=pb_: A.activation(
                        out=u[:, :].rearrange("p (n t) -> p n t", t=292), in_=pp[pb_][:, 0:4, 0:292], func=AF.Copy),
                        reads=[('pp', pb_)], writes=[('uk', k % 2)])
                    wdw = (2, 4, 8, 16)[g]
                    cur, ckey = u, ('uk', k % 2)
                    for s_ in range(int(math.log2(wdw))):
                        sh = 1 << s_
                        nxt, nkey = (sA, 'sA') if s_ % 2 == 0 else (sB, 'sB')
                        P.op('pool', lambda cur=cur, nxt=nxt, sh=sh: G.tensor_tensor(
                            out=nxt[:, sh:1168], in0=cur[:, sh:1168], in1=cur[:, 0:1168 - sh], op=ALU.add),
                            reads=[ckey], writes=[nkey])
                        cur, ckey = nxt, nkey
                    P.op('dve', lambda cur=cur, u=u, k=k, wdw=wdw: V.scalar_tensor_tensor(
                        out=pooledT[:, k, :], in0=cur[:, 16:1168], scalar=1.0 / wdw, in1=u[:, 16:1168],
                        op0=ALU.mult, op1=ALU.subtract), reads=[ckey, ('uk', k % 2)], writes=[('pooledT', k)])
                    P.op('dve', lambda cur=cur, k=k: V.tensor_tensor(out=t16[:, :], in0=cur[:, 144:160], in1=invc_sb[:, k, :], op=ALU.mult),
                         reads=[ckey, 'invc'], writes=['t16'])
                    P.op('dve', lambda u=u, k=k: V.tensor_tensor(out=pooledT[:, k, 128:144], in0=t16[:, :], in1=u[:, 144:160], op=ALU.subtract),
                         reads=['t16', ('uk', k % 2)], writes=[('pooledT', k)])
            for k2 in range(8):
                g, dm = k2 // 2, k2 % 2
                pb_ = next_pp()
                for n in range(3):
                    for cc in range(2):
                        P.op('pe', lambda n=n, cc=cc, g=g, dm=dm, pb_=pb_: T.matmul(
                            pp[pb_][:, n, 0:384], lhsT=Wpl[:, g, cc, dm * 128:(dm + 1) * 128],
                            rhs=pooledT[:, 2 * g + cc, n * 384:(n + 1) * 384], start=(cc == 0), stop=(cc == 1)),
                            reads=['Wpl', ('pooledT', 2 * g + cc)], writes=[('pp', pb_)])
                P.op('act', lambda k2=k2, pb_=pb_: A.activation(
                    out=ypT[:, k2, :].rearrange("p (n t) -> p n t", t=384), in_=pp[pb_][:, 0:3, 0:384],
                    func=AF.Copy, scale=psc_sb[:, k2:k2 + 1]), reads=[('pp', pb_), 'pscale'], writes=[('ypT', k2)])
            idx_scale = (16 ** -0.5) * (64 ** -0.5)
            for i in range(NQB):
                pb_ = next_pp()
                for c in range(16):
                    P.op('pe', lambda c=c, i=i, pb_=pb_: T.matmul(
                        pp[pb_][:, 0, 0:16], lhsT=hM[:, c, 128 + i * 128:256 + i * 128], rhs=Ww[:, c, :],
                        start=(c == 0), stop=(c == 15)), reads=['Ww'], writes=[('pp', pb_)])
                P.op('act', lambda i=i, pb_=pb_: A.activation(out=widx[:, i, :], in_=pp[pb_][:, 0, 0:16], func=AF.Copy, scale=idx_scale),
                     reads=[('pp', pb_)], writes=[('widx', i)])
            P.dma('sp', QT_d[:, :, :], QT[:, :, :], reads=[('fm', id(QT), h_) for h_ in range(8)])
            P.dma('sp', qiT_d[:, :, :], qiT[:, :, :], reads=[('fm', id(qiT), h_) for h_ in range(8)])
            P.dma('sp', ypT_d[:, :, :], ypT[:, :, :], reads=[('ypT', h_) for h_ in range(8)])
            P.dma('sp', widx_d[:, :, :], widx[:, :, :], reads=[('widx', h_) for h_ in range(NQB)])
            P.barrier()
        if stop_after == 'B':
            return nc

        pcd = contextlib.ExitStack()
        es.enter_context(pcd)
        maskT = sb("maskT", [128, 32, NQ], BF16, pcd)
        with contextlib.ExitStack() as pc:
            kidxT = sb("kidxTc", [128, CTX], BF16, pc)
            qiT = sb("qiTc", [128, 8, NQ], BF16, pc)
            widx = sb("widxc", [128, NQB, 16], F32, pc)
            P.dma('sp', kidxT[:, :], kidxT_d[:, :], writes=['kidxT'])
            P.dma('sp', qiT[:, :, :], qiT_d[:, :, :], writes=['qiT'])
            P.dma('sp', widx[:, :, :], widx_d[:, :, :], writes=['widx'])
            slot_sb = sb("slot_sb", [128, CTX], F32, pc)
            causal_sb = sb("causal_sb", [128, 128], F32, pc)
            halfs_sb = sb("halfs_sb", [128, NIT], F32, pc)
            sc = sb("sc", [128, CTX], F32, pc)
            junk = sb("junk", [128, CTX], BF16, pc)
            Rb = [sb("Rb%d" % i, [128, 512], BF16, pc) for i in range(4)]
            Dg = sb("Dg", [128, 16, 128], BF16, pc)
            dgb = sb("dgb", [128, 128], F32, pc)
            bs = sb("bs", [128, 64], F32, pc)
            thr_all = sb("thr_all", [128, NQB], F32, pc)
            pd = [ps("pdC%d" % i, [128, 512], F32, pc) for i in range(4)]
            psc = [ps("pscC%d" % i, [128, 512], F32, pc) for i in range(2)]
            ptm = [ps("ptmC%d" % i, [128, 1024], BF16, pc) for i in range(2)]
            P.dma('sp', slot_sb[:], slotb[:, :], writes=['slot_sb'])
            P.dma('sp', causal_sb[:], causal[:, :], writes=['causal_sb'])
            P.dma('sp', halfs_sb[:], halfs[:, :], writes=['halfs_sb'])
            P.op('pool', lambda: G.memset(maskT[:, :, :].rearrange("p a b -> p (a b)"), 0.0), writes=['maskT'])
            lo = bs[:, 0:1]
            hi = bs[:, 1:2]
            w0 = bs[:, 2:3]
            mid = bs[:, 3:4]
            cntv = bs[:, 4:5]
            gg = bs[:, 5:6]
            mins = bs[:, 8:16]
            hk = bs[:, 16:16 + NIT]
            dcn = 0
            scn = 0
            tcn = 0
            for i in range(NQB):
                E = QS0 + 128 * (i + 1)
                nkt = (E + 511) // 512
                for h in range(16):
                    P.op('dve', lambda h=h, i=i: V.tensor_scalar(out=Dg[:, h, :], in0=ident_f[:], scalar1=widx[:, i, h:h + 1],
                                                                  scalar2=None, op0=ALU.mult),
                         reads=['ident_f', 'widx'], writes=[('Dg', h)])
                P.op('pool', lambda E=E: G.tensor_tensor(out=dgb[:, :], in0=slot_sb[:, E - 128:E], in1=causal_sb[:, :], op=ALU.add),
                     reads=['slot_sb', 'causal_sb'], writes=['dgb'])
                for kt in range(nkt):
                    N = min(512, E - kt * 512)
                    pst = psc[scn % 2]
                    pskey = ('psc', scn % 2)
                    scn += 1
                    pend = []

                    def score_mm(h, rb, N=N, pst=pst, pskey=pskey):
                        P.op('pe', lambda: T.matmul(pst[:, :N], lhsT=Dg[:, h, :], rhs=Rb[rb][:, :N], start=(h == 0), stop=(h == 15)),
                             reads=[('Dg', h), ('Rb', rb)], writes=[pskey])
                    for h in range(16):
                        pdt = pd[dcn % 4]
                        pdk = ('pd', dcn % 4)
                        rb = dcn % 4
                        dcn += 1
                        pr = (h % 2) * 64
                        P.op('pe', lambda h=h, pdt=pdt, pr=pr, N=N, kt=kt, i=i: T.matmul(
                            pdt[:, :N], lhsT=qiT[pr:pr + 64, h // 2, i * 128:(i + 1) * 128],
                            rhs=kidxT[pr:pr + 64, kt * 512:kt * 512 + N], start=True, stop=True),
                            reads=['kidxT', 'qiT'], writes=[pdk])
                        if h % 2 == 0:
                            P.op('act', lambda pdt=pdt, rb=rb, N=N: A.activation(out=Rb[rb][:, :N], in_=pdt[:, :N], func=AF.Relu),
                                 reads=[pdk], writes=[('Rb', rb)])
                        else:
                            P.op('dve', lambda pdt=pdt, rb=rb, N=N: V.tensor_scalar(out=Rb[rb][:, :N], in0=pdt[:, :N], scalar1=0.0,
                                                                                      scalar2=None, op0=ALU.max),
                                 reads=[pdk], writes=[('Rb', rb)])
                        pend.append((h, rb))
                        if len(pend) > 2:
                            score_mm(*pend.pop(0))
                    while pend:
                        score_mm(*pend.pop(0))
                    P.op('dve', lambda pst=pst, N=N, kt=kt: V.tensor_reduce(out=mins[:, kt:kt + 1], in_=pst[:, :N], axis=AX.X, op=ALU.min),
                         reads=[pskey], writes=[('mins', kt)])
                    if kt == nkt - 1:
                        nd = N - 128
                        if nd > 0:
                            P.op('dve', lambda pst=pst, nd=nd, kt=kt: V.tensor_tensor(
                                out=sc[:, kt * 512:kt * 512 + nd], in0=pst[:, :nd], in1=slot_sb[:, kt * 512:kt * 512 + nd], op=ALU.add),
                                reads=[pskey, 'slot_sb'], writes=[('sc', kt)])
                        P.op('dve', lambda pst=pst, nd=nd, N=N, E=E: V.tensor_tensor(
                            out=sc[:, E - 128:E], in0=pst[:, nd:N], in1=dgb[:, :], op=ALU.add),
                            reads=[pskey, 'dgb'], writes=[('sc', kt)])
                    else:
                        P.op('dve', lambda pst=pst, kt=kt: V.tensor_tensor(
                            out=sc[:, kt * 512:(kt + 1) * 512], in0=pst[:, :512], in1=slot_sb[:, kt * 512:(kt + 1) * 512], op=ALU.add),
                            reads=[pskey, 'slot_sb'], writes=[('sc', kt)])
                sckeys = [('sc', kt) for kt in range(nkt)]
                P.op('dve', lambda nkt=nkt: V.tensor_reduce(out=lo, in_=mins[:, 0:nkt], axis=AX.X, op=ALU.min),
                     reads=[('mins', kt) for kt in range(nkt)], writes=['lo'])
                P.op('dve', lambda: V.tensor_scalar(out=lo, in0=lo, scalar1=-1.0, scalar2=None, op0=ALU.add), reads=['lo'], writes=['lo'])
                P.op('dve', lambda E=E: V.tensor_reduce(out=hi, in_=sc[:, 0:E], axis=AX.X, op=ALU.max), reads=sckeys, writes=['hi'])
                P.op('dve', lambda: V.tensor_scalar(out=hi, in0=hi, scalar1=1.0, scalar2=None, op0=ALU.add), reads=['hi'], writes=['hi'])
                P.op('dve', lambda: V.tensor_tensor(out=w0, in0=hi, in1=lo, op=ALU.subtract), reads=['hi', 'lo'], writes=['w0'])
                P.op('dve', lambda: V.tensor_scalar(out=hk, in0=halfs_sb[:, :], scalar1=w0, scalar2=None, op0=ALU.mult),
                     reads=['w0', 'halfs_sb'], writes=['hk'])
                for it in range(NIT):
                    P.op('dve', lambda it=it: V.tensor_tensor(out=mid, in0=lo, in1=hk[:, it:it + 1], op=ALU.add),
                         reads=['lo', 'hk'], writes=['mid'])
                    P.op('dve', lambda E=E: V.tensor_scalar(out=junk[:, 0:E], in0=sc[:, 0:E], scalar1=mid, scalar2=None,
                                                            op0=ALU.is_ge, op1=ALU.add, accum_out=cntv),
                         reads=sckeys + ['mid'], writes=['junk', 'cnt'])
                    P.op('dve', lambda it=it: V.tensor_scalar(out=gg, in0=cntv, scalar1=255.5, scalar2=hk[:, it:it + 1],
                                                               op0=ALU.is_ge, op1=ALU.mult),
                         reads=['cnt', 'hk'], writes=['gg'])
                    P.op('dve', lambda: V.tensor_tensor(out=lo, in0=lo, in1=gg, op=ALU.add), reads=['lo', 'gg'], writes=['lo'])
                P.op('dve', lambda E=E: V.tensor_scalar(out=junk[:, 0:E], in0=sc[:, 0:E], scalar1=lo, scalar2=None, op0=ALU.is_ge),
                     reads=sckeys + ['lo'], writes=['junk'])
                P.op('dve', lambda i=i: V.tensor_copy(out=thr_all[:, i:i + 1], in_=lo), reads=['lo'], writes=['thr_all'])
                nkb = E // 128
                for kb0 in range(0, nkb, 8):
                    n8 = min(8, nkb - kb0)
                    pt = ptm[tcn % 2]
                    ptk = ('ptm', tcn % 2)
                    tcn += 1
                    for r in range(n8):
                        kb = kb0 + r
                        P.op('pe', lambda pt=pt, r=r, kb=kb: T.transpose(out=pt[:, r * 128:(r + 1) * 128],
                                                                        in_=junk[:, kb * 128:(kb + 1) * 128], identity=ident_b[:]),
                             reads=['junk', 'ident_b'], writes=[ptk])
                    P.op('act', lambda pt=pt, n8=n8, kb0=kb0, i=i: A.activation(
                        out=maskT[:, kb0:kb0 + n8, i * 128:(i + 1) * 128],
                        in_=pt[:, 0:n8 * 128].rearrange("p (a b) -> p a b", b=128), func=AF.Copy),
                        reads=[ptk], writes=['maskT'])
            if debug:
                P.dma('sp', dbg['maskT'][:, :, :], maskT[:, :, :], reads=['maskT'])
                P.dma('sp', dbg['thr'][:, :], thr_all[:, :], reads=['thr_all'])
            P.barrier()
        if stop_after == 'C':
            return nc

        SCALE = 128 ** -0.5
        with contextlib.ExitStack() as pdd:
            attnT = sb("attnT", [128, 8, NQ], BF16, pdd)
            QT = sb("QTd", [128, 8, NQ], BF16, pdd)
            P.dma('sp', QT[:, :, :], QT_d[:, :, :], writes=['QT'])
            KTh = [sb("KTh%d" % i, [128, CTX], BF16, pdd) for i in range(2)]
            Vh = [sb("Vh%d" % i, [128, 32, 128], BF16, pdd) for i in range(2)]
            strip_sb = sb("strip_sb", [128, 8, 896], F32, pdd)
            cfar_sb = sb("cfar_sb", [128, 8], F32, pdd)
            Eb = [sb("Eb%d" % i, [128, 384], BF16, pdd) for i in range(3)]
            Pb = [sb("Pb%d" % i, [128, 384], BF16, pdd) for i in range(3)]
            tmpb = [sb("tmpb%d" % i, [128, 384], F32, pdd) for i in range(2)]
            rl = sb("rl", [128, 384], F32, pdd)
            pS = [ps("pS%d" % i, [128, 512], F32, pdd) for i in range(3)]
            pO = [ps("pO%d" % i, [128, 512], F32, pdd) for i in range(2)]
            pL = [ps("pL%d" % i, [128, 512], F32, pdd) for i in range(2)]
            P.dma('sp', strip_sb[:], strip[:, :, :], writes=['strip_sb'])
            P.dma('sp', cfar_sb[:], cfar[:, :], writes=['cfar_sb'])
            scn = 0
            ocn = 0
            tmc = 0
            for h in range(8):
                b = h % 2
                P.dma('sp', KTh[b][:, :], KT_d[h, :, :], writes=[('KTh', b)])
                P.dma('sp', Vh[b][:, :, :], V_d[h, :, :].rearrange("(kb p) d -> p kb d", p=128), writes=[('Vh', b)])
                for qt in range(3):
                    t0 = qt * 384
                    kbmax = 23 + 3 * qt + 2
                    po = pO[ocn % 2]
                    pl = pL[ocn % 2]
                    pok = ('pO', ocn % 2)
                    plk = ('pL', ocn % 2)
                    ocn += 1
                    staged = []

                    def stage1(kb):
                        nonlocal scn, tmc
                        s_i = scn % 3
                        scn += 1
                        pst = pS[s_i]
                        P.op('pe', lambda: T.matmul(pst[:, 0:384], lhsT=KTh[b][:, kb * 128:(kb + 1) * 128],
                                                    rhs=QT[:, h, t0:t0 + 384], start=True, stop=True),
                             reads=[('KTh', b), 'QT'], writes=[('pS', s_i)])
                        D0 = (QS0 + t0) - kb * 128
                        if D0 >= 256:
                            P.op('act', lambda: A.activation(out=Eb[s_i][:, :], in_=pst[:, 0:384], func=AF.Exp,
                                                             scale=SCALE, bias=cfar_sb[:, h:h + 1]),
                                 reads=[('pS', s_i), 'cfar_sb'], writes=[('Eb', s_i)])
                        else:
                            ti = tmc % 2
                            tmc += 1
                            P.op('dve', lambda: V.scalar_tensor_tensor(
                                out=tmpb[ti][:, :], in0=pst[:, 0:384], scalar=SCALE,
                                in1=strip_sb[:, h, D0 + 256:D0 + 256 + 384], op0=ALU.mult, op1=ALU.add),
                                reads=[('pS', s_i), 'strip_sb'], writes=[('tmpb', ti)])
                            P.op('act', lambda: A.activation(out=Eb[s_i][:, :], in_=tmpb[ti][:, :], func=AF.Exp),
                                 reads=[('tmpb', ti)], writes=[('Eb', s_i)])
                        P.op('dve', lambda: V.tensor_tensor(out=Pb[s_i][:, :], in0=Eb[s_i][:, :],
                                                            in1=maskT[:, kb, t0:t0 + 384], op=ALU.mult),
                             reads=[('Eb', s_i)], writes=[('Pb', s_i)])
                        staged.append((kb, s_i))

                    def stage2():
                        kb, s_i = staged.pop(0)
                        P.op('pe', lambda: T.matmul(po[:, 0:384], lhsT=Vh[b][:, kb, :], rhs=Pb[s_i][:, :],
                                                    start=(kb == 0), stop=(kb == kbmax)),
                             reads=[('Vh', b), ('Pb', s_i)], writes=[pok])
                        P.op('pe', lambda: T.matmul(pl[:, 0:384], lhsT=ones_b[:, :], rhs=Pb[s_i][:, :],
                                                    start=(kb == 0), stop=(kb == kbmax)),
                             reads=['ones_b', ('Pb', s_i)], writes=[plk])
                    for kb in range(kbmax + 1):
                        stage1(kb)
                        if len(staged) > 2:
                            stage2()
                    while staged:
                        stage2()
                    P.op('dve', lambda: V.tensor_scalar(out=rl[:, :], in0=pl[:, 0:384], scalar1=1e-30, scalar2=None, op0=ALU.add),
                         reads=[plk], writes=['rl'])
                    P.op('dve', lambda: V.reciprocal(out=rl[:, :], in_=rl[:, :]), reads=['rl'], writes=['rl'])
                    P.op('dve', lambda: V.tensor_tensor(out=attnT[:, h, t0:t0 + 384], in0=po[:, 0:384], in1=rl[:, :], op=ALU.mult),
                         reads=[pok, 'rl'], writes=[('attnT', h)])
            P.dma('sp', attnT_d[:, :, :], attnT[:, :, :], reads=[('attnT', h_) for h_ in range(8)])
            P.barrier()
        pcd.close()
        if stop_after == 'D':
            return nc

        with contextlib.ExitStack() as pf:
            Wo = sb("Wo", [128, 16, 2048], BF16, pf)
            ypT = sb("ypTf", [128, 8, NQ], BF16, pf)
            attnT = sb("attnTf", [128, 8, NQ], BF16, pf)
            P.dma('sp', ypT[:, :, :], ypT_d[:, :, :], writes=['ypT'])
            P.dma('sp', attnT[:, :, :], attnT_d[:, :, :], writes=['attnT'])
            xr = [sb("xrF%d" % i, [128, D], F32, pf) for i in range(2)]
            x1t = [sb("x1tF%d" % i, [128, D], F32, pf) for i in range(2)]
            pw = [ps("pwF%d" % i, [128, 512], F32, pf) for i in range(8)]
            for cg in range(4):
                wload(Wo, ('Wo', cg), w_out, 4 * cg, 4 * cg + 4, 0, 2048)
            pcn = 0
            for i in range(NQB):
                bi = i % 2
                P.dma('sp', xr[bi][:], xctx[QS0 + i * 128:QS0 + (i + 1) * 128, :], writes=[('xr', bi)])
                for nt in range(4):
                    pq = pw[pcn % 8]
                    pk = ('pw', pcn % 8)
                    pcn += 1
                    for c in range(16):
                        src = ypT if c < 8 else attnT
                        P.op('pe', lambda c=c, src=src, pq=pq, nt=nt, i=i: T.matmul(
                            pq[:, :], lhsT=src[:, c % 8, i * 128:(i + 1) * 128], rhs=Wo[:, c, nt * 512:(nt + 1) * 512],
                            start=(c == 0), stop=(c == 15)), reads=[('Wo', c // 4), 'ypT', 'attnT'], writes=[pk])
                    P.op('dve', lambda pq=pq, nt=nt, bi=bi: V.tensor_tensor(
                        out=x1t[bi][:, nt * 512:(nt + 1) * 512], in0=pq[:, :], in1=xr[bi][:, nt * 512:(nt + 1) * 512], op=ALU.add),
                        reads=[pk, ('xr', bi)], writes=[('x1t', bi)])
                P.dma('sp', x1_d[i * 128:(i + 1) * 128, :], x1t[bi][:], reads=[('x1t', bi)], writes=[('x1_d', i)])
            P.barrier()
        if stop_after == 'F':
            return nc


        pgh = contextlib.ExitStack()
        es.enter_context(pgh)
        gT = sb("gT", [128, NFC, 1024], BF16, pgh)
        with contextlib.ExitStack() as pg_:
            h2T = sb("h2T", [128, 16, NQ], BF16, pg_)
            with contextlib.ExitStack() as pg1:
                gsb = sb("gsbG", [128, 16, 128], F32, pg1)
                hf_sb = sb("hf_sb", [128, 1], F32, pg1)
                xb = [sb("xbG%d" % i, [128, D], F32, pg1) for i in range(2)]
                xn = [sb("xnG%d" % i, [128, D], BF16, pg1) for i in range(2)]
                pT = [[ps("pTG%d%d" % (i, h), [128, 1024], BF16, pg1) for h in range(2)] for i in range(2)]
                P.dma('sp', gsb[:].rearrange("p c t -> p (c t)"), gT3[1, :, :], writes=['gsb'])
                P.dma('sp', hf_sb[:], hflag[:, :], writes=['hflag'])
                for j in range(NQB):
                    i = j % 2
                    P.dma('sp', xb[i][:], x1_d[j * 128:(j + 1) * 128, :], writes=[('xb', i)])
                    norm_T(i, xb[i][:], ('xb', i), xn, pT, gsb,
                           lambda h, j=j: h2T[:, 8 * h:8 * h + 8, j * 128:(j + 1) * 128], [('h2T', j)],
                           extra=(hf_sb[:, 0:1] if j == 0 else None))
                P.barrier()
            Wgs = [sb("Wgs%d" % i, [128, 16, 256], BF16, pg_) for i in range(2)]
            Wvs = [sb("Wvs%d" % i, [128, 16, 256], BF16, pg_) for i in range(2)]
            cp = sb("cp", [128, 2 * NFC, 4], F32, pg_)
            rA = [sb("rA%d" % i, [128, 344], F32, pg_) for i in range(4)]
            rB = [sb("rB%d" % i, [128, 344], F32, pg_) for i in range(4)]
            pu = [ps("puG%d" % i, [128, 512], F32, pg_) for i in range(8)]
            P.dma('sp', cp[:], convp[:, :, :], writes=['cp'])
            tiles = [(0, 342), (342, 684), (684, 1024)]
            pcn = 0
            rcn = 0
            for jg in range(NFC // 2):
                wb = jg % 2
                wload(Wgs[wb], ('Wgs', wb), w_up, 0, 16, jg * 256, 256)
                wload(Wvs[wb], ('Wvs', wb), w_up, 0, 16, D_FF + jg * 256, 256)
                for m_ in range(2):
                    j = 2 * jg + m_
                    for (a, b_) in tiles:
                        N = b_ - a + 2
                        c0 = 128 + a - 2
                        res_ = []
                        for (Wt, wk, jj) in ((Wgs[wb], ('Wgs', wb), j), (Wvs[wb], ('Wvs', wb), NFC + j)):
                            pq = pu[pcn % 8]
                            pk = ('pu', pcn % 8)
                            pcn += 1
                            for c in range(16):
                                P.op('pe', lambda c=c, pq=pq, Wt=Wt, N=N, c0=c0: T.matmul(
                                    pq[:, 0:N], lhsT=Wt[:, c, m_ * 128:(m_ + 1) * 128], rhs=h2T[:, c, c0:c0 + N],
                                    start=(c == 0), stop=(c == 15)), reads=[wk], writes=[pk])
                            ri = rcn % 4
                            rcn += 1
                            n2 = N - 2
                            P.op('act', lambda pq=pq, ri=ri, jj=jj, N=N, n2=n2: A.activation(
                                out=rA[ri][:, 0:n2], in_=pq[:, 2:N], func=AF.Identity, scale=cp[:, jj, 2:3], bias=cp[:, jj, 3:4]),
                                reads=[pk, 'cp'], writes=[('rA', ri)])
                            P.op('dve', lambda pq=pq, ri=ri, jj=jj, N=N, n2=n2: V.scalar_tensor_tensor(
                                out=rB[ri][:, 0:n2], in0=pq[:, 1:N - 1], scalar=cp[:, jj, 1:2], in1=rA[ri][:, 0:n2],
                                op0=ALU.mult, op1=ALU.add), reads=[pk, 'cp', ('rA', ri)], writes=[('rB', ri)])
                            P.op('dve', lambda pq=pq, ri=ri, jj=jj, N=N, n2=n2: V.scalar_tensor_tensor(
                                out=rA[ri][:, 0:n2], in0=pq[:, 0:n2], scalar=cp[:, jj, 0:1], in1=rB[ri][:, 0:n2],
                                op0=ALU.mult, op1=ALU.add), reads=[pk, 'cp', ('rB', ri)], writes=[('rA', ri)])
                            res_.append(ri)
                        rg, rv = res_
                        n2 = N - 2
                        P.op('act', lambda rg=rg, n2=n2: A.activation(out=rB[rg][:, 0:n2], in_=rA[rg][:, 0:n2], func=AF.Silu),
                             reads=[('rA', rg)], writes=[('rB', rg)])
                        P.op('pool', lambda rg=rg, rv=rv, n2=n2, j=j, a=a, b_=b_: G.tensor_tensor(
                            out=gT[:, j, a:b_], in0=rB[rg][:, 0:n2], in1=rA[rv][:, 0:n2], op=ALU.mult),
                            reads=[('rB', rg), ('rA', rv)], writes=[('gT', j)])
            if debug:
                P.dma('sp', dbg['gT'][:, :, :], gT[:, :, :], reads=[('gT', j_) for j_ in range(NFC)])
            P.barrier()
        if stop_after == 'G':
            return nc

        with contextlib.ExitStack() as ph:
            Wds = [sb("Wds%d" % i, [128, 4, 512], BF16, ph) for i in range(3)]
            x1s = [sb("x1s%d" % i, [128, 512], F32, ph) for i in range(4)]
            x2s = [sb("x2s%d" % i, [128, 512], F32, ph) for i in range(4)]
            pw = [ps("pwH%d" % i, [128, 512], F32, ph) for i in range(8)]
            wcn = 0
            scn = 0
            pset = 0
            for tg in range(2):
                for nt in range(4):
                    banks = [(pw[4 * (pset % 2) + tb], ('pw', 4 * (pset % 2) + tb)) for tb in range(4)]
                    pset += 1
                    for fg in range(NFC // 4):
                        wb = wcn % 3
                        wcn += 1
                        P.dma('pool', Wds[wb][:, :, :],
                              w_down[fg * 512:(fg + 1) * 512, nt * 512:(nt + 1) * 512].rearrange("(c p) n -> p c n", p=128),
                              writes=[('Wds', wb)])
                        for fl in range(4):
                            f = fg * 4 + fl
                            for tb in range(4):
                                pq, pk = banks[tb]
                                tok0 = (tg * 4 + tb) * 128
                                P.op('pe', lambda pq=pq, f=f, fl=fl, wb=wb, tok0=tok0: T.matmul(
                                    pq[:, :], lhsT=gT[:, f, tok0:tok0 + 128], rhs=Wds[wb][:, fl, :],
                                    start=(f == 0), stop=(f == NFC - 1)), reads=[('Wds', wb)], writes=[pk])
                    for tb in range(4):
                        pq, pk = banks[tb]
                        si = scn % 4
                        scn += 1
                        r0 = (tg * 4 + tb) * 128
                        P.dma('sp', x1s[si][:, :], x1_d[128 + r0:128 + r0 + 128, nt * 512:(nt + 1) * 512], writes=[('x1s', si)])
                        P.op('dve', lambda pq=pq, si=si: V.tensor_tensor(out=x2s[si][:, :], in0=pq[:, :], in1=x1s[si][:, :], op=ALU.add),
                             reads=[pk, ('x1s', si)], writes=[('x2s', si)])
                        P.dma('sp', x2_d[r0:r0 + 128, nt * 512:(nt + 1) * 512], x2s[si][:, :], reads=[('x2s', si)], writes=[('x2_d', r0, nt)])
            P.barrier()
        pgh.close()
        if stop_after == 'H':
            return nc

        with contextlib.ExitStack() as pi:
            x3 = sb("x3", [128, 8, D], F32, pi)
            h3T = sb("h3T", [128, 16, 1024], BF16, pi)
            ppT = sb("ppT", [128, 2, 1024], BF16, pi)
            with contextlib.ExitStack() as pi1:
                gsb = sb("gsbI", [128, 16, 128], F32, pi1)
                p_sb = sb("p_sb", [128, 8, 256], F32, pi1)
                p_bf = sb("p_bf", [128, 8, 256], BF16, pi1)
                xn = [sb("xnI%d" % i, [128, D], BF16, pi1) for i in range(2)]
                pT = [[ps("pTI%d%d" % (i, h), [128, 1024], BF16, pi1) for h in range(2)] for i in range(2)]
                ptp = [ps("ptpI%d" % i, [128, 1024], BF16, pi1) for i in range(2)]
                P.dma('sp', gsb[:].rearrange("p c t -> p (c t)"), gT3[2, :, :], writes=['gsb'])
                P.dma('sp', p_sb[:, :, :], p_own.rearrange("(tb p) c -> p tb c", p=128), writes=['p_sb'])
                P.op('pool', lambda: G.tensor_copy(out=p_bf[:, :, :], in_=p_sb[:, :, :]), reads=['p_sb'], writes=['p_bf'])
                for tb in range(8):
                    P.dma('sp', x3[:, tb, :], x2_d[tb * 128:(tb + 1) * 128, :], writes=[('x3', tb)])
                for tb in range(8):
                    i = tb % 2
                    norm_T(i, x3[:, tb, :], ('x3', tb), xn, pT, gsb,
                           lambda h, tb=tb: h3T[:, 8 * h:8 * h + 8, tb * 128:(tb + 1) * 128], [('h3T', tb)])
                    pt = ptp[i]
                    for cc in range(2):
                        P.op('pe', lambda pt=pt, cc=cc, tb=tb: T.transpose(out=pt[:, cc * 128:(cc + 1) * 128],
                                                                          in_=p_bf[:, tb, cc * 128:(cc + 1) * 128], identity=ident_b[:]),
                             reads=['p_bf', 'ident_b'], writes=[('ptp', i)])
                    P.op('act', lambda pt=pt, tb=tb: A.activation(out=ppT[:, :, tb * 128:(tb + 1) * 128],
                                                                  in_=pt[:, 0:256].rearrange("p (a b) -> p a b", b=128), func=AF.Copy),
                         reads=[('ptp', i)], writes=[('ppT', tb)])
                P.barrier()
            with contextlib.ExitStack() as pi2:
                Wgs = [sb("WgsI%d" % i, [128, 16, 512], BF16, pi2) for i in range(2)]
                Wps = [sb("WpsI%d" % i, [128, 2, 512], BF16, pi2) for i in range(2)]
                sgm = [sb("sgm%d" % i, [128, 512], F32, pi2) for i in range(2)]
                tmpI = [sb("tmpI%d" % i, [128, 512], F32, pi2) for i in range(2)]
                pgt = [ps("pgtI%d" % i, [128, 512], F32, pi2) for i in range(4)]
                ppe = [ps("ppeI%d" % i, [128, 512], F32, pi2) for i in range(4)]
                cn = 0
                for nt in range(4):
                    wb = nt % 2
                    for cg in range(4):
                        P.dma('pool', Wgs[wb][:, 4 * cg:4 * cg + 4, :],
                              w_pg[cg * 512:(cg + 1) * 512, nt * 512:(nt + 1) * 512].rearrange("(c p) n -> p c n", p=128),
                              writes=[('WgsI', wb, cg)])
                    P.dma('pool', Wps[wb][:, :, :], w_pp[:, nt * 512:(nt + 1) * 512].rearrange("(c p) n -> p c n", p=128),
                          writes=[('WpsI', wb)])
                    for tb in range(8):
                        bi = cn % 4
                        si = cn % 2
                        cn += 1
                        for c in range(16):
                            P.op('pe', lambda c=c, bi=bi, tb=tb, wb=wb: T.matmul(
                                pgt[bi][:, :], lhsT=h3T[:, c, tb * 128:(tb + 1) * 128], rhs=Wgs[wb][:, c, :],
                                start=(c == 0), stop=(c == 15)), reads=[('WgsI', wb, c // 4)], writes=[('pgt', bi)])
                        for cc in range(2):
                            P.op('pe', lambda cc=cc, bi=bi, tb=tb, wb=wb: T.matmul(
                                ppe[bi][:, :], lhsT=ppT[:, cc, tb * 128:(tb + 1) * 128], rhs=Wps[wb][:, cc, :],
                                start=(cc == 0), stop=(cc == 1)), reads=[('WpsI', wb)], writes=[('ppe', bi)])
                        P.op('act', lambda bi=bi, si=si: A.activation(out=sgm[si][:, :], in_=pgt[bi][:, :], func=AF.Sigmoid),
                             reads=[('pgt', bi)], writes=[('sgm', si)])
                        P.op('dve', lambda bi=bi, si=si: V.tensor_tensor(out=tmpI[si][:, :], in0=ppe[bi][:, :], in1=sgm[si][:, :], op=ALU.mult),
                             reads=[('ppe', bi), ('sgm', si)], writes=[('tmpI', si)])
                        P.op('pool', lambda si=si, tb=tb, nt=nt: G.tensor_tensor(
                            out=x3[:, tb, nt * 512:(nt + 1) * 512], in0=x3[:, tb, nt * 512:(nt + 1) * 512], in1=tmpI[si][:, :], op=ALU.add),
                            reads=[('tmpI', si)], writes=[('x3o', tb)])
                P.barrier()
            with contextlib.ExitStack() as pi3:
                gf = sb("gf_sb", [128, D], F32, pi3)
                ot = [sb("ot%d" % i, [128, D], F32, pi3) for i in range(2)]
                jk = sb("jkI", [128, D], BF16, pi3)
                P.dma('sp', gf[:, :], gfin[:, :], writes=['gf'])
                for tb in range(8):
                    i = tb % 2
                    ss = st[:, 3 * i:3 * i + 1]
                    sd = st[:, 3 * i + 1:3 * i + 2]
                    rs = st[:, 3 * i + 2:3 * i + 3]
                    P.op('act', lambda tb=tb, ss=ss: A.activation(out=jk[:, :], in_=x3[:, tb, :], func=AF.Square, accum_out=ss),
                         writes=['jk', ('ss', i)])
                    P.op('dve', lambda ss=ss, sd=sd: V.tensor_scalar(out=sd, in0=ss, scalar1=1.0 / D, scalar2=EPS, op0=ALU.mult, op1=ALU.add),
                         reads=[('ss', i)], writes=[('sd', i)])
                    P.op('act', lambda sd=sd: A.activation(out=sd, in_=sd, func=AF.Sqrt), reads=[('sd', i)], writes=[('sd', i)])
                    P.op('dve', lambda sd=sd, rs=rs: V.reciprocal(out=rs, in_=sd), reads=[('sd', i)], writes=[('rs', i)])
                    P.op('dve', lambda tb=tb, rs=rs, i=i: V.scalar_tensor_tensor(
                        out=ot[i][:, :], in0=x3[:, tb, :], scalar=rs, in1=gf[:, :], op0=ALU.mult, op1=ALU.mult),
                        reads=[('rs', i), 'gf'], writes=[('ot', i)])
                    P.dma('sp', out[tb * 128:(tb + 1) * 128, :], ot[i][:, :], reads=[('ot', i)], writes=[('out', tb)])
                P.barrier()
    return nc


def _t5_bucket_static(dist):
    n = np.maximum(dist, 0)
    nf = np.maximum(n, 1).astype(np.float32)
    large = 16 + (np.log(nf / np.float32(16)) / np.float32(math.log(128 / 16)) * 16).astype(np.int32)
    large = np.minimum(large, 31)
    return np.where(n < 16, n, large)


def prep_inputs(x, p, g_mix, w_in, w_pool, pool_scale, rel_bias, w_out, g_ffn, w_up, conv_w, conv_b,
                w_down, g_ple, w_ple_gate, w_ple_proj, g_final):
    f = np.float32
    x = np.asarray(x, f)
    p = np.asarray(p, f)[0]
    shared = {}
    shared["w_in"] = np.ascontiguousarray(np.asarray(w_in, f)[0])
    shared["w_pool"] = np.ascontiguousarray(np.asarray(w_pool, f)[0])
    shared["w_out"] = np.ascontiguousarray(np.asarray(w_out, f)[0])
    shared["w_up"] = np.ascontiguousarray(np.asarray(w_up, f)[0])
    shared["w_down"] = np.ascontiguousarray(np.asarray(w_down, f)[0])
    shared["w_ple_gate"] = np.ascontiguousarray(np.asarray(w_ple_gate, f)[0])
    shared["w_ple_proj"] = np.ascontiguousarray(np.asarray(w_ple_proj, f)[0])

    def fm(v):
        a = np.asarray(v, f).reshape(16, 128).T
        return np.ascontiguousarray(np.repeat(a[:, :, None], 128, axis=2).reshape(128, 2048))
    shared["gT3"] = np.stack([fm(np.asarray(g_mix)[0]), fm(np.asarray(g_ffn)[0]), fm(np.asarray(g_ple)[0])], 0)
    shared["gfin"] = np.ascontiguousarray(np.repeat(np.asarray(g_final, f)[None, :], 128, axis=0))
    shared["pscale"] = np.ascontiguousarray(np.asarray(pool_scale, f)[0].reshape(8, 128).T)
    cw = np.asarray(conv_w, f)[0]
    cb = np.asarray(conv_b, f)[0]
    cp = np.concatenate([cw, cb[None, :]], 0)
    shared["convp"] = np.ascontiguousarray(cp.reshape(4, 2 * NFC, 128).transpose(2, 1, 0))
    rb = np.asarray(rel_bias, f)
    sl = np.arange(128)[:, None]
    dl = np.arange(896)[None, :] - 256
    dist = dl - sl
    bk = _t5_bucket_static(dist)
    stripv = rb[bk]
    shared["strip"] = np.ascontiguousarray(stripv.transpose(0, 2, 1))
    shared["cfar"] = np.ascontiguousarray(np.repeat(rb[31][None, :], 128, axis=0))
    tl = np.arange(128)[:, None]
    s_l = np.arange(128)[None, :]
    shared["causal"] = np.where(s_l <= tl, 0.0, NEG).astype(f)
    shared["ident"] = np.eye(128, dtype=f)
    shared["halfs"] = np.ascontiguousarray(np.repeat((0.5 ** np.arange(1, NIT + 1, dtype=np.float64)).astype(f)[None, :], 128, 0))
    in_maps = []
    for c in range(8):
        b, j = c // 4, c % 4
        T0 = j * 1024
        m = dict(shared)
        xc = np.zeros((CTX, D), f)
        lo = T0 - 3072
        s0 = max(0, -lo)
        xc[s0:] = x[b, lo + s0:T0 + 1024]
        m["xctx"] = xc
        m["p_own"] = np.ascontiguousarray(p[b, T0:T0 + 1024])
        sbias = np.zeros((CTX,), f)
        sbias[:s0] = NEG
        m["slotb"] = np.ascontiguousarray(np.repeat(sbias[None, :], 128, axis=0))
        ic = np.zeros((128, 8, 16), f)
        for k in range(8):
            w = (2, 4, 8, 16)[k // 2]
            tok = T0 + np.arange(16)
            cntv = np.minimum(tok + 1, w).astype(f)
            ic[:, k, :] = (np.float32(1.0) / cntv)[None, :]
        m["invc"] = ic
        m["hflag"] = np.full((128, 1), 1.0 if j > 0 else 0.0, f)
        in_maps.append(m)
    return in_maps


_NC_CACHE = {}


def kernel(**inputs):
    in_maps = prep_inputs(**inputs)
    if 'nc' not in _NC_CACHE:
        _NC_CACHE['nc'] = build()
    nc = _NC_CACHE['nc']
    res = run_bass_kernel_spmd(nc, in_maps, core_ids=list(range(8)))
    outs = [np.asarray(res.results[c]["out"], np.float32).reshape(1024, D) for c in range(8)]
    full = np.zeros((2, SEQ, D), np.float32)
    for c in range(8):
        full[c // 4, (c % 4) * 1024:(c % 4 + 1) * 1024] = outs[c]
    return full
```

```python
import contextlib
import math
import numpy as np
import concourse.bass as bass
import concourse.mybir as mybir
from concourse.bass_utils import run_bass_kernel_spmd

F32 = mybir.dt.float32
BF16 = mybir.dt.bfloat16
AF = mybir.ActivationFunctionType
ALU = mybir.AluOpType
AX = mybir.AxisListType

D = 2048
SEQ = 4096
CTX = 4096
NQB = 9
QS0 = 2944
NQ = NQB * 128
HM0 = 2816
NHM = 1280
D_FF = 5632
NFC = 44
EPS = 1e-6
NEG = -1.0e30
NIT = 16
OFF = dict(pool=0, q=1024, k=2048, v=3072, qi=4096, ki=5120, wi=5184)


class Prog:
    NDS = 12

    def __init__(self, nc, es, same_sync=True):
        self.nc = nc
        self.same_sync = same_sync
        self.eng = {'pe': nc.tensor, 'act': nc.scalar, 'dve': nc.vector, 'pool': nc.gpsimd, 'sp': nc.sync}
        self.csem = {e: es.enter_context(nc.semaphore('c_' + e)) for e in ['pe', 'act', 'dve', 'pool']}
        self.cnt = {e: 0 for e in self.csem}
        self.dsem = {q: [es.enter_context(nc.semaphore('d_%s%d' % (q, i))) for i in range(self.NDS)]
                     for q in ['sp', 'pool']}
        self.duse = {q: [0] * self.NDS for q in self.dsem}
        self.dnext = {q: 0 for q in self.dsem}
        self.seen = {e: {} for e in self.eng}
        self.lastw = {}
        self.rds = {}
        self.nwait = 0

    def _wait(self, e, tok):
        sem, val, sid = tok
        if sid == e and (e == 'pe' or not self.same_sync):
            return
        if self.seen[e].get(sid, 0) >= val:
            return
        self.eng[e].wait_ge(sem, val)
        self.seen[e][sid] = val
        self.nwait += 1

    def _deps(self, reads, writes):
        deps = {}

        def add(tok):
            sid = tok[2]
            if sid not in deps or deps[sid][1] < tok[1]:
                deps[sid] = tok
        for k in reads:
            if k in self.lastw:
                add(self.lastw[k])
        for k in writes:
            if k in self.lastw:
                add(self.lastw[k])
            for tok in self.rds.get(k, {}).values():
                add(tok)
        return deps.values()

    def _record(self, tok, reads, writes):
        sid = tok[2]
        for k in reads:
            d = self.rds.setdefault(k, {})
            if sid not in d or d[sid][1] < tok[1]:
                d[sid] = tok
        for k in writes:
            self.lastw[k] = tok
            self.rds[k] = {}

    def op(self, e, fn, reads=(), writes=()):
        for tok in self._deps(reads, writes):
            self._wait(e, tok)
        ins = fn()
        self.cnt[e] += 1
        ins.then_inc(self.csem[e], 1)
        self._record((self.csem[e], self.cnt[e], e), reads, writes)

    def dma(self, q, out, in_, reads=(), writes=(), **kw):
        for tok in self._deps(reads, writes):
            self._wait(q, tok)
        k = self.dnext[q]
        self.dnext[q] = (k + 1) % self.NDS
        u = self.duse[q][k]
        sem = self.dsem[q][k]
        if u > 0:
            self._wait(q, (sem, 16 * u, (q, k)))
        ins = self.eng[q].dma_start(out=out, in_=in_, **kw)
        ins.then_inc(sem, 16)
        self.duse[q][k] = u + 1
        self._record((sem, 16 * (u + 1), (q, k)), reads, writes)

    def barrier(self):
        for e in self.eng:
            for c in self.csem:
                if self.cnt[c] > 0:
                    self._wait_force(e, (self.csem[c], self.cnt[c], c))
            for q in self.dsem:
                for k in range(self.NDS):
                    if self.duse[q][k] > 0:
                        self._wait_force(e, (self.dsem[q][k], 16 * self.duse[q][k], (q, k)))
        self.lastw.clear()
        self.rds.clear()

    def _wait_force(self, e, tok):
        sem, val, sid = tok
        if self.seen[e].get(sid, 0) >= val:
            return
        self.eng[e].wait_ge(sem, val)
        self.seen[e][sid] = val
        self.nwait += 1


def build(debug=False, stop_after='Z'):
    nc = bass.Bass("TRN2", target_bir_lowering=False)

    def din(name, shape, dt=F32):
        return nc.dram_tensor(name, list(shape), dt, kind="ExternalInput").ap()

    xctx = din("xctx", [CTX, D])
    p_own = din("p_own", [1024, 256])
    w_in = din("w_in", [D, 5200])
    w_pool = din("w_pool", [4, 256, 256])
    w_out = din("w_out", [D, D])
    w_up = din("w_up", [D, 2 * D_FF])
    w_down = din("w_down", [D_FF, D])
    w_pg = din("w_ple_gate", [D, D])
    w_pp = din("w_ple_proj", [256, D])
    gT3 = din("gT3", [3, 128, 2048])
    gfin = din("gfin", [128, 2048])
    pscale = din("pscale", [128, 8])
    convp = din("convp", [128, 2 * NFC, 4])
    strip = din("strip", [128, 8, 896])
    cfar = din("cfar", [128, 8])
    slotb = din("slotb", [128, CTX])
    causal = din("causal", [128, 128])
    ident = din("ident", [128, 128])
    invc = din("invc", [128, 8, 16])
    hflag = din("hflag", [128, 1])
    halfs = din("halfs", [128, NIT + 1])
    out = nc.dram_tensor("out", [1024, D], F32, kind="ExternalOutput").ap()
    ks = "ExternalOutput" if debug else "Internal"
    KT_d = nc.dram_tensor("KT_d", [8, 128, CTX], BF16, kind=ks).ap()
    V_d = nc.dram_tensor("V_d", [8, CTX, 128], BF16, kind=ks).ap()
    x1_d = nc.dram_tensor("x1_d", [NQ, D], F32, kind=ks).ap()
    x2_d = nc.dram_tensor("x2_d", [1024, D], F32, kind=ks).ap()
    kidxT_d = nc.dram_tensor("kidxT_d", [128, CTX], BF16, kind=ks).ap()
    QT_d = nc.dram_tensor("QT_d", [128, 8, NQ], BF16, kind=ks).ap()
    qiT_d = nc.dram_tensor("qiT_d", [128, 8, NQ], BF16, kind=ks).ap()
    ypT_d = nc.dram_tensor("ypT_d", [128, 8, NQ], BF16, kind=ks).ap()
    attnT_d = nc.dram_tensor("attnT_d", [128, 8, NQ], BF16, kind=ks).ap()
    widx_d = nc.dram_tensor("widx_d", [128, NQB, 16], F32, kind=ks).ap()
    dbg = {}
    if debug:
        dbg['maskT'] = nc.dram_tensor("dbg_maskT", [128, 32, NQ], BF16, kind="ExternalOutput").ap()
        dbg['thr'] = nc.dram_tensor("dbg_thr", [128, NQB], F32, kind="ExternalOutput").ap()
        dbg['gT'] = nc.dram_tensor("dbg_gT", [128, NFC, 1024], BF16, kind="ExternalOutput").ap()

    with contextlib.ExitStack() as es:
        P = Prog(nc, es)
        T = nc.tensor
        A = nc.scalar
        V = nc.vector
        G = nc.gpsimd

        def sb(name, shape, dt, stack=es):
            return stack.enter_context(nc.sbuf_tensor(name, list(shape), dt))

        def ps(name, shape, dt, stack):
            return stack.enter_context(nc.psum_tensor(name, list(shape), dt))

        ident_f = sb("ident_f", [128, 128], F32)
        ident_b = sb("ident_b", [128, 128], BF16)
        ones_b = sb("ones_b", [128, 128], BF16)
        st = sb("st", [128, 16], F32)
        P.dma('sp', ident_f[:], ident[:, :], writes=['ident_f'])
        P.op('dve', lambda: V.tensor_copy(out=ident_b[:], in_=ident_f[:]), reads=['ident_f'], writes=['ident_b'])
        P.op('dve', lambda: V.memset(ones_b[:], 1.0), writes=['ones_b'])

        def alloc(name, shape, dt):
            cm = nc.sbuf_tensor(name, list(shape), dt)
            return cm, cm.__enter__()

        def norm_T(i, xin, xin_key, xn, pT, gsb, dst_fn, dst_keys, extra=None):
            ss = st[:, 3 * i:3 * i + 1]
            sd = st[:, 3 * i + 1:3 * i + 2]
            rs = st[:, 3 * i + 2:3 * i + 3]
            P.op('act', lambda: A.activation(out=xn[i][:], in_=xin, func=AF.Square, accum_out=ss),
                 reads=[xin_key], writes=[('xn', i), ('ss', i)])
            P.op('dve', lambda: V.tensor_scalar(out=sd, in0=ss, scalar1=1.0 / D, scalar2=EPS, op0=ALU.mult, op1=ALU.add),
                 reads=[('ss', i)], writes=[('sd', i)])
            P.op('act', lambda: A.activation(out=sd, in_=sd, func=AF.Sqrt), reads=[('sd', i)], writes=[('sd', i)])
            P.op('dve', lambda: V.reciprocal(out=rs, in_=sd), reads=[('sd', i)], writes=[('rs', i)])
            if extra is not None:
                P.op('dve', lambda: V.tensor_tensor(out=rs, in0=rs, in1=extra, op=ALU.mult),
                     reads=[('rs', i), 'hflag'], writes=[('rs', i)])
            P.op('dve', lambda: V.tensor_scalar(out=xn[i][:], in0=xin, scalar1=rs, scalar2=None, op0=ALU.mult),
                 reads=[xin_key, ('rs', i)], writes=[('xn', i)])
            for c in range(16):
                P.op('pe', lambda c=c: T.transpose(out=pT[i][c // 8][:, (c % 8) * 128:(c % 8 + 1) * 128],
                                                   in_=xn[i][:, c * 128:(c + 1) * 128], identity=ident_b[:]),
                     reads=[('xn', i), 'ident_b'], writes=[('pT', i, c // 8)])
            for h in range(2):
                P.op('dve', lambda h=h: V.tensor_tensor(
                    out=dst_fn(h), in0=pT[i][h][:, :].rearrange("p (c t) -> p c t", t=128),
                    in1=gsb[:, 8 * h:8 * h + 8, :], op=ALU.mult),
                    reads=[('pT', i, h), 'gsb'], writes=dst_keys)

        def wload(dst, dkey, src_rows, c0, c1, col0, ncol, q='pool'):
            P.dma(q, dst[:, c0:c1, 0:ncol],
                  src_rows[c0 * 128:c1 * 128, col0:col0 + ncol].rearrange("(c p) n -> p c n", p=128),
                  writes=[dkey])

        with contextlib.ExitStack() as pa:
            kidxT = sb("kidxT", [128, CTX], BF16, pa)
            Wk = sb("Wk", [128, 16, 1024], BF16, pa)
            Wv = sb("Wv", [128, 16, 1024], BF16, pa)
            Wki = sb("Wki", [128, 16, 128], BF16, pa)
            gsb = sb("gsbA", [128, 16, 128], F32, pa)
            xb = [sb("xbA%d" % i, [128, D], F32, pa) for i in range(2)]
            xn = [sb("xnA%d" % i, [128, D], BF16, pa) for i in range(2)]
            hT = [sb("hTA%d" % i, [128, 16, 512], BF16, pa) for i in range(2)]
            KTst = [sb("KTst%d" % i, [128, 8, 512], BF16, pa) for i in range(2)]
            Vst = [sb("Vst%d" % i, [128, 4, 1024], BF16, pa) for i in range(2)]
            pT = [[ps("pTA%d%d" % (i, h), [128, 1024], BF16, pa) for h in range(2)] for i in range(2)]
            pm = [ps("pmA%d" % i, [128, 512], F32, pa) for i in range(4)]

            P.dma('sp', gsb[:].rearrange("p c t -> p (c t)"), gT3[0, :, :], writes=['gsb'])
            for cg in range(4):
                wload(Wk, ('Wk', cg), w_in, 4 * cg, 4 * cg + 4, OFF['k'], 1024)
            for cg in range(4):
                wload(Wv, ('Wv', cg), w_in, 4 * cg, 4 * cg + 4, OFF['v'], 1024)
            P.dma('pool', Wki[:, :, 0:64], w_in[:, OFF['ki']:OFF['ki'] + 64].rearrange("(c p) n -> p c n", p=128),
                  writes=['Wki'])
            P.dma('pool', Wki[:, :, 64:128], w_in[:, OFF['ki']:OFF['ki'] + 64].rearrange("(c p) n -> p c n", p=128),
                  writes=['Wki'])
            pmi = 0
            for ct in range(CTX // 512):
                b = ct % 2
                for sbl in range(4):
                    j = ct * 4 + sbl
                    i = j % 2
                    P.dma('sp', xb[i][:], xctx[j * 128:(j + 1) * 128, :], writes=[('xb', i)])
                    norm_T(i, xb[i][:], ('xb', i), xn, pT, gsb,
                           lambda h, b=b, sbl=sbl: hT[b][:, 8 * h:8 * h + 8, sbl * 128:(sbl + 1) * 128],
                           [('hT', b, sbl)])
                hkeys = [('hT', b, s_) for s_ in range(4)]
                for h in range(8):
                    pq = pm[pmi % 4]
                    pk = ('pm', pmi % 4)
                    pmi += 1
                    for c in range(16):
                        P.op('pe', lambda c=c, h=h, pq=pq: T.matmul(pq[:, :], lhsT=Wk[:, c, h * 128:(h + 1) * 128],
                                                                    rhs=hT[b][:, c, :], start=(c == 0), stop=(c == 15)),
                             reads=hkeys + [('Wk', c // 4)], writes=[pk])
                    P.op('act', lambda h=h, pq=pq: A.activation(out=KTst[b][:, h, :], in_=pq[:, :], func=AF.Copy),
                         reads=[pk], writes=[('KTst', b)])
                pq = pm[pmi % 4]
                pk = ('pm', pmi % 4)
                pmi += 1
                for c in range(16):
                    P.op('pe', lambda c=c, pq=pq: T.matmul(pq[:, :], lhsT=Wki[:, c, :], rhs=hT[b][:, c, :],
                                                           start=(c == 0), stop=(c == 15)),
                         reads=hkeys + ['Wki'], writes=[pk])
                P.op('act', lambda pq=pq, ct=ct: A.activation(out=kidxT[:, ct * 512:(ct + 1) * 512], in_=pq[:, :], func=AF.Copy),
                     reads=[pk], writes=[('kidxT', ct)])
                for sbl in range(4):
                    for hf in range(2):
                        pq = pm[pmi % 4]
                        pk = ('pm', pmi % 4)
                        pmi += 1
                        for c in range(16):
                            P.op('pe', lambda c=c, pq=pq, sbl=sbl, hf=hf: T.matmul(
                                pq[:, :], lhsT=hT[b][:, c, sbl * 128:(sbl + 1) * 128],
                                rhs=Wv[:, c, hf * 512:(hf + 1) * 512], start=(c == 0), stop=(c == 15)),
                                reads=[('hT', b, sbl), ('Wv', c // 4)], writes=[pk])
                        eng = 'act' if hf == 0 else 'dve'
                        if eng == 'act':
                            P.op('act', lambda pq=pq, sbl=sbl, hf=hf: A.activation(
                                out=Vst[b][:, sbl, hf * 512:(hf + 1) * 512], in_=pq[:, :], func=AF.Copy),
                                reads=[pk], writes=[('Vst', b)])
                        else:
                            P.op('dve', lambda pq=pq, sbl=sbl, hf=hf: V.tensor_copy(
                                out=Vst[b][:, sbl, hf * 512:(hf + 1) * 512], in_=pq[:, :]),
                                reads=[pk], writes=[('Vst', b)])
                P.dma('sp', KT_d[:, :, ct * 512:(ct + 1) * 512].rearrange("h d s -> d h s"), KTst[b][:, :, :],
                      reads=[('KTst', b)], writes=[('KT_d', ct)])
                for sbl in range(4):
                    r0 = ct * 512 + sbl * 128
                    P.dma('sp', V_d[:, r0:r0 + 128, :].rearrange("h p d -> p h d"),
                          Vst[b][:, sbl, :].rearrange("p (h d) -> p h d", d=128),
                          reads=[('Vst', b)], writes=[('V_d', ct)])
            P.dma('sp', kidxT_d[:, :], kidxT[:, :], reads=[('kidxT', c_) for c_ in range(8)])
            P.barrier()
        if stop_after == 'A':
            return nc


        with contextlib.ExitStack() as pb:
            QT = sb("QT", [128, 8, NQ], BF16, pb)
            qiT = sb("qiT", [128, 8, NQ], BF16, pb)
            ypT = sb("ypT", [128, 8, NQ], BF16, pb)
            widx = sb("widx", [128, NQB, 16], F32, pb)
            hM = sb("hM", [128, 16, NHM], BF16, pb)
            with contextlib.ExitStack() as pb1:
                gsb = sb("gsbB", [128, 16, 128], F32, pb1)
                xb = [sb("xbB%d" % i, [128, D], F32, pb1) for i in range(2)]
                xn = [sb("xnB%d" % i, [128, D], BF16, pb1) for i in range(2)]
                pT = [[ps("pTB%d%d" % (i, h), [128, 1024], BF16, pb1) for h in range(2)] for i in range(2)]
                P.dma('sp', gsb[:].rearrange("p c t -> p (c t)"), gT3[0, :, :], writes=['gsb'])
                for j in range(NHM // 128):
                    i = j % 2
                    P.dma('sp', xb[i][:], xctx[HM0 + j * 128:HM0 + (j + 1) * 128, :], writes=[('xb', i)])
                    norm_T(i, xb[i][:], ('xb', i), xn, pT, gsb,
                           lambda h, j=j: hM[:, 8 * h:8 * h + 8, j * 128:(j + 1) * 128], [('hM', j)])
                P.barrier()
            Ws = [sb("WsB%d" % i, [128, 16, 512], BF16, pb) for i in range(2)]
            pp = [ps("ppB%d" % i, [128, 4, 512], F32, pb) for i in range(2)]
            Wpl = sb("Wpl", [128, 4, 2, 256], BF16, pb)
            Ww = sb("Ww", [128, 16, 16], BF16, pb)
            psc_sb = sb("pscale_sb", [128, 8], F32, pb)
            invc_sb = sb("invc_sb", [128, 8, 16], F32, pb)
            pooledT = sb("pooledT", [128, 8, NQ], BF16, pb)
            uk = [sb("uk%d" % i, [128, 1168], F32, pb) for i in range(2)]
            sA = sb("sA", [128, 1168], F32, pb)
            sB = sb("sB", [128, 1168], F32, pb)
            t16 = sb("t16", [128, 16], F32, pb)
            P.dma('sp', psc_sb[:], pscale[:, :], writes=['pscale'])
            P.dma('sp', invc_sb[:], invc[:, :, :], writes=['invc'])
            for g in range(4):
                P.dma('pool', Wpl[:, g, :, :], w_pool[g, :, :].rearrange("(cc p) d -> p cc d", p=128), writes=['Wpl'])
            P.dma('pool', Ww[:, :, :], w_in[:, OFF['wi']:OFF['wi'] + 16].rearrange("(c p) n -> p c n", p=128), writes=['Ww'])
            wcnt = [0]
            pcnt = [0]

            def next_w(col0):
                b = wcnt[0] % 2
                wcnt[0] += 1
                wload(Ws[b], ('Ws', b), w_in, 0, 16, col0, 512)
                return b

            def next_pp():
                b = pcnt[0] % 2
                pcnt[0] += 1
                return b

            for (dst, off) in ((QT, OFF['q']), (qiT, OFF['qi'])):
                for g in range(2):
                    wb = next_w(off + 512 * g)
                    for m_ in range(4):
                        pb_ = next_pp()
                        for n in range(3):
                            for c in range(16):
                                P.op('pe', lambda c=c, n=n, wb=wb, m_=m_, pb_=pb_: T.matmul(
                                    pp[pb_][:, n, 0:384], lhsT=Ws[wb][:, c, m_ * 128:(m_ + 1) * 128],
                                    rhs=hM[:, c, 128 + n * 384:128 + (n + 1) * 384], start=(c == 0), stop=(c == 15)),
                                    reads=[('Ws', wb)], writes=[('pp', pb_)])
                        P.op('act', lambda dst=dst, g=g, m_=m_, pb_=pb_: A.activation(
                            out=dst[:, 4 * g + m_, :].rearrange("p (n t) -> p n t", t=384),
                            in_=pp[pb_][:, 0:3, 0:384], func=AF.Copy),
                            reads=[('pp', pb_)], writes=[('fm', id(dst), 4 * g + m_)])
            for g2 in range(2):
                wb = next_w(OFF['pool'] + 512 * g2)
                for m_ in range(4):
                    k = 4 * g2 + m_
                    g = k // 2
                    pb_ = next_pp()
                    for n in range(4):
                        for c in range(16):
                            P.op('pe', lambda c=c, n=n, wb=wb, m_=m_, pb_=pb_: T.matmul(
                                pp[pb_][:, n, 0:292], lhsT=Ws[wb][:, c, m_ * 128:(m_ + 1) * 128],
                                rhs=hM[:, c, 112 + n * 292:112 + (n + 1) * 292], start=(c == 0), stop=(c == 15)),
                                reads=[('Ws', wb)], writes=[('pp', pb_)])
                    u = uk[k % 2]
                    P.op('act', lambda u=u, pb_=pb_: A.activation(
                        out=u[:, :].rearrange("p (n t) -> p n t", t=292), in_=pp[pb_][:, 0:4, 0:292], func=AF.Copy),
                        reads=[('pp', pb_)], writes=[('uk', k % 2)])
                    wdw = (2, 4, 8, 16)[g]
                    cur, ckey = u, ('uk', k % 2)
                    for s_ in range(int(math.log2(wdw))):
                        sh = 1 << s_
                        nxt, nkey = (sA, 'sA') if s_ % 2 == 0 else (sB, 'sB')
                        P.op('pool', lambda cur=cur, nxt=nxt, sh=sh: G.tensor_tensor(
                            out=nxt[:, sh:1168], in0=cur[:, sh:1168], in1=cur[:, 0:1168 - sh], op=ALU.add),
                            reads=[ckey], writes=[nkey])
                        cur, ckey = nxt, nkey
                    P.op('dve', lambda cur=cur, u=u, k=k, wdw=wdw: V.scalar_tensor_tensor(
                        out=pooledT[:, k, :], in0=cur[:, 16:1168], scalar=1.0 / wdw, in1=u[:, 16:1168],
                        op0=ALU.mult, op1=ALU.subtract), reads=[ckey, ('uk', k % 2)], writes=[('pooledT', k)])
                    P.op('dve', lambda cur=cur, k=k: V.tensor_tensor(out=t16[:, :], in0=cur[:, 144:160], in1=invc_sb[:, k, :], op=ALU.mult),
                         reads=[ckey, 'invc'], writes=['t16'])
                    P.op('dve', lambda u=u, k=k: V.tensor_tensor(out=pooledT[:, k, 128:144], in0=t16[:, :], in1=u[:, 144:160], op=ALU.subtract),
                         reads=['t16', ('uk', k % 2)], writes=[('pooledT', k)])
            for k2 in range(8):
                g, dm = k2 // 2, k2 % 2
                pb_ = next_pp()
                for n in range(3):
                    for cc in range(2):
                        P.op('pe', lambda n=n, cc=cc, g=g, dm=dm, pb_=pb_: T.matmul(
                            pp[pb_][:, n, 0:384], lhsT=Wpl[:, g, cc, dm * 128:(dm + 1) * 128],
                            rhs=pooledT[:, 2 * g + cc, n * 384:(n + 1) * 384], start=(cc == 0), stop=(cc == 1)),
                            reads=['Wpl', ('pooledT', 2 * g + cc)], writes=[('pp', pb_)])
                P.op('act', lambda k2=k2, pb_=pb_: A.activation(
                    out=ypT[:, k2, :].rearrange("p (n t) -> p n t", t=384), in_=pp[pb_][:, 0:3, 0:384],
                    func=AF.Copy, scale=psc_sb[:, k2:k2 + 1]), reads=[('pp', pb_), 'pscale'], writes=[('ypT', k2)])
            idx_scale = (16 ** -0.5) * (64 ** -0.5)
            for i in range(NQB):
                pb_ = next_pp()
                for c in range(16):
                    P.op('pe', lambda c=c, i=i, pb_=pb_: T.matmul(
                        pp[pb_][:, 0, 0:16], lhsT=hM[:, c, 128 + i * 128:256 + i * 128], rhs=Ww[:, c, :],
                        start=(c == 0), stop=(c == 15)), reads=['Ww'], writes=[('pp', pb_)])
                P.op('act', lambda i=i, pb_=pb_: A.activation(out=widx[:, i, :], in_=pp[pb_][:, 0, 0:16], func=AF.Copy, scale=idx_scale),
                     reads=[('pp', pb_)], writes=[('widx', i)])
            P.dma('sp', QT_d[:, :, :], QT[:, :, :], reads=[('fm', id(QT), h_) for h_ in range(8)])
            P.dma('sp', qiT_d[:, :, :], qiT[:, :, :], reads=[('fm', id(qiT), h_) for h_ in range(8)])
            P.dma('sp', ypT_d[:, :, :], ypT[:, :, :], reads=[('ypT', h_) for h_ in range(8)])
            P.dma('sp', widx_d[:, :, :], widx[:, :, :], reads=[('widx', h_) for h_ in range(NQB)])
            P.barrier()
        if stop_after == 'B':
            return nc

        pcd = contextlib.ExitStack()
        es.enter_context(pcd)
        maskT = sb("maskT", [128, 32, NQ], BF16, pcd)
        with contextlib.ExitStack() as pc:
            kidxT = sb("kidxTc", [128, CTX], BF16, pc)
            qiT = sb("qiTc", [128, 8, NQ], BF16, pc)
            widx = sb("widxc", [128, NQB, 16], F32, pc)
            P.dma('sp', kidxT[:, :], kidxT_d[:, :], writes=['kidxT'])
            P.dma('sp', qiT[:, :, :], qiT_d[:, :, :], writes=['qiT'])
            P.dma('sp', widx[:, :, :], widx_d[:, :, :], writes=['widx'])
            slot_row = sb("slot_row", [1, CTX], BF16, pc)
            ones_row = sb("ones_row", [1, 128], BF16, pc)
            causal_b = sb("causal_b", [128, 128], BF16, pc)
            halfs_sb = sb("halfs_sb", [128, NIT + 1], F32, pc)
            sc = [sb("sc%d" % i, [128, CTX], F32, pc) for i in range(2)]
            junk = sb("junk", [128, CTX], BF16, pc)
            Rb = [sb("Rb%d" % i, [128, 512], BF16, pc) for i in range(4)]
            Dg = [sb("Dg%d" % i, [128, 16, 128], BF16, pc) for i in range(2)]
            bsx = [sb("bsx%d" % i, [128, 32], F32, pc) for i in range(2)]
            bs = sb("bs", [128, 64], F32, pc)
            thr_all = sb("thr_all", [128, NQB], F32, pc)
            pd = [ps("pdC%d" % i, [128, 512], F32, pc) for i in range(4)]
            psc = [ps("pscC%d" % i, [128, 512], F32, pc) for i in range(3)]
            ptm = [ps("ptmC%d" % i, [128, 1024], BF16, pc) for i in range(1)]
            P.dma('pool', slot_row[0:1, :], slotb[0:1, :], writes=['slot_row'])
            P.dma('pool', causal_b[:, :], causal[:, :], writes=['causal_b'])
            P.dma('sp', halfs_sb[:], halfs[:, :], writes=['halfs_sb'])
            P.op('pool', lambda: G.memset(ones_row[0:1, :], 1.0), writes=['ones_row'])
            P.op('pool', lambda: G.memset(maskT[:, :, :].rearrange("p a b -> p (a b)"), 0.0), writes=['maskT'])
            mid = bs[:, 3:4]
            cntv = bs[:, 4:5]
            gg = bs[:, 5:6]
            lo = bs[:, 6:7]
            hi = bs[:, 7:8]
            w0 = bs[:, 8:9]
            hk = bs[:, 16:16 + NIT + 1]
            cnts = dict(d=0, s=0, t=0)

            def gen_scores(i, bi):
                E = QS0 + 128 * (i + 1)
                nkt = (E + 511) // 512
                mins = bsx[bi][:, 0:8]
                maxs = bsx[bi][:, 8:16]
                for h in range(16):
                    P.op('pool', lambda h=h: G.tensor_scalar(out=Dg[bi][:, h, :], in0=ident_f[:], scalar1=widx[:, i, h:h + 1],
                                                             scalar2=1.0, op0=ALU.mult, op1=ALU.mult),
                         reads=['ident_f', 'widx'], writes=[('Dg', bi, h)])
                prev = None

                def finish(kt, N, pst, pskey):
                    P.op('pe', lambda: T.matmul(pst[:, :N], lhsT=ones_row[0:1, :], rhs=slot_row[0:1, kt * 512:kt * 512 + N],
                                                start=False, stop=(kt != nkt - 1)),
                         reads=['ones_row', 'slot_row', ('mins', bi, kt), ('maxs', bi, kt)], writes=[pskey])
                    if kt == nkt - 1:
                        P.op('pe', lambda: T.matmul(pst[:, N - 128:N], lhsT=ident_b[:, :], rhs=causal_b[:, :], start=False, stop=True),
                             reads=['ident_b', 'causal_b'], writes=[pskey])
                    P.op('act', lambda: A.activation(out=sc[bi][:, kt * 512:kt * 512 + N], in_=pst[:, :N], func=AF.Copy),
                         reads=[pskey], writes=[('sc', bi, kt)])
                for kt in range(nkt):
                    N = min(512, E - kt * 512)
                    si = cnts['s'] % 3
                    cnts['s'] += 1
                    pst = psc[si]
                    pskey = ('psc', si)
                    pend = []

                    def score_mm(h, rb, N=N, pst=pst, pskey=pskey):
                        P.op('pe', lambda: T.matmul(pst[:, :N], lhsT=Dg[bi][:, h, :], rhs=Rb[rb][:, :N], start=(h == 0), stop=False),
                             reads=[('Dg', bi, h), ('Rb', rb)], writes=[pskey])
                    for h in range(16):
                        di = cnts['d'] % 4
                        cnts['d'] += 1
                        pdt = pd[di]
                        pdk = ('pd', di)
                        pr = (h % 2) * 64
                        P.op('pe', lambda h=h, pdt=pdt, pr=pr: T.matmul(
                            pdt[:, :N], lhsT=qiT[pr:pr + 64, h // 2, i * 128:(i + 1) * 128],
                            rhs=kidxT[pr:pr + 64, kt * 512:kt * 512 + N], start=True, stop=True),
                            reads=['kidxT', 'qiT'], writes=[pdk])
                        P.op('act', lambda pdt=pdt, di=di: A.activation(out=Rb[di][:, :N], in_=pdt[:, :N], func=AF.Relu),
                             reads=[pdk], writes=[('Rb', di)])
                        pend.append((h, di))
                        if len(pend) > 2:
                            score_mm(*pend.pop(0))
                    while pend:
                        score_mm(*pend.pop(0))
                    P.op('dve', lambda pst=pst, N=N, kt=kt: V.tensor_reduce(out=mins[:, kt:kt + 1], in_=pst[:, :N], axis=AX.X, op=ALU.min),
                         reads=[pskey], writes=[('mins', bi, kt)])
                    P.op('dve', lambda pst=pst, N=N, kt=kt: V.tensor_reduce(out=maxs[:, kt:kt + 1], in_=pst[:, :N], axis=AX.X, op=ALU.max),
                         reads=[pskey], writes=[('maxs', bi, kt)])
                    if prev is not None:
                        finish(*prev)
                    prev = (kt, N, pst, pskey)
                    yield
                finish(*prev)
                yield

            def bisect(i, bi, nxt):
                E = QS0 + 128 * (i + 1)
                nkt = (E + 511) // 512
                mins = bsx[bi][:, 0:8]
                maxs = bsx[bi][:, 8:16]
                sckeys = [('sc', bi, kt) for kt in range(nkt)]
                P.op('dve', lambda: V.tensor_reduce(out=lo, in_=mins[:, 0:nkt], axis=AX.X, op=ALU.min),
                     reads=[('mins', bi, kt) for kt in range(nkt)], writes=['lo'])
                P.op('dve', lambda: V.tensor_reduce(out=hi, in_=maxs[:, 0:nkt], axis=AX.X, op=ALU.max),
                     reads=[('maxs', bi, kt) for kt in range(nkt)], writes=['hi'])
                P.op('dve', lambda: V.scalar_tensor_tensor(out=w0, in0=hi, scalar=2.0, in1=lo, op0=ALU.add, op1=ALU.subtract),
                     reads=['hi', 'lo'], writes=['w0'])
                P.op('dve', lambda: V.tensor_scalar(out=hk, in0=halfs_sb[:, :], scalar1=w0, scalar2=None, op0=ALU.mult),
                     reads=['w0', 'halfs_sb'], writes=['hk'])
                P.op('dve', lambda: V.scalar_tensor_tensor(out=mid, in0=lo, scalar=-1.0, in1=hk[:, 0:1], op0=ALU.add, op1=ALU.add),
                     reads=['lo', 'hk'], writes=['mid'])
                for it in range(NIT):
                    P.op('dve', lambda: V.tensor_scalar(out=junk[:, 0:E], in0=sc[bi][:, 0:E], scalar1=mid, scalar2=None,
                                                        op0=ALU.is_ge, op1=ALU.add, accum_out=cntv),
                         reads=sckeys + ['mid'], writes=['junk', 'cnt'])
                    P.op('dve', lambda: V.tensor_scalar(out=gg, in0=cntv, scalar1=255.5, scalar2=0.5, op0=ALU.is_ge, op1=ALU.subtract),
                         reads=['cnt'], writes=['gg'])
                    P.op('dve', lambda it=it: V.scalar_tensor_tensor(out=mid, in0=gg, scalar=hk[:, it:it + 1], in1=mid,
                                                                      op0=ALU.mult, op1=ALU.add),
                         reads=['gg', 'hk', 'mid'], writes=['mid'])
                    if nxt is not None and it % 2 == 1:
                        next(nxt, None)
                if nxt is not None:
                    for _ in nxt:
                        pass
                P.op('dve', lambda: V.tensor_tensor(out=lo, in0=mid, in1=hk[:, NIT:NIT + 1], op=ALU.subtract),
                     reads=['mid', 'hk'], writes=['lo'])
                P.op('dve', lambda: V.tensor_scalar(out=junk[:, 0:E], in0=sc[bi][:, 0:E], scalar1=lo, scalar2=None, op0=ALU.is_ge),
                     reads=sckeys + ['lo'], writes=['junk'])
                P.op('dve', lambda: V.tensor_copy(out=thr_all[:, i:i + 1], in_=lo), reads=['lo'], writes=['thr_all'])
                nkb = E // 128
                for kb0 in range(0, nkb, 8):
                    n8 = min(8, nkb - kb0)
                    pt = ptm[0]
                    ptk = ('ptm', 0)
                    for r in range(n8):
                        kb = kb0 + r
                        P.op('pe', lambda r=r, kb=kb: T.transpose(out=pt[:, r * 128:(r + 1) * 128],
                                                                 in_=junk[:, kb * 128:(kb + 1) * 128], identity=ident_b[:]),
                             reads=['junk', 'ident_b'], writes=[ptk])
                    P.op('act', lambda n8=n8, kb0=kb0: A.activation(
                        out=maskT[:, kb0:kb0 + n8, i * 128:(i + 1) * 128],
                        in_=pt[:, 0:n8 * 128].rearrange("p (a b) -> p a b", b=128), func=AF.Copy),
                        reads=[ptk], writes=['maskT'])

            for _ in gen_scores(0, 0):
                pass
            for i in range(NQB):
                nxt = gen_scores(i + 1, (i + 1) % 2) if i + 1 < NQB else None
                bisect(i, i % 2, nxt)
            if debug:
                P.dma('sp', dbg['maskT'][:, :, :], maskT[:, :, :], reads=['maskT'])
                P.dma('sp', dbg['thr'][:, :], thr_all[:, :], reads=['thr_all'])
            P.barrier()
        if stop_after == 'C':
            return nc

        SCALE = 128 ** -0.5
        with contextlib.ExitStack() as pdd:
            attnT = sb("attnT", [128, 8, NQ], BF16, pdd)
            QT = sb("QTd", [128, 8, NQ], BF16, pdd)
            P.dma('sp', QT[:, :, :], QT_d[:, :, :], writes=['QT'])
            KTh = [sb("KTh%d" % i, [128, CTX], BF16, pdd) for i in range(2)]
            Vh = [sb("Vh%d" % i, [128, 32, 128], BF16, pdd) for i in range(2)]
            strip_sb = sb("strip_sb", [128, 8, 896], F32, pdd)
            cfar_sb = sb("cfar_sb", [128, 8], F32, pdd)
            Eb = [sb("Eb%d" % i, [128, 384], BF16, pdd) for i in range(3)]
            Pb = [sb("Pb%d" % i, [128, 384], BF16, pdd) for i in range(3)]
            tmpb = [sb("tmpb%d" % i, [128, 384], F32, pdd) for i in range(2)]
            rl = sb("rl", [128, 384], F32, pdd)
            pS = [ps("pS%d" % i, [128, 512], F32, pdd) for i in range(3)]
            pO = [ps("pO%d" % i, [128, 512], F32, pdd) for i in range(2)]
            pL = [ps("pL%d" % i, [128, 512], F32, pdd) for i in range(2)]
            P.dma('sp', strip_sb[:], strip[:, :, :], writes=['strip_sb'])
            P.dma('sp', cfar_sb[:], cfar[:, :], writes=['cfar_sb'])
            scn = 0
            ocn = 0
            tmc = 0
            for h in range(8):
                b = h % 2
                P.dma('sp', KTh[b][:, :], KT_d[h, :, :], writes=[('KTh', b)])
                P.dma('sp', Vh[b][:, :, :], V_d[h, :, :].rearrange("(kb p) d -> p kb d", p=128), writes=[('Vh', b)])
                for qt in range(3):
                    t0 = qt * 384
                    kbmax = 23 + 3 * qt + 2
                    po = pO[ocn % 2]
                    pl = pL[ocn % 2]
                    pok = ('pO', ocn % 2)
                    plk = ('pL', ocn % 2)
                    ocn += 1
                    staged = []

                    def stage1(kb):
                        nonlocal scn, tmc
                        s_i = scn % 3
                        scn += 1
                        pst = pS[s_i]
                        P.op('pe', lambda: T.matmul(pst[:, 0:384], lhsT=KTh[b][:, kb * 128:(kb + 1) * 128],
                                                    rhs=QT[:, h, t0:t0 + 384], start=True, stop=True),
                             reads=[('KTh', b), 'QT'], writes=[('pS', s_i)])
                        D0 = (QS0 + t0) - kb * 128
                        if D0 >= 256:
                            P.op('act', lambda: A.activation(out=Eb[s_i][:, :], in_=pst[:, 0:384], func=AF.Exp,
                                                             scale=SCALE, bias=cfar_sb[:, h:h + 1]),
                                 reads=[('pS', s_i), 'cfar_sb'], writes=[('Eb', s_i)])
                        else:
                            ti = tmc % 2
                            tmc += 1
                            P.op('dve', lambda: V.scalar_tensor_tensor(
                                out=tmpb[ti][:, :], in0=pst[:, 0:384], scalar=SCALE,
                                in1=strip_sb[:, h, D0 + 256:D0 + 256 + 384], op0=ALU.mult, op1=ALU.add),
                                reads=[('pS', s_i), 'strip_sb'], writes=[('tmpb', ti)])
                            P.op('act', lambda: A.activation(out=Eb[s_i][:, :], in_=tmpb[ti][:, :], func=AF.Exp),
                                 reads=[('tmpb', ti)], writes=[('Eb', s_i)])
                        P.op('dve', lambda: V.tensor_tensor(out=Pb[s_i][:, :], in0=Eb[s_i][:, :],
                                                            in1=maskT[:, kb, t0:t0 + 384], op=ALU.mult),
                             reads=[('Eb', s_i)], writes=[('Pb', s_i)])
                        staged.append((kb, s_i))

                    def stage2():
                        kb, s_i = staged.pop(0)
                        P.op('pe', lambda: T.matmul(po[:, 0:384], lhsT=Vh[b][:, kb, :], rhs=Pb[s_i][:, :],
                                                    start=(kb == 0), stop=(kb == kbmax)),
                             reads=[('Vh', b), ('Pb', s_i)], writes=[pok])
                        P.op('pe', lambda: T.matmul(pl[:, 0:384], lhsT=ones_b[:, :], rhs=Pb[s_i][:, :],
                                                    start=(kb == 0), stop=(kb == kbmax)),
                             reads=['ones_b', ('Pb', s_i)], writes=[plk])
                    for kb in range(kbmax + 1):
                        stage1(kb)
                        if len(staged) > 2:
                            stage2()
                    while staged:
                        stage2()
                    P.op('dve', lambda: V.tensor_scalar(out=rl[:, :], in0=pl[:, 0:384], scalar1=1e-30, scalar2=None, op0=ALU.add),
                         reads=[plk], writes=['rl'])
                    P.op('dve', lambda: V.reciprocal(out=rl[:, :], in_=rl[:, :]), reads=['rl'], writes=['rl'])
                    P.op('dve', lambda: V.tensor_tensor(out=attnT[:, h, t0:t0 + 384], in0=po[:, 0:384], in1=rl[:, :], op=ALU.mult),
                         reads=[pok, 'rl'], writes=[('attnT', h)])
            P.dma('sp', attnT_d[:, :, :], attnT[:, :, :], reads=[('attnT', h_) for h_ in range(8)])
            P.barrier()
        pcd.close()
        if stop_after == 'D':
            return nc

        with contextlib.ExitStack() as pf:
            Wo = sb("Wo", [128, 16, 2048], BF16, pf)
            ypT = sb("ypTf", [128, 8, NQ], BF16, pf)
            attnT = sb("attnTf", [128, 8, NQ], BF16, pf)
            P.dma('sp', ypT[:, :, :], ypT_d[:, :, :], writes=['ypT'])
            P.dma('sp', attnT[:, :, :], attnT_d[:, :, :], writes=['attnT'])
            xr = [sb("xrF%d" % i, [128, D], F32, pf) for i in range(2)]
            x1t = [sb("x1tF%d" % i, [128, D], F32, pf) for i in range(2)]
            pw = [ps("pwF%d" % i, [128, 512], F32, pf) for i in range(8)]
            for cg in range(4):
                wload(Wo, ('Wo', cg), w_out, 4 * cg, 4 * cg + 4, 0, 2048)
            pcn = 0
            for i in range(NQB):
                bi = i % 2
                P.dma('sp', xr[bi][:], xctx[QS0 + i * 128:QS0 + (i + 1) * 128, :], writes=[('xr', bi)])
                for nt in range(4):
                    pq = pw[pcn % 8]
                    pk = ('pw', pcn % 8)
                    pcn += 1
                    for c in range(16):
                        src = ypT if c < 8 else attnT
                        P.op('pe', lambda c=c, src=src, pq=pq, nt=nt, i=i: T.matmul(
                            pq[:, :], lhsT=src[:, c % 8, i * 128:(i + 1) * 128], rhs=Wo[:, c, nt * 512:(nt + 1) * 512],
                            start=(c == 0), stop=(c == 15)), reads=[('Wo', c // 4), 'ypT', 'attnT'], writes=[pk])
                    P.op('dve', lambda pq=pq, nt=nt, bi=bi: V.tensor_tensor(
                        out=x1t[bi][:, nt * 512:(nt + 1) * 512], in0=pq[:, :], in1=xr[bi][:, nt * 512:(nt + 1) * 512], op=ALU.add),
                        reads=[pk, ('xr', bi)], writes=[('x1t', bi)])
                P.dma('sp', x1_d[i * 128:(i + 1) * 128, :], x1t[bi][:], reads=[('x1t', bi)], writes=[('x1_d', i)])
            P.barrier()
        if stop_after == 'F':
            return nc


        pgh = contextlib.ExitStack()
        es.enter_context(pgh)
        gT = sb("gT", [128, NFC, 1024], BF16, pgh)
        with contextlib.ExitStack() as pg_:
            h2T = sb("h2T", [128, 16, NQ], BF16, pg_)
            with contextlib.ExitStack() as pg1:
                gsb = sb("gsbG", [128, 16, 128], F32, pg1)
                hf_sb = sb("hf_sb", [128, 1], F32, pg1)
                xb = [sb("xbG%d" % i, [128, D], F32, pg1) for i in range(2)]
                xn = [sb("xnG%d" % i, [128, D], BF16, pg1) for i in range(2)]
                pT = [[ps("pTG%d%d" % (i, h), [128, 1024], BF16, pg1) for h in range(2)] for i in range(2)]
                P.dma('sp', gsb[:].rearrange("p c t -> p (c t)"), gT3[1, :, :], writes=['gsb'])
                P.dma('sp', hf_sb[:], hflag[:, :], writes=['hflag'])
                for j in range(NQB):
                    i = j % 2
                    P.dma('sp', xb[i][:], x1_d[j * 128:(j + 1) * 128, :], writes=[('xb', i)])
                    norm_T(i, xb[i][:], ('xb', i), xn, pT, gsb,
                           lambda h, j=j: h2T[:, 8 * h:8 * h + 8, j * 128:(j + 1) * 128], [('h2T', j)],
                           extra=(hf_sb[:, 0:1] if j == 0 else None))
                P.barrier()
            Wgv = [sb("Wgv%d" % i, [128, 16, 512], BF16, pg_) for i in range(2)]
            sgT = sb("sgT", [128, 4, 1024], F32, pg_)
            cp = sb("cp", [128, 2 * NFC, 4], F32, pg_)
            rA = [sb("rA%d" % i, [128, 344], F32, pg_) for i in range(3)]
            rB = [sb("rB%d" % i, [128, 344], F32, pg_) for i in range(3)]
            pu = [ps("puG%d" % i, [128, 512], F32, pg_) for i in range(8)]
            P.dma('sp', cp[:], convp[:, :, :], writes=['cp'])
            tiles = [(0, 342), (342, 684), (684, 1024)]
            pcn = 0
            rcn = 0
            for grp in range(2 * (NFC // 4)):
                jg, isval = grp // 2, grp % 2
                wb = grp % 2
                wload(Wgv[wb], ('Wgv', wb), w_up, 0, 16, (D_FF if isval else 0) + jg * 512, 512)
                for m_ in range(4):
                    j = 4 * jg + m_
                    jj = (NFC + j) if isval else j
                    for (a, b_) in tiles:
                        N = b_ - a + 2
                        n2 = N - 2
                        c0 = 128 + a - 2
                        pq = pu[pcn % 8]
                        pk = ('pu', pcn % 8)
                        pcn += 1
                        for c in range(16):
                            P.op('pe', lambda c=c: T.matmul(
                                pq[:, 0:N], lhsT=Wgv[wb][:, c, m_ * 128:(m_ + 1) * 128], rhs=h2T[:, c, c0:c0 + N],
                                start=(c == 0), stop=(c == 15)), reads=[('Wgv', wb)], writes=[pk])
                        ri = rcn % 3
                        rcn += 1
                        P.op('act', lambda: A.activation(
                            out=rA[ri][:, 0:n2], in_=pq[:, 2:N], func=AF.Identity, scale=cp[:, jj, 2:3], bias=cp[:, jj, 3:4]),
                            reads=[pk, 'cp'], writes=[('rA', ri)])
                        P.op('dve', lambda: V.scalar_tensor_tensor(
                            out=rB[ri][:, 0:n2], in0=pq[:, 1:N - 1], scalar=cp[:, jj, 1:2], in1=rA[ri][:, 0:n2],
                            op0=ALU.mult, op1=ALU.add), reads=[pk, 'cp', ('rA', ri)], writes=[('rB', ri)])
                        if not isval:
                            P.op('dve', lambda: V.scalar_tensor_tensor(
                                out=rA[ri][:, 0:n2], in0=pq[:, 0:n2], scalar=cp[:, jj, 0:1], in1=rB[ri][:, 0:n2],
                                op0=ALU.mult, op1=ALU.add), reads=[pk, 'cp', ('rB', ri)], writes=[('rA', ri)])
                            P.op('act', lambda: A.activation(out=sgT[:, m_, a:b_], in_=rA[ri][:, 0:n2], func=AF.Silu),
                                 reads=[('rA', ri)], writes=[('sgT', m_)])
                        else:
                            P.op('dve', lambda: V.scalar_tensor_tensor(
                                out=rA[ri][:, 0:n2], in0=pq[:, 0:n2], scalar=cp[:, jj, 0:1], in1=rB[ri][:, 0:n2],
                                op0=ALU.mult, op1=ALU.add), reads=[pk, 'cp', ('rB', ri)], writes=[('rA', ri)])
                            P.op('pool', lambda: G.tensor_tensor(
                                out=gT[:, j, a:b_], in0=sgT[:, m_, a:b_], in1=rA[ri][:, 0:n2], op=ALU.mult),
                                reads=[('sgT', m_), ('rA', ri)], writes=[('gT', j)])
            if debug:
                P.dma('sp', dbg['gT'][:, :, :], gT[:, :, :], reads=[('gT', j_) for j_ in range(NFC)])
            P.barrier()
        if stop_after == 'G':
            return nc

        with contextlib.ExitStack() as ph:
            Wds = [sb("Wds%d" % i, [128, 4, 512], BF16, ph) for i in range(3)]
            x1s = [sb("x1s%d" % i, [128, 512], F32, ph) for i in range(4)]
            x2s = [sb("x2s%d" % i, [128, 512], F32, ph) for i in range(4)]
            pw = [ps("pwH%d" % i, [128, 512], F32, ph) for i in range(8)]
            wcn = 0
            scn = 0
            pset = 0
            for tg in range(2):
                for nt in range(4):
                    banks = [(pw[4 * (pset % 2) + tb], ('pw', 4 * (pset % 2) + tb)) for tb in range(4)]
                    pset += 1
                    for fg in range(NFC // 4):
                        wb = wcn % 3
                        wcn += 1
                        P.dma('pool', Wds[wb][:, :, :],
                              w_down[fg * 512:(fg + 1) * 512, nt * 512:(nt + 1) * 512].rearrange("(c p) n -> p c n", p=128),
                              writes=[('Wds', wb)])
                        for fl in range(4):
                            f = fg * 4 + fl
                            for tb in range(4):
                                pq, pk = banks[tb]
                                tok0 = (tg * 4 + tb) * 128
                                P.op('pe', lambda pq=pq, f=f, fl=fl, wb=wb, tok0=tok0: T.matmul(
                                    pq[:, :], lhsT=gT[:, f, tok0:tok0 + 128], rhs=Wds[wb][:, fl, :],
                                    start=(f == 0), stop=(f == NFC - 1)), reads=[('Wds', wb)], writes=[pk])
                    for tb in range(4):
                        pq, pk = banks[tb]
                        si = scn % 4
                        scn += 1
                        r0 = (tg * 4 + tb) * 128
                        P.dma('sp', x1s[si][:, :], x1_d[128 + r0:128 + r0 + 128, nt * 512:(nt + 1) * 512], writes=[('x1s', si)])
                        P.op('dve', lambda pq=pq, si=si: V.tensor_tensor(out=x2s[si][:, :], in0=pq[:, :], in1=x1s[si][:, :], op=ALU.add),
                             reads=[pk, ('x1s', si)], writes=[('x2s', si)])
                        P.dma('sp', x2_d[r0:r0 + 128, nt * 512:(nt + 1) * 512], x2s[si][:, :], reads=[('x2s', si)], writes=[('x2_d', r0, nt)])
            P.barrier()
        pgh.close()
        if stop_after == 'H':
            return nc

        with contextlib.ExitStack() as pi:
            x3 = sb("x3", [128, 8, D], F32, pi)
            h3T = sb("h3T", [128, 16, 1024], BF16, pi)
            ppT = sb("ppT", [128, 2, 1024], BF16, pi)
            with contextlib.ExitStack() as pi1:
                gsb = sb("gsbI", [128, 16, 128], F32, pi1)
                p_sb = sb("p_sb", [128, 8, 256], F32, pi1)
                p_bf = sb("p_bf", [128, 8, 256], BF16, pi1)
                xn = [sb("xnI%d" % i, [128, D], BF16, pi1) for i in range(2)]
                pT = [[ps("pTI%d%d" % (i, h), [128, 1024], BF16, pi1) for h in range(2)] for i in range(2)]
                ptp = [ps("ptpI%d" % i, [128, 1024], BF16, pi1) for i in range(2)]
                P.dma('sp', gsb[:].rearrange("p c t -> p (c t)"), gT3[2, :, :], writes=['gsb'])
                P.dma('sp', p_sb[:, :, :], p_own.rearrange("(tb p) c -> p tb c", p=128), writes=['p_sb'])
                P.op('pool', lambda: G.tensor_copy(out=p_bf[:, :, :], in_=p_sb[:, :, :]), reads=['p_sb'], writes=['p_bf'])
                for tb in range(8):
                    P.dma('sp', x3[:, tb, :], x2_d[tb * 128:(tb + 1) * 128, :], writes=[('x3', tb)])
                for tb in range(8):
                    i = tb % 2
                    norm_T(i, x3[:, tb, :], ('x3', tb), xn, pT, gsb,
                           lambda h, tb=tb: h3T[:, 8 * h:8 * h + 8, tb * 128:(tb + 1) * 128], [('h3T', tb)])
                    pt = ptp[i]
                    for cc in range(2):
                        P.op('pe', lambda pt=pt, cc=cc, tb=tb: T.transpose(out=pt[:, cc * 128:(cc + 1) * 128],
                                                                          in_=p_bf[:, tb, cc * 128:(cc + 1) * 128], identity=ident_b[:]),
                             reads=['p_bf', 'ident_b'], writes=[('ptp', i)])
                    P.op('act', lambda pt=pt, tb=tb: A.activation(out=ppT[:, :, tb * 128:(tb + 1) * 128],
                                                                  in_=pt[:, 0:256].rearrange("p (a b) -> p a b", b=128), func=AF.Copy),
                         reads=[('ptp', i)], writes=[('ppT', tb)])
                P.barrier()
            with contextlib.ExitStack() as pi2:
                Wgs = [sb("WgsI%d" % i, [128, 16, 512], BF16, pi2) for i in range(2)]
                Wps = [sb("WpsI%d" % i, [128, 2, 512], BF16, pi2) for i in range(2)]
                sgm = [sb("sgm%d" % i, [128, 512], F32, pi2) for i in range(2)]
                tmpI = [sb("tmpI%d" % i, [128, 512], F32, pi2) for i in range(2)]
                pgt = [ps("pgtI%d" % i, [128, 512], F32, pi2) for i in range(4)]
                ppe = [ps("ppeI%d" % i, [128, 512], F32, pi2) for i in range(4)]
                cn = 0
                for nt in range(4):
                    wb = nt % 2
                    for cg in range(4):
                        P.dma('pool', Wgs[wb][:, 4 * cg:4 * cg + 4, :],
                              w_pg[cg * 512:(cg + 1) * 512, nt * 512:(nt + 1) * 512].rearrange("(c p) n -> p c n", p=128),
                              writes=[('WgsI', wb, cg)])
                    P.dma('pool', Wps[wb][:, :, :], w_pp[:, nt * 512:(nt + 1) * 512].rearrange("(c p) n -> p c n", p=128),
                          writes=[('WpsI', wb)])
                    for tb in range(8):
                        bi = cn % 4
                        si = cn % 2
                        cn += 1
                        for c in range(16):
                            P.op('pe', lambda c=c, bi=bi, tb=tb, wb=wb: T.matmul(
                                pgt[bi][:, :], lhsT=h3T[:, c, tb * 128:(tb + 1) * 128], rhs=Wgs[wb][:, c, :],
                                start=(c == 0), stop=(c == 15)), reads=[('WgsI', wb, c // 4)], writes=[('pgt', bi)])
                        for cc in range(2):
                            P.op('pe', lambda cc=cc, bi=bi, tb=tb, wb=wb: T.matmul(
                                ppe[bi][:, :], lhsT=ppT[:, cc, tb * 128:(tb + 1) * 128], rhs=Wps[wb][:, cc, :],
                                start=(cc == 0), stop=(cc == 1)), reads=[('WpsI', wb)], writes=[('ppe', bi)])
                        P.op('act', lambda bi=bi, si=si: A.activation(out=sgm[si][:, :], in_=pgt[bi][:, :], func=AF.Sigmoid),
                             reads=[('pgt', bi)], writes=[('sgm', si)])
                        P.op('dve', lambda bi=bi, si=si: V.tensor_tensor(out=tmpI[si][:, :], in0=ppe[bi][:, :], in1=sgm[si][:, :], op=ALU.mult),
                             reads=[('ppe', bi), ('sgm', si)], writes=[('tmpI', si)])
                        P.op('pool', lambda si=si, tb=tb, nt=nt: G.tensor_tensor(
                            out=x3[:, tb, nt * 512:(nt + 1) * 512], in0=x3[:, tb, nt * 512:(nt + 1) * 512], in1=tmpI[si][:, :], op=ALU.add),
                            reads=[('tmpI', si)], writes=[('x3o', tb)])
                P.barrier()
            with contextlib.ExitStack() as pi3:
                gf = sb("gf_sb", [128, D], F32, pi3)
                ot = [sb("ot%d" % i, [128, D], F32, pi3) for i in range(2)]
                jk = sb("jkI", [128, D], BF16, pi3)
                P.dma('sp', gf[:, :], gfin[:, :], writes=['gf'])
                for tb in range(8):
                    i = tb % 2
                    ss = st[:, 3 * i:3 * i + 1]
                    sd = st[:, 3 * i + 1:3 * i + 2]
                    rs = st[:, 3 * i + 2:3 * i + 3]
                    P.op('act', lambda tb=tb, ss=ss: A.activation(out=jk[:, :], in_=x3[:, tb, :], func=AF.Square, accum_out=ss),
                         writes=['jk', ('ss', i)])
                    P.op('dve', lambda ss=ss, sd=sd: V.tensor_scalar(out=sd, in0=ss, scalar1=1.0 / D, scalar2=EPS, op0=ALU.mult, op1=ALU.add),
                         reads=[('ss', i)], writes=[('sd', i)])
                    P.op('act', lambda sd=sd: A.activation(out=sd, in_=sd, func=AF.Sqrt), reads=[('sd', i)], writes=[('sd', i)])
                    P.op('dve', lambda sd=sd, rs=rs: V.reciprocal(out=rs, in_=sd), reads=[('sd', i)], writes=[('rs', i)])
                    P.op('dve', lambda tb=tb, rs=rs, i=i: V.scalar_tensor_tensor(
                        out=ot[i][:, :], in0=x3[:, tb, :], scalar=rs, in1=gf[:, :], op0=ALU.mult, op1=ALU.mult),
                        reads=[('rs', i), 'gf'], writes=[('ot', i)])
                    P.dma('sp', out[tb * 128:(tb + 1) * 128, :], ot[i][:, :], reads=[('ot', i)], writes=[('out', tb)])
                P.barrier()
    return nc


def _t5_bucket_static(dist):
    n = np.maximum(dist, 0)
    nf = np.maximum(n, 1).astype(np.float32)
    large = 16 + (np.log(nf / np.float32(16)) / np.float32(math.log(128 / 16)) * 16).astype(np.int32)
    large = np.minimum(large, 31)
    return np.where(n < 16, n, large)


def prep_inputs(x, p, g_mix, w_in, w_pool, pool_scale, rel_bias, w_out, g_ffn, w_up, conv_w, conv_b,
                w_down, g_ple, w_ple_gate, w_ple_proj, g_final):
    f = np.float32
    x = np.asarray(x, f)
    p = np.asarray(p, f)[0]
    shared = {}
    shared["w_in"] = np.ascontiguousarray(np.asarray(w_in, f)[0])
    shared["w_pool"] = np.ascontiguousarray(np.asarray(w_pool, f)[0])
    shared["w_out"] = np.ascontiguousarray(np.asarray(w_out, f)[0])
    shared["w_up"] = np.ascontiguousarray(np.asarray(w_up, f)[0])
    shared["w_down"] = np.ascontiguousarray(np.asarray(w_down, f)[0])
    shared["w_ple_gate"] = np.ascontiguousarray(np.asarray(w_ple_gate, f)[0])
    shared["w_ple_proj"] = np.ascontiguousarray(np.asarray(w_ple_proj, f)[0])

    def fm(v):
        a = np.asarray(v, f).reshape(16, 128).T
        return np.ascontiguousarray(np.repeat(a[:, :, None], 128, axis=2).reshape(128, 2048))
    shared["gT3"] = np.stack([fm(np.asarray(g_mix)[0]), fm(np.asarray(g_ffn)[0]), fm(np.asarray(g_ple)[0])], 0)
    shared["gfin"] = np.ascontiguousarray(np.repeat(np.asarray(g_final, f)[None, :], 128, axis=0))
    shared["pscale"] = np.ascontiguousarray(np.asarray(pool_scale, f)[0].reshape(8, 128).T)
    cw = np.asarray(conv_w, f)[0]
    cb = np.asarray(conv_b, f)[0]
    cp = np.concatenate([cw, cb[None, :]], 0)
    shared["convp"] = np.ascontiguousarray(cp.reshape(4, 2 * NFC, 128).transpose(2, 1, 0))
    rb = np.asarray(rel_bias, f)
    sl = np.arange(128)[:, None]
    dl = np.arange(896)[None, :] - 256
    dist = dl - sl
    bk = _t5_bucket_static(dist)
    stripv = rb[bk]
    shared["strip"] = np.ascontiguousarray(stripv.transpose(0, 2, 1))
    shared["cfar"] = np.ascontiguousarray(np.repeat(rb[31][None, :], 128, axis=0))
    tl = np.arange(128)[:, None]
    s_l = np.arange(128)[None, :]
    shared["causal"] = np.where(s_l <= tl, 0.0, NEG).astype(f)
    shared["ident"] = np.eye(128, dtype=f)
    shared["halfs"] = np.ascontiguousarray(np.repeat((0.5 ** np.arange(1, NIT + 2, dtype=np.float64)).astype(f)[None, :], 128, 0))
    in_maps = []
    for c in range(8):
        b, j = c // 4, c % 4
        T0 = j * 1024
        m = dict(shared)
        xc = np.zeros((CTX, D), f)
        lo = T0 - 3072
        s0 = max(0, -lo)
        xc[s0:] = x[b, lo + s0:T0 + 1024]
        m["xctx"] = xc
        m["p_own"] = np.ascontiguousarray(p[b, T0:T0 + 1024])
        sbias = np.zeros((CTX,), f)
        sbias[:s0] = NEG
        m["slotb"] = np.ascontiguousarray(np.repeat(sbias[None, :], 128, axis=0))
        ic = np.zeros((128, 8, 16), f)
        for k in range(8):
            w = (2, 4, 8, 16)[k // 2]
            tok = T0 + np.arange(16)
            cntv = np.minimum(tok + 1, w).astype(f)
            ic[:, k, :] = (np.float32(1.0) / cntv)[None, :]
        m["invc"] = ic
        m["hflag"] = np.full((128, 1), 1.0 if j > 0 else 0.0, f)
        in_maps.append(m)
    return in_maps


_NC_CACHE = {}


def kernel(**inputs):
    in_maps = prep_inputs(**inputs)
    if 'nc' not in _NC_CACHE:
        _NC_CACHE['nc'] = build()
    nc = _NC_CACHE['nc']
    res = run_bass_kernel_spmd(nc, in_maps, core_ids=list(range(8)))
    outs = [np.asarray(res.results[c]["out"], np.float32).reshape(1024, D) for c in range(8)]
    full = np.zeros((2, SEQ, D), np.float32)
    for c in range(8):
        full[c // 4, (c % 4) * 1024:(c % 4 + 1) * 1024] = outs[c]
    return full
```

```python
import contextlib
import math
import numpy as np
import concourse.bass as bass
import concourse.mybir as mybir
from concourse.bass_utils import run_bass_kernel_spmd

F32 = mybir.dt.float32
BF16 = mybir.dt.bfloat16
AF = mybir.ActivationFunctionType
ALU = mybir.AluOpType
AX = mybir.AxisListType

D = 2048
SEQ = 4096
CTX = 4096
NQB = 9
QS0 = 2944
NQ = NQB * 128
HM0 = 2816
NHM = 1280
D_FF = 5632
NFC = 44
EPS = 1e-6
NEG = -1.0e30
NIT = 16
OFF = dict(pool=0, q=1024, k=2048, v=3072, qi=4096, ki=5120, wi=5184)


class Prog:
    NDS = 12

    def __init__(self, nc, es, same_sync=True):
        self.nc = nc
        self.same_sync = same_sync
        self.eng = {'pe': nc.tensor, 'act': nc.scalar, 'dve': nc.vector, 'pool': nc.gpsimd, 'sp': nc.sync}
        self.csem = {e: es.enter_context(nc.semaphore('c_' + e)) for e in ['pe', 'act', 'dve', 'pool']}
        self.cnt = {e: 0 for e in self.csem}
        self.dsem = {q: [es.enter_context(nc.semaphore('d_%s%d' % (q, i))) for i in range(self.NDS)]
                     for q in ['sp', 'pool']}
        self.duse = {q: [0] * self.NDS for q in self.dsem}
        self.dnext = {q: 0 for q in self.dsem}
        self.seen = {e: {} for e in self.eng}
        self.lastw = {}
        self.rds = {}
        self.nwait = 0

    def _wait(self, e, tok):
        sem, val, sid = tok
        if sid == e and (e == 'pe' or not self.same_sync):
            return
        if self.seen[e].get(sid, 0) >= val:
            return
        self.eng[e].wait_ge(sem, val)
        self.seen[e][sid] = val
        self.nwait += 1

    def _deps(self, reads, writes):
        deps = {}

        def add(tok):
            sid = tok[2]
            if sid not in deps or deps[sid][1] < tok[1]:
                deps[sid] = tok
        for k in reads:
            if k in self.lastw:
                add(self.lastw[k])
        for k in writes:
            if k in self.lastw:
                add(self.lastw[k])
            for tok in self.rds.get(k, {}).values():
                add(tok)
        return deps.values()

    def _record(self, tok, reads, writes):
        sid = tok[2]
        for k in reads:
            d = self.rds.setdefault(k, {})
            if sid not in d or d[sid][1] < tok[1]:
                d[sid] = tok
        for k in writes:
            self.lastw[k] = tok
            self.rds[k] = {}

    def op(self, e, fn, reads=(), writes=()):
        for tok in self._deps(reads, writes):
            self._wait(e, tok)
        ins = fn()
        self.cnt[e] += 1
        ins.then_inc(self.csem[e], 1)
        self._record((self.csem[e], self.cnt[e], e), reads, writes)

    def dma(self, q, out, in_, reads=(), writes=(), **kw):
        for tok in self._deps(reads, writes):
            self._wait(q, tok)
        k = self.dnext[q]
        self.dnext[q] = (k + 1) % self.NDS
        u = self.duse[q][k]
        sem = self.dsem[q][k]
        if u > 0:
            self._wait(q, (sem, 16 * u, (q, k)))
        ins = self.eng[q].dma_start(out=out, in_=in_, **kw)
        ins.then_inc(sem, 16)
        self.duse[q][k] = u + 1
        self._record((sem, 16 * (u + 1), (q, k)), reads, writes)

    def barrier(self):
        for e in self.eng:
            for c in self.csem:
                if self.cnt[c] > 0:
                    self._wait_force(e, (self.csem[c], self.cnt[c], c))
            for q in self.dsem:
                for k in range(self.NDS):
                    if self.duse[q][k] > 0:
                        self._wait_force(e, (self.dsem[q][k], 16 * self.duse[q][k], (q, k)))
        self.lastw.clear()
        self.rds.clear()

    def _wait_force(self, e, tok):
        sem, val, sid = tok
        if self.seen[e].get(sid, 0) >= val:
            return
        self.eng[e].wait_ge(sem, val)
        self.seen[e][sid] = val
        self.nwait += 1


def build(debug=False, stop_after='Z'):
    nc = bass.Bass("TRN2", target_bir_lowering=False)

    def din(name, shape, dt=F32):
        return nc.dram_tensor(name, list(shape), dt, kind="ExternalInput").ap()

    xctx = din("xctx", [CTX, D])
    p_own = din("p_own", [1024, 256])
    w_in = din("w_in", [D, 5200])
    w_pool = din("w_pool", [4, 256, 256])
    w_out = din("w_out", [D, D])
    w_up = din("w_up", [D, 2 * D_FF])
    w_down = din("w_down", [D_FF, D])
    w_pg = din("w_ple_gate", [D, D])
    w_pp = din("w_ple_proj", [256, D])
    gT3 = din("gT3", [3, 128, 2048])
    gfin = din("gfin", [128, 2048])
    pscale = din("pscale", [128, 8])
    convp = din("convp", [128, 2 * NFC, 4])
    strip = din("strip", [128, 8, 896])
    cfar = din("cfar", [128, 8])
    slotb = din("slotb", [128, CTX])
    causal = din("causal", [128, 128])
    ident = din("ident", [128, 128])
    invc = din("invc", [128, 8, 16])
    hflag = din("hflag", [128, 1])
    halfs = din("halfs", [128, NIT + 1])
    out = nc.dram_tensor("out", [1024, D], F32, kind="ExternalOutput").ap()
    ks = "ExternalOutput" if debug else "Internal"
    KT_d = nc.dram_tensor("KT_d", [8, 128, CTX], BF16, kind=ks).ap()
    V_d = nc.dram_tensor("V_d", [8, CTX, 128], BF16, kind=ks).ap()
    x1_d = nc.dram_tensor("x1_d", [NQ, D], F32, kind=ks).ap()
    x2_d = nc.dram_tensor("x2_d", [1024, D], F32, kind=ks).ap()
    kidxT_d = nc.dram_tensor("kidxT_d", [128, CTX], BF16, kind=ks).ap()
    QT_d = nc.dram_tensor("QT_d", [128, 8, NQ], BF16, kind=ks).ap()
    qiT_d = nc.dram_tensor("qiT_d", [128, 8, NQ], BF16, kind=ks).ap()
    ypT_d = nc.dram_tensor("ypT_d", [128, 8, NQ], BF16, kind=ks).ap()
    attnT_d = nc.dram_tensor("attnT_d", [128, 8, NQ], BF16, kind=ks).ap()
    widx_d = nc.dram_tensor("widx_d", [128, NQB, 16], F32, kind=ks).ap()
    dbg = {}
    if debug:
        dbg['maskT'] = nc.dram_tensor("dbg_maskT", [128, 32, NQ], BF16, kind="ExternalOutput").ap()
        dbg['thr'] = nc.dram_tensor("dbg_thr", [128, NQB], F32, kind="ExternalOutput").ap()
        dbg['gT'] = nc.dram_tensor("dbg_gT", [128, NFC, 1024], BF16, kind="ExternalOutput").ap()

    with contextlib.ExitStack() as es:
        P = Prog(nc, es)
        T = nc.tensor
        A = nc.scalar
        V = nc.vector
        G = nc.gpsimd

        def sb(name, shape, dt, stack=es):
            return stack.enter_context(nc.sbuf_tensor(name, list(shape), dt))

        def ps(name, shape, dt, stack):
            return stack.enter_context(nc.psum_tensor(name, list(shape), dt))

        ident_f = sb("ident_f", [128, 128], F32)
        ident_b = sb("ident_b", [128, 128], BF16)
        ones_b = sb("ones_b", [128, 128], BF16)
        st = sb("st", [128, 16], F32)
        P.dma('sp', ident_f[:], ident[:, :], writes=['ident_f'])
        P.op('dve', lambda: V.tensor_copy(out=ident_b[:], in_=ident_f[:]), reads=['ident_f'], writes=['ident_b'])
        P.op('dve', lambda: V.memset(ones_b[:], 1.0), writes=['ones_b'])

        def alloc(name, shape, dt):
            cm = nc.sbuf_tensor(name, list(shape), dt)
            return cm, cm.__enter__()

        def norm_T(i, xin, xin_key, xn, pT, gsb, dst_fn, dst_keys, extra=None):
            ss = st[:, 3 * i:3 * i + 1]
            sd = st[:, 3 * i + 1:3 * i + 2]
            rs = st[:, 3 * i + 2:3 * i + 3]
            P.op('act', lambda: A.activation(out=xn[i][:], in_=xin, func=AF.Square, accum_out=ss),
                 reads=[xin_key], writes=[('xn', i), ('ss', i)])
            P.op('dve', lambda: V.tensor_scalar(out=sd, in0=ss, scalar1=1.0 / D, scalar2=EPS, op0=ALU.mult, op1=ALU.add),
                 reads=[('ss', i)], writes=[('sd', i)])
            P.op('act', lambda: A.activation(out=sd, in_=sd, func=AF.Sqrt), reads=[('sd', i)], writes=[('sd', i)])
            P.op('dve', lambda: V.reciprocal(out=rs, in_=sd), reads=[('sd', i)], writes=[('rs', i)])
            if extra is not None:
                P.op('dve', lambda: V.tensor_tensor(out=rs, in0=rs, in1=extra, op=ALU.mult),
                     reads=[('rs', i), 'hflag'], writes=[('rs', i)])
            P.op('dve', lambda: V.tensor_scalar(out=xn[i][:], in0=xin, scalar1=rs, scalar2=None, op0=ALU.mult),
                 reads=[xin_key, ('rs', i)], writes=[('xn', i)])
            for c in range(16):
                P.op('pe', lambda c=c: T.transpose(out=pT[i][c // 8][:, (c % 8) * 128:(c % 8 + 1) * 128],
                                                   in_=xn[i][:, c * 128:(c + 1) * 128], identity=ident_b[:]),
                     reads=[('xn', i), 'ident_b'], writes=[('pT', i, c // 8)])
            for h in range(2):
                P.op('dve', lambda h=h: V.tensor_tensor(
                    out=dst_fn(h), in0=pT[i][h][:, :].rearrange("p (c t) -> p c t", t=128),
                    in1=gsb[:, 8 * h:8 * h + 8, :], op=ALU.mult),
                    reads=[('pT', i, h), 'gsb'], writes=dst_keys)

        def wload(dst, dkey, src_rows, c0, c1, col0, ncol, q='pool'):
            P.dma(q, dst[:, c0:c1, 0:ncol],
                  src_rows[c0 * 128:c1 * 128, col0:col0 + ncol].rearrange("(c p) n -> p c n", p=128),
                  writes=[dkey])

        with contextlib.ExitStack() as pa:
            kidxT = sb("kidxT", [128, CTX], BF16, pa)
            Wk = sb("Wk", [128, 16, 1024], BF16, pa)
            Wv = sb("Wv", [128, 16, 1024], BF16, pa)
            Wki = sb("Wki", [128, 16, 128], BF16, pa)
            gsb = sb("gsbA", [128, 16, 128], F32, pa)
            xb = [sb("xbA%d" % i, [128, D], F32, pa) for i in range(2)]
            xn = [sb("xnA%d" % i, [128, D], BF16, pa) for i in range(2)]
            hT = [sb("hTA%d" % i, [128, 16, 512], BF16, pa) for i in range(2)]
            KTst = [sb("KTst%d" % i, [128, 8, 512], BF16, pa) for i in range(2)]
            Vst = [sb("Vst%d" % i, [128, 4, 1024], BF16, pa) for i in range(2)]
            pT = [[ps("pTA%d%d" % (i, h), [128, 1024], BF16, pa) for h in range(2)] for i in range(2)]
            pm = [ps("pmA%d" % i, [128, 512], F32, pa) for i in range(4)]

            P.dma('sp', gsb[:].rearrange("p c t -> p (c t)"), gT3[0, :, :], writes=['gsb'])
            for cg in range(4):
                wload(Wk, ('Wk', cg), w_in, 4 * cg, 4 * cg + 4, OFF['k'], 1024)
            for cg in range(4):
                wload(Wv, ('Wv', cg), w_in, 4 * cg, 4 * cg + 4, OFF['v'], 1024)
            P.dma('pool', Wki[:, :, 0:64], w_in[:, OFF['ki']:OFF['ki'] + 64].rearrange("(c p) n -> p c n", p=128),
                  writes=['Wki'])
            P.dma('pool', Wki[:, :, 64:128], w_in[:, OFF['ki']:OFF['ki'] + 64].rearrange("(c p) n -> p c n", p=128),
                  writes=['Wki'])
            pmi = [0]

            def a_norm(ct, sbl):
                b = ct % 2
                j = ct * 4 + sbl
                i = j % 2
                P.dma('sp', xb[i][:], xctx[j * 128:(j + 1) * 128, :], writes=[('xb', i)])
                norm_T(i, xb[i][:], ('xb', i), xn, pT, gsb,
                       lambda h: hT[b][:, 8 * h:8 * h + 8, sbl * 128:(sbl + 1) * 128], [('hT', b, sbl)])

            def a_k_heads(ct, heads):
                b = ct % 2
                hkeys = [('hT', b, s_) for s_ in range(4)]
                for h in heads:
                    pq = pm[pmi[0] % 4]
                    pk = ('pm', pmi[0] % 4)
                    pmi[0] += 1
                    for c in range(16):
                        P.op('pe', lambda c=c: T.matmul(pq[:, :], lhsT=Wk[:, c, h * 128:(h + 1) * 128],
                                                        rhs=hT[b][:, c, :], start=(c == 0), stop=(c == 15)),
                             reads=hkeys + [('Wk', c // 4)], writes=[pk])
                    P.op('act', lambda: A.activation(out=KTst[b][:, h, :], in_=pq[:, :], func=AF.Copy),
                         reads=[pk], writes=[('KTst', b)])

            def a_kidx(ct):
                b = ct % 2
                hkeys = [('hT', b, s_) for s_ in range(4)]
                pq = pm[pmi[0] % 4]
                pk = ('pm', pmi[0] % 4)
                pmi[0] += 1
                for c in range(16):
                    P.op('pe', lambda c=c: T.matmul(pq[:, :], lhsT=Wki[:, c, :], rhs=hT[b][:, c, :],
                                                    start=(c == 0), stop=(c == 15)),
                         reads=hkeys + ['Wki'], writes=[pk])
                P.op('act', lambda: A.activation(out=kidxT[:, ct * 512:(ct + 1) * 512], in_=pq[:, :], func=AF.Copy),
                     reads=[pk], writes=[('kidxT', ct)])
                P.dma('sp', KT_d[:, :, ct * 512:(ct + 1) * 512].rearrange("h d s -> d h s"), KTst[b][:, :, :],
                      reads=[('KTst', b)], writes=[('KT_d', ct)])

            def a_v(ct, sbls):
                b = ct % 2
                for sbl in sbls:
                    for hf in range(2):
                        pq = pm[pmi[0] % 4]
                        pk = ('pm', pmi[0] % 4)
                        pmi[0] += 1
                        for c in range(16):
                            P.op('pe', lambda c=c: T.matmul(
                                pq[:, :], lhsT=hT[b][:, c, sbl * 128:(sbl + 1) * 128],
                                rhs=Wv[:, c, hf * 512:(hf + 1) * 512], start=(c == 0), stop=(c == 15)),
                                reads=[('hT', b, sbl), ('Wv', c // 4)], writes=[pk])
                        if hf == 0:
                            P.op('act', lambda: A.activation(
                                out=Vst[b][:, sbl, hf * 512:(hf + 1) * 512], in_=pq[:, :], func=AF.Copy),
                                reads=[pk], writes=[('Vst', b, sbl)])
                        else:
                            P.op('dve', lambda: V.tensor_copy(
                                out=Vst[b][:, sbl, hf * 512:(hf + 1) * 512], in_=pq[:, :]),
                                reads=[pk], writes=[('Vst', b, sbl)])
                    r0 = ct * 512 + sbl * 128
                    P.dma('sp', V_d[:, r0:r0 + 128, :].rearrange("h p d -> p h d"),
                          Vst[b][:, sbl, :].rearrange("p (h d) -> p h d", d=128),
                          reads=[('Vst', b, sbl)], writes=[('V_d', ct, sbl)])

            for sbl in range(4):
                a_norm(0, sbl)
            NCT = CTX // 512
            for ct in range(NCT):
                parts = [lambda ct=ct: a_k_heads(ct, range(0, 4)),
                         lambda ct=ct: (a_k_heads(ct, range(4, 8)), a_kidx(ct)),
                         lambda ct=ct: a_v(ct, (0, 1)),
                         lambda ct=ct: a_v(ct, (2, 3))]
                for q_ in range(4):
                    parts[q_]()
                    if ct + 1 < NCT:
                        a_norm(ct + 1, q_)
            P.dma('sp', kidxT_d[:, :], kidxT[:, :], reads=[('kidxT', c_) for c_ in range(8)])
            P.barrier()
        if stop_after == 'A':
            return nc


        with contextlib.ExitStack() as pb:
            QT = sb("QT", [128, 8, NQ], BF16, pb)
            qiT = sb("qiT", [128, 8, NQ], BF16, pb)
            ypT = sb("ypT", [128, 8, NQ], BF16, pb)
            widx = sb("widx", [128, NQB, 16], F32, pb)
            hM = sb("hM", [128, 16, NHM], BF16, pb)
            Ws = [sb("WsB%d" % i, [128, 16, 512], BF16, pb) for i in range(2)]
            wgroups = [OFF['q'], OFF['q'] + 512, OFF['qi'], OFF['qi'] + 512, OFF['pool'], OFF['pool'] + 512]
            wissued = [0]

            def issue_w():
                k = wissued[0]
                if k < len(wgroups):
                    wload(Ws[k % 2], ('Ws', k % 2), w_in, 0, 16, wgroups[k], 512)
                    wissued[0] += 1
            issue_w()
            issue_w()
            with contextlib.ExitStack() as pb1:
                gsb = sb("gsbB", [128, 16, 128], F32, pb1)
                xb = [sb("xbB%d" % i, [128, D], F32, pb1) for i in range(2)]
                xn = [sb("xnB%d" % i, [128, D], BF16, pb1) for i in range(2)]
                pT = [[ps("pTB%d%d" % (i, h), [128, 1024], BF16, pb1) for h in range(2)] for i in range(2)]
                P.dma('sp', gsb[:].rearrange("p c t -> p (c t)"), gT3[0, :, :], writes=['gsb'])
                for j in range(NHM // 128):
                    i = j % 2
                    P.dma('sp', xb[i][:], xctx[HM0 + j * 128:HM0 + (j + 1) * 128, :], writes=[('xb', i)])
                    norm_T(i, xb[i][:], ('xb', i), xn, pT, gsb,
                           lambda h, j=j: hM[:, 8 * h:8 * h + 8, j * 128:(j + 1) * 128], [('hM', j)])
                P.barrier()
            pp = [ps("ppB%d" % i, [128, 4, 512], F32, pb) for i in range(2)]
            Wpl = sb("Wpl", [128, 4, 2, 256], BF16, pb)
            Ww = sb("Ww", [128, 16, 16], BF16, pb)
            psc_sb = sb("pscale_sb", [128, 8], F32, pb)
            invc_sb = sb("invc_sb", [128, 8, 16], F32, pb)
            pooledT = sb("pooledT", [128, 8, NQ], BF16, pb)
            uk = [sb("uk%d" % i, [128, 1168], F32, pb) for i in range(2)]
            sA = sb("sA", [128, 1168], F32, pb)
            sB = sb("sB", [128, 1168], F32, pb)
            t16 = sb("t16", [128, 16], F32, pb)
            P.dma('sp', psc_sb[:], pscale[:, :], writes=['pscale'])
            P.dma('sp', invc_sb[:], invc[:, :, :], writes=['invc'])
            for g in range(4):
                P.dma('pool', Wpl[:, g, :, :], w_pool[g, :, :].rearrange("(cc p) d -> p cc d", p=128), writes=['Wpl'])
            P.dma('pool', Ww[:, :, :], w_in[:, OFF['wi']:OFF['wi'] + 16].rearrange("(c p) n -> p c n", p=128), writes=['Ww'])
            wcnt = [0]
            pcnt = [0]

            def next_w(col0):
                b = wcnt[0] % 2
                assert wgroups[wcnt[0]] == col0
                if wcnt[0] >= 1:
                    issue_w()
                wcnt[0] += 1
                return b

            def next_pp():
                b = pcnt[0] % 2
                pcnt[0] += 1
                return b

            for (dst, off) in ((QT, OFF['q']), (qiT, OFF['qi'])):
                for g in range(2):
                    wb = next_w(off + 512 * g)
                    for m_ in range(4):
                        pb_ = next_pp()
                        for n in range(3):
                            for c in range(16):
                                P.op('pe', lambda c=c, n=n, wb=wb, m_=m_, pb_=pb_: T.matmul(
                                    pp[pb_][:, n, 0:384], lhsT=Ws[wb][:, c, m_ * 128:(m_ + 1) * 128],
                                    rhs=hM[:, c, 128 + n * 384:128 + (n + 1) * 384], start=(c == 0), stop=(c == 15)),
                                    reads=[('Ws', wb)], writes=[('pp', pb_)])
                        P.op('act', lambda dst=dst, g=g, m_=m_, pb_=pb_: A.activation(
                            out=dst[:, 4 * g + m_, :].rearrange("p (n t) -> p n t", t=384),
                            in_=pp[pb_][:, 0:3, 0:384], func=AF.Copy),
                            reads=[('pp', pb_)], writes=[('fm', id(dst), 4 * g + m_)])
            for g2 in range(2):
                wb = next_w(OFF['pool'] + 512 * g2)
                for m_ in range(4):
                    k = 4 * g2 + m_
                    g = k // 2
                    pb_ = next_pp()
                    for n in range(4):
                        for c in range(16):
                            P.op('pe', lambda c=c, n=n, wb=wb, m_=m_, pb_=pb_: T.matmul(
                                pp[pb_][:, n, 0:292], lhsT=Ws[wb][:, c, m_ * 128:(m_ + 1) * 128],
                                rhs=hM[:, c, 112 + n * 292:112 + (n + 1) * 292], start=(c == 0), stop=(c == 15)),
                                reads=[('Ws', wb)], writes=[('pp', pb_)])
                    u = uk[k % 2]
                    P.op('act', lambda u=u, pb_=pb_: A.activation(
                        out=u[:, :].rearrange("p (n t) -> p n t", t=292), in_=pp[pb_][:, 0:4, 0:292], func=AF.Copy),
                        reads=[('pp', pb_)], writes=[('uk', k % 2)])
                    wdw = (2, 4, 8, 16)[g]
                    cur, ckey = u, ('uk', k % 2)
                    for s_ in range(int(math.log2(wdw))):
                        sh = 1 << s_
                        nxt, nkey = (sA, 'sA') if s_ % 2 == 0 else (sB, 'sB')
                        P.op('pool', lambda cur=cur, nxt=nxt, sh=sh: G.tensor_tensor(
                            out=nxt[:, sh:1168], in0=cur[:, sh:1168], in1=cur[:, 0:1168 - sh], op=ALU.add),
                            reads=[ckey], writes=[nkey])
                        cur, ckey = nxt, nkey
                    P.op('dve', lambda cur=cur, u=u, k=k, wdw=wdw: V.scalar_tensor_tensor(
                        out=pooledT[:, k, :], in0=cur[:, 16:1168], scalar=1.0 / wdw, in1=u[:, 16:1168],
                        op0=ALU.mult, op1=ALU.subtract), reads=[ckey, ('uk', k % 2)], writes=[('pooledT', k)])
                    P.op('dve', lambda cur=cur, k=k: V.tensor_tensor(out=t16[:, :], in0=cur[:, 144:160], in1=invc_sb[:, k, :], op=ALU.mult),
                         reads=[ckey, 'invc'], writes=['t16'])
                    P.op('dve', lambda u=u, k=k: V.tensor_tensor(out=pooledT[:, k, 128:144], in0=t16[:, :], in1=u[:, 144:160], op=ALU.subtract),
                         reads=['t16', ('uk', k % 2)], writes=[('pooledT', k)])
            for k2 in range(8):
                g, dm = k2 // 2, k2 % 2
                pb_ = next_pp()
                for n in range(3):
                    for cc in range(2):
                        P.op('pe', lambda n=n, cc=cc, g=g, dm=dm, pb_=pb_: T.matmul(
                            pp[pb_][:, n, 0:384], lhsT=Wpl[:, g, cc, dm * 128:(dm + 1) * 128],
                            rhs=pooledT[:, 2 * g + cc, n * 384:(n + 1) * 384], start=(cc == 0), stop=(cc == 1)),
                            reads=['Wpl', ('pooledT', 2 * g + cc)], writes=[('pp', pb_)])
                P.op('act', lambda k2=k2, pb_=pb_: A.activation(
                    out=ypT[:, k2, :].rearrange("p (n t) -> p n t", t=384), in_=pp[pb_][:, 0:3, 0:384],
                    func=AF.Copy, scale=psc_sb[:, k2:k2 + 1]), reads=[('pp', pb_), 'pscale'], writes=[('ypT', k2)])
            idx_scale = (16 ** -0.5) * (64 ** -0.5)
            for i in range(NQB):
                pb_ = next_pp()
                for c in range(16):
                    P.op('pe', lambda c=c, i=i, pb_=pb_: T.matmul(
                        pp[pb_][:, 0, 0:16], lhsT=hM[:, c, 128 + i * 128:256 + i * 128], rhs=Ww[:, c, :],
                        start=(c == 0), stop=(c == 15)), reads=['Ww'], writes=[('pp', pb_)])
                P.op('act', lambda i=i, pb_=pb_: A.activation(out=widx[:, i, :], in_=pp[pb_][:, 0, 0:16], func=AF.Copy, scale=idx_scale),
                     reads=[('pp', pb_)], writes=[('widx', i)])
            P.dma('sp', QT_d[:, :, :], QT[:, :, :], reads=[('fm', id(QT), h_) for h_ in range(8)])
            P.dma('sp', qiT_d[:, :, :], qiT[:, :, :], reads=[('fm', id(qiT), h_) for h_ in range(8)])
            P.dma('sp', ypT_d[:, :, :], ypT[:, :, :], reads=[('ypT', h_) for h_ in range(8)])
            P.dma('sp', widx_d[:, :, :], widx[:, :, :], reads=[('widx', h_) for h_ in range(NQB)])
            P.barrier()
        if stop_after == 'B':
            return nc

        pcd = contextlib.ExitStack()
        es.enter_context(pcd)
        maskT = sb("maskT", [128, 32, NQ], BF16, pcd)
        with contextlib.ExitStack() as pc:
            kidxT = sb("kidxTc", [128, CTX], BF16, pc)
            qiT = sb("qiTc", [128, 8, NQ], BF16, pc)
            widx = sb("widxc", [128, NQB, 16], F32, pc)
            P.dma('sp', kidxT[:, :], kidxT_d[:, :], writes=['kidxT'])
            P.dma('sp', qiT[:, :, :], qiT_d[:, :, :], writes=['qiT'])
            P.dma('sp', widx[:, :, :], widx_d[:, :, :], writes=['widx'])
            slot_row = sb("slot_row", [1, CTX], BF16, pc)
            ones_row = sb("ones_row", [1, 128], BF16, pc)
            causal_b = sb("causal_b", [128, 128], BF16, pc)
            halfs_sb = sb("halfs_sb", [128, NIT + 1], F32, pc)
            sc = [sb("sc%d" % i, [128, CTX], F32, pc) for i in range(2)]
            junk = sb("junk", [128, CTX], BF16, pc)
            Rb = [sb("Rb%d" % i, [128, 512], BF16, pc) for i in range(4)]
            Dg = [sb("Dg%d" % i, [128, 16, 128], BF16, pc) for i in range(2)]
            bsx = [sb("bsx%d" % i, [128, 32], F32, pc) for i in range(2)]
            bs = sb("bs", [128, 64], F32, pc)
            thr_all = sb("thr_all", [128, NQB], F32, pc)
            pd = [ps("pdC%d" % i, [128, 512], F32, pc) for i in range(4)]
            psc = [ps("pscC%d" % i, [128, 512], F32, pc) for i in range(3)]
            ptm = [ps("ptmC%d" % i, [128, 1024], BF16, pc) for i in range(1)]
            P.dma('pool', slot_row[0:1, :], slotb[0:1, :], writes=['slot_row'])
            P.dma('pool', causal_b[:, :], causal[:, :], writes=['causal_b'])
            P.dma('sp', halfs_sb[:], halfs[:, :], writes=['halfs_sb'])
            P.op('pool', lambda: G.memset(ones_row[0:1, :], 1.0), writes=['ones_row'])
            P.op('pool', lambda: G.memset(maskT[:, :, :].rearrange("p a b -> p (a b)"), 0.0), writes=['maskT'])
            mid = bs[:, 3:4]
            cntv = bs[:, 4:5]
            gg = bs[:, 5:6]
            lo = bs[:, 6:7]
            hi = bs[:, 7:8]
            w0 = bs[:, 8:9]
            hk = bs[:, 16:16 + NIT + 1]
            cnts = dict(d=0, s=0, t=0)

            def gen_scores(i, bi):
                E = QS0 + 128 * (i + 1)
                nkt = (E + 511) // 512
                mins = bsx[bi][:, 0:8]
                maxs = bsx[bi][:, 8:16]
                for h in range(16):
                    P.op('pool', lambda h=h: G.tensor_scalar(out=Dg[bi][:, h, :], in0=ident_f[:], scalar1=widx[:, i, h:h + 1],
                                                             scalar2=1.0, op0=ALU.mult, op1=ALU.mult),
                         reads=['ident_f', 'widx'], writes=[('Dg', bi, h)])
                prev = None

                def finish(kt, N, pst, pskey):
                    P.op('pe', lambda: T.matmul(pst[:, :N], lhsT=ones_row[0:1, :], rhs=slot_row[0:1, kt * 512:kt * 512 + N],
                                                start=False, stop=(kt != nkt - 1)),
                         reads=['ones_row', 'slot_row', ('mins', bi, kt), ('maxs', bi, kt)], writes=[pskey])
                    if kt == nkt - 1:
                        P.op('pe', lambda: T.matmul(pst[:, N - 128:N], lhsT=ident_b[:, :], rhs=causal_b[:, :], start=False, stop=True),
                             reads=['ident_b', 'causal_b'], writes=[pskey])
                    P.op('act', lambda: A.activation(out=sc[bi][:, kt * 512:kt * 512 + N], in_=pst[:, :N], func=AF.Copy),
                         reads=[pskey], writes=[('sc', bi, kt)])
                for kt in range(nkt):
                    N = min(512, E - kt * 512)
                    si = cnts['s'] % 3
                    cnts['s'] += 1
                    pst = psc[si]
                    pskey = ('psc', si)
                    pend = []

                    def score_mm(h, rb, N=N, pst=pst, pskey=pskey):
                        P.op('pe', lambda: T.matmul(pst[:, :N], lhsT=Dg[bi][:, h, :], rhs=Rb[rb][:, :N], start=(h == 0), stop=False),
                             reads=[('Dg', bi, h), ('Rb', rb)], writes=[pskey])
                    for h in range(16):
                        di = cnts['d'] % 4
                        cnts['d'] += 1
                        pdt = pd[di]
                        pdk = ('pd', di)
                        pr = (h % 2) * 64
                        P.op('pe', lambda h=h, pdt=pdt, pr=pr: T.matmul(
                            pdt[:, :N], lhsT=qiT[pr:pr + 64, h // 2, i * 128:(i + 1) * 128],
                            rhs=kidxT[pr:pr + 64, kt * 512:kt * 512 + N], start=True, stop=True),
                            reads=['kidxT', 'qiT'], writes=[pdk])
                        P.op('act', lambda pdt=pdt, di=di: A.activation(out=Rb[di][:, :N], in_=pdt[:, :N], func=AF.Relu),
                             reads=[pdk], writes=[('Rb', di)])
                        pend.append((h, di))
                        if len(pend) > 2:
                            score_mm(*pend.pop(0))
                    while pend:
                        score_mm(*pend.pop(0))
                    P.op('dve', lambda pst=pst, N=N, kt=kt: V.tensor_reduce(out=mins[:, kt:kt + 1], in_=pst[:, :N], axis=AX.X, op=ALU.min),
                         reads=[pskey], writes=[('mins', bi, kt)])
                    P.op('dve', lambda pst=pst, N=N, kt=kt: V.tensor_reduce(out=maxs[:, kt:kt + 1], in_=pst[:, :N], axis=AX.X, op=ALU.max),
                         reads=[pskey], writes=[('maxs', bi, kt)])
                    if prev is not None:
                        finish(*prev)
                    prev = (kt, N, pst, pskey)
                    yield
                finish(*prev)
                yield

            def bisect(i, bi, nxt):
                E = QS0 + 128 * (i + 1)
                nkt = (E + 511) // 512
                mins = bsx[bi][:, 0:8]
                maxs = bsx[bi][:, 8:16]
                sckeys = [('sc', bi, kt) for kt in range(nkt)]
                P.op('dve', lambda: V.tensor_reduce(out=lo, in_=mins[:, 0:nkt], axis=AX.X, op=ALU.min),
                     reads=[('mins', bi, kt) for kt in range(nkt)], writes=['lo'])
                P.op('dve', lambda: V.tensor_reduce(out=hi, in_=maxs[:, 0:nkt], axis=AX.X, op=ALU.max),
                     reads=[('maxs', bi, kt) for kt in range(nkt)], writes=['hi'])
                P.op('dve', lambda: V.scalar_tensor_tensor(out=w0, in0=hi, scalar=2.0, in1=lo, op0=ALU.add, op1=ALU.subtract),
                     reads=['hi', 'lo'], writes=['w0'])
                P.op('dve', lambda: V.tensor_scalar(out=hk, in0=halfs_sb[:, :], scalar1=w0, scalar2=None, op0=ALU.mult),
                     reads=['w0', 'halfs_sb'], writes=['hk'])
                P.op('dve', lambda: V.scalar_tensor_tensor(out=mid, in0=lo, scalar=-1.0, in1=hk[:, 0:1], op0=ALU.add, op1=ALU.add),
                     reads=['lo', 'hk'], writes=['mid'])
                for it in range(NIT):
                    P.op('dve', lambda: V.tensor_scalar(out=junk[:, 0:E], in0=sc[bi][:, 0:E], scalar1=mid, scalar2=None,
                                                        op0=ALU.is_ge, op1=ALU.add, accum_out=cntv),
                         reads=sckeys + ['mid'], writes=['junk', 'cnt'])
                    P.op('dve', lambda: V.tensor_scalar(out=gg, in0=cntv, scalar1=255.5, scalar2=0.5, op0=ALU.is_ge, op1=ALU.subtract),
                         reads=['cnt'], writes=['gg'])
                    P.op('dve', lambda it=it: V.scalar_tensor_tensor(out=mid, in0=gg, scalar=hk[:, it:it + 1], in1=mid,
                                                                      op0=ALU.mult, op1=ALU.add),
                         reads=['gg', 'hk', 'mid'], writes=['mid'])
                    if nxt is not None and it % 2 == 1:
                        next(nxt, None)
                if nxt is not None:
                    for _ in nxt:
                        pass
                P.op('dve', lambda: V.tensor_tensor(out=lo, in0=mid, in1=hk[:, NIT:NIT + 1], op=ALU.subtract),
                     reads=['mid', 'hk'], writes=['lo'])
                P.op('dve', lambda: V.tensor_scalar(out=junk[:, 0:E], in0=sc[bi][:, 0:E], scalar1=lo, scalar2=None, op0=ALU.is_ge),
                     reads=sckeys + ['lo'], writes=['junk'])
                P.op('dve', lambda: V.tensor_copy(out=thr_all[:, i:i + 1], in_=lo), reads=['lo'], writes=['thr_all'])
                nkb = E // 128
                for kb0 in range(0, nkb, 8):
                    n8 = min(8, nkb - kb0)
                    pt = ptm[0]
                    ptk = ('ptm', 0)
                    for r in range(n8):
                        kb = kb0 + r
                        P.op('pe', lambda r=r, kb=kb: T.transpose(out=pt[:, r * 128:(r + 1) * 128],
                                                                 in_=junk[:, kb * 128:(kb + 1) * 128], identity=ident_b[:]),
                             reads=['junk', 'ident_b'], writes=[ptk])
                    P.op('act', lambda n8=n8, kb0=kb0: A.activation(
                        out=maskT[:, kb0:kb0 + n8, i * 128:(i + 1) * 128],
                        in_=pt[:, 0:n8 * 128].rearrange("p (a b) -> p a b", b=128), func=AF.Copy),
                        reads=[ptk], writes=['maskT'])

            for _ in gen_scores(0, 0):
                pass
            for i in range(NQB):
                nxt = gen_scores(i + 1, (i + 1) % 2) if i + 1 < NQB else None
                bisect(i, i % 2, nxt)
            if debug:
                P.dma('sp', dbg['maskT'][:, :, :], maskT[:, :, :], reads=['maskT'])
                P.dma('sp', dbg['thr'][:, :], thr_all[:, :], reads=['thr_all'])
            P.barrier()
        if stop_after == 'C':
            return nc

        SCALE = 128 ** -0.5
        with contextlib.ExitStack() as pdd:
            attnT = sb("attnT", [128, 8, NQ], BF16, pdd)
            QT = sb("QTd", [128, 8, NQ], BF16, pdd)
            P.dma('sp', QT[:, :, :], QT_d[:, :, :], writes=['QT'])
            KTh = [sb("KTh%d" % i, [128, CTX], BF16, pdd) for i in range(2)]
            Vh = [sb("Vh%d" % i, [128, 32, 128], BF16, pdd) for i in range(2)]
            strip_sb = sb("strip_sb", [128, 8, 896], F32, pdd)
            cfar_sb = sb("cfar_sb", [128, 8], F32, pdd)
            Eb = [sb("Eb%d" % i, [128, 384], BF16, pdd) for i in range(3)]
            Pb = [sb("Pb%d" % i, [128, 384], BF16, pdd) for i in range(3)]
            tmpb = [sb("tmpb%d" % i, [128, 384], F32, pdd) for i in range(2)]
            rl = sb("rl", [128, 384], F32, pdd)
            pS = [ps("pS%d" % i, [128, 512], F32, pdd) for i in range(3)]
            pO = [ps("pO%d" % i, [128, 512], F32, pdd) for i in range(2)]
            pL = [ps("pL%d" % i, [128, 512], F32, pdd) for i in range(2)]
            P.dma('sp', strip_sb[:], strip[:, :, :], writes=['strip_sb'])
            P.dma('sp', cfar_sb[:], cfar[:, :], writes=['cfar_sb'])
            scn = 0
            ocn = 0
            tmc = 0
            for h in range(8):
                b = h % 2
                P.dma('sp', KTh[b][:, :], KT_d[h, :, :], writes=[('KTh', b)])
                P.dma('sp', Vh[b][:, :, :], V_d[h, :, :].rearrange("(kb p) d -> p kb d", p=128), writes=[('Vh', b)])
                for qt in range(3):
                    t0 = qt * 384
                    kbmax = 23 + 3 * qt + 2
                    po = pO[ocn % 2]
                    pl = pL[ocn % 2]
                    pok = ('pO', ocn % 2)
                    plk = ('pL', ocn % 2)
                    ocn += 1
                    staged = []

                    def stage1(kb):
                        nonlocal scn, tmc
                        s_i = scn % 3
                        scn += 1
                        pst = pS[s_i]
                        P.op('pe', lambda: T.matmul(pst[:, 0:384], lhsT=KTh[b][:, kb * 128:(kb + 1) * 128],
                                                    rhs=QT[:, h, t0:t0 + 384], start=True, stop=True),
                             reads=[('KTh', b), 'QT'], writes=[('pS', s_i)])
                        D0 = (QS0 + t0) - kb * 128
                        if D0 >= 256:
                            P.op('act', lambda: A.activation(out=Eb[s_i][:, :], in_=pst[:, 0:384], func=AF.Exp,
                                                             scale=SCALE, bias=cfar_sb[:, h:h + 1]),
                                 reads=[('pS', s_i), 'cfar_sb'], writes=[('Eb', s_i)])
                        else:
                            ti = tmc % 2
                            tmc += 1
                            P.op('dve', lambda: V.scalar_tensor_tensor(
                                out=tmpb[ti][:, :], in0=pst[:, 0:384], scalar=SCALE,
                                in1=strip_sb[:, h, D0 + 256:D0 + 256 + 384], op0=ALU.mult, op1=ALU.add),
                                reads=[('pS', s_i), 'strip_sb'], writes=[('tmpb', ti)])
                            P.op('act', lambda: A.activation(out=Eb[s_i][:, :], in_=tmpb[ti][:, :], func=AF.Exp),
                                 reads=[('tmpb', ti)], writes=[('Eb', s_i)])
                        P.op('dve', lambda: V.tensor_tensor(out=Pb[s_i][:, :], in0=Eb[s_i][:, :],
                                                            in1=maskT[:, kb, t0:t0 + 384], op=ALU.mult),
                             reads=[('Eb', s_i)], writes=[('Pb', s_i)])
                        staged.append((kb, s_i))

                    def stage2():
                        kb, s_i = staged.pop(0)
                        P.op('pe', lambda: T.matmul(po[:, 0:384], lhsT=Vh[b][:, kb, :], rhs=Pb[s_i][:, :],
                                                    start=(kb == 0), stop=(kb == kbmax)),
                             reads=[('Vh', b), ('Pb', s_i)], writes=[pok])
                        P.op('pe', lambda: T.matmul(pl[:, 0:384], lhsT=ones_b[:, :], rhs=Pb[s_i][:, :],
                                                    start=(kb == 0), stop=(kb == kbmax)),
                             reads=['ones_b', ('Pb', s_i)], writes=[plk])
                    for kb in range(kbmax + 1):
                        stage1(kb)
                        if len(staged) > 2:
                            stage2()
                    while staged:
                        stage2()
                    P.op('dve', lambda: V.tensor_scalar(out=rl[:, :], in0=pl[:, 0:384], scalar1=1e-30, scalar2=None, op0=ALU.add),
                         reads=[plk], writes=['rl'])
                    P.op('dve', lambda: V.reciprocal(out=rl[:, :], in_=rl[:, :]), reads=['rl'], writes=['rl'])
                    P.op('dve', lambda: V.tensor_tensor(out=attnT[:, h, t0:t0 + 384], in0=po[:, 0:384], in1=rl[:, :], op=ALU.mult),
                         reads=[pok, 'rl'], writes=[('attnT', h)])
            P.dma('sp', attnT_d[:, :, :], attnT[:, :, :], reads=[('attnT', h_) for h_ in range(8)])
            P.barrier()
        pcd.close()
        if stop_after == 'D':
            return nc

        with contextlib.ExitStack() as pf:
            Wo = sb("Wo", [128, 16, 2048], BF16, pf)
            ypT = sb("ypTf", [128, 8, NQ], BF16, pf)
            attnT = sb("attnTf", [128, 8, NQ], BF16, pf)
            P.dma('sp', ypT[:, :, :], ypT_d[:, :, :], writes=['ypT'])
            P.dma('sp', attnT[:, :, :], attnT_d[:, :, :], writes=['attnT'])
            xr = [sb("xrF%d" % i, [128, D], F32, pf) for i in range(2)]
            x1t = [sb("x1tF%d" % i, [128, D], F32, pf) for i in range(2)]
            pw = [ps("pwF%d" % i, [128, 512], F32, pf) for i in range(8)]
            for cg in range(4):
                wload(Wo, ('Wo', cg), w_out, 4 * cg, 4 * cg + 4, 0, 2048)
            pcn = 0
            for i in range(NQB):
                bi = i % 2
                P.dma('sp', xr[bi][:], xctx[QS0 + i * 128:QS0 + (i + 1) * 128, :], writes=[('xr', bi)])
                for nt in range(4):
                    pq = pw[pcn % 8]
                    pk = ('pw', pcn % 8)
                    pcn += 1
                    for c in range(16):
                        src = ypT if c < 8 else attnT
                        P.op('pe', lambda c=c, src=src, pq=pq, nt=nt, i=i: T.matmul(
                            pq[:, :], lhsT=src[:, c % 8, i * 128:(i + 1) * 128], rhs=Wo[:, c, nt * 512:(nt + 1) * 512],
                            start=(c == 0), stop=(c == 15)), reads=[('Wo', c // 4), 'ypT', 'attnT'], writes=[pk])
                    P.op('dve', lambda pq=pq, nt=nt, bi=bi: V.tensor_tensor(
                        out=x1t[bi][:, nt * 512:(nt + 1) * 512], in0=pq[:, :], in1=xr[bi][:, nt * 512:(nt + 1) * 512], op=ALU.add),
                        reads=[pk, ('xr', bi)], writes=[('x1t', bi)])
                P.dma('sp', x1_d[i * 128:(i + 1) * 128, :], x1t[bi][:], reads=[('x1t', bi)], writes=[('x1_d', i)])
            P.barrier()
        if stop_after == 'F':
            return nc


        pgh = contextlib.ExitStack()
        es.enter_context(pgh)
        gT = sb("gT", [128, NFC, 1024], BF16, pgh)
        with contextlib.ExitStack() as pg_:
            h2T = sb("h2T", [128, 16, NQ], BF16, pg_)
            Wgv = [sb("Wgv%d" % i, [128, 16, 512], BF16, pg_) for i in range(2)]

            def g_load(grp):
                jg_, isval_ = grp // 2, grp % 2
                wload(Wgv[grp % 2], ('Wgv', grp % 2), w_up, 0, 16, (D_FF if isval_ else 0) + jg_ * 512, 512)
            g_load(0)
            g_load(1)
            with contextlib.ExitStack() as pg1:
                gsb = sb("gsbG", [128, 16, 128], F32, pg1)
                hf_sb = sb("hf_sb", [128, 1], F32, pg1)
                xb = [sb("xbG%d" % i, [128, D], F32, pg1) for i in range(2)]
                xn = [sb("xnG%d" % i, [128, D], BF16, pg1) for i in range(2)]
                pT = [[ps("pTG%d%d" % (i, h), [128, 1024], BF16, pg1) for h in range(2)] for i in range(2)]
                P.dma('sp', gsb[:].rearrange("p c t -> p (c t)"), gT3[1, :, :], writes=['gsb'])
                P.dma('sp', hf_sb[:], hflag[:, :], writes=['hflag'])
                for j in range(NQB):
                    i = j % 2
                    P.dma('sp', xb[i][:], x1_d[j * 128:(j + 1) * 128, :], writes=[('xb', i)])
                    norm_T(i, xb[i][:], ('xb', i), xn, pT, gsb,
                           lambda h, j=j: h2T[:, 8 * h:8 * h + 8, j * 128:(j + 1) * 128], [('h2T', j)],
                           extra=(hf_sb[:, 0:1] if j == 0 else None))
                P.barrier()
            sgT = sb("sgT", [128, 4, 1024], F32, pg_)
            cp = sb("cp", [128, 2 * NFC, 4], F32, pg_)
            rA = [sb("rA%d" % i, [128, 344], F32, pg_) for i in range(3)]
            rB = [sb("rB%d" % i, [128, 344], F32, pg_) for i in range(3)]
            pu = [ps("puG%d" % i, [128, 512], F32, pg_) for i in range(8)]
            P.dma('sp', cp[:], convp[:, :, :], writes=['cp'])
            tiles = [(0, 342), (342, 684), (684, 1024)]
            pcn = 0
            rcn = 0
            for grp in range(2 * (NFC // 4)):
                jg, isval = grp // 2, grp % 2
                wb = grp % 2
                if grp >= 1 and grp + 1 < 2 * (NFC // 4):
                    g_load(grp + 1)
                for m_ in range(4):
                    j = 4 * jg + m_
                    jj = (NFC + j) if isval else j
                    for (a, b_) in tiles:
                        N = b_ - a + 2
                        n2 = N - 2
                        c0 = 128 + a - 2
                        pq = pu[pcn % 8]
                        pk = ('pu', pcn % 8)
                        pcn += 1
                        for c in range(16):
                            P.op('pe', lambda c=c: T.matmul(
                                pq[:, 0:N], lhsT=Wgv[wb][:, c, m_ * 128:(m_ + 1) * 128], rhs=h2T[:, c, c0:c0 + N],
                                start=(c == 0), stop=(c == 15)), reads=[('Wgv', wb)], writes=[pk])
                        ri = rcn % 3
                        rcn += 1
                        P.op('act', lambda: A.activation(
                            out=rA[ri][:, 0:n2], in_=pq[:, 2:N], func=AF.Identity, scale=cp[:, jj, 2:3], bias=cp[:, jj, 3:4]),
                            reads=[pk, 'cp'], writes=[('rA', ri)])
                        P.op('dve', lambda: V.scalar_tensor_tensor(
                            out=rB[ri][:, 0:n2], in0=pq[:, 1:N - 1], scalar=cp[:, jj, 1:2], in1=rA[ri][:, 0:n2],
                            op0=ALU.mult, op1=ALU.add), reads=[pk, 'cp', ('rA', ri)], writes=[('rB', ri)])
                        if not isval:
                            P.op('dve', lambda: V.scalar_tensor_tensor(
                                out=rA[ri][:, 0:n2], in0=pq[:, 0:n2], scalar=cp[:, jj, 0:1], in1=rB[ri][:, 0:n2],
                                op0=ALU.mult, op1=ALU.add), reads=[pk, 'cp', ('rB', ri)], writes=[('rA', ri)])
                            P.op('act', lambda: A.activation(out=sgT[:, m_, a:b_], in_=rA[ri][:, 0:n2], func=AF.Silu),
                                 reads=[('rA', ri)], writes=[('sgT', m_)])
                        else:
                            P.op('dve', lambda: V.scalar_tensor_tensor(
                                out=rA[ri][:, 0:n2], in0=pq[:, 0:n2], scalar=cp[:, jj, 0:1], in1=rB[ri][:, 0:n2],
                                op0=ALU.mult, op1=ALU.add), reads=[pk, 'cp', ('rB', ri)], writes=[('rA', ri)])
                            P.op('dve', lambda: V.tensor_tensor(
                                out=gT[:, j, a:b_], in0=sgT[:, m_, a:b_], in1=rA[ri][:, 0:n2], op=ALU.mult),
                                reads=[('sgT', m_), ('rA', ri)], writes=[('gT', j)])
            if debug:
                P.dma('sp', dbg['gT'][:, :, :], gT[:, :, :], reads=[('gT', j_) for j_ in range(NFC)])
            P.barrier()
        if stop_after == 'G':
            return nc

        with contextlib.ExitStack() as ph:
            Wds = [sb("Wds%d" % i, [128, 4, 512], BF16, ph) for i in range(3)]
            x1s = [sb("x1s%d" % i, [128, 512], F32, ph) for i in range(4)]
            x2s = [sb("x2s%d" % i, [128, 512], F32, ph) for i in range(4)]
            pw = [ps("pwH%d" % i, [128, 512], F32, ph) for i in range(8)]
            wcn = 0
            scn = 0
            pset = 0
            for tg in range(2):
                for nt in range(4):
                    banks = [(pw[4 * (pset % 2) + tb], ('pw', 4 * (pset % 2) + tb)) for tb in range(4)]
                    pset += 1
                    for fg in range(NFC // 4):
                        wb = wcn % 3
                        wcn += 1
                        P.dma('pool', Wds[wb][:, :, :],
                              w_down[fg * 512:(fg + 1) * 512, nt * 512:(nt + 1) * 512].rearrange("(c p) n -> p c n", p=128),
                              writes=[('Wds', wb)])
                        for fl in range(4):
                            f = fg * 4 + fl
                            for tb in range(4):
                                pq, pk = banks[tb]
                                tok0 = (tg * 4 + tb) * 128
                                P.op('pe', lambda pq=pq, f=f, fl=fl, wb=wb, tok0=tok0: T.matmul(
                                    pq[:, :], lhsT=gT[:, f, tok0:tok0 + 128], rhs=Wds[wb][:, fl, :],
                                    start=(f == 0), stop=(f == NFC - 1)), reads=[('Wds', wb)], writes=[pk])
                    for tb in range(4):
                        pq, pk = banks[tb]
                        si = scn % 4
                        scn += 1
                        r0 = (tg * 4 + tb) * 128
                        P.dma('sp', x1s[si][:, :], x1_d[128 + r0:128 + r0 + 128, nt * 512:(nt + 1) * 512], writes=[('x1s', si)])
                        P.op('dve', lambda pq=pq, si=si: V.tensor_tensor(out=x2s[si][:, :], in0=pq[:, :], in1=x1s[si][:, :], op=ALU.add),
                             reads=[pk, ('x1s', si)], writes=[('x2s', si)])
                        P.dma('sp', x2_d[r0:r0 + 128, nt * 512:(nt + 1) * 512], x2s[si][:, :], reads=[('x2s', si)], writes=[('x2_d', r0, nt)])
            P.barrier()
        pgh.close()
        if stop_after == 'H':
            return nc

        with contextlib.ExitStack() as pi:
            x3 = sb("x3", [128, 8, D], F32, pi)
            h3T = sb("h3T", [128, 16, 1024], BF16, pi)
            ppT = sb("ppT", [128, 2, 1024], BF16, pi)
            with contextlib.ExitStack() as pi1:
                gsb = sb("gsbI", [128, 16, 128], F32, pi1)
                p_sb = sb("p_sb", [128, 8, 256], F32, pi1)
                p_bf = sb("p_bf", [128, 8, 256], BF16, pi1)
                xn = [sb("xnI%d" % i, [128, D], BF16, pi1) for i in range(2)]
                pT = [[ps("pTI%d%d" % (i, h), [128, 1024], BF16, pi1) for h in range(2)] for i in range(2)]
                ptp = [ps("ptpI%d" % i, [128, 1024], BF16, pi1) for i in range(2)]
                P.dma('sp', gsb[:].rearrange("p c t -> p (c t)"), gT3[2, :, :], writes=['gsb'])
                P.dma('sp', p_sb[:, :, :], p_own.rearrange("(tb p) c -> p tb c", p=128), writes=['p_sb'])
                P.op('pool', lambda: G.tensor_copy(out=p_bf[:, :, :], in_=p_sb[:, :, :]), reads=['p_sb'], writes=['p_bf'])
                for tb in range(8):
                    P.dma('sp', x3[:, tb, :], x2_d[tb * 128:(tb + 1) * 128, :], writes=[('x3', tb)])
                for tb in range(8):
                    i = tb % 2
                    norm_T(i, x3[:, tb, :], ('x3', tb), xn, pT, gsb,
                           lambda h, tb=tb: h3T[:, 8 * h:8 * h + 8, tb * 128:(tb + 1) * 128], [('h3T', tb)])
                    pt = ptp[i]
                    for cc in range(2):
                        P.op('pe', lambda pt=pt, cc=cc, tb=tb: T.transpose(out=pt[:, cc * 128:(cc + 1) * 128],
                                                                          in_=p_bf[:, tb, cc * 128:(cc + 1) * 128], identity=ident_b[:]),
                             reads=['p_bf', 'ident_b'], writes=[('ptp', i)])
                    P.op('act', lambda pt=pt, tb=tb: A.activation(out=ppT[:, :, tb * 128:(tb + 1) * 128],
                                                                  in_=pt[:, 0:256].rearrange("p (a b) -> p a b", b=128), func=AF.Copy),
                         reads=[('ptp', i)], writes=[('ppT', tb)])
                P.barrier()
            with contextlib.ExitStack() as pi2:
                Wgs = [sb("WgsI%d" % i, [128, 16, 512], BF16, pi2) for i in range(2)]
                Wps = [sb("WpsI%d" % i, [128, 2, 512], BF16, pi2) for i in range(2)]
                sgm = [sb("sgm%d" % i, [128, 512], F32, pi2) for i in range(2)]
                tmpI = [sb("tmpI%d" % i, [128, 512], F32, pi2) for i in range(2)]
                pgt = [ps("pgtI%d" % i, [128, 512], F32, pi2) for i in range(4)]
                ppe = [ps("ppeI%d" % i, [128, 512], F32, pi2) for i in range(4)]
                cn = 0

                def i_load(nt_):
                    wb_ = nt_ % 2
                    for cg in range(4):
                        P.dma('pool', Wgs[wb_][:, 4 * cg:4 * cg + 4, :],
                              w_pg[cg * 512:(cg + 1) * 512, nt_ * 512:(nt_ + 1) * 512].rearrange("(c p) n -> p c n", p=128),
                              writes=[('WgsI', wb_, cg)])
                    P.dma('pool', Wps[wb_][:, :, :], w_pp[:, nt_ * 512:(nt_ + 1) * 512].rearrange("(c p) n -> p c n", p=128),
                          writes=[('WpsI', wb_)])
                i_load(0)
                for nt in range(4):
                    wb = nt % 2
                    if nt + 1 < 4:
                        i_load(nt + 1)
                    for tb in range(8):
                        bi = cn % 4
                        si = cn % 2
                        cn += 1
                        for c in range(16):
                            P.op('pe', lambda c=c, bi=bi, tb=tb, wb=wb: T.matmul(
                                pgt[bi][:, :], lhsT=h3T[:, c, tb * 128:(tb + 1) * 128], rhs=Wgs[wb][:, c, :],
                                start=(c == 0), stop=(c == 15)), reads=[('WgsI', wb, c // 4)], writes=[('pgt', bi)])
                        for cc in range(2):
                            P.op('pe', lambda cc=cc, bi=bi, tb=tb, wb=wb: T.matmul(
                                ppe[bi][:, :], lhsT=ppT[:, cc, tb * 128:(tb + 1) * 128], rhs=Wps[wb][:, cc, :],
                                start=(cc == 0), stop=(cc == 1)), reads=[('WpsI', wb)], writes=[('ppe', bi)])
                        P.op('act', lambda bi=bi, si=si: A.activation(out=sgm[si][:, :], in_=pgt[bi][:, :], func=AF.Sigmoid),
                             reads=[('pgt', bi)], writes=[('sgm', si)])
                        P.op('dve', lambda bi=bi, si=si: V.tensor_tensor(out=tmpI[si][:, :], in0=ppe[bi][:, :], in1=sgm[si][:, :], op=ALU.mult),
                             reads=[('ppe', bi), ('sgm', si)], writes=[('tmpI', si)])
                        P.op('pool', lambda si=si, tb=tb, nt=nt: G.tensor_tensor(
                            out=x3[:, tb, nt * 512:(nt + 1) * 512], in0=x3[:, tb, nt * 512:(nt + 1) * 512], in1=tmpI[si][:, :], op=ALU.add),
                            reads=[('tmpI', si)], writes=[('x3o', tb)])
                P.barrier()
            with contextlib.ExitStack() as pi3:
                gf = sb("gf_sb", [128, D], F32, pi3)
                ot = [sb("ot%d" % i, [128, D], F32, pi3) for i in range(2)]
                jk = sb("jkI", [128, D], BF16, pi3)
                P.dma('sp', gf[:, :], gfin[:, :], writes=['gf'])
                for tb in range(8):
                    i = tb % 2
                    ss = st[:, 3 * i:3 * i + 1]
                    sd = st[:, 3 * i + 1:3 * i + 2]
                    rs = st[:, 3 * i + 2:3 * i + 3]
                    P.op('act', lambda tb=tb, ss=ss: A.activation(out=jk[:, :], in_=x3[:, tb, :], func=AF.Square, accum_out=ss),
                         writes=['jk', ('ss', i)])
                    P.op('dve', lambda ss=ss, sd=sd: V.tensor_scalar(out=sd, in0=ss, scalar1=1.0 / D, scalar2=EPS, op0=ALU.mult, op1=ALU.add),
                         reads=[('ss', i)], writes=[('sd', i)])
                    P.op('act', lambda sd=sd: A.activation(out=sd, in_=sd, func=AF.Sqrt), reads=[('sd', i)], writes=[('sd', i)])
                    P.op('dve', lambda sd=sd, rs=rs: V.reciprocal(out=rs, in_=sd), reads=[('sd', i)], writes=[('rs', i)])
                    P.op('dve', lambda tb=tb, rs=rs, i=i: V.scalar_tensor_tensor(
                        out=ot[i][:, :], in0=x3[:, tb, :], scalar=rs, in1=gf[:, :], op0=ALU.mult, op1=ALU.mult),
                        reads=[('rs', i), 'gf'], writes=[('ot', i)])
                    P.dma('sp', out[tb * 128:(tb + 1) * 128, :], ot[i][:, :], reads=[('ot', i)], writes=[('out', tb)])
                P.barrier()
    return nc


def _t5_bucket_static(dist):
    n = np.maximum(dist, 0)
    nf = np.maximum(n, 1).astype(np.float32)
    large = 16 + (np.log(nf / np.float32(16)) / np.float32(math.log(128 / 16)) * 16).astype(np.int32)
    large = np.minimum(large, 31)
    return np.where(n < 16, n, large)


def prep_inputs(x, p, g_mix, w_in, w_pool, pool_scale, rel_bias, w_out, g_ffn, w_up, conv_w, conv_b,
                w_down, g_ple, w_ple_gate, w_ple_proj, g_final):
    f = np.float32
    x = np.asarray(x, f)
    p = np.asarray(p, f)[0]
    shared = {}
    shared["w_in"] = np.ascontiguousarray(np.asarray(w_in, f)[0])
    shared["w_pool"] = np.ascontiguousarray(np.asarray(w_pool, f)[0])
    shared["w_out"] = np.ascontiguousarray(np.asarray(w_out, f)[0])
    shared["w_up"] = np.ascontiguousarray(np.asarray(w_up, f)[0])
    shared["w_down"] = np.ascontiguousarray(np.asarray(w_down, f)[0])
    shared["w_ple_gate"] = np.ascontiguousarray(np.asarray(w_ple_gate, f)[0])
    shared["w_ple_proj"] = np.ascontiguousarray(np.asarray(w_ple_proj, f)[0])

    def fm(v):
        a = np.asarray(v, f).reshape(16, 128).T
        return np.ascontiguousarray(np.repeat(a[:, :, None], 128, axis=2).reshape(128, 2048))
    shared["gT3"] = np.stack([fm(np.asarray(g_mix)[0]), fm(np.asarray(g_ffn)[0]), fm(np.asarray(g_ple)[0])], 0)
    shared["gfin"] = np.ascontiguousarray(np.repeat(np.asarray(g_final, f)[None, :], 128, axis=0))
    shared["pscale"] = np.ascontiguousarray(np.asarray(pool_scale, f)[0].reshape(8, 128).T)
    cw = np.asarray(conv_w, f)[0]
    cb = np.asarray(conv_b, f)[0]
    cp = np.concatenate([cw, cb[None, :]], 0)
    shared["convp"] = np.ascontiguousarray(cp.reshape(4, 2 * NFC, 128).transpose(2, 1, 0))
    rb = np.asarray(rel_bias, f)
    sl = np.arange(128)[:, None]
    dl = np.arange(896)[None, :] - 256
    dist = dl - sl
    bk = _t5_bucket_static(dist)
    stripv = rb[bk]
    shared["strip"] = np.ascontiguousarray(stripv.transpose(0, 2, 1))
    shared["cfar"] = np.ascontiguousarray(np.repeat(rb[31][None, :], 128, axis=0))
    tl = np.arange(128)[:, None]
    s_l = np.arange(128)[None, :]
    shared["causal"] = np.where(s_l <= tl, 0.0, NEG).astype(f)
    shared["ident"] = np.eye(128, dtype=f)
    shared["halfs"] = np.ascontiguousarray(np.repeat((0.5 ** np.arange(1, NIT + 2, dtype=np.float64)).astype(f)[None, :], 128, 0))
    in_maps = []
    for c in range(8):
        b, j = c // 4, c % 4
        T0 = j * 1024
        m = dict(shared)
        xc = np.zeros((CTX, D), f)
        lo = T0 - 3072
        s0 = max(0, -lo)
        xc[s0:] = x[b, lo + s0:T0 + 1024]
        m["xctx"] = xc
        m["p_own"] = np.ascontiguousarray(p[b, T0:T0 + 1024])
        sbias = np.zeros((CTX,), f)
        sbias[:s0] = NEG
        m["slotb"] = np.ascontiguousarray(np.repeat(sbias[None, :], 128, axis=0))
        ic = np.zeros((128, 8, 16), f)
        for k in range(8):
            w = (2, 4, 8, 16)[k // 2]
            tok = T0 + np.arange(16)
            cntv = np.minimum(tok + 1, w).astype(f)
            ic[:, k, :] = (np.float32(1.0) / cntv)[None, :]
        m["invc"] = ic
        m["hflag"] = np.full((128, 1), 1.0 if j > 0 else 0.0, f)
        in_maps.append(m)
    return in_maps


_NC_CACHE = {}


def kernel(**inputs):
    in_maps = prep_inputs(**inputs)
    if 'nc' not in _NC_CACHE:
        _NC_CACHE['nc'] = build()
    nc = _NC_CACHE['nc']
    res = run_bass_kernel_spmd(nc, in_maps, core_ids=list(range(8)))
    outs = [np.asarray(res.results[c]["out"], np.float32).reshape(1024, D) for c in range(8)]
    full = np.zeros((2, SEQ, D), np.float32)
    for c in range(8):
        full[c // 4, (c % 4) * 1024:(c % 4 + 1) * 1024] = outs[c]
    return full
```

```python
import contextlib
import math
import numpy as np
import concourse.bass as bass
import concourse.mybir as mybir
from concourse.bass_utils import run_bass_kernel_spmd

F32 = mybir.dt.float32
BF16 = mybir.dt.bfloat16
AF = mybir.ActivationFunctionType
ALU = mybir.AluOpType
AX = mybir.AxisListType

D = 2048
SEQ = 4096
CTX = 4096
NQB = 9
QS0 = 2944
NQ = NQB * 128
HM0 = 2816
NHM = 1280
D_FF = 5632
NFC = 44
EPS = 1e-6
NEG = -1.0e30
NIT = 16
OFF = dict(pool=0, q=1024, k=2048, v=3072, qi=4096, ki=5120, wi=5184)


class Prog:
    NDS = 12

    def __init__(self, nc, es, same_sync=True):
        self.nc = nc
        self.same_sync = same_sync
        self.eng = {'pe': nc.tensor, 'act': nc.scalar, 'dve': nc.vector, 'pool': nc.gpsimd, 'sp': nc.sync}
        self.csem = {e: es.enter_context(nc.semaphore('c_' + e)) for e in ['pe', 'act', 'dve', 'pool']}
        self.cnt = {e: 0 for e in self.csem}
        self.dsem = {q: [es.enter_context(nc.semaphore('d_%s%d' % (q, i))) for i in range(self.NDS)]
                     for q in ['sp', 'pool']}
        self.duse = {q: [0] * self.NDS for q in self.dsem}
        self.dnext = {q: 0 for q in self.dsem}
        self.seen = {e: {} for e in self.eng}
        self.lastw = {}
        self.rds = {}
        self.nwait = 0

    def _wait(self, e, tok):
        sem, val, sid = tok
        if sid == e and (e == 'pe' or not self.same_sync):
            return
        if self.seen[e].get(sid, 0) >= val:
            return
        self.eng[e].wait_ge(sem, val)
        self.seen[e][sid] = val
        self.nwait += 1

    def _deps(self, reads, writes):
        deps = {}

        def add(tok):
            sid = tok[2]
            if sid not in deps or deps[sid][1] < tok[1]:
                deps[sid] = tok
        for k in reads:
            if k in self.lastw:
                add(self.lastw[k])
        for k in writes:
            if k in self.lastw:
                add(self.lastw[k])
            for tok in self.rds.get(k, {}).values():
                add(tok)
        return deps.values()

    def _record(self, tok, reads, writes):
        sid = tok[2]
        for k in reads:
            d = self.rds.setdefault(k, {})
            if sid not in d or d[sid][1] < tok[1]:
                d[sid] = tok
        for k in writes:
            self.lastw[k] = tok
            self.rds[k] = {}

    def op(self, e, fn, reads=(), writes=()):
        for tok in self._deps(reads, writes):
            self._wait(e, tok)
        ins = fn()
        self.cnt[e] += 1
        ins.then_inc(self.csem[e], 1)
        self._record((self.csem[e], self.cnt[e], e), reads, writes)

    def dma(self, q, out, in_, reads=(), writes=(), **kw):
        for tok in self._deps(reads, writes):
            self._wait(q, tok)
        k = self.dnext[q]
        self.dnext[q] = (k + 1) % self.NDS
        u = self.duse[q][k]
        sem = self.dsem[q][k]
        if u > 0:
            self._wait(q, (sem, 16 * u, (q, k)))
        ins = self.eng[q].dma_start(out=out, in_=in_, **kw)
        ins.then_inc(sem, 16)
        self.duse[q][k] = u + 1
        self._record((sem, 16 * (u + 1), (q, k)), reads, writes)

    def barrier(self):
        for e in self.eng:
            for c in self.csem:
                if self.cnt[c] > 0:
                    self._wait_force(e, (self.csem[c], self.cnt[c], c))
            for q in self.dsem:
                for k in range(self.NDS):
                    if self.duse[q][k] > 0:
                        self._wait_force(e, (self.dsem[q][k], 16 * self.duse[q][k], (q, k)))
        self.lastw.clear()
        self.rds.clear()

    def _wait_force(self, e, tok):
        sem, val, sid = tok
        if self.seen[e].get(sid, 0) >= val:
            return
        self.eng[e].wait_ge(sem, val)
        self.seen[e][sid] = val
        self.nwait += 1


def build(debug=False, stop_after='Z'):
    nc = bass.Bass("TRN2", target_bir_lowering=False)

    def din(name, shape, dt=F32):
        return nc.dram_tensor(name, list(shape), dt, kind="ExternalInput").ap()

    xctx = din("xctx", [CTX, D])
    p_own = din("p_own", [1024, 256])
    w_in = din("w_in", [D, 5200])
    w_pool = din("w_pool", [4, 256, 256])
    w_out = din("w_out", [D, D])
    w_up = din("w_up", [D, 2 * D_FF])
    w_down = din("w_down", [D_FF, D])
    w_pg = din("w_ple_gate", [D, D])
    w_pp = din("w_ple_proj", [256, D])
    gT3 = din("gT3", [3, 128, 2048])
    gfin = din("gfin", [128, 2048])
    pscale = din("pscale", [128, 8])
    convp = din("convp", [128, 2 * NFC, 4])
    strip = din("strip", [128, 8, 896])
    cfar = din("cfar", [128, 8])
    slotb = din("slotb", [128, CTX])
    causal = din("causal", [128, 128])
    ident = din("ident", [128, 128])
    invc = din("invc", [128, 8, 16])
    hflag = din("hflag", [128, 1])
    halfs = din("halfs", [128, NIT + 1])
    out = nc.dram_tensor("out", [1024, D], F32, kind="ExternalOutput").ap()
    ks = "ExternalOutput" if debug else "Internal"
    KT_d = nc.dram_tensor("KT_d", [8, 128, CTX], BF16, kind=ks).ap()
    V_d = nc.dram_tensor("V_d", [8, CTX, 128], BF16, kind=ks).ap()
    x1_d = nc.dram_tensor("x1_d", [NQ, D], F32, kind=ks).ap()
    x2_d = nc.dram_tensor("x2_d", [1024, D], F32, kind=ks).ap()
    kidxT_d = nc.dram_tensor("kidxT_d", [128, CTX], BF16, kind=ks).ap()
    QT_d = nc.dram_tensor("QT_d", [128, 8, NQ], BF16, kind=ks).ap()
    qiT_d = nc.dram_tensor("qiT_d", [128, 8, NQ], BF16, kind=ks).ap()
    ypT_d = nc.dram_tensor("ypT_d", [128, 8, NQ], BF16, kind=ks).ap()
    attnT_d = nc.dram_tensor("attnT_d", [128, 8, NQ], BF16, kind=ks).ap()
    widx_d = nc.dram_tensor("widx_d", [128, NQB, 16], F32, kind=ks).ap()
    dbg = {}
    if debug:
        dbg['maskT'] = nc.dram_tensor("dbg_maskT", [128, 32, NQ], BF16, kind="ExternalOutput").ap()
        dbg['thr'] = nc.dram_tensor("dbg_thr", [128, NQB], F32, kind="ExternalOutput").ap()
        dbg['gT'] = nc.dram_tensor("dbg_gT", [128, NFC, 1024], BF16, kind="ExternalOutput").ap()

    with contextlib.ExitStack() as es:
        P = Prog(nc, es)
        T = nc.tensor
        A = nc.scalar
        V = nc.vector
        G = nc.gpsimd

        def sb(name, shape, dt, stack=es):
            return stack.enter_context(nc.sbuf_tensor(name, list(shape), dt))

        def ps(name, shape, dt, stack):
            return stack.enter_context(nc.psum_tensor(name, list(shape), dt))

        ident_f = sb("ident_f", [128, 128], F32)
        ident_b = sb("ident_b", [128, 128], BF16)
        ones_b = sb("ones_b", [128, 128], BF16)
        st = sb("st", [128, 16], F32)
        P.dma('sp', ident_f[:], ident[:, :], writes=['ident_f'])
        P.op('dve', lambda: V.tensor_copy(out=ident_b[:], in_=ident_f[:]), reads=['ident_f'], writes=['ident_b'])
        P.op('dve', lambda: V.memset(ones_b[:], 1.0), writes=['ones_b'])

        def alloc(name, shape, dt):
            cm = nc.sbuf_tensor(name, list(shape), dt)
            return cm, cm.__enter__()

        def norm_T(i, xin, xin_key, xn, pT, gsb, dst_fn, dst_keys, extra=None):
            ss = st[:, 3 * i:3 * i + 1]
            sd = st[:, 3 * i + 1:3 * i + 2]
            rs = st[:, 3 * i + 2:3 * i + 3]
            P.op('act', lambda: A.activation(out=xn[i][:], in_=xin, func=AF.Square, accum_out=ss),
                 reads=[xin_key], writes=[('xn', i), ('ss', i)])
            P.op('dve', lambda: V.tensor_scalar(out=sd, in0=ss, scalar1=1.0 / D, scalar2=EPS, op0=ALU.mult, op1=ALU.add),
                 reads=[('ss', i)], writes=[('sd', i)])
            P.op('act', lambda: A.activation(out=sd, in_=sd, func=AF.Sqrt), reads=[('sd', i)], writes=[('sd', i)])
            P.op('dve', lambda: V.reciprocal(out=rs, in_=sd), reads=[('sd', i)], writes=[('rs', i)])
            if extra is not None:
                P.op('dve', lambda: V.tensor_tensor(out=rs, in0=rs, in1=extra, op=ALU.mult),
                     reads=[('rs', i), 'hflag'], writes=[('rs', i)])
            P.op('dve', lambda: V.tensor_scalar(out=xn[i][:], in0=xin, scalar1=rs, scalar2=None, op0=ALU.mult),
                 reads=[xin_key, ('rs', i)], writes=[('xn', i)])
            for c in range(16):
                P.op('pe', lambda c=c: T.transpose(out=pT[i][c // 8][:, (c % 8) * 128:(c % 8 + 1) * 128],
                                                   in_=xn[i][:, c * 128:(c + 1) * 128], identity=ident_b[:]),
                     reads=[('xn', i), 'ident_b'], writes=[('pT', i, c // 8)])
            for h in range(2):
                P.op('dve', lambda h=h: V.tensor_tensor(
                    out=dst_fn(h), in0=pT[i][h][:, :].rearrange("p (c t) -> p c t", t=128),
                    in1=gsb[:, 8 * h:8 * h + 8, :], op=ALU.mult),
                    reads=[('pT', i, h), 'gsb'], writes=dst_keys)

        def wload(dst, dkey, src_rows, c0, c1, col0, ncol, q='pool'):
            P.dma(q, dst[:, c0:c1, 0:ncol],
                  src_rows[c0 * 128:c1 * 128, col0:col0 + ncol].rearrange("(c p) n -> p c n", p=128),
                  writes=[dkey])

        with contextlib.ExitStack() as pa:
            kidxT = sb("kidxT", [128, CTX], BF16, pa)
            Wk = sb("Wk", [128, 16, 1024], BF16, pa)
            Wv = sb("Wv", [128, 16, 1024], BF16, pa)
            Wki = sb("Wki", [128, 16, 128], BF16, pa)
            gsb = sb("gsbA", [128, 16, 128], F32, pa)
            xb = [sb("xbA%d" % i, [128, D], F32, pa) for i in range(2)]
            xn = [sb("xnA%d" % i, [128, D], BF16, pa) for i in range(2)]
            hT = [sb("hTA%d" % i, [128, 16, 512], BF16, pa) for i in range(2)]
            KTst = [sb("KTst%d" % i, [128, 8, 512], BF16, pa) for i in range(2)]
            Vst = [sb("Vst%d" % i, [128, 4, 1024], BF16, pa) for i in range(2)]
            pT = [[ps("pTA%d%d" % (i, h), [128, 1024], BF16, pa) for h in range(2)] for i in range(2)]
            pm = [ps("pmA%d" % i, [128, 512], F32, pa) for i in range(4)]

            P.dma('sp', gsb[:].rearrange("p c t -> p (c t)"), gT3[0, :, :], writes=['gsb'])
            for cg in range(4):
                wload(Wk, ('Wk', cg), w_in, 4 * cg, 4 * cg + 4, OFF['k'], 1024)
            for cg in range(4):
                wload(Wv, ('Wv', cg), w_in, 4 * cg, 4 * cg + 4, OFF['v'], 1024)
            P.dma('pool', Wki[:, :, 0:64], w_in[:, OFF['ki']:OFF['ki'] + 64].rearrange("(c p) n -> p c n", p=128),
                  writes=['Wki'])
            P.dma('pool', Wki[:, :, 64:128], w_in[:, OFF['ki']:OFF['ki'] + 64].rearrange("(c p) n -> p c n", p=128),
                  writes=['Wki'])
            pmi = [0]

            def a_load(ct, sbl):
                j = ct * 4 + sbl
                P.dma('sp', xb[j % 2][:], xctx[j * 128:(j + 1) * 128, :], writes=[('xb', j % 2)])

            def a_norm(ct, sbl):
                b = ct % 2
                j = ct * 4 + sbl
                i = j % 2
                norm_T(i, xb[i][:], ('xb', i), xn, pT, gsb,
                       lambda h: hT[b][:, 8 * h:8 * h + 8, sbl * 128:(sbl + 1) * 128], [('hT', b, sbl)])

            def a_k_heads(ct, heads):
                b = ct % 2
                hkeys = [('hT', b, s_) for s_ in range(4)]
                for h in heads:
                    pq = pm[pmi[0] % 4]
                    pk = ('pm', pmi[0] % 4)
                    pmi[0] += 1
                    for c in range(16):
                        P.op('pe', lambda c=c: T.matmul(pq[:, :], lhsT=Wk[:, c, h * 128:(h + 1) * 128],
                                                        rhs=hT[b][:, c, :], start=(c == 0), stop=(c == 15)),
                             reads=hkeys + [('Wk', c // 4)], writes=[pk])
                    P.op('act', lambda: A.activation(out=KTst[b][:, h, :], in_=pq[:, :], func=AF.Copy),
                         reads=[pk], writes=[('KTst', b)])

            def a_kidx(ct):
                b = ct % 2
                hkeys = [('hT', b, s_) for s_ in range(4)]
                pq = pm[pmi[0] % 4]
                pk = ('pm', pmi[0] % 4)
                pmi[0] += 1
                for c in range(16):
                    P.op('pe', lambda c=c: T.matmul(pq[:, :], lhsT=Wki[:, c, :], rhs=hT[b][:, c, :],
                                                    start=(c == 0), stop=(c == 15)),
                         reads=hkeys + ['Wki'], writes=[pk])
                P.op('act', lambda: A.activation(out=kidxT[:, ct * 512:(ct + 1) * 512], in_=pq[:, :], func=AF.Copy),
                     reads=[pk], writes=[('kidxT', ct)])
                P.dma('pool', KT_d[:, :, ct * 512:(ct + 1) * 512].rearrange("h d s -> d h s"), KTst[b][:, :, :],
                      reads=[('KTst', b)], writes=[('KT_d', ct)])

            def a_v(ct, sbls):
                b = ct % 2
                for sbl in sbls:
                    for hf in range(2):
                        pq = pm[pmi[0] % 4]
                        pk = ('pm', pmi[0] % 4)
                        pmi[0] += 1
                        for c in range(16):
                            P.op('pe', lambda c=c: T.matmul(
                                pq[:, :], lhsT=hT[b][:, c, sbl * 128:(sbl + 1) * 128],
                                rhs=Wv[:, c, hf * 512:(hf + 1) * 512], start=(c == 0), stop=(c == 15)),
                                reads=[('hT', b, sbl), ('Wv', c // 4)], writes=[pk])
                        if hf == 0:
                            P.op('act', lambda: A.activation(
                                out=Vst[b][:, sbl, hf * 512:(hf + 1) * 512], in_=pq[:, :], func=AF.Copy),
                                reads=[pk], writes=[('Vst', b, sbl)])
                        else:
                            P.op('dve', lambda: V.tensor_copy(
                                out=Vst[b][:, sbl, hf * 512:(hf + 1) * 512], in_=pq[:, :]),
                                reads=[pk], writes=[('Vst', b, sbl)])
                    r0 = ct * 512 + sbl * 128
                    P.dma('pool', V_d[:, r0:r0 + 128, :].rearrange("h p d -> p h d"),
                          Vst[b][:, sbl, :].rearrange("p (h d) -> p h d", d=128),
                          reads=[('Vst', b, sbl)], writes=[('V_d', ct, sbl)])

            for sbl in range(4):
                a_load(0, sbl)
                a_norm(0, sbl)
            NCT = CTX // 512
            for ct in range(NCT):
                parts = [lambda ct=ct: a_k_heads(ct, range(0, 4)),
                         lambda ct=ct: (a_k_heads(ct, range(4, 8)), a_kidx(ct)),
                         lambda ct=ct: a_v(ct, (0, 1)),
                         lambda ct=ct: a_v(ct, (2, 3))]
                for q_ in range(4):
                    if ct + 1 < NCT:
                        a_load(ct + 1, q_)
                    parts[q_]()
                    if ct + 1 < NCT:
                        a_norm(ct + 1, q_)
            P.dma('sp', kidxT_d[:, :], kidxT[:, :], reads=[('kidxT', c_) for c_ in range(8)])
            P.barrier()
        if stop_after == 'A':
            return nc


        with contextlib.ExitStack() as pb:
            QT = sb("QT", [128, 8, NQ], BF16, pb)
            qiT = sb("qiT", [128, 8, NQ], BF16, pb)
            ypT = sb("ypT", [128, 8, NQ], BF16, pb)
            widx = sb("widx", [128, NQB, 16], F32, pb)
            hM = sb("hM", [128, 16, NHM], BF16, pb)
            Ws = [sb("WsB%d" % i, [128, 16, 512], BF16, pb) for i in range(2)]
            wgroups = [OFF['q'], OFF['q'] + 512, OFF['qi'], OFF['qi'] + 512, OFF['pool'], OFF['pool'] + 512]
            wissued = [0]

            def issue_w():
                k = wissued[0]
                if k < len(wgroups):
                    wload(Ws[k % 2], ('Ws', k % 2), w_in, 0, 16, wgroups[k], 512)
                    wissued[0] += 1
            issue_w()
            issue_w()
            with contextlib.ExitStack() as pb1:
                gsb = sb("gsbB", [128, 16, 128], F32, pb1)
                xb = [sb("xbB%d" % i, [128, D], F32, pb1) for i in range(2)]
                xn = [sb("xnB%d" % i, [128, D], BF16, pb1) for i in range(2)]
                pT = [[ps("pTB%d%d" % (i, h), [128, 1024], BF16, pb1) for h in range(2)] for i in range(2)]
                P.dma('sp', gsb[:].rearrange("p c t -> p (c t)"), gT3[0, :, :], writes=['gsb'])
                for j in range(NHM // 128):
                    i = j % 2
                    P.dma('sp', xb[i][:], xctx[HM0 + j * 128:HM0 + (j + 1) * 128, :], writes=[('xb', i)])
                    norm_T(i, xb[i][:], ('xb', i), xn, pT, gsb,
                           lambda h, j=j: hM[:, 8 * h:8 * h + 8, j * 128:(j + 1) * 128], [('hM', j)])
                P.barrier()
            pp = [ps("ppB%d" % i, [128, 4, 512], F32, pb) for i in range(2)]
            Wpl = sb("Wpl", [128, 4, 2, 256], BF16, pb)
            Ww = sb("Ww", [128, 16, 16], BF16, pb)
            psc_sb = sb("pscale_sb", [128, 8], F32, pb)
            invc_sb = sb("invc_sb", [128, 8, 16], F32, pb)
            pooledT = sb("pooledT", [128, 8, NQ], BF16, pb)
            uk = [sb("uk%d" % i, [128, 1168], F32, pb) for i in range(2)]
            sA = sb("sA", [128, 1168], F32, pb)
            sB = sb("sB", [128, 1168], F32, pb)
            t16 = sb("t16", [128, 16], F32, pb)
            P.dma('sp', psc_sb[:], pscale[:, :], writes=['pscale'])
            P.dma('sp', invc_sb[:], invc[:, :, :], writes=['invc'])
            for g in range(4):
                P.dma('pool', Wpl[:, g, :, :], w_pool[g, :, :].rearrange("(cc p) d -> p cc d", p=128), writes=['Wpl'])
            P.dma('pool', Ww[:, :, :], w_in[:, OFF['wi']:OFF['wi'] + 16].rearrange("(c p) n -> p c n", p=128), writes=['Ww'])
            wcnt = [0]
            pcnt = [0]

            def next_w(col0):
                b = wcnt[0] % 2
                assert wgroups[wcnt[0]] == col0
                if wcnt[0] >= 1:
                    issue_w()
                wcnt[0] += 1
                return b

            def next_pp():
                b = pcnt[0] % 2
                pcnt[0] += 1
                return b

            for (dst, off) in ((QT, OFF['q']), (qiT, OFF['qi'])):
                for g in range(2):
                    wb = next_w(off + 512 * g)
                    for m_ in range(4):
                        pb_ = next_pp()
                        for n in range(3):
                            for c in range(16):
                                P.op('pe', lambda c=c, n=n, wb=wb, m_=m_, pb_=pb_: T.matmul(
                                    pp[pb_][:, n, 0:384], lhsT=Ws[wb][:, c, m_ * 128:(m_ + 1) * 128],
                                    rhs=hM[:, c, 128 + n * 384:128 + (n + 1) * 384], start=(c == 0), stop=(c == 15)),
                                    reads=[('Ws', wb)], writes=[('pp', pb_)])
                        P.op('act', lambda dst=dst, g=g, m_=m_, pb_=pb_: A.activation(
                            out=dst[:, 4 * g + m_, :].rearrange("p (n t) -> p n t", t=384),
                            in_=pp[pb_][:, 0:3, 0:384], func=AF.Copy),
                            reads=[('pp', pb_)], writes=[('fm', id(dst), 4 * g + m_)])
            for g2 in range(2):
                wb = next_w(OFF['pool'] + 512 * g2)
                for m_ in range(4):
                    k = 4 * g2 + m_
                    g = k // 2
                    pb_ = next_pp()
                    for n in range(4):
                        for c in range(16):
                            P.op('pe', lambda c=c, n=n, wb=wb, m_=m_, pb_=pb_: T.matmul(
                                pp[pb_][:, n, 0:292], lhsT=Ws[wb][:, c, m_ * 128:(m_ + 1) * 128],
                                rhs=hM[:, c, 112 + n * 292:112 + (n + 1) * 292], start=(c == 0), stop=(c == 15)),
                                reads=[('Ws', wb)], writes=[('pp', pb_)])
                    u = uk[k % 2]
                    P.op('act', lambda u=u, pb_=pb_: A.activation(
                        out=u[:, :].rearrange("p (n t) -> p n t", t=292), in_=pp[pb_][:, 0:4, 0:292], func=AF.Copy),
                        reads=[('pp', pb_)], writes=[('uk', k % 2)])
                    wdw = (2, 4, 8, 16)[g]
                    cur, ckey = u, ('uk', k % 2)
                    for s_ in range(int(math.log2(wdw))):
                        sh = 1 << s_
                        nxt, nkey = (sA, 'sA') if s_ % 2 == 0 else (sB, 'sB')
                        P.op('pool', lambda cur=cur, nxt=nxt, sh=sh: G.tensor_tensor(
                            out=nxt[:, sh:1168], in0=cur[:, sh:1168], in1=cur[:, 0:1168 - sh], op=ALU.add),
                            reads=[ckey], writes=[nkey])
                        cur, ckey = nxt, nkey
                    P.op('dve', lambda cur=cur, u=u, k=k, wdw=wdw: V.scalar_tensor_tensor(
                        out=pooledT[:, k, :], in0=cur[:, 16:1168], scalar=1.0 / wdw, in1=u[:, 16:1168],
                        op0=ALU.mult, op1=ALU.subtract), reads=[ckey, ('uk', k % 2)], writes=[('pooledT', k)])
                    P.op('dve', lambda cur=cur, k=k: V.tensor_tensor(out=t16[:, :], in0=cur[:, 144:160], in1=invc_sb[:, k, :], op=ALU.mult),
                         reads=[ckey, 'invc'], writes=['t16'])
                    P.op('dve', lambda u=u, k=k: V.tensor_tensor(out=pooledT[:, k, 128:144], in0=t16[:, :], in1=u[:, 144:160], op=ALU.subtract),
                         reads=['t16', ('uk', k % 2)], writes=[('pooledT', k)])
            for k2 in range(8):
                g, dm = k2 // 2, k2 % 2
                pb_ = next_pp()
                for n in range(3):
                    for cc in range(2):
                        P.op('pe', lambda n=n, cc=cc, g=g, dm=dm, pb_=pb_: T.matmul(
                            pp[pb_][:, n, 0:384], lhsT=Wpl[:, g, cc, dm * 128:(dm + 1) * 128],
                            rhs=pooledT[:, 2 * g + cc, n * 384:(n + 1) * 384], start=(cc == 0), stop=(cc == 1)),
                            reads=['Wpl', ('pooledT', 2 * g + cc)], writes=[('pp', pb_)])
                P.op('act', lambda k2=k2, pb_=pb_: A.activation(
                    out=ypT[:, k2, :].rearrange("p (n t) -> p n t", t=384), in_=pp[pb_][:, 0:3, 0:384],
                    func=AF.Copy, scale=psc_sb[:, k2:k2 + 1]), reads=[('pp', pb_), 'pscale'], writes=[('ypT', k2)])
            idx_scale = (16 ** -0.5) * (64 ** -0.5)
            for i in range(NQB):
                pb_ = next_pp()
                for c in range(16):
                    P.op('pe', lambda c=c, i=i, pb_=pb_: T.matmul(
                        pp[pb_][:, 0, 0:16], lhsT=hM[:, c, 128 + i * 128:256 + i * 128], rhs=Ww[:, c, :],
                        start=(c == 0), stop=(c == 15)), reads=['Ww'], writes=[('pp', pb_)])
                P.op('act', lambda i=i, pb_=pb_: A.activation(out=widx[:, i, :], in_=pp[pb_][:, 0, 0:16], func=AF.Copy, scale=idx_scale),
                     reads=[('pp', pb_)], writes=[('widx', i)])
            P.dma('sp', QT_d[:, :, :], QT[:, :, :], reads=[('fm', id(QT), h_) for h_ in range(8)])
            P.dma('sp', qiT_d[:, :, :], qiT[:, :, :], reads=[('fm', id(qiT), h_) for h_ in range(8)])
            P.dma('sp', ypT_d[:, :, :], ypT[:, :, :], reads=[('ypT', h_) for h_ in range(8)])
            P.dma('sp', widx_d[:, :, :], widx[:, :, :], reads=[('widx', h_) for h_ in range(NQB)])
            P.barrier()
        if stop_after == 'B':
            return nc

        pcd = contextlib.ExitStack()
        es.enter_context(pcd)
        maskT = sb("maskT", [128, 32, NQ], BF16, pcd)
        with contextlib.ExitStack() as pc:
            kidxT = sb("kidxTc", [128, CTX], BF16, pc)
            qiT = sb("qiTc", [128, 8, NQ], BF16, pc)
            widx = sb("widxc", [128, NQB, 16], F32, pc)
            P.dma('sp', kidxT[:, :], kidxT_d[:, :], writes=['kidxT'])
            P.dma('sp', qiT[:, :, :], qiT_d[:, :, :], writes=['qiT'])
            P.dma('sp', widx[:, :, :], widx_d[:, :, :], writes=['widx'])
            slot_row = sb("slot_row", [1, CTX], BF16, pc)
            ones_row = sb("ones_row", [1, 128], BF16, pc)
            causal_b = sb("causal_b", [128, 128], BF16, pc)
            halfs_sb = sb("halfs_sb", [128, NIT + 1], F32, pc)
            sc = [sb("sc%d" % i, [128, CTX], F32, pc) for i in range(2)]
            junk = sb("junk", [128, CTX], BF16, pc)
            Rb = sb("Rb", [128, 4, 512], BF16, pc)
            Dg = [sb("Dg%d" % i, [128, 16, 128], BF16, pc) for i in range(2)]
            bsx = [sb("bsx%d" % i, [128, 32], F32, pc) for i in range(2)]
            bs = sb("bs", [128, 64], F32, pc)
            thr_all = sb("thr_all", [128, NQB], F32, pc)
            pd = ps("pdC", [128, 4, 512], F32, pc)
            psc = [ps("pscC%d" % i, [128, 512], F32, pc) for i in range(3)]
            ptm = [ps("ptmC%d" % i, [128, 1024], BF16, pc) for i in range(1)]
            P.dma('pool', slot_row[0:1, :], slotb[0:1, :], writes=['slot_row'])
            P.dma('pool', causal_b[:, :], causal[:, :], writes=['causal_b'])
            P.dma('sp', halfs_sb[:], halfs[:, :], writes=['halfs_sb'])
            P.op('pool', lambda: G.memset(ones_row[0:1, :], 1.0), writes=['ones_row'])
            P.op('pool', lambda: G.memset(maskT[:, :, :].rearrange("p a b -> p (a b)"), 0.0), writes=['maskT'])
            mid = bs[:, 3:4]
            cntv = bs[:, 4:5]
            gg = bs[:, 5:6]
            lo = bs[:, 6:7]
            hi = bs[:, 7:8]
            w0 = bs[:, 8:9]
            hk = bs[:, 16:16 + NIT + 1]
            cnts = dict(d=0, s=0, t=0)

            def gen_scores(i, bi):
                E = QS0 + 128 * (i + 1)
                nkt = (E + 511) // 512
                mins = bsx[bi][:, 0:8]
                maxs = bsx[bi][:, 8:16]
                for h in range(16):
                    P.op('pool', lambda h=h: G.tensor_scalar(out=Dg[bi][:, h, :], in0=ident_f[:], scalar1=widx[:, i, h:h + 1],
                                                             scalar2=1.0, op0=ALU.mult, op1=ALU.mult),
                         reads=['ident_f', 'widx'], writes=[('Dg', bi, h)])
                prev = None

                def finish(kt, N, pst, pskey):
                    P.op('pe', lambda: T.matmul(pst[:, :N], lhsT=ones_row[0:1, :], rhs=slot_row[0:1, kt * 512:kt * 512 + N],
                                                start=False, stop=(kt != nkt - 1)),
                         reads=['ones_row', 'slot_row', ('mins', bi, kt), ('maxs', bi, kt)], writes=[pskey])
                    if kt == nkt - 1:
                        P.op('pe', lambda: T.matmul(pst[:, N - 128:N], lhsT=ident_b[:, :], rhs=causal_b[:, :], start=False, stop=True),
                             reads=['ident_b', 'causal_b'], writes=[pskey])
                    P.op('act', lambda: A.activation(out=sc[bi][:, kt * 512:kt * 512 + N], in_=pst[:, :N], func=AF.Copy),
                         reads=[pskey], writes=[('sc', bi, kt)])
                for kt in range(nkt):
                    N = min(512, E - kt * 512)
                    si = cnts['s'] % 3
                    cnts['s'] += 1
                    pst = psc[si]
                    pskey = ('psc', si)
                    pend = []

                    def score_mm(hp, di, N=N, pst=pst, pskey=pskey):
                        for q_ in range(2):
                            h = 2 * hp + q_
                            P.op('pe', lambda h=h, q_=q_: T.matmul(pst[:, :N], lhsT=Dg[bi][:, h, :], rhs=Rb[:, 2 * di + q_, :N],
                                                                   start=(h == 0), stop=False),
                                 reads=[('Dg', bi, h), ('Rb', di)], writes=[pskey])
                    for hp in range(8):
                        di = cnts['d'] % 2
                        cnts['d'] += 1
                        pdk = ('pd', di)
                        for q_ in range(2):
                            pr = q_ * 64
                            P.op('pe', lambda q_=q_, pr=pr: T.matmul(
                                pd[:, 2 * di + q_, :N], lhsT=qiT[pr:pr + 64, hp, i * 128:(i + 1) * 128],
                                rhs=kidxT[pr:pr + 64, kt * 512:kt * 512 + N], start=True, stop=True),
                                reads=['kidxT', 'qiT'], writes=[pdk])
                        P.op('act', lambda di=di: A.activation(out=Rb[:, 2 * di:2 * di + 2, :N], in_=pd[:, 2 * di:2 * di + 2, :N], func=AF.Relu),
                             reads=[pdk], writes=[('Rb', di)])
                        pend.append((hp, di))
                        if len(pend) > 1:
                            score_mm(*pend.pop(0))
                    while pend:
                        score_mm(*pend.pop(0))
                    P.op('dve', lambda pst=pst, N=N, kt=kt: V.tensor_reduce(out=mins[:, kt:kt + 1], in_=pst[:, :N], axis=AX.X, op=ALU.min),
                         reads=[pskey], writes=[('mins', bi, kt)])
                    P.op('dve', lambda pst=pst, N=N, kt=kt: V.tensor_reduce(out=maxs[:, kt:kt + 1], in_=pst[:, :N], axis=AX.X, op=ALU.max),
                         reads=[pskey], writes=[('maxs', bi, kt)])
                    if prev is not None:
                        finish(*prev)
                    prev = (kt, N, pst, pskey)
                    yield
                finish(*prev)
                yield

            def bisect(i, bi, nxt):
                E = QS0 + 128 * (i + 1)
                nkt = (E + 511) // 512
                mins = bsx[bi][:, 0:8]
                maxs = bsx[bi][:, 8:16]
                sckeys = [('sc', bi, kt) for kt in range(nkt)]
                P.op('dve', lambda: V.tensor_reduce(out=lo, in_=mins[:, 0:nkt], axis=AX.X, op=ALU.min),
                     reads=[('mins', bi, kt) for kt in range(nkt)], writes=['lo'])
                P.op('dve', lambda: V.tensor_reduce(out=hi, in_=maxs[:, 0:nkt], axis=AX.X, op=ALU.max),
                     reads=[('maxs', bi, kt) for kt in range(nkt)], writes=['hi'])
                P.op('dve', lambda: V.scalar_tensor_tensor(out=w0, in0=hi, scalar=2.0, in1=lo, op0=ALU.add, op1=ALU.subtract),
                     reads=['hi', 'lo'], writes=['w0'])
                P.op('dve', lambda: V.tensor_scalar(out=hk, in0=halfs_sb[:, :], scalar1=w0, scalar2=None, op0=ALU.mult),
                     reads=['w0', 'halfs_sb'], writes=['hk'])
                P.op('dve', lambda: V.scalar_tensor_tensor(out=mid, in0=lo, scalar=-1.0, in1=hk[:, 0:1], op0=ALU.add, op1=ALU.add),
                     reads=['lo', 'hk'], writes=['mid'])
                for it in range(NIT):
                    P.op('dve', lambda: V.tensor_scalar(out=junk[:, 0:E], in0=sc[bi][:, 0:E], scalar1=mid, scalar2=None,
                                                        op0=ALU.is_ge, op1=ALU.add, accum_out=cntv),
                         reads=sckeys + ['mid'], writes=['junk', 'cnt'])
                    P.op('dve', lambda: V.tensor_scalar(out=gg, in0=cntv, scalar1=255.5, scalar2=0.5, op0=ALU.is_ge, op1=ALU.subtract),
                         reads=['cnt'], writes=['gg'])
                    P.op('dve', lambda it=it: V.scalar_tensor_tensor(out=mid, in0=gg, scalar=hk[:, it:it + 1], in1=mid,
                                                                      op0=ALU.mult, op1=ALU.add),
                         reads=['gg', 'hk', 'mid'], writes=['mid'])
                    if nxt is not None and it % 2 == 1:
                        next(nxt, None)
                if nxt is not None:
                    for _ in nxt:
                        pass
                P.op('dve', lambda: V.tensor_tensor(out=lo, in0=mid, in1=hk[:, NIT:NIT + 1], op=ALU.subtract),
                     reads=['mid', 'hk'], writes=['lo'])
                P.op('dve', lambda: V.tensor_scalar(out=junk[:, 0:E], in0=sc[bi][:, 0:E], scalar1=lo, scalar2=None, op0=ALU.is_ge),
                     reads=sckeys + ['lo'], writes=['junk'])
                P.op('dve', lambda: V.tensor_copy(out=thr_all[:, i:i + 1], in_=lo), reads=['lo'], writes=['thr_all'])
                nkb = E // 128
                for kb0 in range(0, nkb, 8):
                    n8 = min(8, nkb - kb0)
                    pt = ptm[0]
                    ptk = ('ptm', 0)
                    for r in range(n8):
                        kb = kb0 + r
                        P.op('pe', lambda r=r, kb=kb: T.transpose(out=pt[:, r * 128:(r + 1) * 128],
                                                                 in_=junk[:, kb * 128:(kb + 1) * 128], identity=ident_b[:]),
                             reads=['junk', 'ident_b'], writes=[ptk])
                    P.op('act', lambda n8=n8, kb0=kb0: A.activation(
                        out=maskT[:, kb0:kb0 + n8, i * 128:(i + 1) * 128],
                        in_=pt[:, 0:n8 * 128].rearrange("p (a b) -> p a b", b=128), func=AF.Copy),
                        reads=[ptk], writes=['maskT'])

            for _ in gen_scores(0, 0):
                pass
            for i in range(NQB):
                nxt = gen_scores(i + 1, (i + 1) % 2) if i + 1 < NQB else None
                bisect(i, i % 2, nxt)
            if debug:
                P.dma('sp', dbg['maskT'][:, :, :], maskT[:, :, :], reads=['maskT'])
                P.dma('sp', dbg['thr'][:, :], thr_all[:, :], reads=['thr_all'])
            P.barrier()
        if stop_after == 'C':
            return nc

        SCALE = 128 ** -0.5
        with contextlib.ExitStack() as pdd:
            attnT = sb("attnT", [128, 8, NQ], BF16, pdd)
            QT = sb("QTd", [128, 8, NQ], BF16, pdd)
            P.dma('sp', QT[:, :, :], QT_d[:, :, :], writes=['QT'])
            KTh = [sb("KTh%d" % i, [128, CTX], BF16, pdd) for i in range(2)]
            Vh = [sb("Vh%d" % i, [128, 32, 128], BF16, pdd) for i in range(2)]
            strip_sb = sb("strip_sb", [128, 8, 896], F32, pdd)
            cfar_sb = sb("cfar_sb", [128, 8], F32, pdd)
            Eb = [sb("Eb%d" % i, [128, 384], BF16, pdd) for i in range(4)]
            Pb = [sb("Pb%d" % i, [128, 384], BF16, pdd) for i in range(4)]
            tmpb = [sb("tmpb%d" % i, [128, 384], F32, pdd) for i in range(2)]
            rl = sb("rl", [128, 384], F32, pdd)
            pS = [ps("pS%d" % i, [128, 512], F32, pdd) for i in range(4)]
            pO = [ps("pO%d" % i, [128, 512], F32, pdd) for i in range(2)]
            pL = [ps("pL%d" % i, [128, 512], F32, pdd) for i in range(2)]
            P.dma('sp', strip_sb[:], strip[:, :, :], writes=['strip_sb'])
            P.dma('sp', cfar_sb[:], cfar[:, :], writes=['cfar_sb'])
            scn = 0
            ocn = 0
            tmc = 0
            for h in range(8):
                b = h % 2
                P.dma('sp', KTh[b][:, :], KT_d[h, :, :], writes=[('KTh', b)])
                P.dma('sp', Vh[b][:, :, :], V_d[h, :, :].rearrange("(kb p) d -> p kb d", p=128), writes=[('Vh', b)])
                for qt in range(3):
                    t0 = qt * 384
                    kbmax = 23 + 3 * qt + 2
                    po = pO[ocn % 2]
                    pl = pL[ocn % 2]
                    pok = ('pO', ocn % 2)
                    plk = ('pL', ocn % 2)
                    ocn += 1
                    staged = []

                    def stage1(kb):
                        nonlocal scn, tmc
                        s_i = scn % 4
                        scn += 1
                        pst = pS[s_i]
                        P.op('pe', lambda: T.matmul(pst[:, 0:384], lhsT=KTh[b][:, kb * 128:(kb + 1) * 128],
                                                    rhs=QT[:, h, t0:t0 + 384], start=True, stop=True),
                             reads=[('KTh', b), 'QT'], writes=[('pS', s_i)])
                        D0 = (QS0 + t0) - kb * 128
                        if D0 >= 256:
                            P.op('act', lambda: A.activation(out=Eb[s_i][:, :], in_=pst[:, 0:384], func=AF.Exp,
                                                             scale=SCALE, bias=cfar_sb[:, h:h + 1]),
                                 reads=[('pS', s_i), 'cfar_sb'], writes=[('Eb', s_i)])
                        else:
                            ti = tmc % 2
                            tmc += 1
                            P.op('dve', lambda: V.scalar_tensor_tensor(
                                out=tmpb[ti][:, :], in0=pst[:, 0:384], scalar=SCALE,
                                in1=strip_sb[:, h, D0 + 256:D0 + 256 + 384], op0=ALU.mult, op1=ALU.add),
                                reads=[('pS', s_i), 'strip_sb'], writes=[('tmpb', ti)])
                            P.op('act', lambda: A.activation(out=Eb[s_i][:, :], in_=tmpb[ti][:, :], func=AF.Exp),
                                 reads=[('tmpb', ti)], writes=[('Eb', s_i)])
                        P.op('dve', lambda: V.tensor_tensor(out=Pb[s_i][:, :], in0=Eb[s_i][:, :],
                                                            in1=maskT[:, kb, t0:t0 + 384], op=ALU.mult),
                             reads=[('Eb', s_i)], writes=[('Pb', s_i)])
                        staged.append((kb, s_i))

                    def stage2():
                        kb, s_i = staged.pop(0)
                        P.op('pe', lambda: T.matmul(po[:, 0:384], lhsT=Vh[b][:, kb, :], rhs=Pb[s_i][:, :],
                                                    start=(kb == 0), stop=(kb == kbmax)),
                             reads=[('Vh', b), ('Pb', s_i)], writes=[pok])
                        P.op('pe', lambda: T.matmul(pl[:, 0:384], lhsT=ones_b[:, :], rhs=Pb[s_i][:, :],
                                                    start=(kb == 0), stop=(kb == kbmax)),
                             reads=['ones_b', ('Pb', s_i)], writes=[plk])
                    for kb in range(kbmax + 1):
                        stage1(kb)
                        if len(staged) > 3:
                            stage2()
                    while staged:
                        stage2()
                    P.op('dve', lambda: V.tensor_scalar(out=rl[:, :], in0=pl[:, 0:384], scalar1=1e-30, scalar2=None, op0=ALU.add),
                         reads=[plk], writes=['rl'])
                    P.op('dve', lambda: V.reciprocal(out=rl[:, :], in_=rl[:, :]), reads=['rl'], writes=['rl'])
                    P.op('dve', lambda: V.tensor_tensor(out=attnT[:, h, t0:t0 + 384], in0=po[:, 0:384], in1=rl[:, :], op=ALU.mult),
                         reads=[pok, 'rl'], writes=[('attnT', h)])
            P.dma('sp', attnT_d[:, :, :], attnT[:, :, :], reads=[('attnT', h_) for h_ in range(8)])
            P.barrier()
        pcd.close()
        if stop_after == 'D':
            return nc

        with contextlib.ExitStack() as pf:
            Wo = sb("Wo", [128, 16, 2048], BF16, pf)
            ypT = sb("ypTf", [128, 8, NQ], BF16, pf)
            attnT = sb("attnTf", [128, 8, NQ], BF16, pf)
            P.dma('sp', ypT[:, :, :], ypT_d[:, :, :], writes=['ypT'])
            P.dma('sp', attnT[:, :, :], attnT_d[:, :, :], writes=['attnT'])
            xr = [sb("xrF%d" % i, [128, D], F32, pf) for i in range(2)]
            x1t = [sb("x1tF%d" % i, [128, D], F32, pf) for i in range(2)]
            pw = [ps("pwF%d" % i, [128, 512], F32, pf) for i in range(8)]
            for cg in range(4):
                wload(Wo, ('Wo', cg), w_out, 4 * cg, 4 * cg + 4, 0, 2048)
            pcn = 0
            for i in range(NQB):
                bi = i % 2
                P.dma('sp', xr[bi][:], xctx[QS0 + i * 128:QS0 + (i + 1) * 128, :], writes=[('xr', bi)])
                for nt in range(4):
                    pq = pw[pcn % 8]
                    pk = ('pw', pcn % 8)
                    pcn += 1
                    for c in range(16):
                        src = ypT if c < 8 else attnT
                        P.op('pe', lambda c=c, src=src, pq=pq, nt=nt, i=i: T.matmul(
                            pq[:, :], lhsT=src[:, c % 8, i * 128:(i + 1) * 128], rhs=Wo[:, c, nt * 512:(nt + 1) * 512],
                            start=(c == 0), stop=(c == 15)), reads=[('Wo', c // 4), 'ypT', 'attnT'], writes=[pk])
                    P.op('dve', lambda pq=pq, nt=nt, bi=bi: V.tensor_tensor(
                        out=x1t[bi][:, nt * 512:(nt + 1) * 512], in0=pq[:, :], in1=xr[bi][:, nt * 512:(nt + 1) * 512], op=ALU.add),
                        reads=[pk, ('xr', bi)], writes=[('x1t', bi)])
                P.dma('sp', x1_d[i * 128:(i + 1) * 128, :], x1t[bi][:], reads=[('x1t', bi)], writes=[('x1_d', i)])
            P.barrier()
        if stop_after == 'F':
            return nc


        pgh = contextlib.ExitStack()
        es.enter_context(pgh)
        gT = sb("gT", [128, NFC, 1024], BF16, pgh)
        with contextlib.ExitStack() as pg_:
            h2T = sb("h2T", [128, 16, NQ], BF16, pg_)
            Wgv = [sb("Wgv%d" % i, [128, 16, 512], BF16, pg_) for i in range(2)]

            def g_load(grp):
                jg_, isval_ = grp // 2, grp % 2
                wload(Wgv[grp % 2], ('Wgv', grp % 2), w_up, 0, 16, (D_FF if isval_ else 0) + jg_ * 512, 512)
            g_load(0)
            g_load(1)
            with contextlib.ExitStack() as pg1:
                gsb = sb("gsbG", [128, 16, 128], F32, pg1)
                hf_sb = sb("hf_sb", [128, 1], F32, pg1)
                xb = [sb("xbG%d" % i, [128, D], F32, pg1) for i in range(2)]
                xn = [sb("xnG%d" % i, [128, D], BF16, pg1) for i in range(2)]
                pT = [[ps("pTG%d%d" % (i, h), [128, 1024], BF16, pg1) for h in range(2)] for i in range(2)]
                P.dma('sp', gsb[:].rearrange("p c t -> p (c t)"), gT3[1, :, :], writes=['gsb'])
                P.dma('sp', hf_sb[:], hflag[:, :], writes=['hflag'])
                for j in range(NQB):
                    i = j % 2
                    P.dma('sp', xb[i][:], x1_d[j * 128:(j + 1) * 128, :], writes=[('xb', i)])
                    norm_T(i, xb[i][:], ('xb', i), xn, pT, gsb,
                           lambda h, j=j: h2T[:, 8 * h:8 * h + 8, j * 128:(j + 1) * 128], [('h2T', j)],
                           extra=(hf_sb[:, 0:1] if j == 0 else None))
                P.barrier()
            sgT = sb("sgT", [128, 4, 1024], F32, pg_)
            cp = sb("cp", [128, 2 * NFC, 4], F32, pg_)
            rA = [sb("rA%d" % i, [128, 344], F32, pg_) for i in range(3)]
            rB = [sb("rB%d" % i, [128, 344], F32, pg_) for i in range(3)]
            pu = [ps("puG%d" % i, [128, 512], F32, pg_) for i in range(8)]
            P.dma('sp', cp[:], convp[:, :, :], writes=['cp'])
            tiles = [(0, 342), (342, 684), (684, 1024)]
            pcn = 0
            rcn = 0
            for grp in range(2 * (NFC // 4)):
                jg, isval = grp // 2, grp % 2
                wb = grp % 2
                if grp >= 1 and grp + 1 < 2 * (NFC // 4):
                    g_load(grp + 1)
                for m_ in range(4):
                    j = 4 * jg + m_
                    jj = (NFC + j) if isval else j
                    for (a, b_) in tiles:
                        N = b_ - a + 2
                        n2 = N - 2
                        c0 = 128 + a - 2
                        pq = pu[pcn % 8]
                        pk = ('pu', pcn % 8)
                        pcn += 1
                        for c in range(16):
                            P.op('pe', lambda c=c: T.matmul(
                                pq[:, 0:N], lhsT=Wgv[wb][:, c, m_ * 128:(m_ + 1) * 128], rhs=h2T[:, c, c0:c0 + N],
                                start=(c == 0), stop=(c == 15)), reads=[('Wgv', wb)], writes=[pk])
                        ri = rcn % 3
                        rcn += 1
                        P.op('act', lambda: A.activation(
                            out=rA[ri][:, 0:n2], in_=pq[:, 2:N], func=AF.Identity, scale=cp[:, jj, 2:3], bias=cp[:, jj, 3:4]),
                            reads=[pk, 'cp'], writes=[('rA', ri)])
                        P.op('dve', lambda: V.scalar_tensor_tensor(
                            out=rB[ri][:, 0:n2], in0=pq[:, 1:N - 1], scalar=cp[:, jj, 1:2], in1=rA[ri][:, 0:n2],
                            op0=ALU.mult, op1=ALU.add), reads=[pk, 'cp', ('rA', ri)], writes=[('rB', ri)])
                        if not isval:
                            P.op('dve', lambda: V.scalar_tensor_tensor(
                                out=rA[ri][:, 0:n2], in0=pq[:, 0:n2], scalar=cp[:, jj, 0:1], in1=rB[ri][:, 0:n2],
                                op0=ALU.mult, op1=ALU.add), reads=[pk, 'cp', ('rB', ri)], writes=[('rA', ri)])
                            P.op('act', lambda: A.activation(out=sgT[:, m_, a:b_], in_=rA[ri][:, 0:n2], func=AF.Silu),
                                 reads=[('rA', ri)], writes=[('sgT', m_)])
                        else:
                            P.op('dve', lambda: V.scalar_tensor_tensor(
                                out=rA[ri][:, 0:n2], in0=pq[:, 0:n2], scalar=cp[:, jj, 0:1], in1=rB[ri][:, 0:n2],
                                op0=ALU.mult, op1=ALU.add), reads=[pk, 'cp', ('rB', ri)], writes=[('rA', ri)])
                            P.op('dve', lambda: V.tensor_tensor(
                                out=gT[:, j, a:b_], in0=sgT[:, m_, a:b_], in1=rA[ri][:, 0:n2], op=ALU.mult),
                                reads=[('sgT', m_), ('rA', ri)], writes=[('gT', j)])
            if debug:
                P.dma('sp', dbg['gT'][:, :, :], gT[:, :, :], reads=[('gT', j_) for j_ in range(NFC)])
            P.barrier()
        if stop_after == 'G':
            return nc

        with contextlib.ExitStack() as ph:
            Wds = [sb("Wds%d" % i, [128, 4, 512], BF16, ph) for i in range(3)]
            x1s = [sb("x1s%d" % i, [128, 512], F32, ph) for i in range(4)]
            x2s = [sb("x2s%d" % i, [128, 512], F32, ph) for i in range(4)]
            pw = [ps("pwH%d" % i, [128, 512], F32, ph) for i in range(8)]
            wcn = 0
            scn = 0
            pset = 0
            for tg in range(2):
                for nt in range(4):
                    banks = [(pw[4 * (pset % 2) + tb], ('pw', 4 * (pset % 2) + tb)) for tb in range(4)]
                    pset += 1
                    for fg in range(NFC // 4):
                        wb = wcn % 3
                        wcn += 1
                        P.dma('pool', Wds[wb][:, :, :],
                              w_down[fg * 512:(fg + 1) * 512, nt * 512:(nt + 1) * 512].rearrange("(c p) n -> p c n", p=128),
                              writes=[('Wds', wb)])
                        for fl in range(4):
                            f = fg * 4 + fl
                            for tb in range(4):
                                pq, pk = banks[tb]
                                tok0 = (tg * 4 + tb) * 128
                                P.op('pe', lambda pq=pq, f=f, fl=fl, wb=wb, tok0=tok0: T.matmul(
                                    pq[:, :], lhsT=gT[:, f, tok0:tok0 + 128], rhs=Wds[wb][:, fl, :],
                                    start=(f == 0), stop=(f == NFC - 1)), reads=[('Wds', wb)], writes=[pk])
                    for tb in range(4):
                        pq, pk = banks[tb]
                        si = scn % 4
                        scn += 1
                        r0 = (tg * 4 + tb) * 128
                        P.dma('sp', x1s[si][:, :], x1_d[128 + r0:128 + r0 + 128, nt * 512:(nt + 1) * 512], writes=[('x1s', si)])
                        P.op('dve', lambda pq=pq, si=si: V.tensor_tensor(out=x2s[si][:, :], in0=pq[:, :], in1=x1s[si][:, :], op=ALU.add),
                             reads=[pk, ('x1s', si)], writes=[('x2s', si)])
                        P.dma('sp', x2_d[r0:r0 + 128, nt * 512:(nt + 1) * 512], x2s[si][:, :], reads=[('x2s', si)], writes=[('x2_d', r0, nt)])
            P.barrier()
        pgh.close()
        if stop_after == 'H':
            return nc

        with contextlib.ExitStack() as pi:
            x3 = sb("x3", [128, 8, D], F32, pi)
            h3T = sb("h3T", [128, 16, 1024], BF16, pi)
            ppT = sb("ppT", [128, 2, 1024], BF16, pi)
            with contextlib.ExitStack() as pi1:
                gsb = sb("gsbI", [128, 16, 128], F32, pi1)
                p_sb = sb("p_sb", [128, 8, 256], F32, pi1)
                p_bf = sb("p_bf", [128, 8, 256], BF16, pi1)
                xn = [sb("xnI%d" % i, [128, D], BF16, pi1) for i in range(2)]
                pT = [[ps("pTI%d%d" % (i, h), [128, 1024], BF16, pi1) for h in range(2)] for i in range(2)]
                ptp = [ps("ptpI%d" % i, [128, 1024], BF16, pi1) for i in range(2)]
                P.dma('sp', gsb[:].rearrange("p c t -> p (c t)"), gT3[2, :, :], writes=['gsb'])
                P.dma('sp', p_sb[:, :, :], p_own.rearrange("(tb p) c -> p tb c", p=128), writes=['p_sb'])
                P.op('pool', lambda: G.tensor_copy(out=p_bf[:, :, :], in_=p_sb[:, :, :]), reads=['p_sb'], writes=['p_bf'])
                for tb in range(8):
                    P.dma('sp', x3[:, tb, :], x2_d[tb * 128:(tb + 1) * 128, :], writes=[('x3', tb)])
                for tb in range(8):
                    i = tb % 2
                    norm_T(i, x3[:, tb, :], ('x3', tb), xn, pT, gsb,
                           lambda h, tb=tb: h3T[:, 8 * h:8 * h + 8, tb * 128:(tb + 1) * 128], [('h3T', tb)])
                    pt = ptp[i]
                    for cc in range(2):
                        P.op('pe', lambda pt=pt, cc=cc, tb=tb: T.transpose(out=pt[:, cc * 128:(cc + 1) * 128],
                                                                          in_=p_bf[:, tb, cc * 128:(cc + 1) * 128], identity=ident_b[:]),
                             reads=['p_bf', 'ident_b'], writes=[('ptp', i)])
                    P.op('act', lambda pt=pt, tb=tb: A.activation(out=ppT[:, :, tb * 128:(tb + 1) * 128],
                                                                  in_=pt[:, 0:256].rearrange("p (a b) -> p a b", b=128), func=AF.Copy),
                         reads=[('ptp', i)], writes=[('ppT', tb)])
                P.barrier()
            with contextlib.ExitStack() as pi2:
                Wgs = [sb("WgsI%d" % i, [128, 16, 512], BF16, pi2) for i in range(2)]
                Wps = [sb("WpsI%d" % i, [128, 2, 512], BF16, pi2) for i in range(2)]
                sgm = [sb("sgm%d" % i, [128, 512], F32, pi2) for i in range(2)]
                tmpI = [sb("tmpI%d" % i, [128, 512], F32, pi2) for i in range(2)]
                pgt = [ps("pgtI%d" % i, [128, 512], F32, pi2) for i in range(4)]
                ppe = [ps("ppeI%d" % i, [128, 512], F32, pi2) for i in range(4)]
                cn = 0

                def i_load(nt_):
                    wb_ = nt_ % 2
                    for cg in range(4):
                        P.dma('pool', Wgs[wb_][:, 4 * cg:4 * cg + 4, :],
                              w_pg[cg * 512:(cg + 1) * 512, nt_ * 512:(nt_ + 1) * 512].rearrange("(c p) n -> p c n", p=128),
                              writes=[('WgsI', wb_, cg)])
                    P.dma('pool', Wps[wb_][:, :, :], w_pp[:, nt_ * 512:(nt_ + 1) * 512].rearrange("(c p) n -> p c n", p=128),
                          writes=[('WpsI', wb_)])
                i_load(0)
                for nt in range(4):
                    wb = nt % 2
                    if nt + 1 < 4:
                        i_load(nt + 1)
                    for tb in range(8):
                        bi = cn % 4
                        si = cn % 2
                        cn += 1
                        for c in range(16):
                            P.op('pe', lambda c=c, bi=bi, tb=tb, wb=wb: T.matmul(
                                pgt[bi][:, :], lhsT=h3T[:, c, tb * 128:(tb + 1) * 128], rhs=Wgs[wb][:, c, :],
                                start=(c == 0), stop=(c == 15)), reads=[('WgsI', wb, c // 4)], writes=[('pgt', bi)])
                        for cc in range(2):
                            P.op('pe', lambda cc=cc, bi=bi, tb=tb, wb=wb: T.matmul(
                                ppe[bi][:, :], lhsT=ppT[:, cc, tb * 128:(tb + 1) * 128], rhs=Wps[wb][:, cc, :],
                                start=(cc == 0), stop=(cc == 1)), reads=[('WpsI', wb)], writes=[('ppe', bi)])
                        P.op('act', lambda bi=bi, si=si: A.activation(out=sgm[si][:, :], in_=pgt[bi][:, :], func=AF.Sigmoid),
                             reads=[('pgt', bi)], writes=[('sgm', si)])
                        P.op('dve', lambda bi=bi, si=si: V.tensor_tensor(out=tmpI[si][:, :], in0=ppe[bi][:, :], in1=sgm[si][:, :], op=ALU.mult),
                             reads=[('ppe', bi), ('sgm', si)], writes=[('tmpI', si)])
                        P.op('pool', lambda si=si, tb=tb, nt=nt: G.tensor_tensor(
                            out=x3[:, tb, nt * 512:(nt + 1) * 512], in0=x3[:, tb, nt * 512:(nt + 1) * 512], in1=tmpI[si][:, :], op=ALU.add),
                            reads=[('tmpI', si)], writes=[('x3o', tb)])
                P.barrier()
            with contextlib.ExitStack() as pi3:
                gf = sb("gf_sb", [128, D], F32, pi3)
                ot = [sb("ot%d" % i, [128, D], F32, pi3) for i in range(2)]
                jk = sb("jkI", [128, D], BF16, pi3)
                P.dma('sp', gf[:, :], gfin[:, :], writes=['gf'])
                for tb in range(8):
                    i = tb % 2
                    ss = st[:, 3 * i:3 * i + 1]
                    sd = st[:, 3 * i + 1:3 * i + 2]
                    rs = st[:, 3 * i + 2:3 * i + 3]
                    P.op('act', lambda tb=tb, ss=ss: A.activation(out=jk[:, :], in_=x3[:, tb, :], func=AF.Square, accum_out=ss),
                         writes=['jk', ('ss', i)])
                    P.op('dve', lambda ss=ss, sd=sd: V.tensor_scalar(out=sd, in0=ss, scalar1=1.0 / D, scalar2=EPS, op0=ALU.mult, op1=ALU.add),
                         reads=[('ss', i)], writes=[('sd', i)])
                    P.op('act', lambda sd=sd: A.activation(out=sd, in_=sd, func=AF.Sqrt), reads=[('sd', i)], writes=[('sd', i)])
                    P.op('dve', lambda sd=sd, rs=rs: V.reciprocal(out=rs, in_=sd), reads=[('sd', i)], writes=[('rs', i)])
                    P.op('dve', lambda tb=tb, rs=rs, i=i: V.scalar_tensor_tensor(
                        out=ot[i][:, :], in0=x3[:, tb, :], scalar=rs, in1=gf[:, :], op0=ALU.mult, op1=ALU.mult),
                        reads=[('rs', i), 'gf'], writes=[('ot', i)])
                    P.dma('sp', out[tb * 128:(tb + 1) * 128, :], ot[i][:, :], reads=[('ot', i)], writes=[('out', tb)])
                P.barrier()
    return nc


def _t5_bucket_static(dist):
    n = np.maximum(dist, 0)
    nf = np.maximum(n, 1).astype(np.float32)
    large = 16 + (np.log(nf / np.float32(16)) / np.float32(math.log(128 / 16)) * 16).astype(np.int32)
    large = np.minimum(large, 31)
    return np.where(n < 16, n, large)


def prep_inputs(x, p, g_mix, w_in, w_pool, pool_scale, rel_bias, w_out, g_ffn, w_up, conv_w, conv_b,
                w_down, g_ple, w_ple_gate, w_ple_proj, g_final):
    f = np.float32
    x = np.asarray(x, f)
    p = np.asarray(p, f)[0]
    shared = {}
    shared["w_in"] = np.ascontiguousarray(np.asarray(w_in, f)[0])
    shared["w_pool"] = np.ascontiguousarray(np.asarray(w_pool, f)[0])
    shared["w_out"] = np.ascontiguousarray(np.asarray(w_out, f)[0])
    shared["w_up"] = np.ascontiguousarray(np.asarray(w_up, f)[0])
    shared["w_down"] = np.ascontiguousarray(np.asarray(w_down, f)[0])
    shared["w_ple_gate"] = np.ascontiguousarray(np.asarray(w_ple_gate, f)[0])
    shared["w_ple_proj"] = np.ascontiguousarray(np.asarray(w_ple_proj, f)[0])

    def fm(v):
        a = np.asarray(v, f).reshape(16, 128).T
        return np.ascontiguousarray(np.repeat(a[:, :, None], 128, axis=2).reshape(128, 2048))
    shared["gT3"] = np.stack([fm(np.asarray(g_mix)[0]), fm(np.asarray(g_ffn)[0]), fm(np.asarray(g_ple)[0])], 0)
    shared["gfin"] = np.ascontiguousarray(np.repeat(np.asarray(g_final, f)[None, :], 128, axis=0))
    shared["pscale"] = np.ascontiguousarray(np.asarray(pool_scale, f)[0].reshape(8, 128).T)
    cw = np.asarray(conv_w, f)[0]
    cb = np.asarray(conv_b, f)[0]
    cp = np.concatenate([cw, cb[None, :]], 0)
    shared["convp"] = np.ascontiguousarray(cp.reshape(4, 2 * NFC, 128).transpose(2, 1, 0))
    rb = np.asarray(rel_bias, f)
    sl = np.arange(128)[:, None]
    dl = np.arange(896)[None, :] - 256
    dist = dl - sl
    bk = _t5_bucket_static(dist)
    stripv = rb[bk]
    shared["strip"] = np.ascontiguousarray(stripv.transpose(0, 2, 1))
    shared["cfar"] = np.ascontiguousarray(np.repeat(rb[31][None, :], 128, axis=0))
    tl = np.arange(128)[:, None]
    s_l = np.arange(128)[None, :]
    shared["causal"] = np.where(s_l <= tl, 0.0, NEG).astype(f)
    shared["ident"] = np.eye(128, dtype=f)
    shared["halfs"] = np.ascontiguousarray(np.repeat((0.5 ** np.arange(1, NIT + 2, dtype=np.float64)).astype(f)[None, :], 128, 0))
    in_maps = []
    for c in range(8):
        b, j = c // 4, c % 4
        T0 = j * 1024
        m = dict(shared)
        xc = np.zeros((CTX, D), f)
        lo = T0 - 3072
        s0 = max(0, -lo)
        xc[s0:] = x[b, lo + s0:T0 + 1024]
        m["xctx"] = xc
        m["p_own"] = np.ascontiguousarray(p[b, T0:T0 + 1024])
        sbias = np.zeros((CTX,), f)
        sbias[:s0] = NEG
        m["slotb"] = np.ascontiguousarray(np.repeat(sbias[None, :], 128, axis=0))
        ic = np.zeros((128, 8, 16), f)
        for k in range(8):
            w = (2, 4, 8, 16)[k // 2]
            tok = T0 + np.arange(16)
            cntv = np.minimum(tok + 1, w).astype(f)
            ic[:, k, :] = (np.float32(1.0) / cntv)[None, :]
        m["invc"] = ic
        m["hflag"] = np.full((128, 1), 1.0 if j > 0 else 0.0, f)
        in_maps.append(m)
    return in_maps


_NC_CACHE = {}


def kernel(**inputs):
    in_maps = prep_inputs(**inputs)
    if 'nc' not in _NC_CACHE:
        _NC_CACHE['nc'] = build()
    nc = _NC_CACHE['nc']
    res = run_bass_kernel_spmd(nc, in_maps, core_ids=list(range(8)))
    outs = [np.asarray(res.results[c]["out"], np.float32).reshape(1024, D) for c in range(8)]
    full = np.zeros((2, SEQ, D), np.float32)
    for c in range(8):
        full[c // 4, (c % 4) * 1024:(c % 4 + 1) * 1024] = outs[c]
    return full
```

```python
import contextlib
import math
import numpy as np
import concourse.bass as bass
import concourse.mybir as mybir
from concourse.bass_utils import run_bass_kernel_spmd

F32 = mybir.dt.float32
BF16 = mybir.dt.bfloat16
AF = mybir.ActivationFunctionType
ALU = mybir.AluOpType
AX = mybir.AxisListType

D = 2048
SEQ = 4096
CTX = 4096
NQB = 9
QS0 = 2944
NQ = NQB * 128
HM0 = 2816
NHM = 1280
D_FF = 5632
NFC = 44
EPS = 1e-6
NEG = -1.0e30
NIT = 16
OFF = dict(pool=0, q=1024, k=2048, v=3072, qi=4096, ki=5120, wi=5184)


class Prog:
    NDS = 12

    def __init__(self, nc, es, same_sync=True):
        self.nc = nc
        self.same_sync = same_sync
        self.eng = {'pe': nc.tensor, 'act': nc.scalar, 'dve': nc.vector, 'pool': nc.gpsimd, 'sp': nc.sync}
        self.csem = {e: es.enter_context(nc.semaphore('c_' + e)) for e in ['pe', 'act', 'dve', 'pool']}
        self.cnt = {e: 0 for e in self.csem}
        self.dsem = {q: [es.enter_context(nc.semaphore('d_%s%d' % (q, i))) for i in range(self.NDS)]
                     for q in ['sp', 'pool']}
        self.duse = {q: [0] * self.NDS for q in self.dsem}
        self.dnext = {q: 0 for q in self.dsem}
        self.seen = {e: {} for e in self.eng}
        self.lastw = {}
        self.rds = {}
        self.nwait = 0

    def _wait(self, e, tok):
        sem, val, sid = tok
        if sid == e and (e == 'pe' or not self.same_sync):
            return
        if self.seen[e].get(sid, 0) >= val:
            return
        self.eng[e].wait_ge(sem, val)
        self.seen[e][sid] = val
        self.nwait += 1

    def _deps(self, reads, writes):
        deps = {}

        def add(tok):
            sid = tok[2]
            if sid not in deps or deps[sid][1] < tok[1]:
                deps[sid] = tok
        for k in reads:
            if k in self.lastw:
                add(self.lastw[k])
        for k in writes:
            if k in self.lastw:
                add(self.lastw[k])
            for tok in self.rds.get(k, {}).values():
                add(tok)
        return deps.values()

    def _record(self, tok, reads, writes):
        sid = tok[2]
        for k in reads:
            d = self.rds.setdefault(k, {})
            if sid not in d or d[sid][1] < tok[1]:
                d[sid] = tok
        for k in writes:
            self.lastw[k] = tok
            self.rds[k] = {}

    def op(self, e, fn, reads=(), writes=()):
        for tok in self._deps(reads, writes):
            self._wait(e, tok)
        ins = fn()
        self.cnt[e] += 1
        ins.then_inc(self.csem[e], 1)
        self._record((self.csem[e], self.cnt[e], e), reads, writes)

    def dma(self, q, out, in_, reads=(), writes=(), **kw):
        for tok in self._deps(reads, writes):
            self._wait(q, tok)
        k = self.dnext[q]
        self.dnext[q] = (k + 1) % self.NDS
        u = self.duse[q][k]
        sem = self.dsem[q][k]
        if u > 0:
            self._wait(q, (sem, 16 * u, (q, k)))
        ins = self.eng[q].dma_start(out=out, in_=in_, **kw)
        ins.then_inc(sem, 16)
        self.duse[q][k] = u + 1
        self._record((sem, 16 * (u + 1), (q, k)), reads, writes)

    def barrier(self):
        for e in self.eng:
            for c in self.csem:
                if self.cnt[c] > 0:
                    self._wait_force(e, (self.csem[c], self.cnt[c], c))
            for q in self.dsem:
                for k in range(self.NDS):
                    if self.duse[q][k] > 0:
                        self._wait_force(e, (self.dsem[q][k], 16 * self.duse[q][k], (q, k)))
        self.lastw.clear()
        self.rds.clear()

    def _wait_force(self, e, tok):
        sem, val, sid = tok
        if self.seen[e].get(sid, 0) >= val:
            return
        self.eng[e].wait_ge(sem, val)
        self.seen[e][sid] = val
        self.nwait += 1


def build(debug=False, stop_after='Z'):
    nc = bass.Bass("TRN2", target_bir_lowering=False)

    def din(name, shape, dt=F32):
        return nc.dram_tensor(name, list(shape), dt, kind="ExternalInput").ap()

    xctx = din("xctx", [CTX, D])
    p_own = din("p_own", [1024, 256])
    w_in = din("w_in", [D, 5200])
    w_pool = din("w_pool", [4, 256, 256])
    w_out = din("w_out", [D, D])
    w_up = din("w_up", [D, 2 * D_FF])
    w_down = din("w_down", [D_FF, D])
    w_pg = din("w_ple_gate", [D, D])
    w_pp = din("w_ple_proj", [256, D])
    gT3 = din("gT3", [3, 128, 2048])
    gfin = din("gfin", [128, 2048])
    pscale = din("pscale", [128, 8])
    convp = din("convp", [128, 2 * NFC, 4])
    strip = din("strip", [128, 8, 896])
    cfar = din("cfar", [128, 8])
    slotb = din("slotb", [128, CTX])
    causal = din("causal", [128, 128])
    ident = din("ident", [128, 128])
    invc = din("invc", [128, 8, 16])
    hflag = din("hflag", [128, 1])
    halfs = din("halfs", [128, NIT + 1])
    out = nc.dram_tensor("out", [1024, D], F32, kind="ExternalOutput").ap()
    ks = "ExternalOutput" if debug else "Internal"
    KT_d = nc.dram_tensor("KT_d", [8, 128, CTX], BF16, kind=ks).ap()
    V_d = nc.dram_tensor("V_d", [8, CTX, 128], BF16, kind=ks).ap()
    x1_d = nc.dram_tensor("x1_d", [NQ, D], F32, kind=ks).ap()
    x2_d = nc.dram_tensor("x2_d", [1024, D], F32, kind=ks).ap()
    kidxT_d = nc.dram_tensor("kidxT_d", [128, CTX], BF16, kind=ks).ap()
    QT_d = nc.dram_tensor("QT_d", [128, 8, NQ], BF16, kind=ks).ap()
    qiT_d = nc.dram_tensor("qiT_d", [128, 8, NQ], BF16, kind=ks).ap()
    ypT_d = nc.dram_tensor("ypT_d", [128, 8, NQ], BF16, kind=ks).ap()
    attnT_d = nc.dram_tensor("attnT_d", [128, 8, NQ], BF16, kind=ks).ap()
    widx_d = nc.dram_tensor("widx_d", [128, NQB, 16], F32, kind=ks).ap()
    dbg = {}
    if debug:
        dbg['maskT'] = nc.dram_tensor("dbg_maskT", [128, 32, NQ], BF16, kind="ExternalOutput").ap()
        dbg['thr'] = nc.dram_tensor("dbg_thr", [128, NQB], F32, kind="ExternalOutput").ap()
        dbg['gT'] = nc.dram_tensor("dbg_gT", [128, NFC, 1024], BF16, kind="ExternalOutput").ap()

    with contextlib.ExitStack() as es:
        P = Prog(nc, es)
        T = nc.tensor
        A = nc.scalar
        V = nc.vector
        G = nc.gpsimd

        def sb(name, shape, dt, stack=es):
            return stack.enter_context(nc.sbuf_tensor(name, list(shape), dt))

        def ps(name, shape, dt, stack):
            return stack.enter_context(nc.psum_tensor(name, list(shape), dt))

        ident_f = sb("ident_f", [128, 128], F32)
        ident_b = sb("ident_b", [128, 128], BF16)
        ones_b = sb("ones_b", [128, 128], BF16)
        st = sb("st", [128, 16], F32)
        P.dma('sp', ident_f[:], ident[:, :], writes=['ident_f'])
        P.op('dve', lambda: V.tensor_copy(out=ident_b[:], in_=ident_f[:]), reads=['ident_f'], writes=['ident_b'])
        P.op('dve', lambda: V.memset(ones_b[:], 1.0), writes=['ones_b'])

        def alloc(name, shape, dt):
            cm = nc.sbuf_tensor(name, list(shape), dt)
            return cm, cm.__enter__()

        def norm_T(i, xin, xin_key, xn, pT, gsb, dst_fn, dst_keys, extra=None):
            ss = st[:, 3 * i:3 * i + 1]
            sd = st[:, 3 * i + 1:3 * i + 2]
            rs = st[:, 3 * i + 2:3 * i + 3]
            P.op('act', lambda: A.activation(out=xn[i][:], in_=xin, func=AF.Square, accum_out=ss),
                 reads=[xin_key], writes=[('xn', i), ('ss', i)])
            P.op('dve', lambda: V.tensor_scalar(out=sd, in0=ss, scalar1=1.0 / D, scalar2=EPS, op0=ALU.mult, op1=ALU.add),
                 reads=[('ss', i)], writes=[('sd', i)])
            P.op('act', lambda: A.activation(out=sd, in_=sd, func=AF.Sqrt), reads=[('sd', i)], writes=[('sd', i)])
            P.op('dve', lambda: V.reciprocal(out=rs, in_=sd), reads=[('sd', i)], writes=[('rs', i)])
            if extra is not None:
                P.op('dve', lambda: V.tensor_tensor(out=rs, in0=rs, in1=extra, op=ALU.mult),
                     reads=[('rs', i), 'hflag'], writes=[('rs', i)])
            P.op('dve', lambda: V.tensor_scalar(out=xn[i][:], in0=xin, scalar1=rs, scalar2=None, op0=ALU.mult),
                 reads=[xin_key, ('rs', i)], writes=[('xn', i)])
            for c in range(16):
                P.op('pe', lambda c=c: T.transpose(out=pT[i][c // 8][:, (c % 8) * 128:(c % 8 + 1) * 128],
                                                   in_=xn[i][:, c * 128:(c + 1) * 128], identity=ident_b[:]),
                     reads=[('xn', i), 'ident_b'], writes=[('pT', i, c // 8)])
            for h in range(2):
                P.op('dve', lambda h=h: V.tensor_tensor(
                    out=dst_fn(h), in0=pT[i][h][:, :].rearrange("p (c t) -> p c t", t=128),
                    in1=gsb[:, 8 * h:8 * h + 8, :], op=ALU.mult),
                    reads=[('pT', i, h), 'gsb'], writes=dst_keys)

        def wload(dst, dkey, src_rows, c0, c1, col0, ncol, q='pool'):
            P.dma(q, dst[:, c0:c1, 0:ncol],
                  src_rows[c0 * 128:c1 * 128, col0:col0 + ncol].rearrange("(c p) n -> p c n", p=128),
                  writes=[dkey])

        with contextlib.ExitStack() as pa:
            kidxT = sb("kidxT", [128, CTX], BF16, pa)
            Wk = sb("Wk", [128, 16, 1024], BF16, pa)
            Wv = sb("Wv", [128, 16, 1024], BF16, pa)
            Wki = sb("Wki", [128, 16, 128], BF16, pa)
            gsb = sb("gsbA", [128, 16, 128], F32, pa)
            xb = [sb("xbA%d" % i, [128, D], F32, pa) for i in range(2)]
            xn = [sb("xnA%d" % i, [128, D], BF16, pa) for i in range(2)]
            hT = [sb("hTA%d" % i, [128, 16, 512], BF16, pa) for i in range(2)]
            KTst = [sb("KTst%d" % i, [128, 8, 512], BF16, pa) for i in range(2)]
            Vst = [sb("Vst%d" % i, [128, 4, 1024], BF16, pa) for i in range(2)]
            pT = [[ps("pTA%d%d" % (i, h), [128, 1024], BF16, pa) for h in range(2)] for i in range(2)]
            pm = [ps("pmA%d" % i, [128, 512], F32, pa) for i in range(4)]

            P.dma('sp', gsb[:].rearrange("p c t -> p (c t)"), gT3[0, :, :], writes=['gsb'])
            for cg in range(4):
                wload(Wk, ('Wk', cg), w_in, 4 * cg, 4 * cg + 4, OFF['k'], 1024)
            for cg in range(4):
                wload(Wv, ('Wv', cg), w_in, 4 * cg, 4 * cg + 4, OFF['v'], 1024)
            P.dma('pool', Wki[:, :, 0:64], w_in[:, OFF['ki']:OFF['ki'] + 64].rearrange("(c p) n -> p c n", p=128),
                  writes=['Wki'])
            P.dma('pool', Wki[:, :, 64:128], w_in[:, OFF['ki']:OFF['ki'] + 64].rearrange("(c p) n -> p c n", p=128),
                  writes=['Wki'])
            pmi = [0]

            def a_load(ct, sbl):
                j = ct * 4 + sbl
                P.dma('sp', xb[j % 2][:], xctx[j * 128:(j + 1) * 128, :], writes=[('xb', j % 2)])

            def a_norm(ct, sbl):
                b = ct % 2
                j = ct * 4 + sbl
                i = j % 2
                norm_T(i, xb[i][:], ('xb', i), xn, pT, gsb,
                       lambda h: hT[b][:, 8 * h:8 * h + 8, sbl * 128:(sbl + 1) * 128], [('hT', b, sbl)])

            def a_k_heads(ct, heads):
                b = ct % 2
                hkeys = [('hT', b, s_) for s_ in range(4)]
                for h in heads:
                    pq = pm[pmi[0] % 4]
                    pk = ('pm', pmi[0] % 4)
                    pmi[0] += 1
                    for c in range(16):
                        P.op('pe', lambda c=c: T.matmul(pq[:, :], lhsT=Wk[:, c, h * 128:(h + 1) * 128],
                                                        rhs=hT[b][:, c, :], start=(c == 0), stop=(c == 15)),
                             reads=hkeys + [('Wk', c // 4)], writes=[pk])
                    P.op('act', lambda: A.activation(out=KTst[b][:, h, :], in_=pq[:, :], func=AF.Copy),
                         reads=[pk], writes=[('KTst', b)])

            def a_kidx(ct):
                b = ct % 2
                hkeys = [('hT', b, s_) for s_ in range(4)]
                pq = pm[pmi[0] % 4]
                pk = ('pm', pmi[0] % 4)
                pmi[0] += 1
                for c in range(16):
                    P.op('pe', lambda c=c: T.matmul(pq[:, :], lhsT=Wki[:, c, :], rhs=hT[b][:, c, :],
                                                    start=(c == 0), stop=(c == 15)),
                         reads=hkeys + ['Wki'], writes=[pk])
                P.op('act', lambda: A.activation(out=kidxT[:, ct * 512:(ct + 1) * 512], in_=pq[:, :], func=AF.Copy),
                     reads=[pk], writes=[('kidxT', ct)])
                P.dma('pool', KT_d[:, :, ct * 512:(ct + 1) * 512].rearrange("h d s -> d h s"), KTst[b][:, :, :],
                      reads=[('KTst', b)], writes=[('KT_d', ct)])

            def a_v(ct, sbls):
                b = ct % 2
                for sbl in sbls:
                    for hf in range(2):
                        pq = pm[pmi[0] % 4]
                        pk = ('pm', pmi[0] % 4)
                        pmi[0] += 1
                        for c in range(16):
                            P.op('pe', lambda c=c: T.matmul(
                                pq[:, :], lhsT=hT[b][:, c, sbl * 128:(sbl + 1) * 128],
                                rhs=Wv[:, c, hf * 512:(hf + 1) * 512], start=(c == 0), stop=(c == 15)),
                                reads=[('hT', b, sbl), ('Wv', c // 4)], writes=[pk])
                        if hf == 0:
                            P.op('act', lambda: A.activation(
                                out=Vst[b][:, sbl, hf * 512:(hf + 1) * 512], in_=pq[:, :], func=AF.Copy),
                                reads=[pk], writes=[('Vst', b, sbl)])
                        else:
                            P.op('dve', lambda: V.tensor_copy(
                                out=Vst[b][:, sbl, hf * 512:(hf + 1) * 512], in_=pq[:, :]),
                                reads=[pk], writes=[('Vst', b, sbl)])
                    r0 = ct * 512 + sbl * 128
                    P.dma('pool', V_d[:, r0:r0 + 128, :].rearrange("h p d -> p h d"),
                          Vst[b][:, sbl, :].rearrange("p (h d) -> p h d", d=128),
                          reads=[('Vst', b, sbl)], writes=[('V_d', ct, sbl)])

            for sbl in range(4):
                a_load(0, sbl)
                a_norm(0, sbl)
            NCT = CTX // 512
            for ct in range(NCT):
                parts = [lambda ct=ct: a_k_heads(ct, range(0, 4)),
                         lambda ct=ct: (a_k_heads(ct, range(4, 8)), a_kidx(ct)),
                         lambda ct=ct: a_v(ct, (0, 1)),
                         lambda ct=ct: a_v(ct, (2, 3))]
                for q_ in range(4):
                    if ct + 1 < NCT:
                        a_load(ct + 1, q_)
                    parts[q_]()
                    if ct + 1 < NCT:
                        a_norm(ct + 1, q_)
            P.dma('sp', kidxT_d[:, :], kidxT[:, :], reads=[('kidxT', c_) for c_ in range(8)])
            P.barrier()
        if stop_after == 'A':
            return nc


        with contextlib.ExitStack() as pb:
            QT = sb("QT", [128, 8, NQ], BF16, pb)
            qiT = sb("qiT", [128, 8, NQ], BF16, pb)
            ypT = sb("ypT", [128, 8, NQ], BF16, pb)
            widx = sb("widx", [128, NQB, 16], F32, pb)
            hM = sb("hM", [128, 16, NHM], BF16, pb)
            Ws = [sb("WsB%d" % i, [128, 16, 512], BF16, pb) for i in range(2)]
            wgroups = [OFF['q'], OFF['q'] + 512, OFF['qi'], OFF['qi'] + 512, OFF['pool'], OFF['pool'] + 512]
            wissued = [0]

            def issue_w():
                k = wissued[0]
                if k < len(wgroups):
                    wload(Ws[k % 2], ('Ws', k % 2), w_in, 0, 16, wgroups[k], 512)
                    wissued[0] += 1
            issue_w()
            issue_w()
            with contextlib.ExitStack() as pb1:
                gsb = sb("gsbB", [128, 16, 128], F32, pb1)
                xb = [sb("xbB%d" % i, [128, D], F32, pb1) for i in range(2)]
                xn = [sb("xnB%d" % i, [128, D], BF16, pb1) for i in range(2)]
                pT = [[ps("pTB%d%d" % (i, h), [128, 1024], BF16, pb1) for h in range(2)] for i in range(2)]
                P.dma('sp', gsb[:].rearrange("p c t -> p (c t)"), gT3[0, :, :], writes=['gsb'])
                for j in range(NHM // 128):
                    i = j % 2
                    P.dma('sp', xb[i][:], xctx[HM0 + j * 128:HM0 + (j + 1) * 128, :], writes=[('xb', i)])
                    norm_T(i, xb[i][:], ('xb', i), xn, pT, gsb,
                           lambda h, j=j: hM[:, 8 * h:8 * h + 8, j * 128:(j + 1) * 128], [('hM', j)])
                P.barrier()
            pp = [ps("ppB%d" % i, [128, 4, 512], F32, pb) for i in range(2)]
            Wpl = sb("Wpl", [128, 4, 2, 256], BF16, pb)
            Ww = sb("Ww", [128, 16, 16], BF16, pb)
            psc_sb = sb("pscale_sb", [128, 8], F32, pb)
            invc_sb = sb("invc_sb", [128, 8, 16], F32, pb)
            pooledT = sb("pooledT", [128, 8, NQ], BF16, pb)
            uk = [sb("uk%d" % i, [128, 1168], F32, pb) for i in range(2)]
            sA = sb("sA", [128, 1168], F32, pb)
            sB = sb("sB", [128, 1168], F32, pb)
            t16 = sb("t16", [128, 16], F32, pb)
            P.dma('sp', psc_sb[:], pscale[:, :], writes=['pscale'])
            P.dma('sp', invc_sb[:], invc[:, :, :], writes=['invc'])
            for g in range(4):
                P.dma('pool', Wpl[:, g, :, :], w_pool[g, :, :].rearrange("(cc p) d -> p cc d", p=128), writes=['Wpl'])
            P.dma('pool', Ww[:, :, :], w_in[:, OFF['wi']:OFF['wi'] + 16].rearrange("(c p) n -> p c n", p=128), writes=['Ww'])
            wcnt = [0]
            pcnt = [0]

            def next_w(col0):
                b = wcnt[0] % 2
                assert wgroups[wcnt[0]] == col0
                if wcnt[0] >= 1:
                    issue_w()
                wcnt[0] += 1
                return b

            def next_pp():
                b = pcnt[0] % 2
                pcnt[0] += 1
                return b

            for (dst, off) in ((QT, OFF['q']), (qiT, OFF['qi'])):
                for g in range(2):
                    wb = next_w(off + 512 * g)
                    for m_ in range(4):
                        pb_ = next_pp()
                        for n in range(3):
                            for c in range(16):
                                P.op('pe', lambda c=c, n=n, wb=wb, m_=m_, pb_=pb_: T.matmul(
                                    pp[pb_][:, n, 0:384], lhsT=Ws[wb][:, c, m_ * 128:(m_ + 1) * 128],
                                    rhs=hM[:, c, 128 + n * 384:128 + (n + 1) * 384], start=(c == 0), stop=(c == 15)),
                                    reads=[('Ws', wb)], writes=[('pp', pb_)])
                        P.op('act', lambda dst=dst, g=g, m_=m_, pb_=pb_: A.activation(
                            out=dst[:, 4 * g + m_, :].rearrange("p (n t) -> p n t", t=384),
                            in_=pp[pb_][:, 0:3, 0:384], func=AF.Copy),
                            reads=[('pp', pb_)], writes=[('fm', id(dst), 4 * g + m_)])
            for g2 in range(2):
                wb = next_w(OFF['pool'] + 512 * g2)
                for m_ in range(4):
                    k = 4 * g2 + m_
                    g = k // 2
                    pb_ = next_pp()
                    for n in range(4):
                        for c in range(16):
                            P.op('pe', lambda c=c, n=n, wb=wb, m_=m_, pb_=pb_: T.matmul(
                                pp[pb_][:, n, 0:292], lhsT=Ws[wb][:, c, m_ * 128:(m_ + 1) * 128],
                                rhs=hM[:, c, 112 + n * 292:112 + (n + 1) * 292], start=(c == 0), stop=(c == 15)),
                                reads=[('Ws', wb)], writes=[('pp', pb_)])
                    u = uk[k % 2]
                    P.op('act', lambda u=u, pb_=pb_: A.activation(
                        out=u[:, :].rearrange("p (n t) -> p n t", t=292), in_=pp[pb_][:, 0:4, 0:292], func=AF.Copy),
                        reads=[('pp', pb_)], writes=[('uk', k % 2)])
                    wdw = (2, 4, 8, 16)[g]
                    cur, ckey = u, ('uk', k % 2)
                    for s_ in range(int(math.log2(wdw))):
                        sh = 1 << s_
                        nxt, nkey = (sA, 'sA') if s_ % 2 == 0 else (sB, 'sB')
                        P.op('pool', lambda cur=cur, nxt=nxt, sh=sh: G.tensor_tensor(
                            out=nxt[:, sh:1168], in0=cur[:, sh:1168], in1=cur[:, 0:1168 - sh], op=ALU.add),
                            reads=[ckey], writes=[nkey])
                        cur, ckey = nxt, nkey
                    P.op('dve', lambda cur=cur, u=u, k=k, wdw=wdw: V.scalar_tensor_tensor(
                        out=pooledT[:, k, :], in0=cur[:, 16:1168], scalar=1.0 / wdw, in1=u[:, 16:1168],
                        op0=ALU.mult, op1=ALU.subtract), reads=[ckey, ('uk', k % 2)], writes=[('pooledT', k)])
                    P.op('dve', lambda cur=cur, k=k: V.tensor_tensor(out=t16[:, :], in0=cur[:, 144:160], in1=invc_sb[:, k, :], op=ALU.mult),
                         reads=[ckey, 'invc'], writes=['t16'])
                    P.op('dve', lambda u=u, k=k: V.tensor_tensor(out=pooledT[:, k, 128:144], in0=t16[:, :], in1=u[:, 144:160], op=ALU.subtract),
                         reads=['t16', ('uk', k % 2)], writes=[('pooledT', k)])
            for k2 in range(8):
                g, dm = k2 // 2, k2 % 2
                pb_ = next_pp()
                for n in range(3):
                    for cc in range(2):
                        P.op('pe', lambda n=n, cc=cc, g=g, dm=dm, pb_=pb_: T.matmul(
                            pp[pb_][:, n, 0:384], lhsT=Wpl[:, g, cc, dm * 128:(dm + 1) * 128],
                            rhs=pooledT[:, 2 * g + cc, n * 384:(n + 1) * 384], start=(cc == 0), stop=(cc == 1)),
                            reads=['Wpl', ('pooledT', 2 * g + cc)], writes=[('pp', pb_)])
                P.op('act', lambda k2=k2, pb_=pb_: A.activation(
                    out=ypT[:, k2, :].rearrange("p (n t) -> p n t", t=384), in_=pp[pb_][:, 0:3, 0:384],
                    func=AF.Copy, scale=psc_sb[:, k2:k2 + 1]), reads=[('pp', pb_), 'pscale'], writes=[('ypT', k2)])
            idx_scale = (16 ** -0.5) * (64 ** -0.5)
            for i in range(NQB):
                pb_ = next_pp()
                for c in range(16):
                    P.op('pe', lambda c=c, i=i, pb_=pb_: T.matmul(
                        pp[pb_][:, 0, 0:16], lhsT=hM[:, c, 128 + i * 128:256 + i * 128], rhs=Ww[:, c, :],
                        start=(c == 0), stop=(c == 15)), reads=['Ww'], writes=[('pp', pb_)])
                P.op('act', lambda i=i, pb_=pb_: A.activation(out=widx[:, i, :], in_=pp[pb_][:, 0, 0:16], func=AF.Copy, scale=idx_scale),
                     reads=[('pp', pb_)], writes=[('widx', i)])
            P.dma('sp', QT_d[:, :, :], QT[:, :, :], reads=[('fm', id(QT), h_) for h_ in range(8)])
            P.dma('sp', qiT_d[:, :, :], qiT[:, :, :], reads=[('fm', id(qiT), h_) for h_ in range(8)])
            P.dma('sp', ypT_d[:, :, :], ypT[:, :, :], reads=[('ypT', h_) for h_ in range(8)])
            P.dma('sp', widx_d[:, :, :], widx[:, :, :], reads=[('widx', h_) for h_ in range(NQB)])
            P.barrier()
        if stop_after == 'B':
            return nc

        pcd = contextlib.ExitStack()
        es.enter_context(pcd)
        maskT = sb("maskT", [128, 32, NQ], BF16, pcd)
        with contextlib.ExitStack() as pc:
            kidxT = sb("kidxTc", [128, CTX], BF16, pc)
            qiT = sb("qiTc", [128, 8, NQ], BF16, pc)
            widx = sb("widxc", [128, NQB, 16], F32, pc)
            P.dma('sp', kidxT[:, :], kidxT_d[:, :], writes=['kidxT'])
            P.dma('sp', qiT[:, :, :], qiT_d[:, :, :], writes=['qiT'])
            P.dma('sp', widx[:, :, :], widx_d[:, :, :], writes=['widx'])
            slot_row = sb("slot_row", [1, CTX], BF16, pc)
            ones_row = sb("ones_row", [1, 128], BF16, pc)
            causal_b = sb("causal_b", [128, 128], BF16, pc)
            halfs_sb = sb("halfs_sb", [128, NIT + 1], F32, pc)
            sc = [sb("sc%d" % i, [128, CTX], F32, pc) for i in range(2)]
            junk = sb("junk", [128, CTX], BF16, pc)
            Rb = sb("Rb", [128, 4, 512], BF16, pc)
            Dg = [sb("Dg%d" % i, [128, 16, 128], BF16, pc) for i in range(2)]
            bsx = [sb("bsx%d" % i, [128, 32], F32, pc) for i in range(2)]
            bs = sb("bs", [128, 64], F32, pc)
            thr_all = sb("thr_all", [128, NQB], F32, pc)
            pd = ps("pdC", [128, 4, 512], F32, pc)
            psc = [ps("pscC%d" % i, [128, 512], F32, pc) for i in range(3)]
            ptm = [ps("ptmC%d" % i, [128, 1024], BF16, pc) for i in range(1)]
            P.dma('pool', slot_row[0:1, :], slotb[0:1, :], writes=['slot_row'])
            P.dma('pool', causal_b[:, :], causal[:, :], writes=['causal_b'])
            P.dma('sp', halfs_sb[:], halfs[:, :], writes=['halfs_sb'])
            P.op('pool', lambda: G.memset(ones_row[0:1, :], 1.0), writes=['ones_row'])
            P.op('pool', lambda: G.memset(maskT[:, :, :].rearrange("p a b -> p (a b)"), 0.0), writes=['maskT'])
            mid = bs[:, 3:4]
            cntv = bs[:, 4:5]
            gg = bs[:, 5:6]
            lo = bs[:, 6:7]
            hi = bs[:, 7:8]
            w0 = bs[:, 8:9]
            hk = bs[:, 16:16 + NIT + 1]
            cnts = dict(d=0, s=0, t=0)

            def gen_scores(i, bi):
                E = QS0 + 128 * (i + 1)
                nkt = (E + 511) // 512
                mins = bsx[bi][:, 0:8]
                maxs = bsx[bi][:, 8:16]
                for h in range(16):
                    P.op('pool', lambda h=h: G.tensor_scalar(out=Dg[bi][:, h, :], in0=ident_f[:], scalar1=widx[:, i, h:h + 1],
                                                             scalar2=1.0, op0=ALU.mult, op1=ALU.mult),
                         reads=['ident_f', 'widx'], writes=[('Dg', bi, h)])
                prev = None

                def finish(kt, N, pst, pskey):
                    P.op('pe', lambda: T.matmul(pst[:, :N], lhsT=ones_row[0:1, :], rhs=slot_row[0:1, kt * 512:kt * 512 + N],
                                                start=False, stop=(kt != nkt - 1)),
                         reads=['ones_row', 'slot_row', ('mins', bi, kt), ('maxs', bi, kt)], writes=[pskey])
                    if kt == nkt - 1:
                        P.op('pe', lambda: T.matmul(pst[:, N - 128:N], lhsT=ident_b[:, :], rhs=causal_b[:, :], start=False, stop=True),
                             reads=['ident_b', 'causal_b'], writes=[pskey])
                    P.op('act', lambda: A.activation(out=sc[bi][:, kt * 512:kt * 512 + N], in_=pst[:, :N], func=AF.Copy),
                         reads=[pskey], writes=[('sc', bi, kt)])
                for kt in range(nkt):
                    N = min(512, E - kt * 512)
                    si = cnts['s'] % 3
                    cnts['s'] += 1
                    pst = psc[si]
                    pskey = ('psc', si)
                    pend = []

                    def score_mm(hp, di, N=N, pst=pst, pskey=pskey):
                        for q_ in range(2):
                            h = 2 * hp + q_
                            P.op('pe', lambda h=h, q_=q_: T.matmul(pst[:, :N], lhsT=Dg[bi][:, h, :], rhs=Rb[:, 2 * di + q_, :N],
                                                                   start=(h == 0), stop=False),
                                 reads=[('Dg', bi, h), ('Rb', di)], writes=[pskey])
                    for hp in range(8):
                        di = cnts['d'] % 2
                        cnts['d'] += 1
                        pdk = ('pd', di)
                        for q_ in range(2):
                            pr = q_ * 64
                            P.op('pe', lambda q_=q_, pr=pr: T.matmul(
                                pd[:, 2 * di + q_, :N], lhsT=qiT[pr:pr + 64, hp, i * 128:(i + 1) * 128],
                                rhs=kidxT[pr:pr + 64, kt * 512:kt * 512 + N], start=True, stop=True),
                                reads=['kidxT', 'qiT'], writes=[pdk])
                        P.op('act', lambda di=di: A.activation(out=Rb[:, 2 * di:2 * di + 2, :N], in_=pd[:, 2 * di:2 * di + 2, :N], func=AF.Relu),
                             reads=[pdk], writes=[('Rb', di)])
                        pend.append((hp, di))
                        if len(pend) > 1:
                            score_mm(*pend.pop(0))
                    while pend:
                        score_mm(*pend.pop(0))
                    P.op('dve', lambda pst=pst, N=N, kt=kt: V.tensor_reduce(out=mins[:, kt:kt + 1], in_=pst[:, :N], axis=AX.X, op=ALU.min),
                         reads=[pskey], writes=[('mins', bi, kt)])
                    P.op('dve', lambda pst=pst, N=N, kt=kt: V.tensor_reduce(out=maxs[:, kt:kt + 1], in_=pst[:, :N], axis=AX.X, op=ALU.max),
                         reads=[pskey], writes=[('maxs', bi, kt)])
                    if prev is not None:
                        finish(*prev)
                    prev = (kt, N, pst, pskey)
                    yield
                finish(*prev)
                yield

            def bisect(i, bi, nxt):
                E = QS0 + 128 * (i + 1)
                nkt = (E + 511) // 512
                mins = bsx[bi][:, 0:8]
                maxs = bsx[bi][:, 8:16]
                sckeys = [('sc', bi, kt) for kt in range(nkt)]
                P.op('dve', lambda: V.tensor_reduce(out=lo, in_=mins[:, 0:nkt], axis=AX.X, op=ALU.min),
                     reads=[('mins', bi, kt) for kt in range(nkt)], writes=['lo'])
                P.op('dve', lambda: V.tensor_reduce(out=hi, in_=maxs[:, 0:nkt], axis=AX.X, op=ALU.max),
                     reads=[('maxs', bi, kt) for kt in range(nkt)], writes=['hi'])
                P.op('dve', lambda: V.scalar_tensor_tensor(out=w0, in0=hi, scalar=2.0, in1=lo, op0=ALU.add, op1=ALU.subtract),
                     reads=['hi', 'lo'], writes=['w0'])
                P.op('dve', lambda: V.tensor_scalar(out=hk, in0=halfs_sb[:, :], scalar1=w0, scalar2=None, op0=ALU.mult),
                     reads=['w0', 'halfs_sb'], writes=['hk'])
                P.op('dve', lambda: V.scalar_tensor_tensor(out=mid, in0=lo, scalar=-1.0, in1=hk[:, 0:1], op0=ALU.add, op1=ALU.add),
                     reads=['lo', 'hk'], writes=['mid'])
                for it in range(NIT):
                    P.op('dve', lambda: V.tensor_scalar(out=junk[:, 0:E], in0=sc[bi][:, 0:E], scalar1=mid, scalar2=None,
                                                        op0=ALU.is_ge, op1=ALU.add, accum_out=cntv),
                         reads=sckeys + ['mid'], writes=['junk', 'cnt'])
                    P.op('dve', lambda: V.tensor_scalar(out=gg, in0=cntv, scalar1=255.5, scalar2=0.5, op0=ALU.is_ge, op1=ALU.subtract),
                         reads=['cnt'], writes=['gg'])
                    P.op('dve', lambda it=it: V.scalar_tensor_tensor(out=mid, in0=gg, scalar=hk[:, it:it + 1], in1=mid,
                                                                      op0=ALU.mult, op1=ALU.add),
                         reads=['gg', 'hk', 'mid'], writes=['mid'])
                    if nxt is not None and it % 2 == 1:
                        next(nxt, None)
                if nxt is not None:
                    for _ in nxt:
                        pass
                P.op('dve', lambda: V.tensor_tensor(out=lo, in0=mid, in1=hk[:, NIT:NIT + 1], op=ALU.subtract),
                     reads=['mid', 'hk'], writes=['lo'])
                P.op('dve', lambda: V.tensor_scalar(out=junk[:, 0:E], in0=sc[bi][:, 0:E], scalar1=lo, scalar2=None, op0=ALU.is_ge),
                     reads=sckeys + ['lo'], writes=['junk'])
                P.op('dve', lambda: V.tensor_copy(out=thr_all[:, i:i + 1], in_=lo), reads=['lo'], writes=['thr_all'])
                nkb = E // 128
                for kb0 in range(0, nkb, 8):
                    n8 = min(8, nkb - kb0)
                    pt = ptm[0]
                    ptk = ('ptm', 0)
                    for r in range(n8):
                        kb = kb0 + r
                        P.op('pe', lambda r=r, kb=kb: T.transpose(out=pt[:, r * 128:(r + 1) * 128],
                                                                 in_=junk[:, kb * 128:(kb + 1) * 128], identity=ident_b[:]),
                             reads=['junk', 'ident_b'], writes=[ptk])
                    P.op('act', lambda n8=n8, kb0=kb0: A.activation(
                        out=maskT[:, kb0:kb0 + n8, i * 128:(i + 1) * 128],
                        in_=pt[:, 0:n8 * 128].rearrange("p (a b) -> p a b", b=128), func=AF.Copy),
                        reads=[ptk], writes=['maskT'])

            for _ in gen_scores(0, 0):
                pass
            for i in range(NQB):
                nxt = gen_scores(i + 1, (i + 1) % 2) if i + 1 < NQB else None
                bisect(i, i % 2, nxt)
            if debug:
                P.dma('sp', dbg['maskT'][:, :, :], maskT[:, :, :], reads=['maskT'])
                P.dma('sp', dbg['thr'][:, :], thr_all[:, :], reads=['thr_all'])
            P.barrier()
        if stop_after == 'C':
            return nc

        SCALE = 128 ** -0.5
        with contextlib.ExitStack() as pdd:
            attnT = sb("attnT", [128, 8, NQ], BF16, pdd)
            QT = sb("QTd", [128, 8, NQ], BF16, pdd)
            P.dma('sp', QT[:, :, :], QT_d[:, :, :], writes=['QT'])
            KTh = [sb("KTh%d" % i, [128, CTX], BF16, pdd) for i in range(2)]
            Vh = [sb("Vh%d" % i, [128, 32, 128], BF16, pdd) for i in range(2)]
            strip_sb = sb("strip_sb", [128, 8, 896], F32, pdd)
            cfar_sb = sb("cfar_sb", [128, 8], F32, pdd)
            Eb = [sb("Eb%d" % i, [128, 384], BF16, pdd) for i in range(4)]
            Pb = [sb("Pb%d" % i, [128, 384], BF16, pdd) for i in range(4)]
            tmpb = [sb("tmpb%d" % i, [128, 384], F32, pdd) for i in range(2)]
            rl = sb("rl", [128, 384], F32, pdd)
            pS = [ps("pS%d" % i, [128, 512], F32, pdd) for i in range(4)]
            pO = [ps("pO%d" % i, [128, 512], F32, pdd) for i in range(2)]
            pL = [ps("pL%d" % i, [128, 512], F32, pdd) for i in range(2)]
            P.dma('sp', strip_sb[:], strip[:, :, :], writes=['strip_sb'])
            P.dma('sp', cfar_sb[:], cfar[:, :], writes=['cfar_sb'])
            steps = []
            for h in range(8):
                for qt in range(3):
                    kbmax = 23 + 3 * qt + 2
                    for kb in range(kbmax, -1, -1):
                        steps.append((h, qt, kb, kb == kbmax, kb == 0))
            loaded = set()

            def load_head(h):
                if h in loaded or h >= 8:
                    return
                loaded.add(h)
                b = h % 2
                P.dma('sp', KTh[b][:, :], KT_d[h, :, :], writes=[('KTh', b)])
                P.dma('sp', Vh[b][:, :, :], V_d[h, :, :].rearrange("(kb p) d -> p kb d", p=128), writes=[('Vh', b)])
            cn = dict(s=0, t=0)
            staged = []

            def stage1(step):
                h, qt, kb, first, last = step
                b = h % 2
                t0 = qt * 384
                if first and qt == 0:
                    load_head(h)
                if qt == 0 and kb == 23 + 2 - 6:
                    load_head(h + 1)
                s_i = cn['s'] % 4
                cn['s'] += 1
                pst = pS[s_i]
                P.op('pe', lambda: T.matmul(pst[:, 0:384], lhsT=KTh[b][:, kb * 128:(kb + 1) * 128],
                                            rhs=QT[:, h, t0:t0 + 384], start=True, stop=True),
                     reads=[('KTh', b), 'QT'], writes=[('pS', s_i)])
                D0 = (QS0 + t0) - kb * 128
                if D0 >= 256:
                    P.op('act', lambda: A.activation(out=Eb[s_i][:, :], in_=pst[:, 0:384], func=AF.Exp,
                                                     scale=SCALE, bias=cfar_sb[:, h:h + 1]),
                         reads=[('pS', s_i), 'cfar_sb'], writes=[('Eb', s_i)])
                else:
                    ti = cn['t'] % 2
                    cn['t'] += 1
                    P.op('dve', lambda: V.scalar_tensor_tensor(
                        out=tmpb[ti][:, :], in0=pst[:, 0:384], scalar=SCALE,
                        in1=strip_sb[:, h, D0 + 256:D0 + 256 + 384], op0=ALU.mult, op1=ALU.add),
                        reads=[('pS', s_i), 'strip_sb'], writes=[('tmpb', ti)])
                    P.op('act', lambda: A.activation(out=Eb[s_i][:, :], in_=tmpb[ti][:, :], func=AF.Exp),
                         reads=[('tmpb', ti)], writes=[('Eb', s_i)])
                P.op('dve', lambda: V.tensor_tensor(out=Pb[s_i][:, :], in0=Eb[s_i][:, :],
                                                    in1=maskT[:, kb, t0:t0 + 384], op=ALU.mult),
                     reads=[('Eb', s_i)], writes=[('Pb', s_i)])
                staged.append((step, s_i))

            def stage2():
                (h, qt, kb, first, last), s_i = staged.pop(0)
                b = h % 2
                t0 = qt * 384
                g_ = (h * 3 + qt) % 2
                po, pl = pO[g_], pL[g_]
                pok, plk = ('pO', g_), ('pL', g_)
                P.op('pe', lambda: T.matmul(po[:, 0:384], lhsT=Vh[b][:, kb, :], rhs=Pb[s_i][:, :], start=first, stop=last),
                     reads=[('Vh', b), ('Pb', s_i)], writes=[pok])
                P.op('pe', lambda: T.matmul(pl[:, 0:384], lhsT=ones_b[:, :], rhs=Pb[s_i][:, :], start=first, stop=last),
                     reads=['ones_b', ('Pb', s_i)], writes=[plk])
                if last:
                    P.op('dve', lambda: V.tensor_scalar(out=rl[:, :], in0=pl[:, 0:384], scalar1=1e-30, scalar2=None, op0=ALU.add),
                         reads=[plk], writes=['rl'])
                    P.op('dve', lambda: V.reciprocal(out=rl[:, :], in_=rl[:, :]), reads=['rl'], writes=['rl'])
                    P.op('dve', lambda: V.tensor_tensor(out=attnT[:, h, t0:t0 + 384], in0=po[:, 0:384], in1=rl[:, :], op=ALU.mult),
                         reads=[pok, 'rl'], writes=[('attnT', h)])
            for step in steps:
                stage1(step)
                if len(staged) > 3:
                    stage2()
            while staged:
                stage2()
            P.dma('sp', attnT_d[:, :, :], attnT[:, :, :], reads=[('attnT', h_) for h_ in range(8)])
            P.barrier()
        pcd.close()
        if stop_after == 'D':
            return nc

        with contextlib.ExitStack() as pf:
            Wo = sb("Wo", [128, 16, 2048], BF16, pf)
            ypT = sb("ypTf", [128, 8, NQ], BF16, pf)
            attnT = sb("attnTf", [128, 8, NQ], BF16, pf)
            P.dma('sp', ypT[:, :, :], ypT_d[:, :, :], writes=['ypT'])
            P.dma('sp', attnT[:, :, :], attnT_d[:, :, :], writes=['attnT'])
            xr = [sb("xrF%d" % i, [128, D], F32, pf) for i in range(2)]
            x1t = [sb("x1tF%d" % i, [128, D], F32, pf) for i in range(2)]
            pw = [ps("pwF%d" % i, [128, 512], F32, pf) for i in range(8)]
            for cg in range(4):
                wload(Wo, ('Wo', cg), w_out, 4 * cg, 4 * cg + 4, 0, 2048)
            pcn = 0
            for i in range(NQB):
                bi = i % 2
                P.dma('sp', xr[bi][:], xctx[QS0 + i * 128:QS0 + (i + 1) * 128, :], writes=[('xr', bi)])
                for nt in range(4):
                    pq = pw[pcn % 8]
                    pk = ('pw', pcn % 8)
                    pcn += 1
                    for c in range(16):
                        src = ypT if c < 8 else attnT
                        P.op('pe', lambda c=c, src=src, pq=pq, nt=nt, i=i: T.matmul(
                            pq[:, :], lhsT=src[:, c % 8, i * 128:(i + 1) * 128], rhs=Wo[:, c, nt * 512:(nt + 1) * 512],
                            start=(c == 0), stop=(c == 15)), reads=[('Wo', c // 4), 'ypT', 'attnT'], writes=[pk])
                    P.op('dve', lambda pq=pq, nt=nt, bi=bi: V.tensor_tensor(
                        out=x1t[bi][:, nt * 512:(nt + 1) * 512], in0=pq[:, :], in1=xr[bi][:, nt * 512:(nt + 1) * 512], op=ALU.add),
                        reads=[pk, ('xr', bi)], writes=[('x1t', bi)])
                P.dma('sp', x1_d[i * 128:(i + 1) * 128, :], x1t[bi][:], reads=[('x1t', bi)], writes=[('x1_d', i)])
            P.barrier()
        if stop_after == 'F':
            return nc


        pgh = contextlib.ExitStack()
        es.enter_context(pgh)
        gT = sb("gT", [128, NFC, 1024], BF16, pgh)
        with contextlib.ExitStack() as pg_:
            h2T = sb("h2T", [128, 16, NQ], BF16, pg_)
            Wgv = [sb("Wgv%d" % i, [128, 16, 512], BF16, pg_) for i in range(2)]

            def g_load(grp):
                jg_, isval_ = grp // 2, grp % 2
                wload(Wgv[grp % 2], ('Wgv', grp % 2), w_up, 0, 16, (D_FF if isval_ else 0) + jg_ * 512, 512)
            g_load(0)
            g_load(1)
            with contextlib.ExitStack() as pg1:
                gsb = sb("gsbG", [128, 16, 128], F32, pg1)
                hf_sb = sb("hf_sb", [128, 1], F32, pg1)
                xb = [sb("xbG%d" % i, [128, D], F32, pg1) for i in range(2)]
                xn = [sb("xnG%d" % i, [128, D], BF16, pg1) for i in range(2)]
                pT = [[ps("pTG%d%d" % (i, h), [128, 1024], BF16, pg1) for h in range(2)] for i in range(2)]
                P.dma('sp', gsb[:].rearrange("p c t -> p (c t)"), gT3[1, :, :], writes=['gsb'])
                P.dma('sp', hf_sb[:], hflag[:, :], writes=['hflag'])
                for j in range(NQB):
                    i = j % 2
                    P.dma('sp', xb[i][:], x1_d[j * 128:(j + 1) * 128, :], writes=[('xb', i)])
                    norm_T(i, xb[i][:], ('xb', i), xn, pT, gsb,
                           lambda h, j=j: h2T[:, 8 * h:8 * h + 8, j * 128:(j + 1) * 128], [('h2T', j)],
                           extra=(hf_sb[:, 0:1] if j == 0 else None))
                P.barrier()
            sgT = sb("sgT", [128, 4, 1024], F32, pg_)
            cp = sb("cp", [128, 2 * NFC, 4], F32, pg_)
            rA = [sb("rA%d" % i, [128, 344], F32, pg_) for i in range(3)]
            rB = [sb("rB%d" % i, [128, 344], F32, pg_) for i in range(3)]
            pu = [ps("puG%d" % i, [128, 512], F32, pg_) for i in range(8)]
            P.dma('sp', cp[:], convp[:, :, :], writes=['cp'])
            tiles = [(0, 342), (342, 684), (684, 1024)]
            pcn = 0
            rcn = 0
            for grp in range(2 * (NFC // 4)):
                jg, isval = grp // 2, grp % 2
                wb = grp % 2
                if grp >= 1 and grp + 1 < 2 * (NFC // 4):
                    g_load(grp + 1)
                for m_ in range(4):
                    j = 4 * jg + m_
                    jj = (NFC + j) if isval else j
                    for (a, b_) in tiles:
                        N = b_ - a + 2
                        n2 = N - 2
                        c0 = 128 + a - 2
                        pq = pu[pcn % 8]
                        pk = ('pu', pcn % 8)
                        pcn += 1
                        for c in range(16):
                            P.op('pe', lambda c=c: T.matmul(
                                pq[:, 0:N], lhsT=Wgv[wb][:, c, m_ * 128:(m_ + 1) * 128], rhs=h2T[:, c, c0:c0 + N],
                                start=(c == 0), stop=(c == 15)), reads=[('Wgv', wb)], writes=[pk])
                        ri = rcn % 3
                        rcn += 1
                        P.op('act', lambda: A.activation(
                            out=rA[ri][:, 0:n2], in_=pq[:, 2:N], func=AF.Identity, scale=cp[:, jj, 2:3], bias=cp[:, jj, 3:4]),
                            reads=[pk, 'cp'], writes=[('rA', ri)])
                        P.op('dve', lambda: V.scalar_tensor_tensor(
                            out=rB[ri][:, 0:n2], in0=pq[:, 1:N - 1], scalar=cp[:, jj, 1:2], in1=rA[ri][:, 0:n2],
                            op0=ALU.mult, op1=ALU.add), reads=[pk, 'cp', ('rA', ri)], writes=[('rB', ri)])
                        if not isval:
                            P.op('dve', lambda: V.scalar_tensor_tensor(
                                out=rA[ri][:, 0:n2], in0=pq[:, 0:n2], scalar=cp[:, jj, 0:1], in1=rB[ri][:, 0:n2],
                                op0=ALU.mult, op1=ALU.add), reads=[pk, 'cp', ('rB', ri)], writes=[('rA', ri)])
                            P.op('act', lambda: A.activation(out=sgT[:, m_, a:b_], in_=rA[ri][:, 0:n2], func=AF.Silu),
                                 reads=[('rA', ri)], writes=[('sgT', m_)])
                        else:
                            P.op('dve', lambda: V.scalar_tensor_tensor(
                                out=rA[ri][:, 0:n2], in0=pq[:, 0:n2], scalar=cp[:, jj, 0:1], in1=rB[ri][:, 0:n2],
                                op0=ALU.mult, op1=ALU.add), reads=[pk, 'cp', ('rB', ri)], writes=[('rA', ri)])
                            P.op('dve', lambda: V.tensor_tensor(
                                out=gT[:, j, a:b_], in0=sgT[:, m_, a:b_], in1=rA[ri][:, 0:n2], op=ALU.mult),
                                reads=[('sgT', m_), ('rA', ri)], writes=[('gT', j)])
            if debug:
                P.dma('sp', dbg['gT'][:, :, :], gT[:, :, :], reads=[('gT', j_) for j_ in range(NFC)])
            P.barrier()
        if stop_after == 'G':
            return nc

        with contextlib.ExitStack() as ph:
            Wds = [sb("Wds%d" % i, [128, 4, 512], BF16, ph) for i in range(3)]
            x1s = [sb("x1s%d" % i, [128, 512], F32, ph) for i in range(4)]
            x2s = [sb("x2s%d" % i, [128, 512], F32, ph) for i in range(4)]
            pw = [ps("pwH%d" % i, [128, 512], F32, ph) for i in range(8)]
            wcn = 0
            scn = 0
            pset = 0
            for tg in range(2):
                for nt in range(4):
                    banks = [(pw[4 * (pset % 2) + tb], ('pw', 4 * (pset % 2) + tb)) for tb in range(4)]
                    pset += 1
                    for fg in range(NFC // 4):
                        wb = wcn % 3
                        wcn += 1
                        P.dma('pool', Wds[wb][:, :, :],
                              w_down[fg * 512:(fg + 1) * 512, nt * 512:(nt + 1) * 512].rearrange("(c p) n -> p c n", p=128),
                              writes=[('Wds', wb)])
                        for fl in range(4):
                            f = fg * 4 + fl
                            for tb in range(4):
                                pq, pk = banks[tb]
                                tok0 = (tg * 4 + tb) * 128
                                P.op('pe', lambda pq=pq, f=f, fl=fl, wb=wb, tok0=tok0: T.matmul(
                                    pq[:, :], lhsT=gT[:, f, tok0:tok0 + 128], rhs=Wds[wb][:, fl, :],
                                    start=(f == 0), stop=(f == NFC - 1)), reads=[('Wds', wb)], writes=[pk])
                    for tb in range(4):
                        pq, pk = banks[tb]
                        si = scn % 4
                        scn += 1
                        r0 = (tg * 4 + tb) * 128
                        P.dma('sp', x1s[si][:, :], x1_d[128 + r0:128 + r0 + 128, nt * 512:(nt + 1) * 512], writes=[('x1s', si)])
                        P.op('dve', lambda pq=pq, si=si: V.tensor_tensor(out=x2s[si][:, :], in0=pq[:, :], in1=x1s[si][:, :], op=ALU.add),
                             reads=[pk, ('x1s', si)], writes=[('x2s', si)])
                        P.dma('sp', x2_d[r0:r0 + 128, nt * 512:(nt + 1) * 512], x2s[si][:, :], reads=[('x2s', si)], writes=[('x2_d', r0, nt)])
            P.barrier()
        pgh.close()
        if stop_after == 'H':
            return nc

        with contextlib.ExitStack() as pi:
            x3 = sb("x3", [128, 8, D], F32, pi)
            h3T = sb("h3T", [128, 16, 1024], BF16, pi)
            ppT = sb("ppT", [128, 2, 1024], BF16, pi)
            with contextlib.ExitStack() as pi1:
                gsb = sb("gsbI", [128, 16, 128], F32, pi1)
                p_sb = sb("p_sb", [128, 8, 256], F32, pi1)
                p_bf = sb("p_bf", [128, 8, 256], BF16, pi1)
                xn = [sb("xnI%d" % i, [128, D], BF16, pi1) for i in range(2)]
                pT = [[ps("pTI%d%d" % (i, h), [128, 1024], BF16, pi1) for h in range(2)] for i in range(2)]
                ptp = [ps("ptpI%d" % i, [128, 1024], BF16, pi1) for i in range(2)]
                P.dma('sp', gsb[:].rearrange("p c t -> p (c t)"), gT3[2, :, :], writes=['gsb'])
                P.dma('sp', p_sb[:, :, :], p_own.rearrange("(tb p) c -> p tb c", p=128), writes=['p_sb'])
                P.op('pool', lambda: G.tensor_copy(out=p_bf[:, :, :], in_=p_sb[:, :, :]), reads=['p_sb'], writes=['p_bf'])
                for tb in range(8):
                    P.dma('sp', x3[:, tb, :], x2_d[tb * 128:(tb + 1) * 128, :], writes=[('x3', tb)])
                for tb in range(8):
                    i = tb % 2
                    norm_T(i, x3[:, tb, :], ('x3', tb), xn, pT, gsb,
                           lambda h, tb=tb: h3T[:, 8 * h:8 * h + 8, tb * 128:(tb + 1) * 128], [('h3T', tb)])
                    pt = ptp[i]
                    for cc in range(2):
                        P.op('pe', lambda pt=pt, cc=cc, tb=tb: T.transpose(out=pt[:, cc * 128:(cc + 1) * 128],
                                                                          in_=p_bf[:, tb, cc * 128:(cc + 1) * 128], identity=ident_b[:]),
                             reads=['p_bf', 'ident_b'], writes=[('ptp', i)])
                    P.op('act', lambda pt=pt, tb=tb: A.activation(out=ppT[:, :, tb * 128:(tb + 1) * 128],
                                                                  in_=pt[:, 0:256].rearrange("p (a b) -> p a b", b=128), func=AF.Copy),
                         reads=[('ptp', i)], writes=[('ppT', tb)])
                P.barrier()
            with contextlib.ExitStack() as pi2:
                Wgs = [sb("WgsI%d" % i, [128, 16, 512], BF16, pi2) for i in range(2)]
                Wps = [sb("WpsI%d" % i, [128, 2, 512], BF16, pi2) for i in range(2)]
                sgm = [sb("sgm%d" % i, [128, 512], F32, pi2) for i in range(2)]
                tmpI = [sb("tmpI%d" % i, [128, 512], F32, pi2) for i in range(2)]
                pgt = [ps("pgtI%d" % i, [128, 512], F32, pi2) for i in range(4)]
                ppe = [ps("ppeI%d" % i, [128, 512], F32, pi2) for i in range(4)]
                cn = 0

                def i_load(nt_):
                    wb_ = nt_ % 2
                    for cg in range(4):
                        P.dma('pool', Wgs[wb_][:, 4 * cg:4 * cg + 4, :],
                              w_pg[cg * 512:(cg + 1) * 512, nt_ * 512:(nt_ + 1) * 512].rearrange("(c p) n -> p c n", p=128),
                              writes=[('WgsI', wb_, cg)])
                    P.dma('pool', Wps[wb_][:, :, :], w_pp[:, nt_ * 512:(nt_ + 1) * 512].rearrange("(c p) n -> p c n", p=128),
                          writes=[('WpsI', wb_)])
                i_load(0)
                for nt in range(4):
                    wb = nt % 2
                    if nt + 1 < 4:
                        i_load(nt + 1)
                    for tb in range(8):
                        bi = cn % 4
                        si = cn % 2
                        cn += 1
                        for c in range(16):
                            P.op('pe', lambda c=c, bi=bi, tb=tb, wb=wb: T.matmul(
                                pgt[bi][:, :], lhsT=h3T[:, c, tb * 128:(tb + 1) * 128], rhs=Wgs[wb][:, c, :],
                                start=(c == 0), stop=(c == 15)), reads=[('WgsI', wb, c // 4)], writes=[('pgt', bi)])
                        for cc in range(2):
                            P.op('pe', lambda cc=cc, bi=bi, tb=tb, wb=wb: T.matmul(
                                ppe[bi][:, :], lhsT=ppT[:, cc, tb * 128:(tb + 1) * 128], rhs=Wps[wb][:, cc, :],
                                start=(cc == 0), stop=(cc == 1)), reads=[('WpsI', wb)], writes=[('ppe', bi)])
                        P.op('act', lambda bi=bi, si=si: A.activation(out=sgm[si][:, :], in_=pgt[bi][:, :], func=AF.Sigmoid),
                             reads=[('pgt', bi)], writes=[('sgm', si)])
                        P.op('dve', lambda bi=bi, si=si: V.tensor_tensor(out=tmpI[si][:, :], in0=ppe[bi][:, :], in1=sgm[si][:, :], op=ALU.mult),
                             reads=[('ppe', bi), ('sgm', si)], writes=[('tmpI', si)])
                        P.op('pool', lambda si=si, tb=tb, nt=nt: G.tensor_tensor(
                            out=x3[:, tb, nt * 512:(nt + 1) * 512], in0=x3[:, tb, nt * 512:(nt + 1) * 512], in1=tmpI[si][:, :], op=ALU.add),
                            reads=[('tmpI', si)], writes=[('x3o', tb)])
                P.barrier()
            with contextlib.ExitStack() as pi3:
                gf = sb("gf_sb", [128, D], F32, pi3)
                ot = [sb("ot%d" % i, [128, D], F32, pi3) for i in range(2)]
                jk = sb("jkI", [128, D], BF16, pi3)
                P.dma('sp', gf[:, :], gfin[:, :], writes=['gf'])
                for tb in range(8):
                    i = tb % 2
                    ss = st[:, 3 * i:3 * i + 1]
                    sd = st[:, 3 * i + 1:3 * i + 2]
                    rs = st[:, 3 * i + 2:3 * i + 3]
                    P.op('act', lambda tb=tb, ss=ss: A.activation(out=jk[:, :], in_=x3[:, tb, :], func=AF.Square, accum_out=ss),
                         writes=['jk', ('ss', i)])
                    P.op('dve', lambda ss=ss, sd=sd: V.tensor_scalar(out=sd, in0=ss, scalar1=1.0 / D, scalar2=EPS, op0=ALU.mult, op1=ALU.add),
                         reads=[('ss', i)], writes=[('sd', i)])
                    P.op('act', lambda sd=sd: A.activation(out=sd, in_=sd, func=AF.Sqrt), reads=[('sd', i)], writes=[('sd', i)])
                    P.op('dve', lambda sd=sd, rs=rs: V.reciprocal(out=rs, in_=sd), reads=[('sd', i)], writes=[('rs', i)])
                    P.op('dve', lambda tb=tb, rs=rs, i=i: V.scalar_tensor_tensor(
                        out=ot[i][:, :], in0=x3[:, tb, :], scalar=rs, in1=gf[:, :], op0=ALU.mult, op1=ALU.mult),
                        reads=[('rs', i), 'gf'], writes=[('ot', i)])
                    P.dma('sp', out[tb * 128:(tb + 1) * 128, :], ot[i][:, :], reads=[('ot', i)], writes=[('out', tb)])
                P.barrier()
    return nc


def _t5_bucket_static(dist):
    n = np.maximum(dist, 0)
    nf = np.maximum(n, 1).astype(np.float32)
    large = 16 + (np.log(nf / np.float32(16)) / np.float32(math.log(128 / 16)) * 16).astype(np.int32)
    large = np.minimum(large, 31)
    return np.where(n < 16, n, large)


def prep_inputs(x, p, g_mix, w_in, w_pool, pool_scale, rel_bias, w_out, g_ffn, w_up, conv_w, conv_b,
                w_down, g_ple, w_ple_gate, w_ple_proj, g_final):
    f = np.float32
    x = np.asarray(x, f)
    p = np.asarray(p, f)[0]
    shared = {}
    shared["w_in"] = np.ascontiguousarray(np.asarray(w_in, f)[0])
    shared["w_pool"] = np.ascontiguousarray(np.asarray(w_pool, f)[0])
    shared["w_out"] = np.ascontiguousarray(np.asarray(w_out, f)[0])
    shared["w_up"] = np.ascontiguousarray(np.asarray(w_up, f)[0])
    shared["w_down"] = np.ascontiguousarray(np.asarray(w_down, f)[0])
    shared["w_ple_gate"] = np.ascontiguousarray(np.asarray(w_ple_gate, f)[0])
    shared["w_ple_proj"] = np.ascontiguousarray(np.asarray(w_ple_proj, f)[0])

    def fm(v):
        a = np.asarray(v, f).reshape(16, 128).T
        return np.ascontiguousarray(np.repeat(a[:, :, None], 128, axis=2).reshape(128, 2048))
    shared["gT3"] = np.stack([fm(np.asarray(g_mix)[0]), fm(np.asarray(g_ffn)[0]), fm(np.asarray(g_ple)[0])], 0)
    shared["gfin"] = np.ascontiguousarray(np.repeat(np.asarray(g_final, f)[None, :], 128, axis=0))
    shared["pscale"] = np.ascontiguousarray(np.asarray(pool_scale, f)[0].reshape(8, 128).T)
    cw = np.asarray(conv_w, f)[0]
    cb = np.asarray(conv_b, f)[0]
    cp = np.concatenate([cw, cb[None, :]], 0)
    shared["convp"] = np.ascontiguousarray(cp.reshape(4, 2 * NFC, 128).transpose(2, 1, 0))
    rb = np.asarray(rel_bias, f)
    sl = np.arange(128)[:, None]
    dl = np.arange(896)[None, :] - 256
    dist = dl - sl
    bk = _t5_bucket_static(dist)
    stripv = rb[bk]
    shared["strip"] = np.ascontiguousarray(stripv.transpose(0, 2, 1))
    shared["cfar"] = np.ascontiguousarray(np.repeat(rb[31][None, :], 128, axis=0))
    tl = np.arange(128)[:, None]
    s_l = np.arange(128)[None, :]
    shared["causal"] = np.where(s_l <= tl, 0.0, NEG).astype(f)
    shared["ident"] = np.eye(128, dtype=f)
    shared["halfs"] = np.ascontiguousarray(np.repeat((0.5 ** np.arange(1, NIT + 2, dtype=np.float64)).astype(f)[None, :], 128, 0))
    in_maps = []
    for c in range(8):
        b, j = c // 4, c % 4
        T0 = j * 1024
        m = dict(shared)
        xc = np.zeros((CTX, D), f)
        lo = T0 - 3072
        s0 = max(0, -lo)
        xc[s0:] = x[b, lo + s0:T0 + 1024]
        m["xctx"] = xc
        m["p_own"] = np.ascontiguousarray(p[b, T0:T0 + 1024])
        sbias = np.zeros((CTX,), f)
        sbias[:s0] = NEG
        m["slotb"] = np.ascontiguousarray(np.repeat(sbias[None, :], 128, axis=0))
        ic = np.zeros((128, 8, 16), f)
        for k in range(8):
            w = (2, 4, 8, 16)[k // 2]
            tok = T0 + np.arange(16)
            cntv = np.minimum(tok + 1, w).astype(f)
            ic[:, k, :] = (np.float32(1.0) / cntv)[None, :]
        m["invc"] = ic
        m["hflag"] = np.full((128, 1), 1.0 if j > 0 else 0.0, f)
        in_maps.append(m)
    return in_maps


_NC_CACHE = {}


def kernel(**inputs):
    in_maps = prep_inputs(**inputs)
    if 'nc' not in _NC_CACHE:
        _NC_CACHE['nc'] = build()
    nc = _NC_CACHE['nc']
    res = run_bass_kernel_spmd(nc, in_maps, core_ids=list(range(8)))
    outs = [np.asarray(res.results[c]["out"], np.float32).reshape(1024, D) for c in range(8)]
    full = np.zeros((2, SEQ, D), np.float32)
    for c in range(8):
        full[c // 4, (c % 4) * 1024:(c % 4 + 1) * 1024] = outs[c]
    return full
```

```python
import contextlib
import math
import numpy as np
import concourse.bass as bass
import concourse.mybir as mybir
from concourse.bass_utils import run_bass_kernel_spmd

F32 = mybir.dt.float32
BF16 = mybir.dt.bfloat16
AF = mybir.ActivationFunctionType
ALU = mybir.AluOpType
AX = mybir.AxisListType

D = 2048
SEQ = 4096
CTX = 4096
NQB = 9
QS0 = 2944
NQ = NQB * 128
HM0 = 2816
NHM = 1280
D_FF = 5632
NFC = 44
EPS = 1e-6
NEG = -1.0e30
NIT = 16
OFF = dict(pool=0, q=1024, k=2048, v=3072, qi=4096, ki=5120, wi=5184)


class Prog:
    NDS = 12

    def __init__(self, nc, es, same_sync=True):
        self.nc = nc
        self.same_sync = same_sync
        self.eng = {'pe': nc.tensor, 'act': nc.scalar, 'dve': nc.vector, 'pool': nc.gpsimd, 'sp': nc.sync}
        self.csem = {e: es.enter_context(nc.semaphore('c_' + e)) for e in ['pe', 'act', 'dve', 'pool']}
        self.cnt = {e: 0 for e in self.csem}
        self.dsem = {q: [es.enter_context(nc.semaphore('d_%s%d' % (q, i))) for i in range(self.NDS)]
                     for q in ['sp', 'pool']}
        self.duse = {q: [0] * self.NDS for q in self.dsem}
        self.dnext = {q: 0 for q in self.dsem}
        self.seen = {e: {} for e in self.eng}
        self.lastw = {}
        self.rds = {}
        self.nwait = 0

    def _wait(self, e, tok):
        sem, val, sid = tok
        if sid == e and (e == 'pe' or not self.same_sync):
            return
        if self.seen[e].get(sid, 0) >= val:
            return
        self.eng[e].wait_ge(sem, val)
        self.seen[e][sid] = val
        self.nwait += 1

    def _deps(self, reads, writes):
        deps = {}

        def add(tok):
            sid = tok[2]
            if sid not in deps or deps[sid][1] < tok[1]:
                deps[sid] = tok
        for k in reads:
            if k in self.lastw:
                add(self.lastw[k])
        for k in writes:
            if k in self.lastw:
                add(self.lastw[k])
            for tok in self.rds.get(k, {}).values():
                add(tok)
        return deps.values()

    def _record(self, tok, reads, writes):
        sid = tok[2]
        for k in reads:
            d = self.rds.setdefault(k, {})
            if sid not in d or d[sid][1] < tok[1]:
                d[sid] = tok
        for k in writes:
            self.lastw[k] = tok
            self.rds[k] = {}

    def op(self, e, fn, reads=(), writes=()):
        for tok in self._deps(reads, writes):
            self._wait(e, tok)
        ins = fn()
        self.cnt[e] += 1
        ins.then_inc(self.csem[e], 1)
        self._record((self.csem[e], self.cnt[e], e), reads, writes)

    def dma(self, q, out, in_, reads=(), writes=(), **kw):
        for tok in self._deps(reads, writes):
            self._wait(q, tok)
        k = self.dnext[q]
        self.dnext[q] = (k + 1) % self.NDS
        u = self.duse[q][k]
        sem = self.dsem[q][k]
        if u > 0:
            self._wait(q, (sem, 16 * u, (q, k)))
        ins = self.eng[q].dma_start(out=out, in_=in_, **kw)
        ins.then_inc(sem, 16)
        self.duse[q][k] = u + 1
        self._record((sem, 16 * (u + 1), (q, k)), reads, writes)

    def barrier(self):
        for e in self.eng:
            for c in self.csem:
                if self.cnt[c] > 0:
                    self._wait_force(e, (self.csem[c], self.cnt[c], c))
            for q in self.dsem:
                for k in range(self.NDS):
                    if self.duse[q][k] > 0:
                        self._wait_force(e, (self.dsem[q][k], 16 * self.duse[q][k], (q, k)))
        self.lastw.clear()
        self.rds.clear()

    def _wait_force(self, e, tok):
        sem, val, sid = tok
        if self.seen[e].get(sid, 0) >= val:
            return
        self.eng[e].wait_ge(sem, val)
        self.seen[e][sid] = val
        self.nwait += 1


def build(debug=False, stop_after='Z'):
    nc = bass.Bass("TRN2", target_bir_lowering=False)

    def din(name, shape, dt=F32):
        return nc.dram_tensor(name, list(shape), dt, kind="ExternalInput").ap()

    xctx = din("xctx", [CTX, D])
    p_own = din("p_own", [1024, 256])
    w_in = din("w_in", [D, 5200])
    w_pool = din("w_pool", [4, 256, 256])
    w_out = din("w_out", [D, D])
    w_up = din("w_up", [D, 2 * D_FF])
    w_down = din("w_down", [D_FF, D])
    w_pg = din("w_ple_gate", [D, D])
    w_pp = din("w_ple_proj", [256, D])
    gT3 = din("gT3", [3, 128, 2048])
    gfin = din("gfin", [128, 2048])
    pscale = din("pscale", [128, 8])
    convp = din("convp", [128, 2 * NFC, 4])
    strip = din("strip", [128, 8, 896])
    cfar = din("cfar", [128, 8])
    slotb = din("slotb", [128, CTX])
    causal = din("causal", [128, 128])
    ident = din("ident", [128, 128])
    invc = din("invc", [128, 8, 16])
    hflag = din("hflag", [128, 1])
    halfs = din("halfs", [128, NIT + 1])
    out = nc.dram_tensor("out", [1024, D], F32, kind="ExternalOutput").ap()
    ks = "ExternalOutput" if debug else "Internal"
    KT_d = nc.dram_tensor("KT_d", [8, 128, CTX], BF16, kind=ks).ap()
    V_d = nc.dram_tensor("V_d", [8, CTX, 128], BF16, kind=ks).ap()
    x1_d = nc.dram_tensor("x1_d", [NQ, D], F32, kind=ks).ap()
    x2_d = nc.dram_tensor("x2_d", [1024, D], F32, kind=ks).ap()
    kidxT_d = nc.dram_tensor("kidxT_d", [128, CTX], BF16, kind=ks).ap()
    QT_d = nc.dram_tensor("QT_d", [128, 8, NQ], BF16, kind=ks).ap()
    qiT_d = nc.dram_tensor("qiT_d", [128, 8, NQ], BF16, kind=ks).ap()
    ypT_d = nc.dram_tensor("ypT_d", [128, 8, NQ], BF16, kind=ks).ap()
    attnT_d = nc.dram_tensor("attnT_d", [128, 8, NQ], BF16, kind=ks).ap()
    widx_d = nc.dram_tensor("widx_d", [128, NQB, 16], F32, kind=ks).ap()
    dbg = {}
    if debug:
        dbg['maskT'] = nc.dram_tensor("dbg_maskT", [128, 32, NQ], BF16, kind="ExternalOutput").ap()
        dbg['thr'] = nc.dram_tensor("dbg_thr", [128, NQB], F32, kind="ExternalOutput").ap()
        dbg['gT'] = nc.dram_tensor("dbg_gT", [128, NFC, 1024], BF16, kind="ExternalOutput").ap()

    with contextlib.ExitStack() as es:
        P = Prog(nc, es)
        T = nc.tensor
        A = nc.scalar
        V = nc.vector
        G = nc.gpsimd

        def sb(name, shape, dt, stack=es):
            return stack.enter_context(nc.sbuf_tensor(name, list(shape), dt))

        def ps(name, shape, dt, stack):
            return stack.enter_context(nc.psum_tensor(name, list(shape), dt))

        ident_f = sb("ident_f", [128, 128], F32)
        ident_b = sb("ident_b", [128, 128], BF16)
        ones_b = sb("ones_b", [128, 128], BF16)
        st = sb("st", [128, 16], F32)
        P.dma('sp', ident_f[:], ident[:, :], writes=['ident_f'])
        P.op('dve', lambda: V.tensor_copy(out=ident_b[:], in_=ident_f[:]), reads=['ident_f'], writes=['ident_b'])
        P.op('dve', lambda: V.memset(ones_b[:], 1.0), writes=['ones_b'])

        def alloc(name, shape, dt):
            cm = nc.sbuf_tensor(name, list(shape), dt)
            return cm, cm.__enter__()

        def norm_T(i, xin, xin_key, xn, pT, gsb, dst_fn, dst_keys, extra=None):
            ss = st[:, 3 * i:3 * i + 1]
            sd = st[:, 3 * i + 1:3 * i + 2]
            rs = st[:, 3 * i + 2:3 * i + 3]
            P.op('act', lambda: A.activation(out=xn[i][:], in_=xin, func=AF.Square, accum_out=ss),
                 reads=[xin_key], writes=[('xn', i), ('ss', i)])
            P.op('dve', lambda: V.tensor_scalar(out=sd, in0=ss, scalar1=1.0 / D, scalar2=EPS, op0=ALU.mult, op1=ALU.add),
                 reads=[('ss', i)], writes=[('sd', i)])
            P.op('act', lambda: A.activation(out=sd, in_=sd, func=AF.Sqrt), reads=[('sd', i)], writes=[('sd', i)])
            P.op('dve', lambda: V.reciprocal(out=rs, in_=sd), reads=[('sd', i)], writes=[('rs', i)])
            if extra is not None:
                P.op('dve', lambda: V.tensor_tensor(out=rs, in0=rs, in1=extra, op=ALU.mult),
                     reads=[('rs', i), 'hflag'], writes=[('rs', i)])
            P.op('dve', lambda: V.tensor_scalar(out=xn[i][:], in0=xin, scalar1=rs, scalar2=None, op0=ALU.mult),
                 reads=[xin_key, ('rs', i)], writes=[('xn', i)])
            for c in range(16):
                P.op('pe', lambda c=c: T.transpose(out=pT[i][c // 8][:, (c % 8) * 128:(c % 8 + 1) * 128],
                                                   in_=xn[i][:, c * 128:(c + 1) * 128], identity=ident_b[:]),
                     reads=[('xn', i), 'ident_b'], writes=[('pT', i, c // 8)])
            for h in range(2):
                P.op('dve', lambda h=h: V.tensor_tensor(
                    out=dst_fn(h), in0=pT[i][h][:, :].rearrange("p (c t) -> p c t", t=128),
                    in1=gsb[:, 8 * h:8 * h + 8, :], op=ALU.mult),
                    reads=[('pT', i, h), 'gsb'], writes=dst_keys)

        def wload(dst, dkey, src_rows, c0, c1, col0, ncol, q='pool'):
            P.dma(q, dst[:, c0:c1, 0:ncol],
                  src_rows[c0 * 128:c1 * 128, col0:col0 + ncol].rearrange("(c p) n -> p c n", p=128),
                  writes=[dkey])

        with contextlib.ExitStack() as pa:
            kidxT = sb("kidxT", [128, CTX], BF16, pa)
            Wk = sb("Wk", [128, 16, 1024], BF16, pa)
            Wv = sb("Wv", [128, 16, 1024], BF16, pa)
            Wki = sb("Wki", [128, 16, 128], BF16, pa)
            gsb = sb("gsbA", [128, 16, 128], F32, pa)
            xb = [sb("xbA%d" % i, [128, D], F32, pa) for i in range(2)]
            xn = [sb("xnA%d" % i, [128, D], BF16, pa) for i in range(2)]
            hT = [sb("hTA%d" % i, [128, 16, 512], BF16, pa) for i in range(2)]
            KTst = [sb("KTst%d" % i, [128, 8, 512], BF16, pa) for i in range(2)]
            Vst = [sb("Vst%d" % i, [128, 4, 1024], BF16, pa) for i in range(2)]
            pT = [[ps("pTA%d%d" % (i, h), [128, 1024], BF16, pa) for h in range(2)] for i in range(2)]
            pm = [ps("pmA%d" % i, [128, 512], F32, pa) for i in range(4)]

            P.dma('sp', gsb[:].rearrange("p c t -> p (c t)"), gT3[0, :, :], writes=['gsb'])
            for cg in range(4):
                wload(Wk, ('Wk', cg), w_in, 4 * cg, 4 * cg + 4, OFF['k'], 1024)
            for cg in range(4):
                wload(Wv, ('Wv', cg), w_in, 4 * cg, 4 * cg + 4, OFF['v'], 1024)
            P.dma('pool', Wki[:, :, 0:64], w_in[:, OFF['ki']:OFF['ki'] + 64].rearrange("(c p) n -> p c n", p=128),
                  writes=['Wki'])
            P.dma('pool', Wki[:, :, 64:128], w_in[:, OFF['ki']:OFF['ki'] + 64].rearrange("(c p) n -> p c n", p=128),
                  writes=['Wki'])
            pmi = [0]

            def a_load(ct, sbl):
                j = ct * 4 + sbl
                P.dma('sp', xb[j % 2][:], xctx[j * 128:(j + 1) * 128, :], writes=[('xb', j % 2)])

            def a_norm(ct, sbl):
                b = ct % 2
                j = ct * 4 + sbl
                i = j % 2
                norm_T(i, xb[i][:], ('xb', i), xn, pT, gsb,
                       lambda h: hT[b][:, 8 * h:8 * h + 8, sbl * 128:(sbl + 1) * 128], [('hT', b, sbl)])

            def a_k_heads(ct, heads):
                b = ct % 2
                hkeys = [('hT', b, s_) for s_ in range(4)]
                for h in heads:
                    pq = pm[pmi[0] % 4]
                    pk = ('pm', pmi[0] % 4)
                    pmi[0] += 1
                    for c in range(16):
                        P.op('pe', lambda c=c: T.matmul(pq[:, :], lhsT=Wk[:, c, h * 128:(h + 1) * 128],
                                                        rhs=hT[b][:, c, :], start=(c == 0), stop=(c == 15)),
                             reads=hkeys + [('Wk', c // 4)], writes=[pk])
                    P.op('act', lambda: A.activation(out=KTst[b][:, h, :], in_=pq[:, :], func=AF.Copy),
                         reads=[pk], writes=[('KTst', b)])

            def a_kidx(ct):
                b = ct % 2
                hkeys = [('hT', b, s_) for s_ in range(4)]
                pq = pm[pmi[0] % 4]
                pk = ('pm', pmi[0] % 4)
                pmi[0] += 1
                for c in range(16):
                    P.op('pe', lambda c=c: T.matmul(pq[:, :], lhsT=Wki[:, c, :], rhs=hT[b][:, c, :],
                                                    start=(c == 0), stop=(c == 15)),
                         reads=hkeys + ['Wki'], writes=[pk])
                P.op('act', lambda: A.activation(out=kidxT[:, ct * 512:(ct + 1) * 512], in_=pq[:, :], func=AF.Copy),
                     reads=[pk], writes=[('kidxT', ct)])
                P.dma('pool', KT_d[:, :, ct * 512:(ct + 1) * 512].rearrange("h d s -> d h s"), KTst[b][:, :, :],
                      reads=[('KTst', b)], writes=[('KT_d', ct)])

            def a_v(ct, sbls):
                b = ct % 2
                for sbl in sbls:
                    for hf in range(2):
                        pq = pm[pmi[0] % 4]
                        pk = ('pm', pmi[0] % 4)
                        pmi[0] += 1
                        for c in range(16):
                            P.op('pe', lambda c=c: T.matmul(
                                pq[:, :], lhsT=hT[b][:, c, sbl * 128:(sbl + 1) * 128],
                                rhs=Wv[:, c, hf * 512:(hf + 1) * 512], start=(c == 0), stop=(c == 15)),
                                reads=[('hT', b, sbl), ('Wv', c // 4)], writes=[pk])
                        if hf == 0:
                            P.op('act', lambda: A.activation(
                                out=Vst[b][:, sbl, hf * 512:(hf + 1) * 512], in_=pq[:, :], func=AF.Copy),
                                reads=[pk], writes=[('Vst', b, sbl)])
                        else:
                            P.op('dve', lambda: V.tensor_copy(
                                out=Vst[b][:, sbl, hf * 512:(hf + 1) * 512], in_=pq[:, :]),
                                reads=[pk], writes=[('Vst', b, sbl)])
                    r0 = ct * 512 + sbl * 128
                    P.dma('pool', V_d[:, r0:r0 + 128, :].rearrange("h p d -> p h d"),
                          Vst[b][:, sbl, :].rearrange("p (h d) -> p h d", d=128),
                          reads=[('Vst', b, sbl)], writes=[('V_d', ct, sbl)])

            for sbl in range(4):
                a_load(0, sbl)
                a_norm(0, sbl)
            NCT = CTX // 512
            for ct in range(NCT):
                parts = [lambda ct=ct: a_k_heads(ct, range(0, 4)),
                         lambda ct=ct: (a_k_heads(ct, range(4, 8)), a_kidx(ct)),
                         lambda ct=ct: a_v(ct, (0, 1)),
                         lambda ct=ct: a_v(ct, (2, 3))]
                for q_ in range(4):
                    if ct + 1 < NCT:
                        a_load(ct + 1, q_)
                    parts[q_]()
                    if ct + 1 < NCT:
                        a_norm(ct + 1, q_)
            P.dma('sp', kidxT_d[:, :], kidxT[:, :], reads=[('kidxT', c_) for c_ in range(8)])
            P.barrier()
        if stop_after == 'A':
            return nc


        with contextlib.ExitStack() as pb:
            QT = sb("QT", [128, 8, NQ], BF16, pb)
            qiT = sb("qiT", [128, 8, NQ], BF16, pb)
            ypT = sb("ypT", [128, 8, NQ], BF16, pb)
            widx = sb("widx", [128, NQB, 16], F32, pb)
            hM = sb("hM", [128, 16, NHM], BF16, pb)
            Ws = [sb("WsB%d" % i, [128, 16, 512], BF16, pb) for i in range(2)]
            wgroups = [OFF['q'], OFF['q'] + 512, OFF['qi'], OFF['qi'] + 512, OFF['pool'], OFF['pool'] + 512]
            wissued = [0]

            def issue_w():
                k = wissued[0]
                if k < len(wgroups):
                    wload(Ws[k % 2], ('Ws', k % 2), w_in, 0, 16, wgroups[k], 512)
                    wissued[0] += 1
            issue_w()
            issue_w()
            with contextlib.ExitStack() as pb1:
                gsb = sb("gsbB", [128, 16, 128], F32, pb1)
                xb = [sb("xbB%d" % i, [128, D], F32, pb1) for i in range(2)]
                xn = [sb("xnB%d" % i, [128, D], BF16, pb1) for i in range(2)]
                pT = [[ps("pTB%d%d" % (i, h), [128, 1024], BF16, pb1) for h in range(2)] for i in range(2)]
                P.dma('sp', gsb[:].rearrange("p c t -> p (c t)"), gT3[0, :, :], writes=['gsb'])
                for j in range(NHM // 128):
                    i = j % 2
                    P.dma('sp', xb[i][:], xctx[HM0 + j * 128:HM0 + (j + 1) * 128, :], writes=[('xb', i)])
                    norm_T(i, xb[i][:], ('xb', i), xn, pT, gsb,
                           lambda h, j=j: hM[:, 8 * h:8 * h + 8, j * 128:(j + 1) * 128], [('hM', j)])
                P.barrier()
            pp = [ps("ppB%d" % i, [128, 4, 512], F32, pb) for i in range(2)]
            Wpl = sb("Wpl", [128, 4, 2, 256], BF16, pb)
            Ww = sb("Ww", [128, 16, 16], BF16, pb)
            psc_sb = sb("pscale_sb", [128, 8], F32, pb)
            invc_sb = sb("invc_sb", [128, 8, 16], F32, pb)
            pooledT = sb("pooledT", [128, 8, NQ], BF16, pb)
            uk = [sb("uk%d" % i, [128, 1168], F32, pb) for i in range(2)]
            sA = sb("sA", [128, 1168], F32, pb)
            sB = sb("sB", [128, 1168], F32, pb)
            t16 = sb("t16", [128, 16], F32, pb)
            P.dma('sp', psc_sb[:], pscale[:, :], writes=['pscale'])
            P.dma('sp', invc_sb[:], invc[:, :, :], writes=['invc'])
            for g in range(4):
                P.dma('pool', Wpl[:, g, :, :], w_pool[g, :, :].rearrange("(cc p) d -> p cc d", p=128), writes=['Wpl'])
            P.dma('pool', Ww[:, :, :], w_in[:, OFF['wi']:OFF['wi'] + 16].rearrange("(c p) n -> p c n", p=128), writes=['Ww'])
            wcnt = [0]
            pcnt = [0]

            def next_w(col0):
                b = wcnt[0] % 2
                assert wgroups[wcnt[0]] == col0
                if wcnt[0] >= 1:
                    issue_w()
                wcnt[0] += 1
                return b

            def next_pp():
                b = pcnt[0] % 2
                pcnt[0] += 1
                return b

            for (dst, off) in ((QT, OFF['q']), (qiT, OFF['qi'])):
                for g in range(2):
                    wb = next_w(off + 512 * g)
                    for m_ in range(4):
                        pb_ = next_pp()
                        for n in range(3):
                            for c in range(16):
                                P.op('pe', lambda c=c, n=n, wb=wb, m_=m_, pb_=pb_: T.matmul(
                                    pp[pb_][:, n, 0:384], lhsT=Ws[wb][:, c, m_ * 128:(m_ + 1) * 128],
                                    rhs=hM[:, c, 128 + n * 384:128 + (n + 1) * 384], start=(c == 0), stop=(c == 15)),
                                    reads=[('Ws', wb)], writes=[('pp', pb_)])
                        P.op('act', lambda dst=dst, g=g, m_=m_, pb_=pb_: A.activation(
                            out=dst[:, 4 * g + m_, :].rearrange("p (n t) -> p n t", t=384),
                            in_=pp[pb_][:, 0:3, 0:384], func=AF.Copy),
                            reads=[('pp', pb_)], writes=[('fm', id(dst), 4 * g + m_)])
            for g2 in range(2):
                wb = next_w(OFF['pool'] + 512 * g2)
                for m_ in range(4):
                    k = 4 * g2 + m_
                    g = k // 2
                    pb_ = next_pp()
                    for n in range(4):
                        for c in range(16):
                            P.op('pe', lambda c=c, n=n, wb=wb, m_=m_, pb_=pb_: T.matmul(
                                pp[pb_][:, n, 0:292], lhsT=Ws[wb][:, c, m_ * 128:(m_ + 1) * 128],
                                rhs=hM[:, c, 112 + n * 292:112 + (n + 1) * 292], start=(c == 0), stop=(c == 15)),
                                reads=[('Ws', wb)], writes=[('pp', pb_)])
                    u = uk[k % 2]
                    P.op('act', lambda u=u, pb_=pb_: A.activation(
                        out=u[:, :].rearrange("p (n t) -> p n t", t=292), in_=pp[pb_][:, 0:4, 0:292], func=AF.Copy),
                        reads=[('pp', pb_)], writes=[('uk', k % 2)])
                    wdw = (2, 4, 8, 16)[g]
                    cur, ckey = u, ('uk', k % 2)
                    for s_ in range(int(math.log2(wdw))):
                        sh = 1 << s_
                        nxt, nkey = (sA, 'sA') if s_ % 2 == 0 else (sB, 'sB')
                        P.op('pool', lambda cur=cur, nxt=nxt, sh=sh: G.tensor_tensor(
                            out=nxt[:, sh:1168], in0=cur[:, sh:1168], in1=cur[:, 0:1168 - sh], op=ALU.add),
                            reads=[ckey], writes=[nkey])
                        cur, ckey = nxt, nkey
                    P.op('dve', lambda cur=cur, u=u, k=k, wdw=wdw: V.scalar_tensor_tensor(
                        out=pooledT[:, k, :], in0=cur[:, 16:1168], scalar=1.0 / wdw, in1=u[:, 16:1168],
                        op0=ALU.mult, op1=ALU.subtract), reads=[ckey, ('uk', k % 2)], writes=[('pooledT', k)])
                    P.op('dve', lambda cur=cur, k=k: V.tensor_tensor(out=t16[:, :], in0=cur[:, 144:160], in1=invc_sb[:, k, :], op=ALU.mult),
                         reads=[ckey, 'invc'], writes=['t16'])
                    P.op('dve', lambda u=u, k=k: V.tensor_tensor(out=pooledT[:, k, 128:144], in0=t16[:, :], in1=u[:, 144:160], op=ALU.subtract),
                         reads=['t16', ('uk', k % 2)], writes=[('pooledT', k)])
            for k2 in range(8):
                g, dm = k2 // 2, k2 % 2
                pb_ = next_pp()
                for n in range(3):
                    for cc in range(2):
                        P.op('pe', lambda n=n, cc=cc, g=g, dm=dm, pb_=pb_: T.matmul(
                            pp[pb_][:, n, 0:384], lhsT=Wpl[:, g, cc, dm * 128:(dm + 1) * 128],
                            rhs=pooledT[:, 2 * g + cc, n * 384:(n + 1) * 384], start=(cc == 0), stop=(cc == 1)),
                            reads=['Wpl', ('pooledT', 2 * g + cc)], writes=[('pp', pb_)])
                P.op('act', lambda k2=k2, pb_=pb_: A.activation(
                    out=ypT[:, k2, :].rearrange("p (n t) -> p n t", t=384), in_=pp[pb_][:, 0:3, 0:384],
                    func=AF.Copy, scale=psc_sb[:, k2:k2 + 1]), reads=[('pp', pb_), 'pscale'], writes=[('ypT', k2)])
            idx_scale = (16 ** -0.5) * (64 ** -0.5)
            for i in range(NQB):
                pb_ = next_pp()
                for c in range(16):
                    P.op('pe', lambda c=c, i=i, pb_=pb_: T.matmul(
                        pp[pb_][:, 0, 0:16], lhsT=hM[:, c, 128 + i * 128:256 + i * 128], rhs=Ww[:, c, :],
                        start=(c == 0), stop=(c == 15)), reads=['Ww'], writes=[('pp', pb_)])
                P.op('act', lambda i=i, pb_=pb_: A.activation(out=widx[:, i, :], in_=pp[pb_][:, 0, 0:16], func=AF.Copy, scale=idx_scale),
                     reads=[('pp', pb_)], writes=[('widx', i)])
            P.dma('sp', QT_d[:, :, :], QT[:, :, :], reads=[('fm', id(QT), h_) for h_ in range(8)])
            P.dma('sp', qiT_d[:, :, :], qiT[:, :, :], reads=[('fm', id(qiT), h_) for h_ in range(8)])
            P.dma('sp', ypT_d[:, :, :], ypT[:, :, :], reads=[('ypT', h_) for h_ in range(8)])
            P.dma('sp', widx_d[:, :, :], widx[:, :, :], reads=[('widx', h_) for h_ in range(NQB)])
            P.barrier()
        if stop_after == 'B':
            return nc

        pcd = contextlib.ExitStack()
        es.enter_context(pcd)
        maskT = sb("maskT", [128, 32, NQ], BF16, pcd)
        with contextlib.ExitStack() as pc:
            kidxT = sb("kidxTc", [128, CTX], BF16, pc)
            qiT = sb("qiTc", [128, 8, NQ], BF16, pc)
            widx = sb("widxc", [128, NQB, 16], F32, pc)
            P.dma('sp', kidxT[:, :], kidxT_d[:, :], writes=['kidxT'])
            P.dma('sp', qiT[:, :, :], qiT_d[:, :, :], writes=['qiT'])
            P.dma('sp', widx[:, :, :], widx_d[:, :, :], writes=['widx'])
            slot_row = sb("slot_row", [1, CTX], BF16, pc)
            ones_row = sb("ones_row", [1, 128], BF16, pc)
            causal_b = sb("causal_b", [128, 128], BF16, pc)
            halfs_sb = sb("halfs_sb", [128, NIT + 1], F32, pc)
            sc = [sb("sc%d" % i, [128, CTX], F32, pc) for i in range(2)]
            junk = sb("junk", [128, CTX], BF16, pc)
            Rb = sb("Rb", [128, 4, 512], BF16, pc)
            Dg = [sb("Dg%d" % i, [128, 16, 128], BF16, pc) for i in range(2)]
            bsx = [sb("bsx%d" % i, [128, 32], F32, pc) for i in range(2)]
            bs = sb("bs", [128, 64], F32, pc)
            thr_all = sb("thr_all", [128, NQB], F32, pc)
            pd = ps("pdC", [128, 4, 512], F32, pc)
            psc = [ps("pscC%d" % i, [128, 512], F32, pc) for i in range(3)]
            ptm = [ps("ptmC%d" % i, [128, 1024], BF16, pc) for i in range(1)]
            P.dma('pool', slot_row[0:1, :], slotb[0:1, :], writes=['slot_row'])
            P.dma('pool', causal_b[:, :], causal[:, :], writes=['causal_b'])
            P.dma('sp', halfs_sb[:], halfs[:, :], writes=['halfs_sb'])
            P.op('pool', lambda: G.memset(ones_row[0:1, :], 1.0), writes=['ones_row'])
            P.op('pool', lambda: G.memset(maskT[:, :, :].rearrange("p a b -> p (a b)"), 0.0), writes=['maskT'])
            mid = bs[:, 3:4]
            cntv = bs[:, 4:5]
            gg = bs[:, 5:6]
            lo = bs[:, 6:7]
            hi = bs[:, 7:8]
            w0 = bs[:, 8:9]
            hk = bs[:, 16:16 + NIT + 1]
            cnts = dict(d=0, s=0, t=0)

            def gen_scores(i, bi):
                E = QS0 + 128 * (i + 1)
                nkt = (E + 511) // 512
                mins = bsx[bi][:, 0:8]
                maxs = bsx[bi][:, 8:16]
                for h in range(16):
                    P.op('pool', lambda h=h: G.tensor_scalar(out=Dg[bi][:, h, :], in0=ident_f[:], scalar1=widx[:, i, h:h + 1],
                                                             scalar2=1.0, op0=ALU.mult, op1=ALU.mult),
                         reads=['ident_f', 'widx'], writes=[('Dg', bi, h)])
                prev = None

                def finish(kt, N, pst, pskey):
                    P.op('pe', lambda: T.matmul(pst[:, :N], lhsT=ones_row[0:1, :], rhs=slot_row[0:1, kt * 512:kt * 512 + N],
                                                start=False, stop=(kt != nkt - 1)),
                         reads=['ones_row', 'slot_row', ('mins', bi, kt), ('maxs', bi, kt)], writes=[pskey])
                    if kt == nkt - 1:
                        P.op('pe', lambda: T.matmul(pst[:, N - 128:N], lhsT=ident_b[:, :], rhs=causal_b[:, :], start=False, stop=True),
                             reads=['ident_b', 'causal_b'], writes=[pskey])
                    P.op('act', lambda: A.activation(out=sc[bi][:, kt * 512:kt * 512 + N], in_=pst[:, :N], func=AF.Copy),
                         reads=[pskey], writes=[('sc', bi, kt)])
                for kt in range(nkt):
                    N = min(512, E - kt * 512)
                    si = cnts['s'] % 3
                    cnts['s'] += 1
                    pst = psc[si]
                    pskey = ('psc', si)
                    pend = []

                    def score_mm(hp, di, N=N, pst=pst, pskey=pskey):
                        for q_ in range(2):
                            h = 2 * hp + q_
                            P.op('pe', lambda h=h, q_=q_: T.matmul(pst[:, :N], lhsT=Dg[bi][:, h, :], rhs=Rb[:, 2 * di + q_, :N],
                                                                   start=(h == 0), stop=False),
                                 reads=[('Dg', bi, h), ('Rb', di)], writes=[pskey])
                    for hp in range(8):
                        di = cnts['d'] % 2
                        cnts['d'] += 1
                        pdk = ('pd', di)
                        for q_ in range(2):
                            pr = q_ * 64
                            P.op('pe', lambda q_=q_, pr=pr: T.matmul(
                                pd[:, 2 * di + q_, :N], lhsT=qiT[pr:pr + 64, hp, i * 128:(i + 1) * 128],
                                rhs=kidxT[pr:pr + 64, kt * 512:kt * 512 + N], start=True, stop=True),
                                reads=['kidxT', 'qiT'], writes=[pdk])
                        P.op('act', lambda di=di: A.activation(out=Rb[:, 2 * di:2 * di + 2, :N], in_=pd[:, 2 * di:2 * di + 2, :N], func=AF.Relu),
                             reads=[pdk], writes=[('Rb', di)])
                        pend.append((hp, di))
                        if len(pend) > 1:
                            score_mm(*pend.pop(0))
                    while pend:
                        score_mm(*pend.pop(0))
                    P.op('dve', lambda pst=pst, N=N, kt=kt: V.tensor_reduce(out=mins[:, kt:kt + 1], in_=pst[:, :N], axis=AX.X, op=ALU.min),
                         reads=[pskey], writes=[('mins', bi, kt)])
                    P.op('dve', lambda pst=pst, N=N, kt=kt: V.tensor_reduce(out=maxs[:, kt:kt + 1], in_=pst[:, :N], axis=AX.X, op=ALU.max),
                         reads=[pskey], writes=[('maxs', bi, kt)])
                    if prev is not None:
                        finish(*prev)
                    prev = (kt, N, pst, pskey)
                    yield
                finish(*prev)
                yield

            def bisect(i, bi, nxt):
                E = QS0 + 128 * (i + 1)
                nkt = (E + 511) // 512
                mins = bsx[bi][:, 0:8]
                maxs = bsx[bi][:, 8:16]
                sckeys = [('sc', bi, kt) for kt in range(nkt)]
                P.op('dve', lambda: V.tensor_reduce(out=lo, in_=mins[:, 0:nkt], axis=AX.X, op=ALU.min),
                     reads=[('mins', bi, kt) for kt in range(nkt)], writes=['lo'])
                P.op('dve', lambda: V.tensor_reduce(out=hi, in_=maxs[:, 0:nkt], axis=AX.X, op=ALU.max),
                     reads=[('maxs', bi, kt) for kt in range(nkt)], writes=['hi'])
                P.op('dve', lambda: V.scalar_tensor_tensor(out=w0, in0=hi, scalar=2.0, in1=lo, op0=ALU.add, op1=ALU.subtract),
                     reads=['hi', 'lo'], writes=['w0'])
                P.op('dve', lambda: V.tensor_scalar(out=hk, in0=halfs_sb[:, :], scalar1=w0, scalar2=None, op0=ALU.mult),
                     reads=['w0', 'halfs_sb'], writes=['hk'])
                P.op('dve', lambda: V.scalar_tensor_tensor(out=mid, in0=lo, scalar=-1.0, in1=hk[:, 0:1], op0=ALU.add, op1=ALU.add),
                     reads=['lo', 'hk'], writes=['mid'])
                for it in range(NIT):
                    P.op('dve', lambda: V.tensor_scalar(out=junk[:, 0:E], in0=sc[bi][:, 0:E], scalar1=mid, scalar2=None,
                                                        op0=ALU.is_ge, op1=ALU.add, accum_out=cntv),
                         reads=sckeys + ['mid'], writes=['junk', 'cnt'])
                    P.op('dve', lambda: V.tensor_scalar(out=gg, in0=cntv, scalar1=255.5, scalar2=0.5, op0=ALU.is_ge, op1=ALU.subtract),
                         reads=['cnt'], writes=['gg'])
                    P.op('dve', lambda it=it: V.scalar_tensor_tensor(out=mid, in0=gg, scalar=hk[:, it:it + 1], in1=mid,
                                                                      op0=ALU.mult, op1=ALU.add),
                         reads=['gg', 'hk', 'mid'], writes=['mid'])
                    if nxt is not None and it % 2 == 1:
                        next(nxt, None)
                if nxt is not None:
                    for _ in nxt:
                        pass
                P.op('dve', lambda: V.tensor_tensor(out=lo, in0=mid, in1=hk[:, NIT:NIT + 1], op=ALU.subtract),
                     reads=['mid', 'hk'], writes=['lo'])
                P.op('dve', lambda: V.tensor_scalar(out=junk[:, 0:E], in0=sc[bi][:, 0:E], scalar1=lo, scalar2=None, op0=ALU.is_ge),
                     reads=sckeys + ['lo'], writes=['junk'])
                P.op('dve', lambda: V.tensor_copy(out=thr_all[:, i:i + 1], in_=lo), reads=['lo'], writes=['thr_all'])
                nkb = E // 128
                for kb0 in range(0, nkb, 8):
                    n8 = min(8, nkb - kb0)
                    pt = ptm[0]
                    ptk = ('ptm', 0)
                    for r in range(n8):
                        kb = kb0 + r
                        P.op('pe', lambda r=r, kb=kb: T.transpose(out=pt[:, r * 128:(r + 1) * 128],
                                                                 in_=junk[:, kb * 128:(kb + 1) * 128], identity=ident_b[:]),
                             reads=['junk', 'ident_b'], writes=[ptk])
                    P.op('act', lambda n8=n8, kb0=kb0: A.activation(
                        out=maskT[:, kb0:kb0 + n8, i * 128:(i + 1) * 128],
                        in_=pt[:, 0:n8 * 128].rearrange("p (a b) -> p a b", b=128), func=AF.Copy),
                        reads=[ptk], writes=['maskT'])

            for _ in gen_scores(0, 0):
                pass
            for i in range(NQB):
                nxt = gen_scores(i + 1, (i + 1) % 2) if i + 1 < NQB else None
                bisect(i, i % 2, nxt)
            if debug:
                P.dma('sp', dbg['maskT'][:, :, :], maskT[:, :, :], reads=['maskT'])
                P.dma('sp', dbg['thr'][:, :], thr_all[:, :], reads=['thr_all'])
            P.barrier()
        if stop_after == 'C':
            return nc

        SCALE = 128 ** -0.5
        with contextlib.ExitStack() as pdd:
            attnT = sb("attnT", [128, 8, NQ], BF16, pdd)
            QT = sb("QTd", [128, 8, NQ], BF16, pdd)
            P.dma('sp', QT[:, :, :], QT_d[:, :, :], writes=['QT'])
            KTh = [sb("KTh%d" % i, [128, CTX], BF16, pdd) for i in range(2)]
            Vh = [sb("Vh%d" % i, [128, 32, 128], BF16, pdd) for i in range(2)]
            strip_sb = sb("strip_sb", [128, 8, 896], F32, pdd)
            cfar_sb = sb("cfar_sb", [128, 8], F32, pdd)
            Eb = [sb("Eb%d" % i, [128, 384], BF16, pdd) for i in range(4)]
            Pb = [sb("Pb%d" % i, [128, 384], BF16, pdd) for i in range(4)]
            tmpb = [sb("tmpb%d" % i, [128, 384], F32, pdd) for i in range(2)]
            rl = sb("rl", [128, 384], F32, pdd)
            tiny_sb = sb("tiny_sb", [128, 1], F32, pdd)
            P.op('pool', lambda: G.memset(tiny_sb[:, :], 1e-18), writes=['tiny'])
            pS = [ps("pS%d" % i, [128, 512], F32, pdd) for i in range(4)]
            pO = [ps("pO%d" % i, [128, 512], F32, pdd) for i in range(2)]
            pL = [ps("pL%d" % i, [128, 512], F32, pdd) for i in range(2)]
            P.dma('sp', strip_sb[:], strip[:, :, :], writes=['strip_sb'])
            P.dma('sp', cfar_sb[:], cfar[:, :], writes=['cfar_sb'])
            steps = []
            for h in range(8):
                for qt in range(3):
                    kbmax = 23 + 3 * qt + 2
                    near = list(range(kbmax, kbmax - 4, -1))
                    far = list(range(kbmax - 4, -1, -1))
                    order = []
                    for nk in near:
                        order.append(nk)
                        order.extend(far[:4])
                        far = far[4:]
                    order.extend(far)
                    for n_, kb in enumerate(order):
                        steps.append((h, qt, kb, n_ == 0, n_ == len(order) - 1))
            loaded = set()

            def load_head(h):
                if h in loaded or h >= 8:
                    return
                loaded.add(h)
                b = h % 2
                P.dma('sp', KTh[b][:, :], KT_d[h, :, :], writes=[('KTh', b)])
                P.dma('sp', Vh[b][:, :, :], V_d[h, :, :].rearrange("(kb p) d -> p kb d", p=128), writes=[('Vh', b)])
            cn = dict(s=0, t=0)
            staged = []

            def stage1(step):
                h, qt, kb, first, last = step
                b = h % 2
                t0 = qt * 384
                if first and qt == 0:
                    load_head(h)
                if qt == 1 and first:
                    load_head(h + 1)
                s_i = cn['s'] % 4
                cn['s'] += 1
                pst = pS[s_i]
                P.op('pe', lambda: T.matmul(pst[:, 0:384], lhsT=KTh[b][:, kb * 128:(kb + 1) * 128],
                                            rhs=QT[:, h, t0:t0 + 384], start=True, stop=True),
                     reads=[('KTh', b), 'QT'], writes=[('pS', s_i)])
                D0 = (QS0 + t0) - kb * 128
                if D0 >= 256:
                    P.op('act', lambda: A.activation(out=Eb[s_i][:, :], in_=pst[:, 0:384], func=AF.Exp,
                                                     scale=SCALE, bias=cfar_sb[:, h:h + 1]),
                         reads=[('pS', s_i), 'cfar_sb'], writes=[('Eb', s_i)])
                else:
                    ti = cn['t'] % 2
                    cn['t'] += 1
                    P.op('dve', lambda: V.scalar_tensor_tensor(
                        out=tmpb[ti][:, :], in0=pst[:, 0:384], scalar=SCALE,
                        in1=strip_sb[:, h, D0 + 256:D0 + 256 + 384], op0=ALU.mult, op1=ALU.add),
                        reads=[('pS', s_i), 'strip_sb'], writes=[('tmpb', ti)])
                    P.op('act', lambda: A.activation(out=Eb[s_i][:, :], in_=tmpb[ti][:, :], func=AF.Exp),
                         reads=[('tmpb', ti)], writes=[('Eb', s_i)])
                P.op('dve', lambda: V.tensor_tensor(out=Pb[s_i][:, :], in0=Eb[s_i][:, :],
                                                    in1=maskT[:, kb, t0:t0 + 384], op=ALU.mult),
                     reads=[('Eb', s_i)], writes=[('Pb', s_i)])
                staged.append((step, s_i))

            def stage2():
                (h, qt, kb, first, last), s_i = staged.pop(0)
                b = h % 2
                t0 = qt * 384
                g_ = (h * 3 + qt) % 2
                po, pl = pO[g_], pL[g_]
                pok, plk = ('pO', g_), ('pL', g_)
                P.op('pe', lambda: T.matmul(po[:, 0:384], lhsT=Vh[b][:, kb, :], rhs=Pb[s_i][:, :], start=first, stop=last),
                     reads=[('Vh', b), ('Pb', s_i)], writes=[pok])
                P.op('pe', lambda: T.matmul(pl[:, 0:384], lhsT=ones_b[:, :], rhs=Pb[s_i][:, :], start=first, stop=last),
                     reads=['ones_b', ('Pb', s_i)], writes=[plk])
                if last:
                    P.op('act', lambda: A.activation(out=rl[:, :], in_=pl[:, 0:384], func=AF.Ln, bias=tiny_sb[:, 0:1]),
                         reads=[plk, 'tiny'], writes=['rl'])
                    P.op('act', lambda: A.activation(out=rl[:, :], in_=rl[:, :], func=AF.Exp, scale=-1.0), reads=['rl'], writes=['rl'])
                    P.op('dve', lambda: V.tensor_tensor(out=attnT[:, h, t0:t0 + 384], in0=po[:, 0:384], in1=rl[:, :], op=ALU.mult),
                         reads=[pok, 'rl'], writes=[('attnT', h)])
            for step in steps:
                stage1(step)
                if len(staged) > 3:
                    stage2()
            while staged:
                stage2()
            P.dma('sp', attnT_d[:, :, :], attnT[:, :, :], reads=[('attnT', h_) for h_ in range(8)])
            P.barrier()
        pcd.close()
        if stop_after == 'D':
            return nc

        with contextlib.ExitStack() as pf:
            Wo = sb("Wo", [128, 16, 2048], BF16, pf)
            ypT = sb("ypTf", [128, 8, NQ], BF16, pf)
            attnT = sb("attnTf", [128, 8, NQ], BF16, pf)
            P.dma('sp', ypT[:, :, :], ypT_d[:, :, :], writes=['ypT'])
            P.dma('sp', attnT[:, :, :], attnT_d[:, :, :], writes=['attnT'])
            xr = [sb("xrF%d" % i, [128, D], F32, pf) for i in range(2)]
            x1t = [sb("x1tF%d" % i, [128, D], F32, pf) for i in range(2)]
            pw = [ps("pwF%d" % i, [128, 512], F32, pf) for i in range(8)]
            for cg in range(4):
                wload(Wo, ('Wo', cg), w_out, 4 * cg, 4 * cg + 4, 0, 2048)
            pcn = 0
            for i in range(NQB):
                bi = i % 2
                P.dma('sp', xr[bi][:], xctx[QS0 + i * 128:QS0 + (i + 1) * 128, :], writes=[('xr', bi)])
                for nt in range(4):
                    pq = pw[pcn % 8]
                    pk = ('pw', pcn % 8)
                    pcn += 1
                    for c in range(16):
                        src = ypT if c < 8 else attnT
                        P.op('pe', lambda c=c, src=src, pq=pq, nt=nt, i=i: T.matmul(
                            pq[:, :], lhsT=src[:, c % 8, i * 128:(i + 1) * 128], rhs=Wo[:, c, nt * 512:(nt + 1) * 512],
                            start=(c == 0), stop=(c == 15)), reads=[('Wo', c // 4), 'ypT', 'attnT'], writes=[pk])
                    P.op('dve', lambda pq=pq, nt=nt, bi=bi: V.tensor_tensor(
                        out=x1t[bi][:, nt * 512:(nt + 1) * 512], in0=pq[:, :], in1=xr[bi][:, nt * 512:(nt + 1) * 512], op=ALU.add),
                        reads=[pk, ('xr', bi)], writes=[('x1t', bi)])
                P.dma('sp', x1_d[i * 128:(i + 1) * 128, :], x1t[bi][:], reads=[('x1t', bi)], writes=[('x1_d', i)])
            P.barrier()
        if stop_after == 'F':
            return nc


        pgh = contextlib.ExitStack()
        es.enter_context(pgh)
        gT = sb("gT", [128, NFC, 1024], BF16, pgh)
        with contextlib.ExitStack() as pg_:
            h2T = sb("h2T", [128, 16, NQ], BF16, pg_)
            Wgv = [sb("Wgv%d" % i, [128, 16, 512], BF16, pg_) for i in range(2)]

            def g_load(grp):
                jg_, isval_ = grp // 2, grp % 2
                wload(Wgv[grp % 2], ('Wgv', grp % 2), w_up, 0, 16, (D_FF if isval_ else 0) + jg_ * 512, 512)
            g_load(0)
            g_load(1)
            with contextlib.ExitStack() as pg1:
                gsb = sb("gsbG", [128, 16, 128], F32, pg1)
                hf_sb = sb("hf_sb", [128, 1], F32, pg1)
                xb = [sb("xbG%d" % i, [128, D], F32, pg1) for i in range(2)]
                xn = [sb("xnG%d" % i, [128, D], BF16, pg1) for i in range(2)]
                pT = [[ps("pTG%d%d" % (i, h), [128, 1024], BF16, pg1) for h in range(2)] for i in range(2)]
                P.dma('sp', gsb[:].rearrange("p c t -> p (c t)"), gT3[1, :, :], writes=['gsb'])
                P.dma('sp', hf_sb[:], hflag[:, :], writes=['hflag'])
                for j in range(NQB):
                    i = j % 2
                    P.dma('sp', xb[i][:], x1_d[j * 128:(j + 1) * 128, :], writes=[('xb', i)])
                    norm_T(i, xb[i][:], ('xb', i), xn, pT, gsb,
                           lambda h, j=j: h2T[:, 8 * h:8 * h + 8, j * 128:(j + 1) * 128], [('h2T', j)],
                           extra=(hf_sb[:, 0:1] if j == 0 else None))
                P.barrier()
            sgT = sb("sgT", [128, 4, 1024], F32, pg_)
            cp = sb("cp", [128, 2 * NFC, 4], F32, pg_)
            rA = [sb("rA%d" % i, [128, 344], F32, pg_) for i in range(3)]
            rB = [sb("rB%d" % i, [128, 344], F32, pg_) for i in range(3)]
            pu = [ps("puG%d" % i, [128, 512], F32, pg_) for i in range(8)]
            P.dma('sp', cp[:], convp[:, :, :], writes=['cp'])
            tiles = [(0, 342), (342, 684), (684, 1024)]
            pcn = 0
            rcn = 0
            for grp in range(2 * (NFC // 4)):
                jg, isval = grp // 2, grp % 2
                wb = grp % 2
                if grp >= 1 and grp + 1 < 2 * (NFC // 4):
                    g_load(grp + 1)
                for m_ in range(4):
                    j = 4 * jg + m_
                    jj = (NFC + j) if isval else j
                    for (a, b_) in tiles:
                        N = b_ - a + 2
                        n2 = N - 2
                        c0 = 128 + a - 2
                        pq = pu[pcn % 8]
                        pk = ('pu', pcn % 8)
                        pcn += 1
                        for c in range(16):
                            P.op('pe', lambda c=c: T.matmul(
                                pq[:, 0:N], lhsT=Wgv[wb][:, c, m_ * 128:(m_ + 1) * 128], rhs=h2T[:, c, c0:c0 + N],
                                start=(c == 0), stop=(c == 15)), reads=[('Wgv', wb)], writes=[pk])
                        ri = rcn % 3
                        rcn += 1
                        P.op('act', lambda: A.activation(
                            out=rA[ri][:, 0:n2], in_=pq[:, 2:N], func=AF.Identity, scale=cp[:, jj, 2:3], bias=cp[:, jj, 3:4]),
                            reads=[pk, 'cp'], writes=[('rA', ri)])
                        P.op('dve', lambda: V.scalar_tensor_tensor(
                            out=rB[ri][:, 0:n2], in0=pq[:, 1:N - 1], scalar=cp[:, jj, 1:2], in1=rA[ri][:, 0:n2],
                            op0=ALU.mult, op1=ALU.add), reads=[pk, 'cp', ('rA', ri)], writes=[('rB', ri)])
                        if not isval:
                            P.op('dve', lambda: V.scalar_tensor_tensor(
                                out=rA[ri][:, 0:n2], in0=pq[:, 0:n2], scalar=cp[:, jj, 0:1], in1=rB[ri][:, 0:n2],
                                op0=ALU.mult, op1=ALU.add), reads=[pk, 'cp', ('rB', ri)], writes=[('rA', ri)])
                            P.op('act', lambda: A.activation(out=sgT[:, m_, a:b_], in_=rA[ri][:, 0:n2], func=AF.Silu),
                                 reads=[('rA', ri)], writes=[('sgT', m_)])
                        else:
                            P.op('dve', lambda: V.scalar_tensor_tensor(
                                out=rA[ri][:, 0:n2], in0=pq[:, 0:n2], scalar=cp[:, jj, 0:1], in1=rB[ri][:, 0:n2],
                                op0=ALU.mult, op1=ALU.add), reads=[pk, 'cp', ('rB', ri)], writes=[('rA', ri)])
                            P.op('dve', lambda: V.tensor_tensor(
                                out=gT[:, j, a:b_], in0=sgT[:, m_, a:b_], in1=rA[ri][:, 0:n2], op=ALU.mult),
                                reads=[('sgT', m_), ('rA', ri)], writes=[('gT', j)])
            if debug:
                P.dma('sp', dbg['gT'][:, :, :], gT[:, :, :], reads=[('gT', j_) for j_ in range(NFC)])
            P.barrier()
        if stop_after == 'G':
            return nc

        with contextlib.ExitStack() as ph:
            Wds = [sb("Wds%d" % i, [128, 4, 512], BF16, ph) for i in range(3)]
            x1s = [sb("x1s%d" % i, [128, 512], F32, ph) for i in range(4)]
            x2s = [sb("x2s%d" % i, [128, 512], F32, ph) for i in range(4)]
            pw = [ps("pwH%d" % i, [128, 512], F32, ph) for i in range(8)]
            wcn = 0
            scn = 0
            pset = 0
            for tg in range(2):
                for nt in range(4):
                    banks = [(pw[4 * (pset % 2) + tb], ('pw', 4 * (pset % 2) + tb)) for tb in range(4)]
                    pset += 1
                    for fg in range(NFC // 4):
                        wb = wcn % 3
                        wcn += 1
                        P.dma('pool', Wds[wb][:, :, :],
                              w_down[fg * 512:(fg + 1) * 512, nt * 512:(nt + 1) * 512].rearrange("(c p) n -> p c n", p=128),
                              writes=[('Wds', wb)])
                        for fl in range(4):
                            f = fg * 4 + fl
                            for tb in range(4):
                                pq, pk = banks[tb]
                                tok0 = (tg * 4 + tb) * 128
                                P.op('pe', lambda pq=pq, f=f, fl=fl, wb=wb, tok0=tok0: T.matmul(
                                    pq[:, :], lhsT=gT[:, f, tok0:tok0 + 128], rhs=Wds[wb][:, fl, :],
                                    start=(f == 0), stop=(f == NFC - 1)), reads=[('Wds', wb)], writes=[pk])
                    for tb in range(4):
                        pq, pk = banks[tb]
                        si = scn % 4
                        scn += 1
                        r0 = (tg * 4 + tb) * 128
                        P.dma('sp', x1s[si][:, :], x1_d[128 + r0:128 + r0 + 128, nt * 512:(nt + 1) * 512], writes=[('x1s', si)])
                        P.op('dve', lambda pq=pq, si=si: V.tensor_tensor(out=x2s[si][:, :], in0=pq[:, :], in1=x1s[si][:, :], op=ALU.add),
                             reads=[pk, ('x1s', si)], writes=[('x2s', si)])
                        P.dma('sp', x2_d[r0:r0 + 128, nt * 512:(nt + 1) * 512], x2s[si][:, :], reads=[('x2s', si)], writes=[('x2_d', r0, nt)])
            P.barrier()
        pgh.close()
        if stop_after == 'H':
            return nc

        with contextlib.ExitStack() as pi:
            x3 = sb("x3", [128, 8, D], F32, pi)
            h3T = sb("h3T", [128, 16, 1024], BF16, pi)
            ppT = sb("ppT", [128, 2, 1024], BF16, pi)
            with contextlib.ExitStack() as pi1:
                gsb = sb("gsbI", [128, 16, 128], F32, pi1)
                p_sb = sb("p_sb", [128, 8, 256], F32, pi1)
                p_bf = sb("p_bf", [128, 8, 256], BF16, pi1)
                xn = [sb("xnI%d" % i, [128, D], BF16, pi1) for i in range(2)]
                pT = [[ps("pTI%d%d" % (i, h), [128, 1024], BF16, pi1) for h in range(2)] for i in range(2)]
                ptp = [ps("ptpI%d" % i, [128, 1024], BF16, pi1) for i in range(2)]
                P.dma('sp', gsb[:].rearrange("p c t -> p (c t)"), gT3[2, :, :], writes=['gsb'])
                P.dma('sp', p_sb[:, :, :], p_own.rearrange("(tb p) c -> p tb c", p=128), writes=['p_sb'])
                P.op('pool', lambda: G.tensor_copy(out=p_bf[:, :, :], in_=p_sb[:, :, :]), reads=['p_sb'], writes=['p_bf'])
                for tb in range(8):
                    P.dma('sp', x3[:, tb, :], x2_d[tb * 128:(tb + 1) * 128, :], writes=[('x3', tb)])
                for tb in range(8):
                    i = tb % 2
                    norm_T(i, x3[:, tb, :], ('x3', tb), xn, pT, gsb,
                           lambda h, tb=tb: h3T[:, 8 * h:8 * h + 8, tb * 128:(tb + 1) * 128], [('h3T', tb)])
                    pt = ptp[i]
                    for cc in range(2):
                        P.op('pe', lambda pt=pt, cc=cc, tb=tb: T.transpose(out=pt[:, cc * 128:(cc + 1) * 128],
                                                                          in_=p_bf[:, tb, cc * 128:(cc + 1) * 128], identity=ident_b[:]),
                             reads=['p_bf', 'ident_b'], writes=[('ptp', i)])
                    P.op('act', lambda pt=pt, tb=tb: A.activation(out=ppT[:, :, tb * 128:(tb + 1) * 128],
                                                                  in_=pt[:, 0:256].rearrange("p (a b) -> p a b", b=128), func=AF.Copy),
                         reads=[('ptp', i)], writes=[('ppT', tb)])
                P.barrier()
            with contextlib.ExitStack() as pi2:
                Wgs = [sb("WgsI%d" % i, [128, 16, 512], BF16, pi2) for i in range(2)]
                Wps = [sb("WpsI%d" % i, [128, 2, 512], BF16, pi2) for i in range(2)]
                sgm = [sb("sgm%d" % i, [128, 512], F32, pi2) for i in range(2)]
                tmpI = [sb("tmpI%d" % i, [128, 512], F32, pi2) for i in range(2)]
                pgt = [ps("pgtI%d" % i, [128, 512], F32, pi2) for i in range(4)]
                ppe = [ps("ppeI%d" % i, [128, 512], F32, pi2) for i in range(4)]
                cn = 0

                def i_load(nt_):
                    wb_ = nt_ % 2
                    for cg in range(4):
                        P.dma('pool', Wgs[wb_][:, 4 * cg:4 * cg + 4, :],
                              w_pg[cg * 512:(cg + 1) * 512, nt_ * 512:(nt_ + 1) * 512].rearrange("(c p) n -> p c n", p=128),
                              writes=[('WgsI', wb_, cg)])
                    P.dma('pool', Wps[wb_][:, :, :], w_pp[:, nt_ * 512:(nt_ + 1) * 512].rearrange("(c p) n -> p c n", p=128),
                          writes=[('WpsI', wb_)])
                i_load(0)
                for nt in range(4):
                    wb = nt % 2
                    if nt + 1 < 4:
                        i_load(nt + 1)
                    for tb in range(8):
                        bi = cn % 4
                        si = cn % 2
                        cn += 1
                        for c in range(16):
                            P.op('pe', lambda c=c, bi=bi, tb=tb, wb=wb: T.matmul(
                                pgt[bi][:, :], lhsT=h3T[:, c, tb * 128:(tb + 1) * 128], rhs=Wgs[wb][:, c, :],
                                start=(c == 0), stop=(c == 15)), reads=[('WgsI', wb, c // 4)], writes=[('pgt', bi)])
                        for cc in range(2):
                            P.op('pe', lambda cc=cc, bi=bi, tb=tb, wb=wb: T.matmul(
                                ppe[bi][:, :], lhsT=ppT[:, cc, tb * 128:(tb + 1) * 128], rhs=Wps[wb][:, cc, :],
                                start=(cc == 0), stop=(cc == 1)), reads=[('WpsI', wb)], writes=[('ppe', bi)])
                        P.op('act', lambda bi=bi, si=si: A.activation(out=sgm[si][:, :], in_=pgt[bi][:, :], func=AF.Sigmoid),
                             reads=[('pgt', bi)], writes=[('sgm', si)])
                        P.op('dve', lambda bi=bi, si=si: V.tensor_tensor(out=tmpI[si][:, :], in0=ppe[bi][:, :], in1=sgm[si][:, :], op=ALU.mult),
                             reads=[('ppe', bi), ('sgm', si)], writes=[('tmpI', si)])
                        P.op('pool', lambda si=si, tb=tb, nt=nt: G.tensor_tensor(
                            out=x3[:, tb, nt * 512:(nt + 1) * 512], in0=x3[:, tb, nt * 512:(nt + 1) * 512], in1=tmpI[si][:, :], op=ALU.add),
                            reads=[('tmpI', si)], writes=[('x3o', tb)])
                P.barrier()
            with contextlib.ExitStack() as pi3:
                gf = sb("gf_sb", [128, D], F32, pi3)
                ot = [sb("ot%d" % i, [128, D], F32, pi3) for i in range(2)]
                jk = sb("jkI", [128, D], BF16, pi3)
                P.dma('sp', gf[:, :], gfin[:, :], writes=['gf'])
                for tb in range(8):
                    i = tb % 2
                    ss = st[:, 3 * i:3 * i + 1]
                    sd = st[:, 3 * i + 1:3 * i + 2]
                    rs = st[:, 3 * i + 2:3 * i + 3]
                    P.op('act', lambda tb=tb, ss=ss: A.activation(out=jk[:, :], in_=x3[:, tb, :], func=AF.Square, accum_out=ss),
                         writes=['jk', ('ss', i)])
                    P.op('dve', lambda ss=ss, sd=sd: V.tensor_scalar(out=sd, in0=ss, scalar1=1.0 / D, scalar2=EPS, op0=ALU.mult, op1=ALU.add),
                         reads=[('ss', i)], writes=[('sd', i)])
                    P.op('act', lambda sd=sd: A.activation(out=sd, in_=sd, func=AF.Sqrt), reads=[('sd', i)], writes=[('sd', i)])
                    P.op('dve', lambda sd=sd, rs=rs: V.reciprocal(out=rs, in_=sd), reads=[('sd', i)], writes=[('rs', i)])
                    P.op('dve', lambda tb=tb, rs=rs, i=i: V.scalar_tensor_tensor(
                        out=ot[i][:, :], in0=x3[:, tb, :], scalar=rs, in1=gf[:, :], op0=ALU.mult, op1=ALU.mult),
                        reads=[('rs', i), 'gf'], writes=[('ot', i)])
                    P.dma('sp', out[tb * 128:(tb + 1) * 128, :], ot[i][:, :], reads=[('ot', i)], writes=[('out', tb)])
                P.barrier()
    return nc


def _t5_bucket_static(dist):
    n = np.maximum(dist, 0)
    nf = np.maximum(n, 1).astype(np.float32)
    large = 16 + (np.log(nf / np.float32(16)) / np.float32(math.log(128 / 16)) * 16).astype(np.int32)
    large = np.minimum(large, 31)
    return np.where(n < 16, n, large)


def prep_inputs(x, p, g_mix, w_in, w_pool, pool_scale, rel_bias, w_out, g_ffn, w_up, conv_w, conv_b,
                w_down, g_ple, w_ple_gate, w_ple_proj, g_final):
    f = np.float32
    x = np.asarray(x, f)
    p = np.asarray(p, f)[0]
    shared = {}
    shared["w_in"] = np.ascontiguousarray(np.asarray(w_in, f)[0])
    shared["w_pool"] = np.ascontiguousarray(np.asarray(w_pool, f)[0])
    shared["w_out"] = np.ascontiguousarray(np.asarray(w_out, f)[0])
    shared["w_up"] = np.ascontiguousarray(np.asarray(w_up, f)[0])
    shared["w_down"] = np.ascontiguousarray(np.asarray(w_down, f)[0])
    shared["w_ple_gate"] = np.ascontiguousarray(np.asarray(w_ple_gate, f)[0])
    shared["w_ple_proj"] = np.ascontiguousarray(np.asarray(w_ple_proj, f)[0])

    def fm(v):
        a = np.asarray(v, f).reshape(16, 128).T
        return np.ascontiguousarray(np.repeat(a[:, :, None], 128, axis=2).reshape(128, 2048))
    shared["gT3"] = np.stack([fm(np.asarray(g_mix)[0]), fm(np.asarray(g_ffn)[0]), fm(np.asarray(g_ple)[0])], 0)
    shared["gfin"] = np.ascontiguousarray(np.repeat(np.asarray(g_final, f)[None, :], 128, axis=0))
    shared["pscale"] = np.ascontiguousarray(np.asarray(pool_scale, f)[0].reshape(8, 128).T)
    cw = np.asarray(conv_w, f)[0]
    cb = np.asarray(conv_b, f)[0]
    cp = np.concatenate([cw, cb[None, :]], 0)
    shared["convp"] = np.ascontiguousarray(cp.reshape(4, 2 * NFC, 128).transpose(2, 1, 0))
    rb = np.asarray(rel_bias, f)
    sl = np.arange(128)[:, None]
    dl = np.arange(896)[None, :] - 256
    dist = dl - sl
    bk = _t5_bucket_static(dist)
    stripv = rb[bk]
    shared["strip"] = np.ascontiguousarray(stripv.transpose(0, 2, 1))
    shared["cfar"] = np.ascontiguousarray(np.repeat(rb[31][None, :], 128, axis=0))
    tl = np.arange(128)[:, None]
    s_l = np.arange(128)[None, :]
    shared["causal"] = np.where(s_l <= tl, 0.0, NEG).astype(f)
    shared["ident"] = np.eye(128, dtype=f)
    shared["halfs"] = np.ascontiguousarray(np.repeat((0.5 ** np.arange(1, NIT + 2, dtype=np.float64)).astype(f)[None, :], 128, 0))
    in_maps = []
    for c in range(8):
        b, j = c // 4, c % 4
        T0 = j * 1024
        m = dict(shared)
        xc = np.zeros((CTX, D), f)
        lo = T0 - 3072
        s0 = max(0, -lo)
        xc[s0:] = x[b, lo + s0:T0 + 1024]
        m["xctx"] = xc
        m["p_own"] = np.ascontiguousarray(p[b, T0:T0 + 1024])
        sbias = np.zeros((CTX,), f)
        sbias[:s0] = NEG
        m["slotb"] = np.ascontiguousarray(np.repeat(sbias[None, :], 128, axis=0))
        ic = np.zeros((128, 8, 16), f)
        for k in range(8):
            w = (2, 4, 8, 16)[k // 2]
            tok = T0 + np.arange(16)
            cntv = np.minimum(tok + 1, w).astype(f)
            ic[:, k, :] = (np.float32(1.0) / cntv)[None, :]
        m["invc"] = ic
        m["hflag"] = np.full((128, 1), 1.0 if j > 0 else 0.0, f)
        in_maps.append(m)
    return in_maps


_NC_CACHE = {}


def kernel(**inputs):
    in_maps = prep_inputs(**inputs)
    if 'nc' not in _NC_CACHE:
        _NC_CACHE['nc'] = build()
    nc = _NC_CACHE['nc']
    res = run_bass_kernel_spmd(nc, in_maps, core_ids=list(range(8)))
    outs = [np.asarray(res.results[c]["out"], np.float32).reshape(1024, D) for c in range(8)]
    full = np.zeros((2, SEQ, D), np.float32)
    for c in range(8):
        full[c // 4, (c % 4) * 1024:(c % 4 + 1) * 1024] = outs[c]
    return full
```

```python
import contextlib
import math
import numpy as np
import concourse.bass as bass
import concourse.mybir as mybir
from concourse.bass_utils import run_bass_kernel_spmd

F32 = mybir.dt.float32
BF16 = mybir.dt.bfloat16
AF = mybir.ActivationFunctionType
ALU = mybir.AluOpType
AX = mybir.AxisListType

D = 2048
SEQ = 4096
CTX = 4096
NQB = 9
QS0 = 2944
NQ = NQB * 128
HM0 = 2816
NHM = 1280
D_FF = 5632
NFC = 44
EPS = 1e-6
NEG = -1.0e30
NIT = 15
OFF = dict(pool=0, q=1024, k=2048, v=3072, qi=4096, ki=5120, wi=5184)


class Prog:
    NDS = 12

    def __init__(self, nc, es, same_sync=True):
        self.nc = nc
        self.same_sync = same_sync
        self.eng = {'pe': nc.tensor, 'act': nc.scalar, 'dve': nc.vector, 'pool': nc.gpsimd, 'sp': nc.sync}
        self.csem = {e: es.enter_context(nc.semaphore('c_' + e)) for e in ['pe', 'act', 'dve', 'pool']}
        self.cnt = {e: 0 for e in self.csem}
        self.dsem = {q: [es.enter_context(nc.semaphore('d_%s%d' % (q, i))) for i in range(self.NDS)]
                     for q in ['sp', 'pool']}
        self.duse = {q: [0] * self.NDS for q in self.dsem}
        self.dnext = {q: 0 for q in self.dsem}
        self.seen = {e: {} for e in self.eng}
        self.lastw = {}
        self.rds = {}
        self.nwait = 0

    def _wait(self, e, tok):
        sem, val, sid = tok
        if sid == e and (e == 'pe' or not self.same_sync):
            return
        if self.seen[e].get(sid, 0) >= val:
            return
        self.eng[e].wait_ge(sem, val)
        self.seen[e][sid] = val
        self.nwait += 1

    def _deps(self, reads, writes):
        deps = {}

        def add(tok):
            sid = tok[2]
            if sid not in deps or deps[sid][1] < tok[1]:
                deps[sid] = tok
        for k in reads:
            if k in self.lastw:
                add(self.lastw[k])
        for k in writes:
            if k in self.lastw:
                add(self.lastw[k])
            for tok in self.rds.get(k, {}).values():
                add(tok)
        return deps.values()

    def _record(self, tok, reads, writes):
        sid = tok[2]
        for k in reads:
            d = self.rds.setdefault(k, {})
            if sid not in d or d[sid][1] < tok[1]:
                d[sid] = tok
        for k in writes:
            self.lastw[k] = tok
            self.rds[k] = {}

    def op(self, e, fn, reads=(), writes=()):
        for tok in self._deps(reads, writes):
            self._wait(e, tok)
        ins = fn()
        self.cnt[e] += 1
        ins.then_inc(self.csem[e], 1)
        self._record((self.csem[e], self.cnt[e], e), reads, writes)

    def dma(self, q, out, in_, reads=(), writes=(), **kw):
        for tok in self._deps(reads, writes):
            self._wait(q, tok)
        k = self.dnext[q]
        self.dnext[q] = (k + 1) % self.NDS
        u = self.duse[q][k]
        sem = self.dsem[q][k]
        if u > 0:
            self._wait(q, (sem, 16 * u, (q, k)))
        ins = self.eng[q].dma_start(out=out, in_=in_, **kw)
        ins.then_inc(sem, 16)
        self.duse[q][k] = u + 1
        self._record((sem, 16 * (u + 1), (q, k)), reads, writes)

    def barrier(self):
        for e in self.eng:
            for c in self.csem:
                if self.cnt[c] > 0:
                    self._wait_force(e, (self.csem[c], self.cnt[c], c))
            for q in self.dsem:
                for k in range(self.NDS):
                    if self.duse[q][k] > 0:
                        self._wait_force(e, (self.dsem[q][k], 16 * self.duse[q][k], (q, k)))
        self.lastw.clear()
        self.rds.clear()

    def _wait_force(self, e, tok):
        sem, val, sid = tok
        if self.seen[e].get(sid, 0) >= val:
            return
        self.eng[e].wait_ge(sem, val)
        self.seen[e][sid] = val
        self.nwait += 1


def build(debug=False, stop_after='Z'):
    nc = bass.Bass("TRN2", target_bir_lowering=False)

    def din(name, shape, dt=F32):
        return nc.dram_tensor(name, list(shape), dt, kind="ExternalInput").ap()

    xctx = din("xctx", [CTX, D])
    p_own = din("p_own", [1024, 256])
    w_in = din("w_in", [D, 5200])
    w_pool = din("w_pool", [4, 256, 256])
    w_out = din("w_out", [D, D])
    w_up = din("w_up", [D, 2 * D_FF])
    w_down = din("w_down", [D_FF, D])
    w_pg = din("w_ple_gate", [D, D])
    w_pp = din("w_ple_proj", [256, D])
    gT3 = din("gT3", [3, 128, 2048])
    gfin = din("gfin", [128, 2048])
    pscale = din("pscale", [128, 8])
    convp = din("convp", [128, 2 * NFC, 4])
    strip = din("strip", [128, 8, 896])
    cfar = din("cfar", [128, 8])
    slotb = din("slotb", [128, CTX])
    causal = din("causal", [128, 128])
    ident = din("ident", [128, 128])
    invc = din("invc", [128, 8, 16])
    hflag = din("hflag", [128, 1])
    halfs = din("halfs", [128, NIT + 1])
    out = nc.dram_tensor("out", [1024, D], F32, kind="ExternalOutput").ap()
    ks = "ExternalOutput" if debug else "Internal"
    KT_d = nc.dram_tensor("KT_d", [8, 128, CTX], BF16, kind=ks).ap()
    V_d = nc.dram_tensor("V_d", [8, CTX, 128], BF16, kind=ks).ap()
    x1_d = nc.dram_tensor("x1_d", [NQ, D], F32, kind=ks).ap()
    x2_d = nc.dram_tensor("x2_d", [1024, D], F32, kind=ks).ap()
    kidxT_d = nc.dram_tensor("kidxT_d", [128, CTX], BF16, kind=ks).ap()
    QT_d = nc.dram_tensor("QT_d", [128, 8, NQ], BF16, kind=ks).ap()
    qiT_d = nc.dram_tensor("qiT_d", [128, 8, NQ], BF16, kind=ks).ap()
    ypT_d = nc.dram_tensor("ypT_d", [128, 8, NQ], BF16, kind=ks).ap()
    attnT_d = nc.dram_tensor("attnT_d", [128, 8, NQ], BF16, kind=ks).ap()
    widx_d = nc.dram_tensor("widx_d", [128, NQB, 16], F32, kind=ks).ap()
    dbg = {}
    if debug:
        dbg['maskT'] = nc.dram_tensor("dbg_maskT", [128, 32, NQ], BF16, kind="ExternalOutput").ap()
        dbg['thr'] = nc.dram_tensor("dbg_thr", [128, NQB], F32, kind="ExternalOutput").ap()
        dbg['gT'] = nc.dram_tensor("dbg_gT", [128, NFC, 1024], BF16, kind="ExternalOutput").ap()

    with contextlib.ExitStack() as es:
        P = Prog(nc, es)
        T = nc.tensor
        A = nc.scalar
        V = nc.vector
        G = nc.gpsimd

        def sb(name, shape, dt, stack=es):
            return stack.enter_context(nc.sbuf_tensor(name, list(shape), dt))

        def ps(name, shape, dt, stack):
            return stack.enter_context(nc.psum_tensor(name, list(shape), dt))

        ident_f = sb("ident_f", [128, 128], F32)
        ident_b = sb("ident_b", [128, 128], BF16)
        ones_b = sb("ones_b", [128, 128], BF16)
        st = sb("st", [128, 16], F32)
        P.dma('sp', ident_f[:], ident[:, :], writes=['ident_f'])
        P.op('dve', lambda: V.tensor_copy(out=ident_b[:], in_=ident_f[:]), reads=['ident_f'], writes=['ident_b'])
        P.op('dve', lambda: V.memset(ones_b[:], 1.0), writes=['ones_b'])

        def alloc(name, shape, dt):
            cm = nc.sbuf_tensor(name, list(shape), dt)
            return cm, cm.__enter__()

        def norm_T(i, xin, xin_key, xn, pT, gsb, dst_fn, dst_keys, extra=None):
            ss = st[:, 3 * i:3 * i + 1]
            sd = st[:, 3 * i + 1:3 * i + 2]
            rs = st[:, 3 * i + 2:3 * i + 3]
            P.op('act', lambda: A.activation(out=xn[i][:], in_=xin, func=AF.Square, accum_out=ss),
                 reads=[xin_key], writes=[('xn', i), ('ss', i)])
            P.op('dve', lambda: V.tensor_scalar(out=sd, in0=ss, scalar1=1.0 / D, scalar2=EPS, op0=ALU.mult, op1=ALU.add),
                 reads=[('ss', i)], writes=[('sd', i)])
            P.op('act', lambda: A.activation(out=sd, in_=sd, func=AF.Sqrt), reads=[('sd', i)], writes=[('sd', i)])
            P.op('dve', lambda: V.reciprocal(out=rs, in_=sd), reads=[('sd', i)], writes=[('rs', i)])
            if extra is not None:
                P.op('dve', lambda: V.tensor_tensor(out=rs, in0=rs, in1=extra, op=ALU.mult),
                     reads=[('rs', i), 'hflag'], writes=[('rs', i)])
            P.op('dve', lambda: V.tensor_scalar(out=xn[i][:], in0=xin, scalar1=rs, scalar2=None, op0=ALU.mult),
                 reads=[xin_key, ('rs', i)], writes=[('xn', i)])
            for c in range(16):
                P.op('pe', lambda c=c: T.transpose(out=pT[i][c // 8][:, (c % 8) * 128:(c % 8 + 1) * 128],
                                                   in_=xn[i][:, c * 128:(c + 1) * 128], identity=ident_b[:]),
                     reads=[('xn', i), 'ident_b'], writes=[('pT', i, c // 8)])
            for h in range(2):
                P.op('dve', lambda h=h: V.tensor_tensor(
                    out=dst_fn(h), in0=pT[i][h][:, :].rearrange("p (c t) -> p c t", t=128),
                    in1=gsb[:, 8 * h:8 * h + 8, :], op=ALU.mult),
                    reads=[('pT', i, h), 'gsb'], writes=dst_keys)

        def wload(dst, dkey, src_rows, c0, c1, col0, ncol, q='pool'):
            P.dma(q, dst[:, c0:c1, 0:ncol],
                  src_rows[c0 * 128:c1 * 128, col0:col0 + ncol].rearrange("(c p) n -> p c n", p=128),
                  writes=[dkey])

        with contextlib.ExitStack() as pa:
            kidxT = sb("kidxT", [128, CTX], BF16, pa)
            Wk = sb("Wk", [128, 16, 1024], BF16, pa)
            Wv = sb("Wv", [128, 16, 1024], BF16, pa)
            Wki = sb("Wki", [128, 16, 128], BF16, pa)
            gsb = sb("gsbA", [128, 16, 128], F32, pa)
            xb = [sb("xbA%d" % i, [128, D], F32, pa) for i in range(2)]
            xn = [sb("xnA%d" % i, [128, D], BF16, pa) for i in range(2)]
            hT = [sb("hTA%d" % i, [128, 16, 512], BF16, pa) for i in range(2)]
            KTst = [sb("KTst%d" % i, [128, 8, 512], BF16, pa) for i in range(2)]
            Vst = [sb("Vst%d" % i, [128, 4, 1024], BF16, pa) for i in range(2)]
            pT = [[ps("pTA%d%d" % (i, h), [128, 1024], BF16, pa) for h in range(2)] for i in range(2)]
            pm = [ps("pmA%d" % i, [128, 512], F32, pa) for i in range(4)]

            P.dma('sp', gsb[:].rearrange("p c t -> p (c t)"), gT3[0, :, :], writes=['gsb'])
            for cg in range(4):
                wload(Wk, ('Wk', cg), w_in, 4 * cg, 4 * cg + 4, OFF['k'], 1024)
            for cg in range(4):
                wload(Wv, ('Wv', cg), w_in, 4 * cg, 4 * cg + 4, OFF['v'], 1024)
            P.dma('pool', Wki[:, :, 0:64], w_in[:, OFF['ki']:OFF['ki'] + 64].rearrange("(c p) n -> p c n", p=128),
                  writes=['Wki'])
            P.dma('pool', Wki[:, :, 64:128], w_in[:, OFF['ki']:OFF['ki'] + 64].rearrange("(c p) n -> p c n", p=128),
                  writes=['Wki'])
            pmi = [0]

            def a_load(ct, sbl):
                j = ct * 4 + sbl
                P.dma('sp', xb[j % 2][:], xctx[j * 128:(j + 1) * 128, :], writes=[('xb', j % 2)])

            def a_norm(ct, sbl):
                b = ct % 2
                j = ct * 4 + sbl
                i = j % 2
                norm_T(i, xb[i][:], ('xb', i), xn, pT, gsb,
                       lambda h: hT[b][:, 8 * h:8 * h + 8, sbl * 128:(sbl + 1) * 128], [('hT', b, sbl)])

            def a_k_heads(ct, heads):
                b = ct % 2
                hkeys = [('hT', b, s_) for s_ in range(4)]
                for h in heads:
                    pq = pm[pmi[0] % 4]
                    pk = ('pm', pmi[0] % 4)
                    pmi[0] += 1
                    for c in range(16):
                        P.op('pe', lambda c=c: T.matmul(pq[:, :], lhsT=Wk[:, c, h * 128:(h + 1) * 128],
                                                        rhs=hT[b][:, c, :], start=(c == 0), stop=(c == 15)),
                             reads=hkeys + [('Wk', c // 4)], writes=[pk])
                    P.op('act', lambda: A.activation(out=KTst[b][:, h, :], in_=pq[:, :], func=AF.Copy),
                         reads=[pk], writes=[('KTst', b)])

            def a_kidx(ct):
                b = ct % 2
                hkeys = [('hT', b, s_) for s_ in range(4)]
                pq = pm[pmi[0] % 4]
                pk = ('pm', pmi[0] % 4)
                pmi[0] += 1
                for c in range(16):
                    P.op('pe', lambda c=c: T.matmul(pq[:, :], lhsT=Wki[:, c, :], rhs=hT[b][:, c, :],
                                                    start=(c == 0), stop=(c == 15)),
                         reads=hkeys + ['Wki'], writes=[pk])
                P.op('act', lambda: A.activation(out=kidxT[:, ct * 512:(ct + 1) * 512], in_=pq[:, :], func=AF.Copy),
                     reads=[pk], writes=[('kidxT', ct)])
                P.dma('pool', KT_d[:, :, ct * 512:(ct + 1) * 512].rearrange("h d s -> d h s"), KTst[b][:, :, :],
                      reads=[('KTst', b)], writes=[('KT_d', ct)])

            def a_v(ct, sbls):
                b = ct % 2
                for sbl in sbls:
                    for hf in range(2):
                        pq = pm[pmi[0] % 4]
                        pk = ('pm', pmi[0] % 4)
                        pmi[0] += 1
                        for c in range(16):
                            P.op('pe', lambda c=c: T.matmul(
                                pq[:, :], lhsT=hT[b][:, c, sbl * 128:(sbl + 1) * 128],
                                rhs=Wv[:, c, hf * 512:(hf + 1) * 512], start=(c == 0), stop=(c == 15)),
                                reads=[('hT', b, sbl), ('Wv', c // 4)], writes=[pk])
                        if hf == 0:
                            P.op('act', lambda: A.activation(
                                out=Vst[b][:, sbl, hf * 512:(hf + 1) * 512], in_=pq[:, :], func=AF.Copy),
                                reads=[pk], writes=[('Vst', b, sbl)])
                        else:
                            P.op('dve', lambda: V.tensor_copy(
                                out=Vst[b][:, sbl, hf * 512:(hf + 1) * 512], in_=pq[:, :]),
                                reads=[pk], writes=[('Vst', b, sbl)])
                    r0 = ct * 512 + sbl * 128
                    P.dma('pool', V_d[:, r0:r0 + 128, :].rearrange("h p d -> p h d"),
                          Vst[b][:, sbl, :].rearrange("p (h d) -> p h d", d=128),
                          reads=[('Vst', b, sbl)], writes=[('V_d', ct, sbl)])

            for sbl in range(4):
                a_load(0, sbl)
                a_norm(0, sbl)
            NCT = CTX // 512
            for ct in range(NCT):
                parts = [lambda ct=ct: a_k_heads(ct, range(0, 4)),
                         lambda ct=ct: (a_k_heads(ct, range(4, 8)), a_kidx(ct)),
                         lambda ct=ct: a_v(ct, (0, 1)),
                         lambda ct=ct: a_v(ct, (2, 3))]
                for q_ in range(4):
                    if ct + 1 < NCT:
                        a_load(ct + 1, q_)
                    parts[q_]()
                    if ct + 1 < NCT:
                        a_norm(ct + 1, q_)
            P.dma('sp', kidxT_d[:, :], kidxT[:, :], reads=[('kidxT', c_) for c_ in range(8)])
            P.barrier()
        if stop_after == 'A':
            return nc


        with contextlib.ExitStack() as pb:
            QT = sb("QT", [128, 8, NQ], BF16, pb)
            qiT = sb("qiT", [128, 8, NQ], BF16, pb)
            ypT = sb("ypT", [128, 8, NQ], BF16, pb)
            widx = sb("widx", [128, NQB, 16], F32, pb)
            hM = sb("hM", [128, 16, NHM], BF16, pb)
            Ws = [sb("WsB%d" % i, [128, 16, 512], BF16, pb) for i in range(2)]
            wgroups = [OFF['q'], OFF['q'] + 512, OFF['qi'], OFF['qi'] + 512, OFF['pool'], OFF['pool'] + 512]
            wissued = [0]

            def issue_w():
                k = wissued[0]
                if k < len(wgroups):
                    wload(Ws[k % 2], ('Ws', k % 2), w_in, 0, 16, wgroups[k], 512)
                    wissued[0] += 1
            issue_w()
            issue_w()
            with contextlib.ExitStack() as pb1:
                gsb = sb("gsbB", [128, 16, 128], F32, pb1)
                xb = [sb("xbB%d" % i, [128, D], F32, pb1) for i in range(2)]
                xn = [sb("xnB%d" % i, [128, D], BF16, pb1) for i in range(2)]
                pT = [[ps("pTB%d%d" % (i, h), [128, 1024], BF16, pb1) for h in range(2)] for i in range(2)]
                P.dma('sp', gsb[:].rearrange("p c t -> p (c t)"), gT3[0, :, :], writes=['gsb'])
                for j in range(NHM // 128):
                    i = j % 2
                    P.dma('sp', xb[i][:], xctx[HM0 + j * 128:HM0 + (j + 1) * 128, :], writes=[('xb', i)])
                    norm_T(i, xb[i][:], ('xb', i), xn, pT, gsb,
                           lambda h, j=j: hM[:, 8 * h:8 * h + 8, j * 128:(j + 1) * 128], [('hM', j)])
                P.barrier()
            pp = [ps("ppB%d" % i, [128, 4, 512], F32, pb) for i in range(2)]
            Wpl = sb("Wpl", [128, 4, 2, 256], BF16, pb)
            Ww = sb("Ww", [128, 16, 16], BF16, pb)
            psc_sb = sb("pscale_sb", [128, 8], F32, pb)
            invc_sb = sb("invc_sb", [128, 8, 16], F32, pb)
            pooledT = sb("pooledT", [128, 8, NQ], BF16, pb)
            uk = [sb("uk%d" % i, [128, 1168], F32, pb) for i in range(2)]
            sA = sb("sA", [128, 1168], F32, pb)
            sB = sb("sB", [128, 1168], F32, pb)
            t16 = sb("t16", [128, 16], F32, pb)
            P.dma('sp', psc_sb[:], pscale[:, :], writes=['pscale'])
            P.dma('sp', invc_sb[:], invc[:, :, :], writes=['invc'])
            for g in range(4):
                P.dma('pool', Wpl[:, g, :, :], w_pool[g, :, :].rearrange("(cc p) d -> p cc d", p=128), writes=['Wpl'])
            P.dma('pool', Ww[:, :, :], w_in[:, OFF['wi']:OFF['wi'] + 16].rearrange("(c p) n -> p c n", p=128), writes=['Ww'])
            wcnt = [0]
            pcnt = [0]

            def next_w(col0):
                b = wcnt[0] % 2
                assert wgroups[wcnt[0]] == col0
                if wcnt[0] >= 1:
                    issue_w()
                wcnt[0] += 1
                return b

            def next_pp():
                b = pcnt[0] % 2
                pcnt[0] += 1
                return b

            for (dst, off) in ((QT, OFF['q']), (qiT, OFF['qi'])):
                for g in range(2):
                    wb = next_w(off + 512 * g)
                    for m_ in range(4):
                        pb_ = next_pp()
                        for n in range(3):
                            for c in range(16):
                                P.op('pe', lambda c=c, n=n, wb=wb, m_=m_, pb_=pb_: T.matmul(
                                    pp[pb_][:, n, 0:384], lhsT=Ws[wb][:, c, m_ * 128:(m_ + 1) * 128],
                                    rhs=hM[:, c, 128 + n * 384:128 + (n + 1) * 384], start=(c == 0), stop=(c == 15)),
                                    reads=[('Ws', wb)], writes=[('pp', pb_)])
                        P.op('act', lambda dst=dst, g=g, m_=m_, pb_=pb_: A.activation(
                            out=dst[:, 4 * g + m_, :].rearrange("p (n t) -> p n t", t=384),
                            in_=pp[pb_][:, 0:3, 0:384], func=AF.Copy),
                            reads=[('pp', pb_)], writes=[('fm', id(dst), 4 * g + m_)])
            for g2 in range(2):
                wb = next_w(OFF['pool'] + 512 * g2)
                for m_ in range(4):
                    k = 4 * g2 + m_
                    g = k // 2
                    pb_ = next_pp()
                    for n in range(4):
                        for c in range(16):
                            P.op('pe', lambda c=c, n=n, wb=wb, m_=m_, pb_=pb_: T.matmul(
                                pp[pb_][:, n, 0:292], lhsT=Ws[wb][:, c, m_ * 128:(m_ + 1) * 128],
                                rhs=hM[:, c, 112 + n * 292:112 + (n + 1) * 292], start=(c == 0), stop=(c == 15)),
                                reads=[('Ws', wb)], writes=[('pp', pb_)])
                    u = uk[k % 2]
                    P.op('act', lambda u=u, pb_=pb_: A.activation(
                        out=u[:, :].rearrange("p (n t) -> p n t", t=292), in_=pp[pb_][:, 0:4, 0:292], func=AF.Copy),
                        reads=[('pp', pb_)], writes=[('uk', k % 2)])
                    wdw = (2, 4, 8, 16)[g]
                    cur, ckey = u, ('uk', k % 2)
                    for s_ in range(int(math.log2(wdw))):
                        sh = 1 << s_
                        nxt, nkey = (sA, 'sA') if s_ % 2 == 0 else (sB, 'sB')
                        P.op('pool', lambda cur=cur, nxt=nxt, sh=sh: G.tensor_tensor(
                            out=nxt[:, sh:1168], in0=cur[:, sh:1168], in1=cur[:, 0:1168 - sh], op=ALU.add),
                            reads=[ckey], writes=[nkey])
                        cur, ckey = nxt, nkey
                    P.op('dve', lambda cur=cur, u=u, k=k, wdw=wdw: V.scalar_tensor_tensor(
                        out=pooledT[:, k, :], in0=cur[:, 16:1168], scalar=1.0 / wdw, in1=u[:, 16:1168],
                        op0=ALU.mult, op1=ALU.subtract), reads=[ckey, ('uk', k % 2)], writes=[('pooledT', k)])
                    P.op('dve', lambda cur=cur, k=k: V.tensor_tensor(out=t16[:, :], in0=cur[:, 144:160], in1=invc_sb[:, k, :], op=ALU.mult),
                         reads=[ckey, 'invc'], writes=['t16'])
                    P.op('dve', lambda u=u, k=k: V.tensor_tensor(out=pooledT[:, k, 128:144], in0=t16[:, :], in1=u[:, 144:160], op=ALU.subtract),
                         reads=['t16', ('uk', k % 2)], writes=[('pooledT', k)])
            for k2 in range(8):
                g, dm = k2 // 2, k2 % 2
                pb_ = next_pp()
                for n in range(3):
                    for cc in range(2):
                        P.op('pe', lambda n=n, cc=cc, g=g, dm=dm, pb_=pb_: T.matmul(
                            pp[pb_][:, n, 0:384], lhsT=Wpl[:, g, cc, dm * 128:(dm + 1) * 128],
                            rhs=pooledT[:, 2 * g + cc, n * 384:(n + 1) * 384], start=(cc == 0), stop=(cc == 1)),
                            reads=['Wpl', ('pooledT', 2 * g + cc)], writes=[('pp', pb_)])
                P.op('act', lambda k2=k2, pb_=pb_: A.activation(
                    out=ypT[:, k2, :].rearrange("p (n t) -> p n t", t=384), in_=pp[pb_][:, 0:3, 0:384],
                    func=AF.Copy, scale=psc_sb[:, k2:k2 + 1]), reads=[('pp', pb_), 'pscale'], writes=[('ypT', k2)])
            idx_scale = (16 ** -0.5) * (64 ** -0.5)
            for i in range(NQB):
                pb_ = next_pp()
                for c in range(16):
                    P.op('pe', lambda c=c, i=i, pb_=pb_: T.matmul(
                        pp[pb_][:, 0, 0:16], lhsT=hM[:, c, 128 + i * 128:256 + i * 128], rhs=Ww[:, c, :],
                        start=(c == 0), stop=(c == 15)), reads=['Ww'], writes=[('pp', pb_)])
                P.op('act', lambda i=i, pb_=pb_: A.activation(out=widx[:, i, :], in_=pp[pb_][:, 0, 0:16], func=AF.Copy, scale=idx_scale),
                     reads=[('pp', pb_)], writes=[('widx', i)])
            P.dma('sp', QT_d[:, :, :], QT[:, :, :], reads=[('fm', id(QT), h_) for h_ in range(8)])
            P.dma('sp', qiT_d[:, :, :], qiT[:, :, :], reads=[('fm', id(qiT), h_) for h_ in range(8)])
            P.dma('sp', ypT_d[:, :, :], ypT[:, :, :], reads=[('ypT', h_) for h_ in range(8)])
            P.dma('sp', widx_d[:, :, :], widx[:, :, :], reads=[('widx', h_) for h_ in range(NQB)])
            P.barrier()
        if stop_after == 'B':
            return nc

        pcd = contextlib.ExitStack()
        es.enter_context(pcd)
        maskT = sb("maskT", [128, 32, NQ], BF16, pcd)
        with contextlib.ExitStack() as pc:
            kidxT = sb("kidxTc", [128, CTX], BF16, pc)
            qiT = sb("qiTc", [128, 8, NQ], BF16, pc)
            widx = sb("widxc", [128, NQB, 16], F32, pc)
            P.dma('sp', kidxT[:, :], kidxT_d[:, :], writes=['kidxT'])
            P.dma('sp', qiT[:, :, :], qiT_d[:, :, :], writes=['qiT'])
            P.dma('sp', widx[:, :, :], widx_d[:, :, :], writes=['widx'])
            slot_row = sb("slot_row", [1, CTX], BF16, pc)
            ones_row = sb("ones_row", [1, 128], BF16, pc)
            causal_b = sb("causal_b", [128, 128], BF16, pc)
            halfs_sb = sb("halfs_sb", [128, NIT + 1], F32, pc)
            sc = [sb("sc%d" % i, [128, CTX], F32, pc) for i in range(2)]
            junk = sb("junk", [128, CTX], BF16, pc)
            Rb = sb("Rb", [128, 4, 512], BF16, pc)
            Dg = [sb("Dg%d" % i, [128, 16, 128], BF16, pc) for i in range(2)]
            bsx = [sb("bsx%d" % i, [128, 32], F32, pc) for i in range(2)]
            bs = sb("bs", [128, 64], F32, pc)
            thr_all = sb("thr_all", [128, NQB], F32, pc)
            pd = ps("pdC", [128, 4, 512], F32, pc)
            psc = [ps("pscC%d" % i, [128, 512], F32, pc) for i in range(3)]
            ptm = [ps("ptmC%d" % i, [128, 1024], BF16, pc) for i in range(1)]
            P.dma('pool', slot_row[0:1, :], slotb[0:1, :], writes=['slot_row'])
            P.dma('pool', causal_b[:, :], causal[:, :], writes=['causal_b'])
            P.dma('sp', halfs_sb[:], halfs[:, :], writes=['halfs_sb'])
            P.op('pool', lambda: G.memset(ones_row[0:1, :], 1.0), writes=['ones_row'])
            P.op('pool', lambda: G.memset(maskT[:, :, :].rearrange("p a b -> p (a b)"), 0.0), writes=['maskT'])
            mid = bs[:, 3:4]
            cntv = bs[:, 4:5]
            gg = bs[:, 5:6]
            lo = bs[:, 6:7]
            hi = bs[:, 7:8]
            w0 = bs[:, 8:9]
            hk = bs[:, 16:16 + NIT + 1]
            cnts = dict(d=0, s=0, t=0)

            def gen_scores(i, bi):
                E = QS0 + 128 * (i + 1)
                nkt = (E + 511) // 512
                mins = bsx[bi][:, 0:8]
                maxs = bsx[bi][:, 8:16]
                for h in range(16):
                    P.op('pool', lambda h=h: G.tensor_scalar(out=Dg[bi][:, h, :], in0=ident_f[:], scalar1=widx[:, i, h:h + 1],
                                                             scalar2=1.0, op0=ALU.mult, op1=ALU.mult),
                         reads=['ident_f', 'widx'], writes=[('Dg', bi, h)])
                prev = None

                def finish(kt, N, pst, pskey):
                    P.op('pe', lambda: T.matmul(pst[:, :N], lhsT=ones_row[0:1, :], rhs=slot_row[0:1, kt * 512:kt * 512 + N],
                                                start=False, stop=(kt != nkt - 1)),
                         reads=['ones_row', 'slot_row', ('mins', bi, kt), ('maxs', bi, kt)], writes=[pskey])
                    if kt == nkt - 1:
                        P.op('pe', lambda: T.matmul(pst[:, N - 128:N], lhsT=ident_b[:, :], rhs=causal_b[:, :], start=False, stop=True),
                             reads=['ident_b', 'causal_b'], writes=[pskey])
                    P.op('act', lambda: A.activation(out=sc[bi][:, kt * 512:kt * 512 + N], in_=pst[:, :N], func=AF.Copy),
                         reads=[pskey], writes=[('sc', bi, kt)])
                for kt in range(nkt):
                    N = min(512, E - kt * 512)
                    si = cnts['s'] % 3
                    cnts['s'] += 1
                    pst = psc[si]
                    pskey = ('psc', si)
                    pend = []

                    def score_mm(hp, di, N=N, pst=pst, pskey=pskey):
                        for q_ in range(2):
                            h = 2 * hp + q_
                            P.op('pe', lambda h=h, q_=q_: T.matmul(pst[:, :N], lhsT=Dg[bi][:, h, :], rhs=Rb[:, 2 * di + q_, :N],
                                                                   start=(h == 0), stop=False),
                                 reads=[('Dg', bi, h), ('Rb', di)], writes=[pskey])
                    for hp in range(8):
                        di = cnts['d'] % 2
                        cnts['d'] += 1
                        pdk = ('pd', di)
                        for q_ in range(2):
                            pr = q_ * 64
                            P.op('pe', lambda q_=q_, pr=pr: T.matmul(
                                pd[:, 2 * di + q_, :N], lhsT=qiT[pr:pr + 64, hp, i * 128:(i + 1) * 128],
                                rhs=kidxT[pr:pr + 64, kt * 512:kt * 512 + N], start=True, stop=True),
                                reads=['kidxT', 'qiT'], writes=[pdk])
                        P.op('act', lambda di=di: A.activation(out=Rb[:, 2 * di:2 * di + 2, :N], in_=pd[:, 2 * di:2 * di + 2, :N], func=AF.Relu),
                             reads=[pdk], writes=[('Rb', di)])
                        pend.append((hp, di))
                        if len(pend) > 1:
                            score_mm(*pend.pop(0))
                    while pend:
                        score_mm(*pend.pop(0))
                    P.op('dve', lambda pst=pst, N=N, kt=kt: V.tensor_reduce(out=mins[:, kt:kt + 1], in_=pst[:, :N], axis=AX.X, op=ALU.min),
                         reads=[pskey], writes=[('mins', bi, kt)])
                    P.op('dve', lambda pst=pst, N=N, kt=kt: V.tensor_reduce(out=maxs[:, kt:kt + 1], in_=pst[:, :N], axis=AX.X, op=ALU.max),
                         reads=[pskey], writes=[('maxs', bi, kt)])
                    if prev is not None:
                        finish(*prev)
                    prev = (kt, N, pst, pskey)
                    yield
                finish(*prev)
                yield

            def bisect(i, bi, nxt):
                E = QS0 + 128 * (i + 1)
                nkt = (E + 511) // 512
                mins = bsx[bi][:, 0:8]
                maxs = bsx[bi][:, 8:16]
                sckeys = [('sc', bi, kt) for kt in range(nkt)]
                P.op('dve', lambda: V.tensor_reduce(out=lo, in_=mins[:, 0:nkt], axis=AX.X, op=ALU.min),
                     reads=[('mins', bi, kt) for kt in range(nkt)], writes=['lo'])
                P.op('dve', lambda: V.tensor_reduce(out=hi, in_=maxs[:, 0:nkt], axis=AX.X, op=ALU.max),
                     reads=[('maxs', bi, kt) for kt in range(nkt)], writes=['hi'])
                P.op('dve', lambda: V.scalar_tensor_tensor(out=w0, in0=hi, scalar=0.01, in1=lo, op0=ALU.add, op1=ALU.subtract),
                     reads=['hi', 'lo'], writes=['w0'])
                P.op('dve', lambda: V.tensor_scalar(out=hk, in0=halfs_sb[:, :], scalar1=w0, scalar2=None, op0=ALU.mult),
                     reads=['w0', 'halfs_sb'], writes=['hk'])
                P.op('dve', lambda: V.scalar_tensor_tensor(out=mid, in0=lo, scalar=-0.01, in1=hk[:, 0:1], op0=ALU.add, op1=ALU.add),
                     reads=['lo', 'hk'], writes=['mid'])
                for it in range(NIT):
                    P.op('dve', lambda: V.tensor_scalar(out=junk[:, 0:E], in0=sc[bi][:, 0:E], scalar1=mid, scalar2=None,
                                                        op0=ALU.is_ge, op1=ALU.add, accum_out=cntv),
                         reads=sckeys + ['mid'], writes=['junk', 'cnt'])
                    P.op('dve', lambda: V.tensor_scalar(out=gg, in0=cntv, scalar1=255.5, scalar2=0.5, op0=ALU.is_ge, op1=ALU.subtract),
                         reads=['cnt'], writes=['gg'])
                    P.op('dve', lambda it=it: V.scalar_tensor_tensor(out=mid, in0=gg, scalar=hk[:, it:it + 1], in1=mid,
                                                                      op0=ALU.mult, op1=ALU.add),
                         reads=['gg', 'hk', 'mid'], writes=['mid'])
                    if nxt is not None and it % 2 == 1:
                        next(nxt, None)
                if nxt is not None:
                    for _ in nxt:
                        pass
                P.op('dve', lambda: V.tensor_tensor(out=lo, in0=mid, in1=hk[:, NIT:NIT + 1], op=ALU.subtract),
                     reads=['mid', 'hk'], writes=['lo'])
                P.op('dve', lambda: V.tensor_scalar(out=junk[:, 0:E], in0=sc[bi][:, 0:E], scalar1=lo, scalar2=None, op0=ALU.is_ge),
                     reads=sckeys + ['lo'], writes=['junk'])
                P.op('dve', lambda: V.tensor_copy(out=thr_all[:, i:i + 1], in_=lo), reads=['lo'], writes=['thr_all'])
                nkb = E // 128
                for kb0 in range(0, nkb, 8):
                    n8 = min(8, nkb - kb0)
                    pt = ptm[0]
                    ptk = ('ptm', 0)
                    for r in range(n8):
                        kb = kb0 + r
                        P.op('pe', lambda r=r, kb=kb: T.transpose(out=pt[:, r * 128:(r + 1) * 128],
                                                                 in_=junk[:, kb * 128:(kb + 1) * 128], identity=ident_b[:]),
                             reads=['junk', 'ident_b'], writes=[ptk])
                    P.op('act', lambda n8=n8, kb0=kb0: A.activation(
                        out=maskT[:, kb0:kb0 + n8, i * 128:(i + 1) * 128],
                        in_=pt[:, 0:n8 * 128].rearrange("p (a b) -> p a b", b=128), func=AF.Copy),
                        reads=[ptk], writes=['maskT'])

            for _ in gen_scores(0, 0):
                pass
            for i in range(NQB):
                nxt = gen_scores(i + 1, (i + 1) % 2) if i + 1 < NQB else None
                bisect(i, i % 2, nxt)
            if debug:
                P.dma('sp', dbg['maskT'][:, :, :], maskT[:, :, :], reads=['maskT'])
                P.dma('sp', dbg['thr'][:, :], thr_all[:, :], reads=['thr_all'])
            P.barrier()
        if stop_after == 'C':
            return nc

        SCALE = 128 ** -0.5
        with contextlib.ExitStack() as pdd:
            attnT = sb("attnT", [128, 8, NQ], BF16, pdd)
            QT = sb("QTd", [128, 8, NQ], BF16, pdd)
            P.dma('sp', QT[:, :, :], QT_d[:, :, :], writes=['QT'])
            KTh = [sb("KTh%d" % i, [128, CTX], BF16, pdd) for i in range(2)]
            Vh = [sb("Vh%d" % i, [128, 32, 128], BF16, pdd) for i in range(2)]
            strip_sb = sb("strip_sb", [128, 8, 896], F32, pdd)
            cfar_sb = sb("cfar_sb", [128, 8], F32, pdd)
            Eb = [sb("Eb%d" % i, [128, 384], BF16, pdd) for i in range(4)]
            Pb = [sb("Pb%d" % i, [128, 384], BF16, pdd) for i in range(4)]
            tmpb = [sb("tmpb%d" % i, [128, 384], F32, pdd) for i in range(2)]
            rl = sb("rl", [128, 384], F32, pdd)
            tiny_sb = sb("tiny_sb", [128, 1], F32, pdd)
            P.op('pool', lambda: G.memset(tiny_sb[:, :], 1e-18), writes=['tiny'])
            pS = [ps("pS%d" % i, [128, 512], F32, pdd) for i in range(4)]
            pO = [ps("pO%d" % i, [128, 512], F32, pdd) for i in range(2)]
            pL = [ps("pL%d" % i, [128, 512], F32, pdd) for i in range(2)]
            P.dma('sp', strip_sb[:], strip[:, :, :], writes=['strip_sb'])
            P.dma('sp', cfar_sb[:], cfar[:, :], writes=['cfar_sb'])
            steps = []
            for h in range(8):
                for qt in range(3):
                    kbmax = 23 + 3 * qt + 2
                    near = list(range(kbmax, kbmax - 4, -1))
                    far = list(range(kbmax - 4, -1, -1))
                    order = []
                    for nk in near:
                        order.append(nk)
                        order.extend(far[:4])
                        far = far[4:]
                    order.extend(far)
                    for n_, kb in enumerate(order):
                        steps.append((h, qt, kb, n_ == 0, n_ == len(order) - 1))
            loaded = set()

            def load_head(h):
                if h in loaded or h >= 8:
                    return
                loaded.add(h)
                b = h % 2
                P.dma('sp', KTh[b][:, :], KT_d[h, :, :], writes=[('KTh', b)])
                P.dma('sp', Vh[b][:, :, :], V_d[h, :, :].rearrange("(kb p) d -> p kb d", p=128), writes=[('Vh', b)])
            cn = dict(s=0, t=0)
            staged = []

            def stage1(step):
                h, qt, kb, first, last = step
                b = h % 2
                t0 = qt * 384
                if first and qt == 0:
                    load_head(h)
                if qt == 1 and first:
                    load_head(h + 1)
                s_i = cn['s'] % 4
                cn['s'] += 1
                pst = pS[s_i]
                P.op('pe', lambda: T.matmul(pst[:, 0:384], lhsT=KTh[b][:, kb * 128:(kb + 1) * 128],
                                            rhs=QT[:, h, t0:t0 + 384], start=True, stop=True),
                     reads=[('KTh', b), 'QT'], writes=[('pS', s_i)])
                D0 = (QS0 + t0) - kb * 128
                if D0 >= 256:
                    P.op('act', lambda: A.activation(out=Eb[s_i][:, :], in_=pst[:, 0:384], func=AF.Exp,
                                                     scale=SCALE, bias=cfar_sb[:, h:h + 1]),
                         reads=[('pS', s_i), 'cfar_sb'], writes=[('Eb', s_i)])
                else:
                    ti = cn['t'] % 2
                    cn['t'] += 1
                    P.op('dve', lambda: V.scalar_tensor_tensor(
                        out=tmpb[ti][:, :], in0=pst[:, 0:384], scalar=SCALE,
                        in1=strip_sb[:, h, D0 + 256:D0 + 256 + 384], op0=ALU.mult, op1=ALU.add),
                        reads=[('pS', s_i), 'strip_sb'], writes=[('tmpb', ti)])
                    P.op('act', lambda: A.activation(out=Eb[s_i][:, :], in_=tmpb[ti][:, :], func=AF.Exp),
                         reads=[('tmpb', ti)], writes=[('Eb', s_i)])
                P.op('dve', lambda: V.tensor_tensor(out=Pb[s_i][:, :], in0=Eb[s_i][:, :],
                                                    in1=maskT[:, kb, t0:t0 + 384], op=ALU.mult),
                     reads=[('Eb', s_i)], writes=[('Pb', s_i)])
                staged.append((step, s_i))

            def stage2():
                (h, qt, kb, first, last), s_i = staged.pop(0)
                b = h % 2
                t0 = qt * 384
                g_ = (h * 3 + qt) % 2
                po, pl = pO[g_], pL[g_]
                pok, plk = ('pO', g_), ('pL', g_)
                P.op('pe', lambda: T.matmul(po[:, 0:384], lhsT=Vh[b][:, kb, :], rhs=Pb[s_i][:, :], start=first, stop=last),
                     reads=[('Vh', b), ('Pb', s_i)], writes=[pok])
                P.op('pe', lambda: T.matmul(pl[:, 0:384], lhsT=ones_b[:, :], rhs=Pb[s_i][:, :], start=first, stop=last),
                     reads=['ones_b', ('Pb', s_i)], writes=[plk])
                if last:
                    P.op('act', lambda: A.activation(out=rl[:, :], in_=pl[:, 0:384], func=AF.Ln, bias=tiny_sb[:, 0:1]),
                         reads=[plk, 'tiny'], writes=['rl'])
                    P.op('act', lambda: A.activation(out=rl[:, :], in_=rl[:, :], func=AF.Exp, scale=-1.0), reads=['rl'], writes=['rl'])
                    P.op('dve', lambda: V.tensor_tensor(out=attnT[:, h, t0:t0 + 384], in0=po[:, 0:384], in1=rl[:, :], op=ALU.mult),
                         reads=[pok, 'rl'], writes=[('attnT', h)])
            for step in steps:
                stage1(step)
                if len(staged) > 3:
                    stage2()
            while staged:
                stage2()
            P.dma('sp', attnT_d[:, :, :], attnT[:, :, :], reads=[('attnT', h_) for h_ in range(8)])
            P.barrier()
        pcd.close()
        if stop_after == 'D':
            return nc

        with contextlib.ExitStack() as pf:
            Wo = sb("Wo", [128, 16, 2048], BF16, pf)
            ypT = sb("ypTf", [128, 8, NQ], BF16, pf)
            attnT = sb("attnTf", [128, 8, NQ], BF16, pf)
            P.dma('sp', ypT[:, :, :], ypT_d[:, :, :], writes=['ypT'])
            P.dma('sp', attnT[:, :, :], attnT_d[:, :, :], writes=['attnT'])
            xr = [sb("xrF%d" % i, [128, D], F32, pf) for i in range(2)]
            x1t = [sb("x1tF%d" % i, [128, D], F32, pf) for i in range(2)]
            pw = [ps("pwF%d" % i, [128, 512], F32, pf) for i in range(8)]
            for cg in range(4):
                wload(Wo, ('Wo', cg), w_out, 4 * cg, 4 * cg + 4, 0, 2048)
            pcn = 0
            for i in range(NQB):
                bi = i % 2
                P.dma('sp', xr[bi][:], xctx[QS0 + i * 128:QS0 + (i + 1) * 128, :], writes=[('xr', bi)])
                for nt in range(4):
                    pq = pw[pcn % 8]
                    pk = ('pw', pcn % 8)
                    pcn += 1
                    for c in range(16):
                        src = ypT if c < 8 else attnT
                        P.op('pe', lambda c=c, src=src, pq=pq, nt=nt, i=i: T.matmul(
                            pq[:, :], lhsT=src[:, c % 8, i * 128:(i + 1) * 128], rhs=Wo[:, c, nt * 512:(nt + 1) * 512],
                            start=(c == 0), stop=(c == 15)), reads=[('Wo', c // 4), 'ypT', 'attnT'], writes=[pk])
                    P.op('dve', lambda pq=pq, nt=nt, bi=bi: V.tensor_tensor(
                        out=x1t[bi][:, nt * 512:(nt + 1) * 512], in0=pq[:, :], in1=xr[bi][:, nt * 512:(nt + 1) * 512], op=ALU.add),
                        reads=[pk, ('xr', bi)], writes=[('x1t', bi)])
                P.dma('sp', x1_d[i * 128:(i + 1) * 128, :], x1t[bi][:], reads=[('x1t', bi)], writes=[('x1_d', i)])
            P.barrier()
        if stop_after == 'F':
            return nc


        pgh = contextlib.ExitStack()
        es.enter_context(pgh)
        gT = sb("gT", [128, NFC, 1024], BF16, pgh)
        with contextlib.ExitStack() as pg_:
            h2T = sb("h2T", [128, 16, NQ], BF16, pg_)
            Wgv = [sb("Wgv%d" % i, [128, 16, 512], BF16, pg_) for i in range(2)]

            def g_load(grp):
                jg_, isval_ = grp // 2, grp % 2
                wload(Wgv[grp % 2], ('Wgv', grp % 2), w_up, 0, 16, (D_FF if isval_ else 0) + jg_ * 512, 512)
            g_load(0)
            g_load(1)
            with contextlib.ExitStack() as pg1:
                gsb = sb("gsbG", [128, 16, 128], F32, pg1)
                hf_sb = sb("hf_sb", [128, 1], F32, pg1)
                xb = [sb("xbG%d" % i, [128, D], F32, pg1) for i in range(2)]
                xn = [sb("xnG%d" % i, [128, D], BF16, pg1) for i in range(2)]
                pT = [[ps("pTG%d%d" % (i, h), [128, 1024], BF16, pg1) for h in range(2)] for i in range(2)]
                P.dma('sp', gsb[:].rearrange("p c t -> p (c t)"), gT3[1, :, :], writes=['gsb'])
                P.dma('sp', hf_sb[:], hflag[:, :], writes=['hflag'])
                for j in range(NQB):
                    i = j % 2
                    P.dma('sp', xb[i][:], x1_d[j * 128:(j + 1) * 128, :], writes=[('xb', i)])
                    norm_T(i, xb[i][:], ('xb', i), xn, pT, gsb,
                           lambda h, j=j: h2T[:, 8 * h:8 * h + 8, j * 128:(j + 1) * 128], [('h2T', j)],
                           extra=(hf_sb[:, 0:1] if j == 0 else None))
                P.barrier()
            sgT = sb("sgT", [128, 4, 1024], F32, pg_)
            cp = sb("cp", [128, 2 * NFC, 4], F32, pg_)
            rA = [sb("rA%d" % i, [128, 344], F32, pg_) for i in range(3)]
            rB = [sb("rB%d" % i, [128, 344], F32, pg_) for i in range(3)]
            pu = [ps("puG%d" % i, [128, 512], F32, pg_) for i in range(8)]
            P.dma('sp', cp[:], convp[:, :, :], writes=['cp'])
            tiles = [(0, 342), (342, 684), (684, 1024)]
            pcn = 0
            rcn = 0
            for grp in range(2 * (NFC // 4)):
                jg, isval = grp // 2, grp % 2
                wb = grp % 2
                if grp >= 1 and grp + 1 < 2 * (NFC // 4):
                    g_load(grp + 1)
                for m_ in range(4):
                    j = 4 * jg + m_
                    jj = (NFC + j) if isval else j
                    for (a, b_) in tiles:
                        N = b_ - a + 2
                        n2 = N - 2
                        c0 = 128 + a - 2
                        pq = pu[pcn % 8]
                        pk = ('pu', pcn % 8)
                        pcn += 1
                        for c in range(16):
                            P.op('pe', lambda c=c: T.matmul(
                                pq[:, 0:N], lhsT=Wgv[wb][:, c, m_ * 128:(m_ + 1) * 128], rhs=h2T[:, c, c0:c0 + N],
                                start=(c == 0), stop=(c == 15)), reads=[('Wgv', wb)], writes=[pk])
                        ri = rcn % 3
                        rcn += 1
                        P.op('act', lambda: A.activation(
                            out=rA[ri][:, 0:n2], in_=pq[:, 2:N], func=AF.Identity, scale=cp[:, jj, 2:3], bias=cp[:, jj, 3:4]),
                            reads=[pk, 'cp'], writes=[('rA', ri)])
                        P.op('dve', lambda: V.scalar_tensor_tensor(
                            out=rB[ri][:, 0:n2], in0=pq[:, 1:N - 1], scalar=cp[:, jj, 1:2], in1=rA[ri][:, 0:n2],
                            op0=ALU.mult, op1=ALU.add), reads=[pk, 'cp', ('rA', ri)], writes=[('rB', ri)])
                        if not isval:
                            P.op('dve', lambda: V.scalar_tensor_tensor(
                                out=rA[ri][:, 0:n2], in0=pq[:, 0:n2], scalar=cp[:, jj, 0:1], in1=rB[ri][:, 0:n2],
                                op0=ALU.mult, op1=ALU.add), reads=[pk, 'cp', ('rB', ri)], writes=[('rA', ri)])
                            P.op('act', lambda: A.activation(out=sgT[:, m_, a:b_], in_=rA[ri][:, 0:n2], func=AF.Silu),
                                 reads=[('rA', ri)], writes=[('sgT', m_)])
                        else:
                            P.op('dve', lambda: V.scalar_tensor_tensor(
                                out=rA[ri][:, 0:n2], in0=pq[:, 0:n2], scalar=cp[:, jj, 0:1], in1=rB[ri][:, 0:n2],
                                op0=ALU.mult, op1=ALU.add), reads=[pk, 'cp', ('rB', ri)], writes=[('rA', ri)])
                            P.op('dve', lambda: V.tensor_tensor(
                                out=gT[:, j, a:b_], in0=sgT[:, m_, a:b_], in1=rA[ri][:, 0:n2], op=ALU.mult),
                                reads=[('sgT', m_), ('rA', ri)], writes=[('gT', j)])
            if debug:
                P.dma('sp', dbg['gT'][:, :, :], gT[:, :, :], reads=[('gT', j_) for j_ in range(NFC)])
            P.barrier()
        if stop_after == 'G':
            return nc

        with contextlib.ExitStack() as ph:
            Wdr = sb("Wdr", [128, NFC, 512], BF16, ph)
            x1s = [sb("x1s%d" % i, [128, 512], F32, ph) for i in range(4)]
            x2s = [sb("x2s%d" % i, [128, 512], F32, ph) for i in range(4)]
            pw = [ps("pwH%d" % i, [128, 512], F32, ph) for i in range(8)]
            NFG = NFC // 4

            def h_load(nt_, fg_):
                P.dma('pool', Wdr[:, 4 * fg_:4 * fg_ + 4, :],
                      w_down[fg_ * 512:(fg_ + 1) * 512, nt_ * 512:(nt_ + 1) * 512].rearrange("(c p) n -> p c n", p=128),
                      writes=[('Wdr', fg_)])
            for fg in range(NFG):
                h_load(0, fg)
            scn = 0
            pset = 0
            for nt in range(4):
                for tg in range(2):
                    banks = [(pw[4 * (pset % 2) + tb], ('pw', 4 * (pset % 2) + tb)) for tb in range(4)]
                    pset += 1
                    for fg in range(NFG):
                        for fl in range(4):
                            f = fg * 4 + fl
                            for tb in range(4):
                                pq, pk = banks[tb]
                                tok0 = (tg * 4 + tb) * 128
                                P.op('pe', lambda pq=pq, f=f, tok0=tok0: T.matmul(
                                    pq[:, :], lhsT=gT[:, f, tok0:tok0 + 128], rhs=Wdr[:, f, :],
                                    start=(f == 0), stop=(f == NFC - 1)), reads=[('Wdr', fg)], writes=[pk])
                        if tg == 1 and nt + 1 < 4:
                            h_load(nt + 1, fg)
                    for tb in range(4):
                        pq, pk = banks[tb]
                        si = scn % 4
                        scn += 1
                        r0 = (tg * 4 + tb) * 128
                        P.dma('sp', x1s[si][:, :], x1_d[128 + r0:128 + r0 + 128, nt * 512:(nt + 1) * 512], writes=[('x1s', si)])
                        P.op('dve', lambda pq=pq, si=si: V.tensor_tensor(out=x2s[si][:, :], in0=pq[:, :], in1=x1s[si][:, :], op=ALU.add),
                             reads=[pk, ('x1s', si)], writes=[('x2s', si)])
                        P.dma('sp', x2_d[r0:r0 + 128, nt * 512:(nt + 1) * 512], x2s[si][:, :], reads=[('x2s', si)], writes=[('x2_d', r0, nt)])
            P.barrier()
        pgh.close()
        if stop_after == 'H':
            return nc

        with contextlib.ExitStack() as pi:
            x3 = sb("x3", [128, 8, D], F32, pi)
            h3T = sb("h3T", [128, 16, 1024], BF16, pi)
            ppT = sb("ppT", [128, 2, 1024], BF16, pi)
            with contextlib.ExitStack() as pi1:
                gsb = sb("gsbI", [128, 16, 128], F32, pi1)
                p_sb = sb("p_sb", [128, 8, 256], F32, pi1)
                p_bf = sb("p_bf", [128, 8, 256], BF16, pi1)
                xn = [sb("xnI%d" % i, [128, D], BF16, pi1) for i in range(2)]
                pT = [[ps("pTI%d%d" % (i, h), [128, 1024], BF16, pi1) for h in range(2)] for i in range(2)]
                ptp = [ps("ptpI%d" % i, [128, 1024], BF16, pi1) for i in range(2)]
                P.dma('sp', gsb[:].rearrange("p c t -> p (c t)"), gT3[2, :, :], writes=['gsb'])
                P.dma('sp', p_sb[:, :, :], p_own.rearrange("(tb p) c -> p tb c", p=128), writes=['p_sb'])
                P.op('pool', lambda: G.tensor_copy(out=p_bf[:, :, :], in_=p_sb[:, :, :]), reads=['p_sb'], writes=['p_bf'])
                for tb in range(8):
                    P.dma('sp', x3[:, tb, :], x2_d[tb * 128:(tb + 1) * 128, :], writes=[('x3', tb)])
                for tb in range(8):
                    i = tb % 2
                    norm_T(i, x3[:, tb, :], ('x3', tb), xn, pT, gsb,
                           lambda h, tb=tb: h3T[:, 8 * h:8 * h + 8, tb * 128:(tb + 1) * 128], [('h3T', tb)])
                    pt = ptp[i]
                    for cc in range(2):
                        P.op('pe', lambda pt=pt, cc=cc, tb=tb: T.transpose(out=pt[:, cc * 128:(cc + 1) * 128],
                                                                          in_=p_bf[:, tb, cc * 128:(cc + 1) * 128], identity=ident_b[:]),
                             reads=['p_bf', 'ident_b'], writes=[('ptp', i)])
                    P.op('act', lambda pt=pt, tb=tb: A.activation(out=ppT[:, :, tb * 128:(tb + 1) * 128],
                                                                  in_=pt[:, 0:256].rearrange("p (a b) -> p a b", b=128), func=AF.Copy),
                         reads=[('ptp', i)], writes=[('ppT', tb)])
                P.barrier()
            with contextlib.ExitStack() as pi2:
                Wgs = [sb("WgsI%d" % i, [128, 16, 512], BF16, pi2) for i in range(2)]
                Wps = [sb("WpsI%d" % i, [128, 2, 512], BF16, pi2) for i in range(2)]
                sgm = [sb("sgm%d" % i, [128, 512], F32, pi2) for i in range(2)]
                tmpI = [sb("tmpI%d" % i, [128, 512], F32, pi2) for i in range(2)]
                pgt = [ps("pgtI%d" % i, [128, 512], F32, pi2) for i in range(4)]
                ppe = [ps("ppeI%d" % i, [128, 512], F32, pi2) for i in range(4)]
                cn = 0

                def i_load(nt_):
                    wb_ = nt_ % 2
                    for cg in range(4):
                        P.dma('pool', Wgs[wb_][:, 4 * cg:4 * cg + 4, :],
                              w_pg[cg * 512:(cg + 1) * 512, nt_ * 512:(nt_ + 1) * 512].rearrange("(c p) n -> p c n", p=128),
                              writes=[('WgsI', wb_, cg)])
                    P.dma('pool', Wps[wb_][:, :, :], w_pp[:, nt_ * 512:(nt_ + 1) * 512].rearrange("(c p) n -> p c n", p=128),
                          writes=[('WpsI', wb_)])
                i_load(0)
                for nt in range(4):
                    wb = nt % 2
                    if nt + 1 < 4:
                        i_load(nt + 1)
                    for tb in range(8):
                        bi = cn % 4
                        si = cn % 2
                        cn += 1
                        for c in range(16):
                            P.op('pe', lambda c=c, bi=bi, tb=tb, wb=wb: T.matmul(
                                pgt[bi][:, :], lhsT=h3T[:, c, tb * 128:(tb + 1) * 128], rhs=Wgs[wb][:, c, :],
                                start=(c == 0), stop=(c == 15)), reads=[('WgsI', wb, c // 4)], writes=[('pgt', bi)])
                        for cc in range(2):
                            P.op('pe', lambda cc=cc, bi=bi, tb=tb, wb=wb: T.matmul(
                                ppe[bi][:, :], lhsT=ppT[:, cc, tb * 128:(tb + 1) * 128], rhs=Wps[wb][:, cc, :],
                                start=(cc == 0), stop=(cc == 1)), reads=[('WpsI', wb)], writes=[('ppe', bi)])
                        P.op('act', lambda bi=bi, si=si: A.activation(out=sgm[si][:, :], in_=pgt[bi][:, :], func=AF.Sigmoid),
                             reads=[('pgt', bi)], writes=[('sgm', si)])
                        P.op('dve', lambda bi=bi, si=si: V.tensor_tensor(out=tmpI[si][:, :], in0=ppe[bi][:, :], in1=sgm[si][:, :], op=ALU.mult),
                             reads=[('ppe', bi), ('sgm', si)], writes=[('tmpI', si)])
                        P.op('pool', lambda si=si, tb=tb, nt=nt: G.tensor_tensor(
                            out=x3[:, tb, nt * 512:(nt + 1) * 512], in0=x3[:, tb, nt * 512:(nt + 1) * 512], in1=tmpI[si][:, :], op=ALU.add),
                            reads=[('tmpI', si)], writes=[('x3o', tb)])
                P.barrier()
            with contextlib.ExitStack() as pi3:
                gf = sb("gf_sb", [128, D], F32, pi3)
                ot = [sb("ot%d" % i, [128, D], F32, pi3) for i in range(2)]
                jk = sb("jkI", [128, D], BF16, pi3)
                P.dma('sp', gf[:, :], gfin[:, :], writes=['gf'])
                for tb in range(8):
                    i = tb % 2
                    ss = st[:, 3 * i:3 * i + 1]
                    sd = st[:, 3 * i + 1:3 * i + 2]
                    rs = st[:, 3 * i + 2:3 * i + 3]
                    P.op('act', lambda tb=tb, ss=ss: A.activation(out=jk[:, :], in_=x3[:, tb, :], func=AF.Square, accum_out=ss),
                         writes=['jk', ('ss', i)])
                    P.op('dve', lambda ss=ss, sd=sd: V.tensor_scalar(out=sd, in0=ss, scalar1=1.0 / D, scalar2=EPS, op0=ALU.mult, op1=ALU.add),
                         reads=[('ss', i)], writes=[('sd', i)])
                    P.op('act', lambda sd=sd: A.activation(out=sd, in_=sd, func=AF.Sqrt), reads=[('sd', i)], writes=[('sd', i)])
                    P.op('dve', lambda sd=sd, rs=rs: V.reciprocal(out=rs, in_=sd), reads=[('sd', i)], writes=[('rs', i)])
                    P.op('dve', lambda tb=tb, rs=rs, i=i: V.scalar_tensor_tensor(
                        out=ot[i][:, :], in0=x3[:, tb, :], scalar=rs, in1=gf[:, :], op0=ALU.mult, op1=ALU.mult),
                        reads=[('rs', i), 'gf'], writes=[('ot', i)])
                    P.dma('sp', out[tb * 128:(tb + 1) * 128, :], ot[i][:, :], reads=[('ot', i)], writes=[('out', tb)])
                P.barrier()
    return nc


def _t5_bucket_static(dist):
    n = np.maximum(dist, 0)
    nf = np.maximum(n, 1).astype(np.float32)
    large = 16 + (np.log(nf / np.float32(16)) / np.float32(math.log(128 / 16)) * 16).astype(np.int32)
    large = np.minimum(large, 31)
    return np.where(n < 16, n, large)


def prep_inputs(x, p, g_mix, w_in, w_pool, pool_scale, rel_bias, w_out, g_ffn, w_up, conv_w, conv_b,
                w_down, g_ple, w_ple_gate, w_ple_proj, g_final):
    f = np.float32
    x = np.asarray(x, f)
    p = np.asarray(p, f)[0]
    shared = {}
    shared["w_in"] = np.ascontiguousarray(np.asarray(w_in, f)[0])
    shared["w_pool"] = np.ascontiguousarray(np.asarray(w_pool, f)[0])
    shared["w_out"] = np.ascontiguousarray(np.asarray(w_out, f)[0])
    shared["w_up"] = np.ascontiguousarray(np.asarray(w_up, f)[0])
    shared["w_down"] = np.ascontiguousarray(np.asarray(w_down, f)[0])
    shared["w_ple_gate"] = np.ascontiguousarray(np.asarray(w_ple_gate, f)[0])
    shared["w_ple_proj"] = np.ascontiguousarray(np.asarray(w_ple_proj, f)[0])

    def fm(v):
        a = np.asarray(v, f).reshape(16, 128).T
        return np.ascontiguousarray(np.repeat(a[:, :, None], 128, axis=2).reshape(128, 2048))
    shared["gT3"] = np.stack([fm(np.asarray(g_mix)[0]), fm(np.asarray(g_ffn)[0]), fm(np.asarray(g_ple)[0])], 0)
    shared["gfin"] = np.ascontiguousarray(np.repeat(np.asarray(g_final, f)[None, :], 128, axis=0))
    shared["pscale"] = np.ascontiguousarray(np.asarray(pool_scale, f)[0].reshape(8, 128).T)
    cw = np.asarray(conv_w, f)[0]
    cb = np.asarray(conv_b, f)[0]
    cp = np.concatenate([cw, cb[None, :]], 0)
    shared["convp"] = np.ascontiguousarray(cp.reshape(4, 2 * NFC, 128).transpose(2, 1, 0))
    rb = np.asarray(rel_bias, f)
    sl = np.arange(128)[:, None]
    dl = np.arange(896)[None, :] - 256
    dist = dl - sl
    bk = _t5_bucket_static(dist)
    stripv = rb[bk]
    shared["strip"] = np.ascontiguousarray(stripv.transpose(0, 2, 1))
    shared["cfar"] = np.ascontiguousarray(np.repeat(rb[31][None, :], 128, axis=0))
    tl = np.arange(128)[:, None]
    s_l = np.arange(128)[None, :]
    shared["causal"] = np.where(s_l <= tl, 0.0, NEG).astype(f)
    shared["ident"] = np.eye(128, dtype=f)
    shared["halfs"] = np.ascontiguousarray(np.repeat((0.5 ** np.arange(1, NIT + 2, dtype=np.float64)).astype(f)[None, :], 128, 0))
    in_maps = []
    for c in range(8):
        b, j = c // 4, c % 4
        T0 = j * 1024
        m = dict(shared)
        xc = np.zeros((CTX, D), f)
        lo = T0 - 3072
        s0 = max(0, -lo)
        xc[s0:] = x[b, lo + s0:T0 + 1024]
        m["xctx"] = xc
        m["p_own"] = np.ascontiguousarray(p[b, T0:T0 + 1024])
        sbias = np.zeros((CTX,), f)
        sbias[:s0] = NEG
        m["slotb"] = np.ascontiguousarray(np.repeat(sbias[None, :], 128, axis=0))
        ic = np.zeros((128, 8, 16), f)
        for k in range(8):
            w = (2, 4, 8, 16)[k // 2]
            tok = T0 + np.arange(16)
            cntv = np.minimum(tok + 1, w).astype(f)
            ic[:, k, :] = (np.float32(1.0) / cntv)[None, :]
        m["invc"] = ic
        m["hflag"] = np.full((128, 1), 1.0 if j > 0 else 0.0, f)
        in_maps.append(m)
    return in_maps


_NC_CACHE = {}


def kernel(**inputs):
    in_maps = prep_inputs(**inputs)
    if 'nc' not in _NC_CACHE:
        _NC_CACHE['nc'] = build()
    nc = _NC_CACHE['nc']
    res = run_bass_kernel_spmd(nc, in_maps, core_ids=list(range(8)))
    outs = [np.asarray(res.results[c]["out"], np.float32).reshape(1024, D) for c in range(8)]
    full = np.zeros((2, SEQ, D), np.float32)
    for c in range(8):
        full[c // 4, (c % 4) * 1024:(c % 4 + 1) * 1024] = outs[c]
    return full
```

```python
import contextlib
import math
import numpy as np
import concourse.bass as bass
import concourse.mybir as mybir
from concourse.bass_utils import run_bass_kernel_spmd

F32 = mybir.dt.float32
BF16 = mybir.dt.bfloat16
AF = mybir.ActivationFunctionType
ALU = mybir.AluOpType
AX = mybir.AxisListType

D = 2048
SEQ = 4096
CTX = 4096
NQB = 9
QS0 = 2944
NQ = NQB * 128
HM0 = 2816
NHM = 1280
D_FF = 5632
NFC = 44
EPS = 1e-6
NEG = -1.0e30
NIT = 15
OFF = dict(pool=0, q=1024, k=2048, v=3072, qi=4096, ki=5120, wi=5184)


class Prog:
    NDS = 12

    def __init__(self, nc, es, same_sync=True):
        self.nc = nc
        self.same_sync = same_sync
        self.eng = {'pe': nc.tensor, 'act': nc.scalar, 'dve': nc.vector, 'pool': nc.gpsimd, 'sp': nc.sync}
        self.csem = {e: es.enter_context(nc.semaphore('c_' + e)) for e in ['pe', 'act', 'dve', 'pool']}
        self.cnt = {e: 0 for e in self.csem}
        self.dsem = {q: [es.enter_context(nc.semaphore('d_%s%d' % (q, i))) for i in range(self.NDS)]
                     for q in ['sp', 'pool']}
        self.duse = {q: [0] * self.NDS for q in self.dsem}
        self.dnext = {q: 0 for q in self.dsem}
        self.seen = {e: {} for e in self.eng}
        self.lastw = {}
        self.rds = {}
        self.nwait = 0

    def _wait(self, e, tok):
        sem, val, sid = tok
        if sid == e and (e == 'pe' or not self.same_sync):
            return
        if self.seen[e].get(sid, 0) >= val:
            return
        self.eng[e].wait_ge(sem, val)
        self.seen[e][sid] = val
        self.nwait += 1

    def _deps(self, reads, writes):
        deps = {}

        def add(tok):
            sid = tok[2]
            if sid not in deps or deps[sid][1] < tok[1]:
                deps[sid] = tok
        for k in reads:
            if k in self.lastw:
                add(self.lastw[k])
        for k in writes:
            if k in self.lastw:
                add(self.lastw[k])
            for tok in self.rds.get(k, {}).values():
                add(tok)
        return deps.values()

    def _record(self, tok, reads, writes):
        sid = tok[2]
        for k in reads:
            d = self.rds.setdefault(k, {})
            if sid not in d or d[sid][1] < tok[1]:
                d[sid] = tok
        for k in writes:
            self.lastw[k] = tok
            self.rds[k] = {}

    def op(self, e, fn, reads=(), writes=()):
        for tok in self._deps(reads, writes):
            self._wait(e, tok)
        ins = fn()
        self.cnt[e] += 1
        ins.then_inc(self.csem[e], 1)
        self._record((self.csem[e], self.cnt[e], e), reads, writes)

    def dma(self, q, out, in_, reads=(), writes=(), **kw):
        for tok in self._deps(reads, writes):
            self._wait(q, tok)
        k = self.dnext[q]
        self.dnext[q] = (k + 1) % self.NDS
        u = self.duse[q][k]
        sem = self.dsem[q][k]
        if u > 0:
            self._wait(q, (sem, 16 * u, (q, k)))
        ins = self.eng[q].dma_start(out=out, in_=in_, **kw)
        ins.then_inc(sem, 16)
        self.duse[q][k] = u + 1
        self._record((sem, 16 * (u + 1), (q, k)), reads, writes)

    def barrier(self):
        for e in self.eng:
            for c in self.csem:
                if self.cnt[c] > 0:
                    self._wait_force(e, (self.csem[c], self.cnt[c], c))
            for q in self.dsem:
                for k in range(self.NDS):
                    if self.duse[q][k] > 0:
                        self._wait_force(e, (self.dsem[q][k], 16 * self.duse[q][k], (q, k)))
        self.lastw.clear()
        self.rds.clear()

    def _wait_force(self, e, tok):
        sem, val, sid = tok
        if self.seen[e].get(sid, 0) >= val:
            return
        self.eng[e].wait_ge(sem, val)
        self.seen[e][sid] = val
        self.nwait += 1


def build(debug=False, stop_after='Z'):
    nc = bass.Bass("TRN2", target_bir_lowering=False)

    def din(name, shape, dt=F32):
        return nc.dram_tensor(name, list(shape), dt, kind="ExternalInput").ap()

    xctx = din("xctx", [CTX, D])
    p_own = din("p_own", [1024, 256])
    w_in = din("w_in", [D, 5200])
    w_pool = din("w_pool", [4, 256, 256])
    w_out = din("w_out", [D, D])
    w_up = din("w_up", [D, 2 * D_FF])
    w_down = din("w_down", [D_FF, D])
    w_pg = din("w_ple_gate", [D, D])
    w_pp = din("w_ple_proj", [256, D])
    gT3 = din("gT3", [3, 128, 2048])
    gfin = din("gfin", [128, 2048])
    pscale = din("pscale", [128, 8])
    convp = din("convp", [128, 2 * NFC, 4])
    strip = din("strip", [128, 8, 896])
    cfar = din("cfar", [128, 8])
    slotb = din("slotb", [128, CTX])
    causal = din("causal", [128, 128])
    ident = din("ident", [128, 128])
    invc = din("invc", [128, 8, 16])
    hflag = din("hflag", [128, 1])
    halfs = din("halfs", [128, NIT + 1])
    out = nc.dram_tensor("out", [1024, D], F32, kind="ExternalOutput").ap()
    ks = "ExternalOutput" if debug else "Internal"
    KT_d = nc.dram_tensor("KT_d", [8, 128, CTX], BF16, kind=ks).ap()
    V_d = nc.dram_tensor("V_d", [8, CTX, 128], BF16, kind=ks).ap()
    x1_d = nc.dram_tensor("x1_d", [NQ, D], F32, kind=ks).ap()
    x2_d = nc.dram_tensor("x2_d", [1024, D], F32, kind=ks).ap()
    kidxT_d = nc.dram_tensor("kidxT_d", [128, CTX], BF16, kind=ks).ap()
    QT_d = nc.dram_tensor("QT_d", [128, 8, NQ], BF16, kind=ks).ap()
    qiT_d = nc.dram_tensor("qiT_d", [128, 8, NQ], BF16, kind=ks).ap()
    ypT_d = nc.dram_tensor("ypT_d", [128, 8, NQ], BF16, kind=ks).ap()
    attnT_d = nc.dram_tensor("attnT_d", [128, 8, NQ], BF16, kind=ks).ap()
    widx_d = nc.dram_tensor("widx_d", [128, NQB, 16], F32, kind=ks).ap()
    dbg = {}
    if debug:
        dbg['maskT'] = nc.dram_tensor("dbg_maskT", [128, 32, NQ], BF16, kind="ExternalOutput").ap()
        dbg['thr'] = nc.dram_tensor("dbg_thr", [128, NQB], F32, kind="ExternalOutput").ap()
        dbg['gT'] = nc.dram_tensor("dbg_gT", [128, NFC, 1024], BF16, kind="ExternalOutput").ap()

    with contextlib.ExitStack() as es:
        P = Prog(nc, es)
        T = nc.tensor
        A = nc.scalar
        V = nc.vector
        G = nc.gpsimd

        def sb(name, shape, dt, stack=es):
            return stack.enter_context(nc.sbuf_tensor(name, list(shape), dt))

        def ps(name, shape, dt, stack):
            return stack.enter_context(nc.psum_tensor(name, list(shape), dt))

        ident_f = sb("ident_f", [128, 128], F32)
        ident_b = sb("ident_b", [128, 128], BF16)
        ones_b = sb("ones_b", [128, 128], BF16)
        st = sb("st", [128, 16], F32)
        P.dma('sp', ident_f[:], ident[:, :], writes=['ident_f'])
        P.op('dve', lambda: V.tensor_copy(out=ident_b[:], in_=ident_f[:]), reads=['ident_f'], writes=['ident_b'])
        P.op('dve', lambda: V.memset(ones_b[:], 1.0), writes=['ones_b'])

        def alloc(name, shape, dt):
            cm = nc.sbuf_tensor(name, list(shape), dt)
            return cm, cm.__enter__()

        def norm_T(i, xin, xin_key, xn, pT, gsb, dst_fn, dst_keys, extra=None, stage='all'):
            ss = st[:, 3 * i:3 * i + 1]
            sd = st[:, 3 * i + 1:3 * i + 2]
            rs = st[:, 3 * i + 2:3 * i + 3]
            if stage == 'post':
                return norm_T_post(i, xn, pT, gsb, dst_fn, dst_keys)
            P.op('act', lambda: A.activation(out=xn[i][:], in_=xin, func=AF.Square, accum_out=ss),
                 reads=[xin_key], writes=[('xn', i), ('ss', i)])
            P.op('dve', lambda: V.tensor_scalar(out=sd, in0=ss, scalar1=1.0 / D, scalar2=EPS, op0=ALU.mult, op1=ALU.add),
                 reads=[('ss', i)], writes=[('sd', i)])
            P.op('act', lambda: A.activation(out=sd, in_=sd, func=AF.Sqrt), reads=[('sd', i)], writes=[('sd', i)])
            P.op('dve', lambda: V.reciprocal(out=rs, in_=sd), reads=[('sd', i)], writes=[('rs', i)])
            if extra is not None:
                P.op('dve', lambda: V.tensor_tensor(out=rs, in0=rs, in1=extra, op=ALU.mult),
                     reads=[('rs', i), 'hflag'], writes=[('rs', i)])
            P.op('dve', lambda: V.tensor_scalar(out=xn[i][:], in0=xin, scalar1=rs, scalar2=None, op0=ALU.mult),
                 reads=[xin_key, ('rs', i)], writes=[('xn', i)])
            if stage == 'pre':
                return
            norm_T_post(i, xn, pT, gsb, dst_fn, dst_keys)

        def norm_T_post(i, xn, pT, gsb, dst_fn, dst_keys):
            for c in range(16):
                P.op('pe', lambda c=c: T.transpose(out=pT[i][c // 8][:, (c % 8) * 128:(c % 8 + 1) * 128],
                                                   in_=xn[i][:, c * 128:(c + 1) * 128], identity=ident_b[:]),
                     reads=[('xn', i), 'ident_b'], writes=[('pT', i, c // 8)])
            for h in range(2):
                P.op('dve', lambda h=h: V.tensor_tensor(
                    out=dst_fn(h), in0=pT[i][h][:, :].rearrange("p (c t) -> p c t", t=128),
                    in1=gsb[:, 8 * h:8 * h + 8, :], op=ALU.mult),
                    reads=[('pT', i, h), 'gsb'], writes=dst_keys)

        def wload(dst, dkey, src_rows, c0, c1, col0, ncol, q='pool'):
            P.dma(q, dst[:, c0:c1, 0:ncol],
                  src_rows[c0 * 128:c1 * 128, col0:col0 + ncol].rearrange("(c p) n -> p c n", p=128),
                  writes=[dkey])

        with contextlib.ExitStack() as pa:
            kidxT = sb("kidxT", [128, CTX], BF16, pa)
            Wk = sb("Wk", [128, 16, 1024], BF16, pa)
            Wv = sb("Wv", [128, 16, 1024], BF16, pa)
            Wki = sb("Wki", [128, 16, 128], BF16, pa)
            gsb = sb("gsbA", [128, 16, 128], F32, pa)
            xb = [sb("xbA%d" % i, [128, D], F32, pa) for i in range(2)]
            xn = [sb("xnA%d" % i, [128, D], BF16, pa) for i in range(2)]
            hT = [sb("hTA%d" % i, [128, 16, 512], BF16, pa) for i in range(2)]
            KTst = [sb("KTst%d" % i, [128, 8, 512], BF16, pa) for i in range(2)]
            Vst = [sb("Vst%d" % i, [128, 4, 1024], BF16, pa) for i in range(2)]
            pT = [[ps("pTA%d%d" % (i, h), [128, 1024], BF16, pa) for h in range(2)] for i in range(2)]
            pm = [ps("pmA%d" % i, [128, 512], F32, pa) for i in range(4)]

            P.dma('sp', gsb[:].rearrange("p c t -> p (c t)"), gT3[0, :, :], writes=['gsb'])
            for cg in range(4):
                wload(Wk, ('Wk', cg), w_in, 4 * cg, 4 * cg + 4, OFF['k'], 1024)
            for cg in range(4):
                wload(Wv, ('Wv', cg), w_in, 4 * cg, 4 * cg + 4, OFF['v'], 1024)
            P.dma('pool', Wki[:, :, 0:64], w_in[:, OFF['ki']:OFF['ki'] + 64].rearrange("(c p) n -> p c n", p=128),
                  writes=['Wki'])
            P.dma('pool', Wki[:, :, 64:128], w_in[:, OFF['ki']:OFF['ki'] + 64].rearrange("(c p) n -> p c n", p=128),
                  writes=['Wki'])
            pmi = [0]

            def a_load(ct, sbl):
                j = ct * 4 + sbl
                P.dma('sp', xb[j % 2][:], xctx[j * 128:(j + 1) * 128, :], writes=[('xb', j % 2)])

            def a_norm(ct, sbl, stage='all'):
                b = ct % 2
                j = ct * 4 + sbl
                i = j % 2
                norm_T(i, xb[i][:], ('xb', i), xn, pT, gsb,
                       lambda h: hT[b][:, 8 * h:8 * h + 8, sbl * 128:(sbl + 1) * 128], [('hT', b, sbl)], stage=stage)

            def a_k_heads(ct, heads):
                b = ct % 2
                hkeys = [('hT', b, s_) for s_ in range(4)]
                for h in heads:
                    pq = pm[pmi[0] % 4]
                    pk = ('pm', pmi[0] % 4)
                    pmi[0] += 1
                    for c in range(16):
                        P.op('pe', lambda c=c: T.matmul(pq[:, :], lhsT=Wk[:, c, h * 128:(h + 1) * 128],
                                                        rhs=hT[b][:, c, :], start=(c == 0), stop=(c == 15)),
                             reads=hkeys + [('Wk', c // 4)], writes=[pk])
                    P.op('act', lambda: A.activation(out=KTst[b][:, h, :], in_=pq[:, :], func=AF.Copy),
                         reads=[pk], writes=[('KTst', b)])

            def a_kidx(ct):
                b = ct % 2
                hkeys = [('hT', b, s_) for s_ in range(4)]
                pq = pm[pmi[0] % 4]
                pk = ('pm', pmi[0] % 4)
                pmi[0] += 1
                for c in range(16):
                    P.op('pe', lambda c=c: T.matmul(pq[:, :], lhsT=Wki[:, c, :], rhs=hT[b][:, c, :],
                                                    start=(c == 0), stop=(c == 15)),
                         reads=hkeys + ['Wki'], writes=[pk])
                P.op('act', lambda: A.activation(out=kidxT[:, ct * 512:(ct + 1) * 512], in_=pq[:, :], func=AF.Copy),
                     reads=[pk], writes=[('kidxT', ct)])
                P.dma('pool', KT_d[:, :, ct * 512:(ct + 1) * 512].rearrange("h d s -> d h s"), KTst[b][:, :, :],
                      reads=[('KTst', b)], writes=[('KT_d', ct)])

            def a_v(ct, sbls):
                b = ct % 2
                for sbl in sbls:
                    for hf in range(2):
                        pq = pm[pmi[0] % 4]
                        pk = ('pm', pmi[0] % 4)
                        pmi[0] += 1
                        for c in range(16):
                            P.op('pe', lambda c=c: T.matmul(
                                pq[:, :], lhsT=hT[b][:, c, sbl * 128:(sbl + 1) * 128],
                                rhs=Wv[:, c, hf * 512:(hf + 1) * 512], start=(c == 0), stop=(c == 15)),
                                reads=[('hT', b, sbl), ('Wv', c // 4)], writes=[pk])
                        if hf == 0:
                            P.op('act', lambda: A.activation(
                                out=Vst[b][:, sbl, hf * 512:(hf + 1) * 512], in_=pq[:, :], func=AF.Copy),
                                reads=[pk], writes=[('Vst', b, sbl)])
                        else:
                            P.op('dve', lambda: V.tensor_copy(
                                out=Vst[b][:, sbl, hf * 512:(hf + 1) * 512], in_=pq[:, :]),
                                reads=[pk], writes=[('Vst', b, sbl)])
                    r0 = ct * 512 + sbl * 128
                    P.dma('pool', V_d[:, r0:r0 + 128, :].rearrange("h p d -> p h d"),
                          Vst[b][:, sbl, :].rearrange("p (h d) -> p h d", d=128),
                          reads=[('Vst', b, sbl)], writes=[('V_d', ct, sbl)])

            for sbl in range(4):
                a_load(0, sbl)
                a_norm(0, sbl)
            NCT = CTX // 512
            for ct in range(NCT):
                parts = [lambda ct=ct: a_k_heads(ct, range(0, 4)),
                         lambda ct=ct: (a_k_heads(ct, range(4, 8)), a_kidx(ct)),
                         lambda ct=ct: a_v(ct, (0, 1)),
                         lambda ct=ct: a_v(ct, (2, 3))]
                for q_ in range(4):
                    if ct + 1 < NCT:
                        a_load(ct + 1, q_)
                        a_norm(ct + 1, q_, 'pre')
                    parts[q_]()
                    if ct + 1 < NCT:
                        a_norm(ct + 1, q_, 'post')
            P.dma('sp', kidxT_d[:, :], kidxT[:, :], reads=[('kidxT', c_) for c_ in range(8)])
            P.barrier()
        if stop_after == 'A':
            return nc


        with contextlib.ExitStack() as pb:
            QT = sb("QT", [128, 8, NQ], BF16, pb)
            qiT = sb("qiT", [128, 8, NQ], BF16, pb)
            ypT = sb("ypT", [128, 8, NQ], BF16, pb)
            widx = sb("widx", [128, NQB, 16], F32, pb)
            hM = sb("hM", [128, 16, NHM], BF16, pb)
            Ws = [sb("WsB%d" % i, [128, 16, 512], BF16, pb) for i in range(2)]
            wgroups = [OFF['q'], OFF['q'] + 512, OFF['qi'], OFF['qi'] + 512, OFF['pool'], OFF['pool'] + 512]
            wissued = [0]

            def issue_w():
                k = wissued[0]
                if k < len(wgroups):
                    wload(Ws[k % 2], ('Ws', k % 2), w_in, 0, 16, wgroups[k], 512)
                    wissued[0] += 1
            issue_w()
            issue_w()
            with contextlib.ExitStack() as pb1:
                gsb = sb("gsbB", [128, 16, 128], F32, pb1)
                xb = [sb("xbB%d" % i, [128, D], F32, pb1) for i in range(2)]
                xn = [sb("xnB%d" % i, [128, D], BF16, pb1) for i in range(2)]
                pT = [[ps("pTB%d%d" % (i, h), [128, 1024], BF16, pb1) for h in range(2)] for i in range(2)]
                P.dma('sp', gsb[:].rearrange("p c t -> p (c t)"), gT3[0, :, :], writes=['gsb'])
                for j in range(NHM // 128):
                    i = j % 2
                    P.dma('sp', xb[i][:], xctx[HM0 + j * 128:HM0 + (j + 1) * 128, :], writes=[('xb', i)])
                    norm_T(i, xb[i][:], ('xb', i), xn, pT, gsb,
                           lambda h, j=j: hM[:, 8 * h:8 * h + 8, j * 128:(j + 1) * 128], [('hM', j)])
                P.barrier()
            pp = [ps("ppB%d" % i, [128, 4, 512], F32, pb) for i in range(2)]
            Wpl = sb("Wpl", [128, 4, 2, 256], BF16, pb)
            Ww = sb("Ww", [128, 16, 16], BF16, pb)
            psc_sb = sb("pscale_sb", [128, 8], F32, pb)
            invc_sb = sb("invc_sb", [128, 8, 16], F32, pb)
            pooledT = sb("pooledT", [128, 8, NQ], BF16, pb)
            uk = [sb("uk%d" % i, [128, 1168], F32, pb) for i in range(2)]
            sA = sb("sA", [128, 1168], F32, pb)
            sB = sb("sB", [128, 1168], F32, pb)
            t16 = sb("t16", [128, 16], F32, pb)
            P.dma('sp', psc_sb[:], pscale[:, :], writes=['pscale'])
            P.dma('sp', invc_sb[:], invc[:, :, :], writes=['invc'])
            for g in range(4):
                P.dma('pool', Wpl[:, g, :, :], w_pool[g, :, :].rearrange("(cc p) d -> p cc d", p=128), writes=['Wpl'])
            P.dma('pool', Ww[:, :, :], w_in[:, OFF['wi']:OFF['wi'] + 16].rearrange("(c p) n -> p c n", p=128), writes=['Ww'])
            wcnt = [0]
            pcnt = [0]

            def next_w(col0):
                b = wcnt[0] % 2
                assert wgroups[wcnt[0]] == col0
                if wcnt[0] >= 1:
                    issue_w()
                wcnt[0] += 1
                return b

            def next_pp():
                b = pcnt[0] % 2
                pcnt[0] += 1
                return b

            for (dst, off) in ((QT, OFF['q']), (qiT, OFF['qi'])):
                for g in range(2):
                    wb = next_w(off + 512 * g)
                    for m_ in range(4):
                        pb_ = next_pp()
                        for n in range(3):
                            for c in range(16):
                                P.op('pe', lambda c=c, n=n, wb=wb, m_=m_, pb_=pb_: T.matmul(
                                    pp[pb_][:, n, 0:384], lhsT=Ws[wb][:, c, m_ * 128:(m_ + 1) * 128],
                                    rhs=hM[:, c, 128 + n * 384:128 + (n + 1) * 384], start=(c == 0), stop=(c == 15)),
                                    reads=[('Ws', wb)], writes=[('pp', pb_)])
                        P.op('act', lambda dst=dst, g=g, m_=m_, pb_=pb_: A.activation(
                            out=dst[:, 4 * g + m_, :].rearrange("p (n t) -> p n t", t=384),
                            in_=pp[pb_][:, 0:3, 0:384], func=AF.Copy),
                            reads=[('pp', pb_)], writes=[('fm', id(dst), 4 * g + m_)])
            for g2 in range(2):
                wb = next_w(OFF['pool'] + 512 * g2)
                for m_ in range(4):
                    k = 4 * g2 + m_
                    g = k // 2
                    pb_ = next_pp()
                    for n in range(4):
                        for c in range(16):
                            P.op('pe', lambda c=c, n=n, wb=wb, m_=m_, pb_=pb_: T.matmul(
                                pp[pb_][:, n, 0:292], lhsT=Ws[wb][:, c, m_ * 128:(m_ + 1) * 128],
                                rhs=hM[:, c, 112 + n * 292:112 + (n + 1) * 292], start=(c == 0), stop=(c == 15)),
                                reads=[('Ws', wb)], writes=[('pp', pb_)])
                    u = uk[k % 2]
                    P.op('act', lambda u=u, pb_=pb_: A.activation(
                        out=u[:, :].rearrange("p (n t) -> p n t", t=292), in_=pp[pb_][:, 0:4, 0:292], func=AF.Copy),
                        reads=[('pp', pb_)], writes=[('uk', k % 2)])
                    wdw = (2, 4, 8, 16)[g]
                    cur, ckey = u, ('uk', k % 2)
                    for s_ in range(int(math.log2(wdw))):
                        sh = 1 << s_
                        nxt, nkey = (sA, 'sA') if s_ % 2 == 0 else (sB, 'sB')
                        P.op('pool', lambda cur=cur, nxt=nxt, sh=sh: G.tensor_tensor(
                            out=nxt[:, sh:1168], in0=cur[:, sh:1168], in1=cur[:, 0:1168 - sh], op=ALU.add),
                            reads=[ckey], writes=[nkey])
                        cur, ckey = nxt, nkey
                    P.op('dve', lambda cur=cur, u=u, k=k, wdw=wdw: V.scalar_tensor_tensor(
                        out=pooledT[:, k, :], in0=cur[:, 16:1168], scalar=1.0 / wdw, in1=u[:, 16:1168],
                        op0=ALU.mult, op1=ALU.subtract), reads=[ckey, ('uk', k % 2)], writes=[('pooledT', k)])
                    P.op('dve', lambda cur=cur, k=k: V.tensor_tensor(out=t16[:, :], in0=cur[:, 144:160], in1=invc_sb[:, k, :], op=ALU.mult),
                         reads=[ckey, 'invc'], writes=['t16'])
                    P.op('dve', lambda u=u, k=k: V.tensor_tensor(out=pooledT[:, k, 128:144], in0=t16[:, :], in1=u[:, 144:160], op=ALU.subtract),
                         reads=['t16', ('uk', k % 2)], writes=[('pooledT', k)])
            for k2 in range(8):
                g, dm = k2 // 2, k2 % 2
                pb_ = next_pp()
                for n in range(3):
                    for cc in range(2):
                        P.op('pe', lambda n=n, cc=cc, g=g, dm=dm, pb_=pb_: T.matmul(
                            pp[pb_][:, n, 0:384], lhsT=Wpl[:, g, cc, dm * 128:(dm + 1) * 128],
                            rhs=pooledT[:, 2 * g + cc, n * 384:(n + 1) * 384], start=(cc == 0), stop=(cc == 1)),
                            reads=['Wpl', ('pooledT', 2 * g + cc)], writes=[('pp', pb_)])
                P.op('act', lambda k2=k2, pb_=pb_: A.activation(
                    out=ypT[:, k2, :].rearrange("p (n t) -> p n t", t=384), in_=pp[pb_][:, 0:3, 0:384],
                    func=AF.Copy, scale=psc_sb[:, k2:k2 + 1]), reads=[('pp', pb_), 'pscale'], writes=[('ypT', k2)])
            idx_scale = (16 ** -0.5) * (64 ** -0.5)
            for i in range(NQB):
                pb_ = next_pp()
                for c in range(16):
                    P.op('pe', lambda c=c, i=i, pb_=pb_: T.matmul(
                        pp[pb_][:, 0, 0:16], lhsT=hM[:, c, 128 + i * 128:256 + i * 128], rhs=Ww[:, c, :],
                        start=(c == 0), stop=(c == 15)), reads=['Ww'], writes=[('pp', pb_)])
                P.op('act', lambda i=i, pb_=pb_: A.activation(out=widx[:, i, :], in_=pp[pb_][:, 0, 0:16], func=AF.Copy, scale=idx_scale),
                     reads=[('pp', pb_)], writes=[('widx', i)])
            P.dma('sp', QT_d[:, :, :], QT[:, :, :], reads=[('fm', id(QT), h_) for h_ in range(8)])
            P.dma('sp', qiT_d[:, :, :], qiT[:, :, :], reads=[('fm', id(qiT), h_) for h_ in range(8)])
            P.dma('sp', ypT_d[:, :, :], ypT[:, :, :], reads=[('ypT', h_) for h_ in range(8)])
            P.dma('sp', widx_d[:, :, :], widx[:, :, :], reads=[('widx', h_) for h_ in range(NQB)])
            P.barrier()
        if stop_after == 'B':
            return nc

        pcd = contextlib.ExitStack()
        es.enter_context(pcd)
        maskT = sb("maskT", [128, 32, NQ], BF16, pcd)
        with contextlib.ExitStack() as pc:
            kidxT = sb("kidxTc", [128, CTX], BF16, pc)
            qiT = sb("qiTc", [128, 8, NQ], BF16, pc)
            widx = sb("widxc", [128, NQB, 16], F32, pc)
            P.dma('sp', kidxT[:, :], kidxT_d[:, :], writes=['kidxT'])
            P.dma('sp', qiT[:, :, :], qiT_d[:, :, :], writes=['qiT'])
            P.dma('sp', widx[:, :, :], widx_d[:, :, :], writes=['widx'])
            slot_row = sb("slot_row", [1, CTX], BF16, pc)
            ones_row = sb("ones_row", [1, 128], BF16, pc)
            causal_b = sb("causal_b", [128, 128], BF16, pc)
            halfs_sb = sb("halfs_sb", [128, NIT + 1], F32, pc)
            sc = [sb("sc%d" % i, [128, CTX], F32, pc) for i in range(2)]
            junk = sb("junk", [128, CTX], BF16, pc)
            Rb = sb("Rb", [128, 4, 512], BF16, pc)
            Dg = [sb("Dg%d" % i, [128, 16, 128], BF16, pc) for i in range(2)]
            bsx = [sb("bsx%d" % i, [128, 32], F32, pc) for i in range(2)]
            bs = sb("bs", [128, 64], F32, pc)
            thr_all = sb("thr_all", [128, NQB], F32, pc)
            pd = ps("pdC", [128, 4, 512], F32, pc)
            psc = [ps("pscC%d" % i, [128, 512], F32, pc) for i in range(3)]
            ptm = [ps("ptmC%d" % i, [128, 1024], BF16, pc) for i in range(1)]
            P.dma('pool', slot_row[0:1, :], slotb[0:1, :], writes=['slot_row'])
            P.dma('pool', causal_b[:, :], causal[:, :], writes=['causal_b'])
            P.dma('sp', halfs_sb[:], halfs[:, :], writes=['halfs_sb'])
            P.op('pool', lambda: G.memset(ones_row[0:1, :], 1.0), writes=['ones_row'])
            P.op('pool', lambda: G.memset(maskT[:, :, :].rearrange("p a b -> p (a b)"), 0.0), writes=['maskT'])
            mid = bs[:, 3:4]
            cntv = bs[:, 4:5]
            gg = bs[:, 5:6]
            lo = bs[:, 6:7]
            hi = bs[:, 7:8]
            w0 = bs[:, 8:9]
            hk = bs[:, 16:16 + NIT + 1]
            cnts = dict(d=0, s=0, t=0)

            def gen_scores(i, bi):
                E = QS0 + 128 * (i + 1)
                nkt = (E + 511) // 512
                mins = bsx[bi][:, 0:8]
                maxs = bsx[bi][:, 8:16]
                for h in range(16):
                    P.op('pool', lambda h=h: G.tensor_scalar(out=Dg[bi][:, h, :], in0=ident_f[:], scalar1=widx[:, i, h:h + 1],
                                                             scalar2=1.0, op0=ALU.mult, op1=ALU.mult),
                         reads=['ident_f', 'widx'], writes=[('Dg', bi, h)])
                prev = None

                def finish(kt, N, pst, pskey):
                    P.op('pe', lambda: T.matmul(pst[:, :N], lhsT=ones_row[0:1, :], rhs=slot_row[0:1, kt * 512:kt * 512 + N],
                                                start=False, stop=(kt != nkt - 1)),
                         reads=['ones_row', 'slot_row', ('mins', bi, kt), ('maxs', bi, kt)], writes=[pskey])
                    if kt == nkt - 1:
                        P.op('pe', lambda: T.matmul(pst[:, N - 128:N], lhsT=ident_b[:, :], rhs=causal_b[:, :], start=False, stop=True),
                             reads=['ident_b', 'causal_b'], writes=[pskey])
                    P.op('act', lambda: A.activation(out=sc[bi][:, kt * 512:kt * 512 + N], in_=pst[:, :N], func=AF.Copy),
                         reads=[pskey], writes=[('sc', bi, kt)])
                for kt in range(nkt):
                    N = min(512, E - kt * 512)
                    si = cnts['s'] % 3
                    cnts['s'] += 1
                    pst = psc[si]
                    pskey = ('psc', si)
                    pend = []

                    def score_mm(hp, di, N=N, pst=pst, pskey=pskey):
                        for q_ in range(2):
                            h = 2 * hp + q_
                            P.op('pe', lambda h=h, q_=q_: T.matmul(pst[:, :N], lhsT=Dg[bi][:, h, :], rhs=Rb[:, 2 * di + q_, :N],
                                                                   start=(h == 0), stop=False),
                                 reads=[('Dg', bi, h), ('Rb', di)], writes=[pskey])
                    for hp in range(8):
                        di = cnts['d'] % 2
                        cnts['d'] += 1
                        pdk = ('pd', di)
                        for q_ in range(2):
                            pr = q_ * 64
                            P.op('pe', lambda q_=q_, pr=pr: T.matmul(
                                pd[:, 2 * di + q_, :N], lhsT=qiT[pr:pr + 64, hp, i * 128:(i + 1) * 128],
                                rhs=kidxT[pr:pr + 64, kt * 512:kt * 512 + N], start=True, stop=True),
                                reads=['kidxT', 'qiT'], writes=[pdk])
                        P.op('act', lambda di=di: A.activation(out=Rb[:, 2 * di:2 * di + 2, :N], in_=pd[:, 2 * di:2 * di + 2, :N], func=AF.Relu),
                             reads=[pdk], writes=[('Rb', di)])
                        pend.append((hp, di))
                        if len(pend) > 1:
                            score_mm(*pend.pop(0))
                    while pend:
                        score_mm(*pend.pop(0))
                    P.op('dve', lambda pst=pst, N=N, kt=kt: V.tensor_reduce(out=mins[:, kt:kt + 1], in_=pst[:, :N], axis=AX.X, op=ALU.min),
                         reads=[pskey], writes=[('mins', bi, kt)])
                    P.op('dve', lambda pst=pst, N=N, kt=kt: V.tensor_reduce(out=maxs[:, kt:kt + 1], in_=pst[:, :N], axis=AX.X, op=ALU.max),
                         reads=[pskey], writes=[('maxs', bi, kt)])
                    if prev is not None:
                        finish(*prev)
                    prev = (kt, N, pst, pskey)
                    yield
                finish(*prev)
                yield

            def bisect(i, bi, nxt):
                E = QS0 + 128 * (i + 1)
                nkt = (E + 511) // 512
                mins = bsx[bi][:, 0:8]
                maxs = bsx[bi][:, 8:16]
                sckeys = [('sc', bi, kt) for kt in range(nkt)]
                P.op('dve', lambda: V.tensor_reduce(out=lo, in_=mins[:, 0:nkt], axis=AX.X, op=ALU.min),
                     reads=[('mins', bi, kt) for kt in range(nkt)], writes=['lo'])
                P.op('dve', lambda: V.tensor_reduce(out=hi, in_=maxs[:, 0:nkt], axis=AX.X, op=ALU.max),
                     reads=[('maxs', bi, kt) for kt in range(nkt)], writes=['hi'])
                P.op('dve', lambda: V.scalar_tensor_tensor(out=w0, in0=hi, scalar=0.01, in1=lo, op0=ALU.add, op1=ALU.subtract),
                     reads=['hi', 'lo'], writes=['w0'])
                P.op('dve', lambda: V.tensor_scalar(out=hk, in0=halfs_sb[:, :], scalar1=w0, scalar2=None, op0=ALU.mult),
                     reads=['w0', 'halfs_sb'], writes=['hk'])
                P.op('dve', lambda: V.scalar_tensor_tensor(out=mid, in0=lo, scalar=-0.01, in1=hk[:, 0:1], op0=ALU.add, op1=ALU.add),
                     reads=['lo', 'hk'], writes=['mid'])
                for it in range(NIT):
                    P.op('dve', lambda: V.tensor_scalar(out=junk[:, 0:E], in0=sc[bi][:, 0:E], scalar1=mid, scalar2=None,
                                                        op0=ALU.is_ge, op1=ALU.add, accum_out=cntv),
                         reads=sckeys + ['mid'], writes=['junk', 'cnt'])
                    P.op('dve', lambda: V.tensor_scalar(out=gg, in0=cntv, scalar1=255.5, scalar2=0.5, op0=ALU.is_ge, op1=ALU.subtract),
                         reads=['cnt'], writes=['gg'])
                    P.op('dve', lambda it=it: V.scalar_tensor_tensor(out=mid, in0=gg, scalar=hk[:, it:it + 1], in1=mid,
                                                                      op0=ALU.mult, op1=ALU.add),
                         reads=['gg', 'hk', 'mid'], writes=['mid'])
                    if nxt is not None and it % 2 == 1:
                        next(nxt, None)
                if nxt is not None:
                    for _ in nxt:
                        pass
                P.op('dve', lambda: V.tensor_tensor(out=lo, in0=mid, in1=hk[:, NIT:NIT + 1], op=ALU.subtract),
                     reads=['mid', 'hk'], writes=['lo'])
                P.op('dve', lambda: V.tensor_scalar(out=junk[:, 0:E], in0=sc[bi][:, 0:E], scalar1=lo, scalar2=None, op0=ALU.is_ge),
                     reads=sckeys + ['lo'], writes=['junk'])
                P.op('dve', lambda: V.tensor_copy(out=thr_all[:, i:i + 1], in_=lo), reads=['lo'], writes=['thr_all'])
                nkb = E // 128
                for kb0 in range(0, nkb, 8):
                    n8 = min(8, nkb - kb0)
                    pt = ptm[0]
                    ptk = ('ptm', 0)
                    for r in range(n8):
                        kb = kb0 + r
                        P.op('pe', lambda r=r, kb=kb: T.transpose(out=pt[:, r * 128:(r + 1) * 128],
                                                                 in_=junk[:, kb * 128:(kb + 1) * 128], identity=ident_b[:]),
                             reads=['junk', 'ident_b'], writes=[ptk])
                    P.op('act', lambda n8=n8, kb0=kb0: A.activation(
                        out=maskT[:, kb0:kb0 + n8, i * 128:(i + 1) * 128],
                        in_=pt[:, 0:n8 * 128].rearrange("p (a b) -> p a b", b=128), func=AF.Copy),
                        reads=[ptk], writes=['maskT'])

            for _ in gen_scores(0, 0):
                pass
            for i in range(NQB):
                nxt = gen_scores(i + 1, (i + 1) % 2) if i + 1 < NQB else None
                bisect(i, i % 2, nxt)
            if debug:
                P.dma('sp', dbg['maskT'][:, :, :], maskT[:, :, :], reads=['maskT'])
                P.dma('sp', dbg['thr'][:, :], thr_all[:, :], reads=['thr_all'])
            P.barrier()
        if stop_after == 'C':
            return nc

        SCALE = 128 ** -0.5
        with contextlib.ExitStack() as pdd:
            attnT = sb("attnT", [128, 8, NQ], BF16, pdd)
            QT = sb("QTd", [128, 8, NQ], BF16, pdd)
            P.dma('sp', QT[:, :, :], QT_d[:, :, :], writes=['QT'])
            KTh = [sb("KTh%d" % i, [128, CTX], BF16, pdd) for i in range(2)]
            Vh = [sb("Vh%d" % i, [128, 32, 128], BF16, pdd) for i in range(2)]
            strip_sb = sb("strip_sb", [128, 8, 896], F32, pdd)
            cfar_sb = sb("cfar_sb", [128, 8], F32, pdd)
            Eb = [sb("Eb%d" % i, [128, 384], BF16, pdd) for i in range(4)]
            Pb = [sb("Pb%d" % i, [128, 384], BF16, pdd) for i in range(4)]
            tmpb = [sb("tmpb%d" % i, [128, 384], F32, pdd) for i in range(2)]
            rl = sb("rl", [128, 384], F32, pdd)
            tiny_sb = sb("tiny_sb", [128, 1], F32, pdd)
            P.op('pool', lambda: G.memset(tiny_sb[:, :], 1e-18), writes=['tiny'])
            pS = [ps("pS%d" % i, [128, 512], F32, pdd) for i in range(4)]
            pO = [ps("pO%d" % i, [128, 512], F32, pdd) for i in range(2)]
            pL = [ps("pL%d" % i, [128, 512], F32, pdd) for i in range(2)]
            P.dma('sp', strip_sb[:], strip[:, :, :], writes=['strip_sb'])
            P.dma('sp', cfar_sb[:], cfar[:, :], writes=['cfar_sb'])
            steps = []
            for h in range(8):
                for qt in range(3):
                    kbmax = 23 + 3 * qt + 2
                    near = list(range(kbmax, kbmax - 4, -1))
                    far = list(range(kbmax - 4, -1, -1))
                    order = []
                    for nk in near:
                        order.append(nk)
                        order.extend(far[:4])
                        far = far[4:]
                    order.extend(far)
                    for n_, kb in enumerate(order):
                        steps.append((h, qt, kb, n_ == 0, n_ == len(order) - 1))
            loaded = set()

            def load_head(h):
                if h in loaded or h >= 8:
                    return
                loaded.add(h)
                b = h % 2
                P.dma('sp', KTh[b][:, :], KT_d[h, :, :], writes=[('KTh', b)])
                P.dma('sp', Vh[b][:, :, :], V_d[h, :, :].rearrange("(kb p) d -> p kb d", p=128), writes=[('Vh', b)])
            cn = dict(s=0, t=0)
            staged = []

            def stage1(step):
                h, qt, kb, first, last = step
                b = h % 2
                t0 = qt * 384
                if first and qt == 0:
                    load_head(h)
                if qt == 1 and first:
                    load_head(h + 1)
                s_i = cn['s'] % 4
                cn['s'] += 1
                pst = pS[s_i]
                P.op('pe', lambda: T.matmul(pst[:, 0:384], lhsT=KTh[b][:, kb * 128:(kb + 1) * 128],
                                            rhs=QT[:, h, t0:t0 + 384], start=True, stop=True),
                     reads=[('KTh', b), 'QT'], writes=[('pS', s_i)])
                D0 = (QS0 + t0) - kb * 128
                if D0 >= 256:
                    P.op('act', lambda: A.activation(out=Eb[s_i][:, :], in_=pst[:, 0:384], func=AF.Exp,
                                                     scale=SCALE, bias=cfar_sb[:, h:h + 1]),
                         reads=[('pS', s_i), 'cfar_sb'], writes=[('Eb', s_i)])
                else:
                    ti = cn['t'] % 2
                    cn['t'] += 1
                    P.op('dve', lambda: V.scalar_tensor_tensor(
                        out=tmpb[ti][:, :], in0=pst[:, 0:384], scalar=SCALE,
                        in1=strip_sb[:, h, D0 + 256:D0 + 256 + 384], op0=ALU.mult, op1=ALU.add),
                        reads=[('pS', s_i), 'strip_sb'], writes=[('tmpb', ti)])
                    P.op('act', lambda: A.activation(out=Eb[s_i][:, :], in_=tmpb[ti][:, :], func=AF.Exp),
                         reads=[('tmpb', ti)], writes=[('Eb', s_i)])
                P.op('dve', lambda: V.tensor_tensor(out=Pb[s_i][:, :], in0=Eb[s_i][:, :],
                                                    in1=maskT[:, kb, t0:t0 + 384], op=ALU.mult),
                     reads=[('Eb', s_i)], writes=[('Pb', s_i)])
                staged.append((step, s_i))

            def stage2():
                (h, qt, kb, first, last), s_i = staged.pop(0)
                b = h % 2
                t0 = qt * 384
                g_ = (h * 3 + qt) % 2
                po, pl = pO[g_], pL[g_]
                pok, plk = ('pO', g_), ('pL', g_)
                P.op('pe', lambda: T.matmul(po[:, 0:384], lhsT=Vh[b][:, kb, :], rhs=Pb[s_i][:, :], start=first, stop=last),
                     reads=[('Vh', b), ('Pb', s_i)], writes=[pok])
                P.op('pe', lambda: T.matmul(pl[:, 0:384], lhsT=ones_b[:, :], rhs=Pb[s_i][:, :], start=first, stop=last),
                     reads=['ones_b', ('Pb', s_i)], writes=[plk])
                if last:
                    P.op('act', lambda: A.activation(out=rl[:, :], in_=pl[:, 0:384], func=AF.Ln, bias=tiny_sb[:, 0:1]),
                         reads=[plk, 'tiny'], writes=['rl'])
                    P.op('act', lambda: A.activation(out=rl[:, :], in_=rl[:, :], func=AF.Exp, scale=-1.0), reads=['rl'], writes=['rl'])
                    P.op('dve', lambda: V.tensor_tensor(out=attnT[:, h, t0:t0 + 384], in0=po[:, 0:384], in1=rl[:, :], op=ALU.mult),
                         reads=[pok, 'rl'], writes=[('attnT', h)])
            for step in steps:
                stage1(step)
                if len(staged) > 3:
                    stage2()
            while staged:
                stage2()
            P.dma('sp', attnT_d[:, :, :], attnT[:, :, :], reads=[('attnT', h_) for h_ in range(8)])
            P.barrier()
        pcd.close()
        if stop_after == 'D':
            return nc

        with contextlib.ExitStack() as pf:
            Wo = sb("Wo", [128, 16, 2048], BF16, pf)
            ypT = sb("ypTf", [128, 8, NQ], BF16, pf)
            attnT = sb("attnTf", [128, 8, NQ], BF16, pf)
            P.dma('sp', ypT[:, :, :], ypT_d[:, :, :], writes=['ypT'])
            P.dma('sp', attnT[:, :, :], attnT_d[:, :, :], writes=['attnT'])
            xr = [sb("xrF%d" % i, [128, D], F32, pf) for i in range(2)]
            x1t = [sb("x1tF%d" % i, [128, D], F32, pf) for i in range(2)]
            pw = [ps("pwF%d" % i, [128, 512], F32, pf) for i in range(8)]
            for cg in range(4):
                wload(Wo, ('Wo', cg), w_out, 4 * cg, 4 * cg + 4, 0, 2048)
            pcn = 0
            for i in range(NQB):
                bi = i % 2
                P.dma('sp', xr[bi][:], xctx[QS0 + i * 128:QS0 + (i + 1) * 128, :], writes=[('xr', bi)])
                for nt in range(4):
                    pq = pw[pcn % 8]
                    pk = ('pw', pcn % 8)
                    pcn += 1
                    for c in range(16):
                        src = ypT if c < 8 else attnT
                        P.op('pe', lambda c=c, src=src, pq=pq, nt=nt, i=i: T.matmul(
                            pq[:, :], lhsT=src[:, c % 8, i * 128:(i + 1) * 128], rhs=Wo[:, c, nt * 512:(nt + 1) * 512],
                            start=(c == 0), stop=(c == 15)), reads=[('Wo', c // 4), 'ypT', 'attnT'], writes=[pk])
                    P.op('dve', lambda pq=pq, nt=nt, bi=bi: V.tensor_tensor(
                        out=x1t[bi][:, nt * 512:(nt + 1) * 512], in0=pq[:, :], in1=xr[bi][:, nt * 512:(nt + 1) * 512], op=ALU.add),
                        reads=[pk, ('xr', bi)], writes=[('x1t', bi)])
                P.dma('sp', x1_d[i * 128:(i + 1) * 128, :], x1t[bi][:], reads=[('x1t', bi)], writes=[('x1_d', i)])
            P.barrier()
        if stop_after == 'F':
            return nc


        pgh = contextlib.ExitStack()
        es.enter_context(pgh)
        gT = sb("gT", [128, NFC, 1024], BF16, pgh)
        with contextlib.ExitStack() as pg_:
            h2T = sb("h2T", [128, 16, NQ], BF16, pg_)
            Wgv = [sb("Wgv%d" % i, [128, 16, 512], BF16, pg_) for i in range(2)]

            def g_load(grp):
                jg_, isval_ = grp // 2, grp % 2
                wload(Wgv[grp % 2], ('Wgv', grp % 2), w_up, 0, 16, (D_FF if isval_ else 0) + jg_ * 512, 512)
            g_load(0)
            g_load(1)
            with contextlib.ExitStack() as pg1:
                gsb = sb("gsbG", [128, 16, 128], F32, pg1)
                hf_sb = sb("hf_sb", [128, 1], F32, pg1)
                xb = [sb("xbG%d" % i, [128, D], F32, pg1) for i in range(2)]
                xn = [sb("xnG%d" % i, [128, D], BF16, pg1) for i in range(2)]
                pT = [[ps("pTG%d%d" % (i, h), [128, 1024], BF16, pg1) for h in range(2)] for i in range(2)]
                P.dma('sp', gsb[:].rearrange("p c t -> p (c t)"), gT3[1, :, :], writes=['gsb'])
                P.dma('sp', hf_sb[:], hflag[:, :], writes=['hflag'])
                for j in range(NQB):
                    i = j % 2
                    P.dma('sp', xb[i][:], x1_d[j * 128:(j + 1) * 128, :], writes=[('xb', i)])
                    norm_T(i, xb[i][:], ('xb', i), xn, pT, gsb,
                           lambda h, j=j: h2T[:, 8 * h:8 * h + 8, j * 128:(j + 1) * 128], [('h2T', j)],
                           extra=(hf_sb[:, 0:1] if j == 0 else None))
                P.barrier()
            sgT = sb("sgT", [128, 4, 1024], F32, pg_)
            cp = sb("cp", [128, 2 * NFC, 4], F32, pg_)
            rA = [sb("rA%d" % i, [128, 344], F32, pg_) for i in range(3)]
            rB = [sb("rB%d" % i, [128, 344], F32, pg_) for i in range(3)]
            pu = [ps("puG%d" % i, [128, 512], F32, pg_) for i in range(8)]
            P.dma('sp', cp[:], convp[:, :, :], writes=['cp'])
            tiles = [(0, 342), (342, 684), (684, 1024)]
            pcn = 0
            rcn = 0
            for grp in range(2 * (NFC // 4)):
                jg, isval = grp // 2, grp % 2
                wb = grp % 2
                if grp >= 1 and grp + 1 < 2 * (NFC // 4):
                    g_load(grp + 1)
                for m_ in range(4):
                    j = 4 * jg + m_
                    jj = (NFC + j) if isval else j
                    for (a, b_) in tiles:
                        N = b_ - a + 2
                        n2 = N - 2
                        c0 = 128 + a - 2
                        pq = pu[pcn % 8]
                        pk = ('pu', pcn % 8)
                        pcn += 1
                        for c in range(16):
                            P.op('pe', lambda c=c: T.matmul(
                                pq[:, 0:N], lhsT=Wgv[wb][:, c, m_ * 128:(m_ + 1) * 128], rhs=h2T[:, c, c0:c0 + N],
                                start=(c == 0), stop=(c == 15)), reads=[('Wgv', wb)], writes=[pk])
                        ri = rcn % 3
                        rcn += 1
                        P.op('act', lambda: A.activation(
                            out=rA[ri][:, 0:n2], in_=pq[:, 2:N], func=AF.Identity, scale=cp[:, jj, 2:3], bias=cp[:, jj, 3:4]),
                            reads=[pk, 'cp'], writes=[('rA', ri)])
                        P.op('dve', lambda: V.scalar_tensor_tensor(
                            out=rB[ri][:, 0:n2], in0=pq[:, 1:N - 1], scalar=cp[:, jj, 1:2], in1=rA[ri][:, 0:n2],
                            op0=ALU.mult, op1=ALU.add), reads=[pk, 'cp', ('rA', ri)], writes=[('rB', ri)])
                        if not isval:
                            P.op('dve', lambda: V.scalar_tensor_tensor(
                                out=rA[ri][:, 0:n2], in0=pq[:, 0:n2], scalar=cp[:, jj, 0:1], in1=rB[ri][:, 0:n2],
                                op0=ALU.mult, op1=ALU.add), reads=[pk, 'cp', ('rB', ri)], writes=[('rA', ri)])
                            P.op('act', lambda: A.activation(out=sgT[:, m_, a:b_], in_=rA[ri][:, 0:n2], func=AF.Silu),
                                 reads=[('rA', ri)], writes=[('sgT', m_)])
                        else:
                            P.op('dve', lambda: V.scalar_tensor_tensor(
                                out=rA[ri][:, 0:n2], in0=pq[:, 0:n2], scalar=cp[:, jj, 0:1], in1=rB[ri][:, 0:n2],
                                op0=ALU.mult, op1=ALU.add), reads=[pk, 'cp', ('rB', ri)], writes=[('rA', ri)])
                            P.op('dve', lambda: V.tensor_tensor(
                                out=gT[:, j, a:b_], in0=sgT[:, m_, a:b_], in1=rA[ri][:, 0:n2], op=ALU.mult),
                                reads=[('sgT', m_), ('rA', ri)], writes=[('gT', j)])
            if debug:
                P.dma('sp', dbg['gT'][:, :, :], gT[:, :, :], reads=[('gT', j_) for j_ in range(NFC)])
            P.barrier()
        if stop_after == 'G':
            return nc

        with contextlib.ExitStack() as ph:
            Wdr = sb("Wdr", [128, NFC, 512], BF16, ph)
            x1s = [sb("x1s%d" % i, [128, 512], F32, ph) for i in range(4)]
            x2s = [sb("x2s%d" % i, [128, 512], F32, ph) for i in range(4)]
            pw = [ps("pwH%d" % i, [128, 512], F32, ph) for i in range(8)]
            NFG = NFC // 4

            def h_load(nt_, fg_):
                P.dma('pool', Wdr[:, 4 * fg_:4 * fg_ + 4, :],
                      w_down[fg_ * 512:(fg_ + 1) * 512, nt_ * 512:(nt_ + 1) * 512].rearrange("(c p) n -> p c n", p=128),
                      writes=[('Wdr', fg_)])
            for fg in range(NFG):
                h_load(0, fg)
            scn = 0
            pset = 0
            for nt in range(4):
                for tg in range(2):
                    banks = [(pw[4 * (pset % 2) + tb], ('pw', 4 * (pset % 2) + tb)) for tb in range(4)]
                    pset += 1
                    for fg in range(NFG):
                        for fl in range(4):
                            f = fg * 4 + fl
                            for tb in range(4):
                                pq, pk = banks[tb]
                                tok0 = (tg * 4 + tb) * 128
                                P.op('pe', lambda pq=pq, f=f, tok0=tok0: T.matmul(
                                    pq[:, :], lhsT=gT[:, f, tok0:tok0 + 128], rhs=Wdr[:, f, :],
                                    start=(f == 0), stop=(f == NFC - 1)), reads=[('Wdr', fg)], writes=[pk])
                        if tg == 1 and nt + 1 < 4:
                            h_load(nt + 1, fg)
                    for tb in range(4):
                        pq, pk = banks[tb]
                        si = scn % 4
                        scn += 1
                        r0 = (tg * 4 + tb) * 128
                        P.dma('sp', x1s[si][:, :], x1_d[128 + r0:128 + r0 + 128, nt * 512:(nt + 1) * 512], writes=[('x1s', si)])
                        P.op('dve', lambda pq=pq, si=si: V.tensor_tensor(out=x2s[si][:, :], in0=pq[:, :], in1=x1s[si][:, :], op=ALU.add),
                             reads=[pk, ('x1s', si)], writes=[('x2s', si)])
                        P.dma('sp', x2_d[r0:r0 + 128, nt * 512:(nt + 1) * 512], x2s[si][:, :], reads=[('x2s', si)], writes=[('x2_d', r0, nt)])
            P.barrier()
        pgh.close()
        if stop_after == 'H':
            return nc

        with contextlib.ExitStack() as pi:
            x3 = sb("x3", [128, 8, D], F32, pi)
            h3T = sb("h3T", [128, 16, 1024], BF16, pi)
            ppT = sb("ppT", [128, 2, 1024], BF16, pi)
            with contextlib.ExitStack() as pi1:
                gsb = sb("gsbI", [128, 16, 128], F32, pi1)
                p_sb = sb("p_sb", [128, 8, 256], F32, pi1)
                p_bf = sb("p_bf", [128, 8, 256], BF16, pi1)
                xn = [sb("xnI%d" % i, [128, D], BF16, pi1) for i in range(2)]
                pT = [[ps("pTI%d%d" % (i, h), [128, 1024], BF16, pi1) for h in range(2)] for i in range(2)]
                ptp = [ps("ptpI%d" % i, [128, 1024], BF16, pi1) for i in range(2)]
                P.dma('sp', gsb[:].rearrange("p c t -> p (c t)"), gT3[2, :, :], writes=['gsb'])
                P.dma('sp', p_sb[:, :, :], p_own.rearrange("(tb p) c -> p tb c", p=128), writes=['p_sb'])
                P.op('pool', lambda: G.tensor_copy(out=p_bf[:, :, :], in_=p_sb[:, :, :]), reads=['p_sb'], writes=['p_bf'])
                for tb in range(8):
                    P.dma('sp', x3[:, tb, :], x2_d[tb * 128:(tb + 1) * 128, :], writes=[('x3', tb)])
                for tb in range(8):
                    i = tb % 2
                    norm_T(i, x3[:, tb, :], ('x3', tb), xn, pT, gsb,
                           lambda h, tb=tb: h3T[:, 8 * h:8 * h + 8, tb * 128:(tb + 1) * 128], [('h3T', tb)])
                    pt = ptp[i]
                    for cc in range(2):
                        P.op('pe', lambda pt=pt, cc=cc, tb=tb: T.transpose(out=pt[:, cc * 128:(cc + 1) * 128],
                                                                          in_=p_bf[:, tb, cc * 128:(cc + 1) * 128], identity=ident_b[:]),
                             reads=['p_bf', 'ident_b'], writes=[('ptp', i)])
                    P.op('act', lambda pt=pt, tb=tb: A.activation(out=ppT[:, :, tb * 128:(tb + 1) * 128],
                                                                  in_=pt[:, 0:256].rearrange("p (a b) -> p a b", b=128), func=AF.Copy),
                         reads=[('ptp', i)], writes=[('ppT', tb)])
                P.barrier()
            with contextlib.ExitStack() as pi2:
                Wgs = [sb("WgsI%d" % i, [128, 16, 512], BF16, pi2) for i in range(2)]
                Wps = [sb("WpsI%d" % i, [128, 2, 512], BF16, pi2) for i in range(2)]
                sgm = [sb("sgm%d" % i, [128, 512], F32, pi2) for i in range(2)]
                tmpI = [sb("tmpI%d" % i, [128, 512], F32, pi2) for i in range(2)]
                pgt = [ps("pgtI%d" % i, [128, 512], F32, pi2) for i in range(4)]
                ppe = [ps("ppeI%d" % i, [128, 512], F32, pi2) for i in range(4)]
                cn = 0

                def i_load(nt_):
                    wb_ = nt_ % 2
                    for cg in range(4):
                        P.dma('pool', Wgs[wb_][:, 4 * cg:4 * cg + 4, :],
                              w_pg[cg * 512:(cg + 1) * 512, nt_ * 512:(nt_ + 1) * 512].rearrange("(c p) n -> p c n", p=128),
                              writes=[('WgsI', wb_, cg)])
                    P.dma('pool', Wps[wb_][:, :, :], w_pp[:, nt_ * 512:(nt_ + 1) * 512].rearrange("(c p) n -> p c n", p=128),
                          writes=[('WpsI', wb_)])
                i_load(0)
                for nt in range(4):
                    wb = nt % 2
                    if nt + 1 < 4:
                        i_load(nt + 1)
                    for tb in range(8):
                        bi = cn % 4
                        si = cn % 2
                        cn += 1
                        for c in range(16):
                            P.op('pe', lambda c=c, bi=bi, tb=tb, wb=wb: T.matmul(
                                pgt[bi][:, :], lhsT=h3T[:, c, tb * 128:(tb + 1) * 128], rhs=Wgs[wb][:, c, :],
                                start=(c == 0), stop=(c == 15)), reads=[('WgsI', wb, c // 4)], writes=[('pgt', bi)])
                        for cc in range(2):
                            P.op('pe', lambda cc=cc, bi=bi, tb=tb, wb=wb: T.matmul(
                                ppe[bi][:, :], lhsT=ppT[:, cc, tb * 128:(tb + 1) * 128], rhs=Wps[wb][:, cc, :],
                                start=(cc == 0), stop=(cc == 1)), reads=[('WpsI', wb)], writes=[('ppe', bi)])
                        P.op('act', lambda bi=bi, si=si: A.activation(out=sgm[si][:, :], in_=pgt[bi][:, :], func=AF.Sigmoid),
                             reads=[('pgt', bi)], writes=[('sgm', si)])
                        P.op('dve', lambda bi=bi, si=si: V.tensor_tensor(out=tmpI[si][:, :], in0=ppe[bi][:, :], in1=sgm[si][:, :], op=ALU.mult),
                             reads=[('ppe', bi), ('sgm', si)], writes=[('tmpI', si)])
                        P.op('pool', lambda si=si, tb=tb, nt=nt: G.tensor_tensor(
                            out=x3[:, tb, nt * 512:(nt + 1) * 512], in0=x3[:, tb, nt * 512:(nt + 1) * 512], in1=tmpI[si][:, :], op=ALU.add),
                            reads=[('tmpI', si)], writes=[('x3o', tb)])
                P.barrier()
            with contextlib.ExitStack() as pi3:
                gf = sb("gf_sb", [128, D], F32, pi3)
                ot = [sb("ot%d" % i, [128, D], F32, pi3) for i in range(2)]
                jk = sb("jkI", [128, D], BF16, pi3)
                P.dma('sp', gf[:, :], gfin[:, :], writes=['gf'])
                for tb in range(8):
                    i = tb % 2
                    ss = st[:, 3 * i:3 * i + 1]
                    sd = st[:, 3 * i + 1:3 * i + 2]
                    rs = st[:, 3 * i + 2:3 * i + 3]
                    P.op('act', lambda tb=tb, ss=ss: A.activation(out=jk[:, :], in_=x3[:, tb, :], func=AF.Square, accum_out=ss),
                         writes=['jk', ('ss', i)])
                    P.op('dve', lambda ss=ss, sd=sd: V.tensor_scalar(out=sd, in0=ss, scalar1=1.0 / D, scalar2=EPS, op0=ALU.mult, op1=ALU.add),
                         reads=[('ss', i)], writes=[('sd', i)])
                    P.op('act', lambda sd=sd: A.activation(out=sd, in_=sd, func=AF.Sqrt), reads=[('sd', i)], writes=[('sd', i)])
                    P.op('dve', lambda sd=sd, rs=rs: V.reciprocal(out=rs, in_=sd), reads=[('sd', i)], writes=[('rs', i)])
                    P.op('dve', lambda tb=tb, rs=rs, i=i: V.scalar_tensor_tensor(
                        out=ot[i][:, :], in0=x3[:, tb, :], scalar=rs, in1=gf[:, :], op0=ALU.mult, op1=ALU.mult),
                        reads=[('rs', i), 'gf'], writes=[('ot', i)])
                    P.dma('sp', out[tb * 128:(tb + 1) * 128, :], ot[i][:, :], reads=[('ot', i)], writes=[('out', tb)])
                P.barrier()
    return nc


def _t5_bucket_static(dist):
    n = np.maximum(dist, 0)
    nf = np.maximum(n, 1).astype(np.float32)
    large = 16 + (np.log(nf / np.float32(16)) / np.float32(math.log(128 / 16)) * 16).astype(np.int32)
    large = np.minimum(large, 31)
    return np.where(n < 16, n, large)


def prep_inputs(x, p, g_mix, w_in, w_pool, pool_scale, rel_bias, w_out, g_ffn, w_up, conv_w, conv_b,
                w_down, g_ple, w_ple_gate, w_ple_proj, g_final):
    f = np.float32
    x = np.asarray(x, f)
    p = np.asarray(p, f)[0]
    shared = {}
    shared["w_in"] = np.ascontiguousarray(np.asarray(w_in, f)[0])
    shared["w_pool"] = np.ascontiguousarray(np.asarray(w_pool, f)[0])
    shared["w_out"] = np.ascontiguousarray(np.asarray(w_out, f)[0])
    shared["w_up"] = np.ascontiguousarray(np.asarray(w_up, f)[0])
    shared["w_down"] = np.ascontiguousarray(np.asarray(w_down, f)[0])
    shared["w_ple_gate"] = np.ascontiguousarray(np.asarray(w_ple_gate, f)[0])
    shared["w_ple_proj"] = np.ascontiguousarray(np.asarray(w_ple_proj, f)[0])

    def fm(v):
        a = np.asarray(v, f).reshape(16, 128).T
        return np.ascontiguousarray(np.repeat(a[:, :, None], 128, axis=2).reshape(128, 2048))
    shared["gT3"] = np.stack([fm(np.asarray(g_mix)[0]), fm(np.asarray(g_ffn)[0]), fm(np.asarray(g_ple)[0])], 0)
    shared["gfin"] = np.ascontiguousarray(np.repeat(np.asarray(g_final, f)[None, :], 128, axis=0))
    shared["pscale"] = np.ascontiguousarray(np.asarray(pool_scale, f)[0].reshape(8, 128).T)
    cw = np.asarray(conv_w, f)[0]
    cb = np.asarray(conv_b, f)[0]
    cp = np.concatenate([cw, cb[None, :]], 0)
    shared["convp"] = np.ascontiguousarray(cp.reshape(4, 2 * NFC, 128).transpose(2, 1, 0))
    rb = np.asarray(rel_bias, f)
    sl = np.arange(128)[:, None]
    dl = np.arange(896)[None, :] - 256
    dist = dl - sl
    bk = _t5_bucket_static(dist)
    stripv = rb[bk]
    shared["strip"] = np.ascontiguousarray(stripv.transpose(0, 2, 1))
    shared["cfar"] = np.ascontiguousarray(np.repeat(rb[31][None, :], 128, axis=0))
    tl = np.arange(128)[:, None]
    s_l = np.arange(128)[None, :]
    shared["causal"] = np.where(s_l <= tl, 0.0, NEG).astype(f)
    shared["ident"] = np.eye(128, dtype=f)
    shared["halfs"] = np.ascontiguousarray(np.repeat((0.5 ** np.arange(1, NIT + 2, dtype=np.float64)).astype(f)[None, :], 128, 0))
    in_maps = []
    for c in range(8):
        b, j = c // 4, c % 4
        T0 = j * 1024
        m = dict(shared)
        xc = np.zeros((CTX, D), f)
        lo = T0 - 3072
        s0 = max(0, -lo)
        xc[s0:] = x[b, lo + s0:T0 + 1024]
        m["xctx"] = xc
        m["p_own"] = np.ascontiguousarray(p[b, T0:T0 + 1024])
        sbias = np.zeros((CTX,), f)
        sbias[:s0] = NEG
        m["slotb"] = np.ascontiguousarray(np.repeat(sbias[None, :], 128, axis=0))
        ic = np.zeros((128, 8, 16), f)
        for k in range(8):
            w = (2, 4, 8, 16)[k // 2]
            tok = T0 + np.arange(16)
            cntv = np.minimum(tok + 1, w).astype(f)
            ic[:, k, :] = (np.float32(1.0) / cntv)[None, :]
        m["invc"] = ic
        m["hflag"] = np.full((128, 1), 1.0 if j > 0 else 0.0, f)
        in_maps.append(m)
    return in_maps


_NC_CACHE = {}


def kernel(**inputs):
    in_maps = prep_inputs(**inputs)
    if 'nc' not in _NC_CACHE:
        _NC_CACHE['nc'] = build()
    nc = _NC_CACHE['nc']
    res = run_bass_kernel_spmd(nc, in_maps, core_ids=list(range(8)))
    outs = [np.asarray(res.results[c]["out"], np.float32).reshape(1024, D) for c in range(8)]
    full = np.zeros((2, SEQ, D), np.float32)
    for c in range(8):
        full[c // 4, (c % 4) * 1024:(c % 4 + 1) * 1024] = outs[c]
    return full
```
